# Optimizing a Trainium2 kernel written in Bass

```python
import math
import jax, jax.numpy as jnp
from jax import lax
import numpy as np

D_MODEL = 2048
BATCH = 2
SEQ = 4096
DEPTH = 2

GRID_W = 64
CTX_LEN = 256
N_GROUPS = 4
GROUP_W = D_MODEL // N_GROUPS
MIX_W = N_GROUPS * GROUP_W
NORM_EPS = 1e-6
RW_HEAD = 64
RW_HEADS = GROUP_W // RW_HEAD
RW_DECAY_RANK = 64
RW_ICLR_RANK = 64
RW_GATE_RANK = 128
RW_DECAY_SCALE = math.exp(-0.5)
RW_GN_EPS = 64e-5
RW_COLS = 3 * GROUP_W + RW_DECAY_RANK + RW_ICLR_RANK + RW_GATE_RANK
ML_HEAD = 128
ML_HEADS = GROUP_W // ML_HEAD
ML_CHUNK = 64
ML_COLS = 4 * GROUP_W + 4 * ML_HEADS
GD_HEAD = 128
GD_HEADS = GROUP_W // GD_HEAD
GD_CHUNK = 64
GD_CONV = 3
GD_COLS = 4 * GROUP_W + 4 * GD_HEADS
AT_HEAD = 128
AT_Q_HEADS = GROUP_W // AT_HEAD
AT_KV_HEADS = 2
AT_BLOCK = 128
ROPE_THETA = 10000.0
AT_COLS = (AT_Q_HEADS + 2 * AT_KV_HEADS) * AT_HEAD
N_IN = RW_COLS + ML_COLS + GD_COLS + AT_COLS
N_EXPERTS = 16
EC_FACTOR = 2
EXPERT_FF = D_MODEL

kernel_name = 'hybrid_parallel_heads_ec_moe_dit'


def split_sizes(a, sizes):
    cuts = [int(s) for s in np.cumsum(sizes)[:-1]]
    return jnp.split(a, cuts, axis=-1)


def rms_norm(x, g, eps=NORM_EPS):
    xf = x.astype(jnp.float32)
    y = xf * lax.rsqrt(jnp.mean(xf * xf, axis=-1, keepdims=True) + eps)
    return (y * g.astype(jnp.float32)).astype(x.dtype)


def l2_normalize(x, eps=1e-6):
    xf = x.astype(jnp.float32)
    return (xf * lax.rsqrt(jnp.sum(xf * xf, axis=-1, keepdims=True) + eps)).astype(x.dtype)


def to_heads(x, n):
    b, l, w = x.shape
    return x.reshape(b, l, n, w // n).transpose(0, 2, 1, 3)


def from_heads(x):
    b, n, l, d = x.shape
    return x.transpose(0, 2, 1, 3).reshape(b, l, n * d)


def shift_mix(x, mu):
    prev = jnp.pad(x[:, :-1], ((0, 0), (1, 0), (0, 0)))
    nxt = jnp.pad(x[:, 1:], ((0, 0), (0, 1), (0, 0)))
    return x + mu[0] * (prev - x) + mu[1] * (nxt - x)


def centred_conv(x, w):
    k = w.shape[0]
    pad = k // 2
    n = x.shape[1]
    xp = jnp.pad(x, ((0, 0), (pad, pad), (0, 0)))
    return sum(xp[:, j:j + n] * w[j] for j in range(k))


def rope_half(x, ang):
    h = x.shape[-1] // 2
    x1, x2 = x[..., :h], x[..., h:]
    cs, sn = jnp.cos(ang), jnp.sin(ang)
    return jnp.concatenate([x1 * cs - x2 * sn, x2 * cs + x1 * sn], axis=-1).astype(x.dtype)


def rope_2d(x, ang_r, ang_c):
    h = x.shape[-1] // 2
    return jnp.concatenate([rope_half(x[..., :h], ang_r), rope_half(x[..., h:], ang_c)], axis=-1)


def prefix_then_latent(run, ctx_args, lat_args, init):
    y_c, state = run(ctx_args, init)
    y_l, _ = run(lat_args, state)
    return y_c, y_l


def bidirectional(run, ctx_fwd, lat_fwd, ctx_bwd, lat_bwd, init):
    flip = lambda args: tuple(jnp.flip(a, axis=2) for a in args)
    yc_f, yl_f = prefix_then_latent(run, ctx_fwd, lat_fwd, init)
    yc_b, yl_b = prefix_then_latent(run, flip(ctx_bwd), flip(lat_bwd), init)
    return yc_f + jnp.flip(yc_b, axis=2), yl_f + jnp.flip(yl_b, axis=2)


def rwkv_run(args, s0):
    xs = tuple(jnp.moveaxis(a.astype(jnp.float32), 2, 0) for a in args)

    def step(s, inp):
        r, lw, kt, v, kk, a = inp
        sk = jnp.einsum('bhvk,bhk->bhv', s, kk)
        s = s * jnp.exp(lw)[:, :, None, :] - sk[..., None] * (kk * a)[:, :, None, :] + v[..., None] * kt[:, :, None, :]
        return s, jnp.einsum('bhvk,bhk->bhv', s, r)

    s, ys = lax.scan(step, s0, xs)
    return jnp.moveaxis(ys, 0, 2), s


def rwkv_prep(pp, p):
    pp = shift_mix(pp, p['rw_mu'])
    r, k, v, wd, ad, gd = split_sizes(pp, [GROUP_W, GROUP_W, GROUP_W, RW_DECAY_RANK, RW_ICLR_RANK, RW_GATE_RANK])
    g = jax.nn.sigmoid(gd) @ p['rw_g_up']
    kk = from_heads(l2_normalize(to_heads(k * p['rw_k_k'], RW_HEADS)))
    per_dir = []
    for d in range(2):
        logw = -RW_DECAY_SCALE * jax.nn.sigmoid(p['rw_w0'][d] + jnp.tanh(wd) @ p['rw_w_up'][d])
        a = jax.nn.sigmoid(p['rw_a0'][d] + ad @ p['rw_a_up'][d])
        kt = k * (1.0 + (a - 1.0) * p['rw_k_a'])
        per_dir.append((logw, a, kt))
    return r, v, kk, g, per_dir


def rwkv_group(pc, pl, p):
    prep_c, prep_l = rwkv_prep(pc, p), rwkv_prep(pl, p)

    def args(prep, d):
        r, v, kk, _, per_dir = prep
        logw, a, kt = per_dir[d]
        return tuple(to_heads(t, RW_HEADS) for t in (r, logw, kt, v, kk, a))

    s0 = jnp.zeros((pl.shape[0], RW_HEADS, RW_HEAD, RW_HEAD), jnp.float32)
    yc, yl = bidirectional(rwkv_run, args(prep_c, 0), args(prep_l, 0), args(prep_c, 1), args(prep_l, 1), s0)

    def post(y, prep):
        r, v, _, g, per_dir = prep
        y = y.transpose(0, 2, 1, 3)
        mean = jnp.mean(y, axis=-1, keepdims=True)
        var = jnp.mean(jnp.square(y - mean), axis=-1, keepdims=True)
        y = ((y - mean) * lax.rsqrt(var + RW_GN_EPS)).reshape(y.shape[0], y.shape[1], GROUP_W)
        y = y * p['rw_ln_w'] + p['rw_ln_b']
        bh = lambda t: t.reshape(t.shape[0], t.shape[1], RW_HEADS, RW_HEAD)
        bonus = sum(jnp.sum(bh(r) * bh(kt) * p['rw_r_k'], axis=-1, keepdims=True) * bh(v) for _, _, kt in per_dir)
        return ((y + bonus.reshape(y.shape)) * g).astype(pl.dtype)

    return post(yc, prep_c), post(yl, prep_l)


def mlstm_run(args, state):
    q, k, v, li, lf = (a.astype(jnp.float32) for a in args)
    b, h, n, d = q.shape
    nc = n // ML_CHUNK

    def chunks(a):
        return jnp.moveaxis(a.reshape(a.shape[:2] + (nc, ML_CHUNK) + a.shape[3:]), 2, 0)

    tri = jnp.tril(jnp.ones((ML_CHUNK, ML_CHUNK), dtype=bool))

    def step(carry, inp):
        cm, nv, m = carry
        qc, kc, vc, ic, fc = inp
        bcum = jnp.cumsum(fc, axis=-1)
        dlog = jnp.where(tri, bcum[..., :, None] - bcum[..., None, :] + ic[..., None, :], -jnp.inf)
        inter = bcum + m[..., None]
        mt = jnp.maximum(inter, jnp.max(dlog, axis=-1))
        s = jnp.einsum('bhtd,bhsd->bhts', qc, kc) * jnp.exp(dlog - mt[..., None])
        wi = jnp.exp(inter - mt)
        num = jnp.einsum('bhts,bhse->bhte', s, vc) + wi[..., None] * jnp.einsum('bhed,bhtd->bhte', cm, qc)
        den = jnp.sum(s, axis=-1) + wi * jnp.einsum('bhd,bhtd->bht', nv, qc)
        out = num / jnp.maximum(jnp.abs(den), jnp.exp(-mt))[..., None]
        m_new = mt[..., -1]
        wk = jnp.exp(bcum[..., -1:] - bcum + ic - m_new[..., None])
        dc = jnp.exp(bcum[..., -1] + m - m_new)
        cm = dc[..., None, None] * cm + jnp.einsum('bhs,bhse,bhsd->bhed', wk, vc, kc)
        nv = dc[..., None] * nv + jnp.einsum('bhs,bhsd->bhd', wk, kc)
        return (cm, nv, m_new), out

    state, outs = lax.scan(step, state, tuple(chunks(a) for a in (q, k, v, li, lf)))
    return jnp.moveaxis(outs, 0, 2).reshape(b, h, n, d), state


def mlstm_prep(pp, p):
    q, k, v, o, gts = split_sizes(pp, [GROUP_W, GROUP_W, GROUP_W, GROUP_W, 4 * ML_HEADS])
    qh, kh, vh = to_heads(q, ML_HEADS), to_heads(k, ML_HEADS) * ML_HEAD ** -0.5, to_heads(v, ML_HEADS)
    gh = jnp.swapaxes(gts, 1, 2)
    dirs = []
    for d in range(2):
        li = gh[:, d * ML_HEADS:(d + 1) * ML_HEADS] + p['ml_ib'][d][:, None]
        lf = jax.nn.log_sigmoid(gh[:, (2 + d) * ML_HEADS:(3 + d) * ML_HEADS] + p['ml_fb'][d][:, None])
        dirs.append((qh, kh, vh, li, lf))
    return dirs, o


def mlstm_group(pc, pl, p):
    (dc, oc), (dl, ol) = mlstm_prep(pc, p), mlstm_prep(pl, p)
    b = pl.shape[0]
    init = (jnp.zeros((b, ML_HEADS, ML_HEAD, ML_HEAD), jnp.float32),
            jnp.zeros((b, ML_HEADS, ML_HEAD), jnp.float32),
            jnp.zeros((b, ML_HEADS), jnp.float32))
    hc, hl = bidirectional(mlstm_run, dc[0], dl[0], dc[1], dl[1], init)

    def post(hh, o):
        y = hh.transpose(0, 2, 1, 3)
        mean = jnp.mean(y, axis=-1, keepdims=True)
        var = jnp.mean(jnp.square(y - mean), axis=-1, keepdims=True)
        y = ((y - mean) * lax.rsqrt(var + NORM_EPS)).reshape(y.shape[0], y.shape[1], GROUP_W) * p['ml_norm_g']
        return (y * jax.nn.sigmoid(o)).astype(o.dtype)

    return post(hc, oc), post(hl, ol)


def gdn_run(args, state):
    q, k, v, lg, beta = (a.astype(jnp.float32) for a in args)
    b, h, n, _ = q.shape
    dv = v.shape[-1]
    nc, c = n // GD_CHUNK, GD_CHUNK
    blk = lambda a: a.reshape(a.shape[:2] + (nc, c) + a.shape[3:])
    q, k, v, lg, beta = blk(q), blk(k), blk(v), blk(lg), blk(beta)
    gc = jnp.cumsum(lg, axis=-1)
    incl = jnp.tril(jnp.ones((c, c), dtype=bool))
    strict = jnp.tril(jnp.ones((c, c), dtype=bool), k=-1)
    decay = jnp.exp(jnp.where(incl, gc[..., :, None] - gc[..., None, :], -jnp.inf))
    kb = k * beta[..., None]
    a_low = jnp.where(strict, jnp.einsum('bhnid,bhnjd->bhnij', kb, k) * decay, 0.0)
    rhs = jnp.concatenate([v * beta[..., None], kb * jnp.exp(gc)[..., None]], axis=-1)
    sol = lax.linalg.triangular_solve(a_low, rhs, left_side=True, lower=True, unit_diagonal=True)
    u, w = sol[..., :dv], sol[..., dv:]
    attn = jnp.einsum('bhnid,bhnjd->bhnij', q, k) * decay
    qg = q * jnp.exp(gc)[..., None]
    kd = k * jnp.exp(gc[..., -1:] - gc)[..., None]
    gl = jnp.exp(gc[..., -1])

    def step(s, inp):
        uc, wc, qc, kc, ac, glc = inp
        v_new = uc - jnp.einsum('bhld,bhde->bhle', wc, s)
        out = jnp.einsum('bhld,bhde->bhle', qc, s) + jnp.einsum('bhij,bhje->bhie', ac, v_new)
        s = s * glc[..., None, None] + jnp.einsum('bhld,bhle->bhde', kc, v_new)
        return s, out

    xs = tuple(jnp.moveaxis(a, 2, 0) for a in (u, w, qg, kd, attn, gl))
    state, outs = lax.scan(step, state, xs)
    return jnp.moveaxis(outs, 0, 2).reshape(b, h, n, dv), state


def gdn_prep(pp, p):
    qkv, g, gts = split_sizes(pp, [3 * GROUP_W, GROUP_W, 4 * GD_HEADS])
    qkv = jax.nn.silu(centred_conv(qkv, p['gd_conv']))
    q, k, v = split_sizes(qkv, [GROUP_W, GROUP_W, GROUP_W])
    qh = l2_normalize(to_heads(q, GD_HEADS)) * GD_HEAD ** -0.5
    kh = l2_normalize(to_heads(k, GD_HEADS))
    vh = to_heads(v, GD_HEADS)
    gh = jnp.swapaxes(gts, 1, 2)
    dirs = []
    for d in range(2):
        lg = -jnp.exp(p['gd_a_log'][d])[:, None] * jax.nn.softplus(
            gh[:, d * GD_HEADS:(d + 1) * GD_HEADS] + p['gd_dt_bias'][d][:, None])
        beta = jax.nn.sigmoid(gh[:, (2 + d) * GD_HEADS:(3 + d) * GD_HEADS])
        dirs.append((qh, kh, vh, lg, beta))
    return dirs, g


def gdn_group(pc, pl, p):
    (dc, gc_), (dl, gl_) = gdn_prep(pc, p), gdn_prep(pl, p)
    s0 = jnp.zeros((pl.shape[0], GD_HEADS, GD_HEAD, GD_HEAD), jnp.float32)
    oc, ol = bidirectional(gdn_run, dc[0], dl[0], dc[1], dl[1], s0)

    def post(oh, g):
        gh = g.reshape(g.shape[0], g.shape[1], GD_HEADS, GD_HEAD)
        y = rms_norm(oh.transpose(0, 2, 1, 3), p['gd_norm_g']) * jax.nn.silu(gh)
        return y.reshape(g.shape).astype(g.dtype)

    return post(oc, gc_), post(ol, gl_)


def gqa(q, k, v):
    b, hq, lq, d = q.shape
    hkv = k.shape[1]
    qg = q.reshape(b, hkv, hq // hkv, lq, d)
    s = jnp.einsum('bkgqd,bksd->bkgqs', qg, k).astype(jnp.float32) * d ** -0.5
    pr = jax.nn.softmax(s, axis=-1).astype(v.dtype)
    return jnp.einsum('bkgqs,bksd->bkgqd', pr, v).reshape(b, hq, lq, d)


def blocked_gqa(q, k, v):
    b, hq, n, d = q.shape
    nb = n // AT_BLOCK
    qb = jnp.moveaxis(q.reshape(b, hq, nb, AT_BLOCK, d), 2, 0)
    ob = lax.map(lambda blk: gqa(blk, k, v), qb)
    return jnp.moveaxis(ob, 0, 2).reshape(b, hq, n, d)


def attn_group(pc, pl, p, ang_r, ang_c, need_ctx):
    sizes = [AT_Q_HEADS * AT_HEAD, AT_KV_HEADS * AT_HEAD, AT_KV_HEADS * AT_HEAD]
    qc, kc, vc = split_sizes(pc, sizes)
    ql, kl, vl = split_sizes(pl, sizes)
    kc = rms_norm(to_heads(kc, AT_KV_HEADS), p['at_k_norm'])
    vc = to_heads(vc, AT_KV_HEADS)
    ql = rope_2d(rms_norm(to_heads(ql, AT_Q_HEADS), p['at_q_norm']), ang_r, ang_c)
    kl = rope_2d(rms_norm(to_heads(kl, AT_KV_HEADS), p['at_k_norm']), ang_r, ang_c)
    vl = to_heads(vl, AT_KV_HEADS)
    k_all = jnp.concatenate([kc, kl], axis=2)
    v_all = jnp.concatenate([vc, vl], axis=2)
    out_l = from_heads(blocked_gqa(ql, k_all, v_all))
    if not need_ctx:
        return None, out_l
    qc = rms_norm(to_heads(qc, AT_Q_HEADS), p['at_q_norm'])
    return from_heads(gqa(qc, kc, vc)), out_l


def token_mixer(hc, hl, p, ang_r, ang_c, need_ctx):
    pc = hc @ p['w_in']
    pl = hl @ p['w_in']
    sizes = [RW_COLS, ML_COLS, GD_COLS, AT_COLS]
    ac, bc, cc, dc = split_sizes(pc, sizes)
    al, bl, cl, dl = split_sizes(pl, sizes)
    oa = rwkv_group(ac, al, p)
    ob = mlstm_group(bc, bl, p)
    oc = gdn_group(cc, cl, p)
    od = attn_group(dc, dl, p, ang_r, ang_c, need_ctx)
    yl = jnp.concatenate([oa[1], ob[1], oc[1], od[1]], axis=-1) @ p['w_out']
    if not need_ctx:
        return None, yl
    yc = jnp.concatenate([oa[0], ob[0], oc[0], od[0]], axis=-1) @ p['w_out']
    return yc, yl


def expert_choice_ffn(h, w_router, w1, w3, w2):
    b, l, _ = h.shape
    cap = EC_FACTOR * l // N_EXPERTS
    aff = jax.nn.softmax(jnp.einsum('bld,de->ble', h, w_router).astype(jnp.float32), axis=-1)
    gate, idx = lax.top_k(jnp.swapaxes(aff, 1, 2), cap)
    bidx = jnp.arange(b)[:, None, None]
    xs = h[bidx, idx]
    hid = jax.nn.silu(jnp.einsum('becd,edf->becf', xs, w1)) * jnp.einsum('becd,edf->becf', xs, w3)
    y = jnp.einsum('becf,efd->becd', hid, w2) * gate[..., None].astype(h.dtype)
    return jnp.zeros_like(h).at[bidx, idx].add(y)


def modulation(cond, p):
    return jnp.split(jax.nn.silu(cond) @ p['ada_w'] + p['ada_b'], 6, axis=-1)


def modulate(x, g, shift, scale):
    return rms_norm(x, g) * (1.0 + scale) + shift


def trunk_layer(xl, xc, c, c_ctx, p, ang_r, ang_c, last):
    ml = [t[:, None, :] for t in modulation(c, p)]
    mc = modulation(c_ctx, p)
    hl = modulate(xl, p['norm1_g'], ml[0], ml[1])
    hc = modulate(xc, p['norm1_g'], mc[0], mc[1])
    yc, yl = token_mixer(hc, hl, p, ang_r, ang_c, not last)
    xl = xl + ml[2] * yl
    xl = xl + ml[5] * expert_choice_ffn(modulate(xl, p['norm2_g'], ml[3], ml[4]),
                                        p['w_router'], p['w_exp1'], p['w_exp3'], p['w_exp2'])
    if not last:
        xc = xc + mc[2] * yc
        xc = xc + mc[5] * expert_choice_ffn(modulate(xc, p['norm2_g'], mc[3], mc[4]),
                                            p['w_router'], p['w_exp1'], p['w_exp3'], p['w_exp2'])
    return xl, xc


def setup_inputs(seed: int = 0) -> dict:
    key = jax.random.key(seed)

    def nrm(i, shape, scale):
        return jax.random.normal(jax.random.fold_in(key, i), shape, jnp.float32) * scale

    def uni(i, shape, lo, hi):
        return jax.random.uniform(jax.random.fold_in(key, i), shape, jnp.float32, lo, hi)

    d = D_MODEL
    dt = jnp.exp(uni(26, (DEPTH, 2, GD_HEADS), math.log(1e-3), math.log(1e-1)))
    return {
        'x': nrm(0, (BATCH, SEQ, d), 1.0),
        'c': nrm(1, (BATCH, d), 1.0),
        'ctx': nrm(2, (BATCH, CTX_LEN, d), 1.0),
        'c_ctx': nrm(3, (d,), 1.0),
        'ada_w': nrm(4, (DEPTH, d, 6 * d), 0.5 * d ** -0.5),
        'ada_b': nrm(5, (DEPTH, 6 * d), 0.01),
        'norm1_g': 1.0 + nrm(6, (DEPTH, d), 0.02),
        'norm2_g': 1.0 + nrm(7, (DEPTH, d), 0.02),
        'w_in': nrm(8, (DEPTH, d, N_IN), d ** -0.5),
        'w_out': nrm(9, (DEPTH, MIX_W, d), MIX_W ** -0.5),
        'rw_mu': uni(10, (DEPTH, 2, RW_COLS), 0.0, 0.5),
        'rw_w0': nrm(11, (DEPTH, 2, GROUP_W), 0.5),
        'rw_w_up': nrm(12, (DEPTH, 2, RW_DECAY_RANK, GROUP_W), 0.1),
        'rw_a0': nrm(13, (DEPTH, 2, GROUP_W), 0.5),
        'rw_a_up': nrm(14, (DEPTH, 2, RW_ICLR_RANK, GROUP_W), 0.1),
        'rw_g_up': nrm(15, (DEPTH, RW_GATE_RANK, GROUP_W), RW_GATE_RANK ** -0.5),
        'rw_k_k': 0.85 + nrm(16, (DEPTH, GROUP_W), 0.05),
        'rw_k_a': 1.0 + nrm(17, (DEPTH, GROUP_W), 0.05),
        'rw_r_k': nrm(18, (DEPTH, RW_HEADS, RW_HEAD), 0.1),
        'rw_ln_w': 1.0 + nrm(19, (DEPTH, GROUP_W), 0.02),
        'rw_ln_b': nrm(20, (DEPTH, GROUP_W), 0.01),
        'ml_ib': nrm(21, (DEPTH, 2, ML_HEADS), 0.1),
        'ml_fb': jnp.linspace(3.0, 6.0, ML_HEADS, dtype=jnp.float32) + nrm(22, (DEPTH, 2, ML_HEADS), 0.1),
        'ml_norm_g': 1.0 + nrm(23, (DEPTH, GROUP_W), 0.02),
        'gd_conv': nrm(24, (DEPTH, GD_CONV, 3 * GROUP_W), GD_CONV ** -0.5),
        'gd_a_log': jnp.log(uni(25, (DEPTH, 2, GD_HEADS), 1.0, 16.0)),
        'gd_dt_bias': dt + jnp.log(-jnp.expm1(-dt)),
        'gd_norm_g': 1.0 + nrm(27, (DEPTH, GD_HEAD), 0.02),
        'at_q_norm': 1.0 + nrm(28, (DEPTH, AT_HEAD), 0.02),
        'at_k_norm': 1.0 + nrm(29, (DEPTH, AT_HEAD), 0.02),
        'w_router': nrm(30, (DEPTH, d, N_EXPERTS), d ** -0.5),
        'w_exp1': nrm(31, (DEPTH, N_EXPERTS, d, EXPERT_FF), d ** -0.5),
        'w_exp3': nrm(32, (DEPTH, N_EXPERTS, d, EXPERT_FF), d ** -0.5),
        'w_exp2': nrm(33, (DEPTH, N_EXPERTS, EXPERT_FF, d), EXPERT_FF ** -0.5),
        'final_g': 1.0 + nrm(34, (d,), 0.02),
    }


def reference(x, c, ctx, c_ctx, ada_w, ada_b, norm1_g, norm2_g, w_in, w_out,
              rw_mu, rw_w0, rw_w_up, rw_a0, rw_a_up, rw_g_up, rw_k_k, rw_k_a, rw_r_k, rw_ln_w, rw_ln_b,
              ml_ib, ml_fb, ml_norm_g, gd_conv, gd_a_log, gd_dt_bias, gd_norm_g,
              at_q_norm, at_k_norm, w_router, w_exp1, w_exp3, w_exp2, final_g):
    n_lat = x.shape[1]
    ROWS = n_lat // GRID_W
    row = jnp.broadcast_to(jnp.arange(ROWS)[:, None], (ROWS, GRID_W)).reshape(-1).astype(jnp.float32)
    col = jnp.broadcast_to(jnp.arange(GRID_W)[None, :], (ROWS, GRID_W)).reshape(-1).astype(jnp.float32)
    axis_dim = AT_HEAD // 2
    inv_freq = ROPE_THETA ** (-jnp.arange(0, axis_dim, 2, dtype=jnp.float32) / axis_dim)
    ang_r = row[:, None] * inv_freq[None, :]
    ang_c = col[:, None] * inv_freq[None, :]
    xl, xc = x, ctx
    for layer in range(DEPTH):
        p = {
            'ada_w': ada_w[layer], 'ada_b': ada_b[layer],
            'norm1_g': norm1_g[layer], 'norm2_g': norm2_g[layer],
            'w_in': w_in[layer], 'w_out': w_out[layer],
            'rw_mu': rw_mu[layer], 'rw_w0': rw_w0[layer], 'rw_w_up': rw_w_up[layer],
            'rw_a0': rw_a0[layer], 'rw_a_up': rw_a_up[layer], 'rw_g_up': rw_g_up[layer],
            'rw_k_k': rw_k_k[layer], 'rw_k_a': rw_k_a[layer], 'rw_r_k': rw_r_k[layer],
            'rw_ln_w': rw_ln_w[layer], 'rw_ln_b': rw_ln_b[layer],
            'ml_ib': ml_ib[layer], 'ml_fb': ml_fb[layer], 'ml_norm_g': ml_norm_g[layer],
            'gd_conv': gd_conv[layer], 'gd_a_log': gd_a_log[layer], 'gd_dt_bias': gd_dt_bias[layer],
            'gd_norm_g': gd_norm_g[layer],
            'at_q_norm': at_q_norm[layer], 'at_k_norm': at_k_norm[layer],
            'w_router': w_router[layer], 'w_exp1': w_exp1[layer], 'w_exp3': w_exp3[layer],
            'w_exp2': w_exp2[layer],
        }
        xl, xc = trunk_layer(xl, xc, c, c_ctx, p, ang_r, ang_c, layer == DEPTH - 1)
    return rms_norm(xl, final_g)
```

```python
import math
from contextlib import ExitStack

import numpy as np
import concourse.bass as bass
import concourse.mybir as mybir
from concourse.bass_utils import run_bass_kernel_spmd

F32 = mybir.dt.float32
U32 = mybir.dt.uint32
I32 = mybir.dt.int32
AF = mybir.ActivationFunctionType
ALU = mybir.AluOpType
AX = mybir.AxisListType

NCORES = 8
D = 2048
B = 2
SEQ = 4096
CTX = 256
DEPTH = 2
GW = 512
RW_COLS = 3 * GW + 64 + 64 + 128
ML_COLS = 4 * GW + 16
GD_COLS = 4 * GW + 16
AT_COLS = 1024
N_IN = RW_COLS + ML_COLS + GD_COLS + AT_COLS
NE = 16
EPS = 1e-6


class KB:
    NDSEM = 8

    def __init__(self):
        self.nc = bass.Bass("TRN2", target_bir_lowering=False)
        self.es = ExitStack()
        nc = self.nc
        self.eng = {"pe": nc.tensor, "dve": nc.vector, "act": nc.scalar, "pool": nc.gpsimd, "sp": nc.sync}
        self.sem = {e: self.es.enter_context(nc.semaphore("s_" + e)) for e in self.eng}
        self.cnt = {e: 0 for e in self.eng}
        self.waited = {e: {} for e in self.eng}
        self.dsem = {}
        self.dcnt = {}
        self.dnext = {}
        for q in ("sp", "pool", "act"):
            self.dsem[q] = [self.es.enter_context(nc.semaphore(f"d_{q}{i}")) for i in range(self.NDSEM)]
            self.dcnt[q] = [0] * self.NDSEM
            self.dnext[q] = 0
        self.last_w = {}
        self.readers = {}
        self.excl = set()
        self.ninst = 0
        self.out_tokens = []

    def sb(self, name, shape, dt=F32):
        return self.es.enter_context(self.nc.sbuf_tensor(name, list(shape), dt))

    def ps(self, name, shape, dt=F32):
        self.excl.add(name)
        return self.es.enter_context(self.nc.psum_tensor(name, list(shape), dt))

    def dram_in(self, name, shape, dt=F32):
        return self.nc.dram_tensor(name, list(shape), dt, kind="ExternalInput").ap()

    def dram_out(self, name, shape, dt=F32):
        return self.nc.dram_tensor(name, list(shape), dt, kind="ExternalOutput").ap()

    def _wait(self, e, tok):
        if tok is None:
            return
        kind, key, val = tok
        w = self.waited[e]
        k = (kind, key if kind == "c" else id(key))
        if w.get(k, 0) >= val:
            return
        w[k] = val
        sem = self.sem[key] if kind == "c" else key
        self.eng[e].wait_ge(sem, val)

    def _deps(self, e, reads, writes):
        deps = []
        for k in reads:
            t = self.last_w.get(k)
            if t is not None:
                deps.append(t)
        for k in writes:
            t = self.last_w.get(k)
            if t is not None:
                deps.append(t)
            deps.extend(self.readers.get(k, ()))
        for t in deps:
            if e == "pe" and t[0] == "c" and t[1] == "pe":
                continue
            self._wait(e, t)

    def _record(self, tok, reads, writes):
        for k in reads:
            self.readers.setdefault(k, []).append(tok)
        for k in writes:
            self.last_w[k] = tok
            self.readers[k] = []

    def _x(self, reads, writes):
        ex = [k for k in reads if k in self.excl]
        if ex:
            reads = [k for k in reads if k not in self.excl]
            writes = list(writes) + ex
        return reads, writes

    def op(self, e, fn, reads=(), writes=()):
        reads, writes = self._x(reads, writes)
        self._deps(e, reads, writes)
        inst = fn(self.eng[e])
        self.cnt[e] += 1
        inst.then_inc(self.sem[e], 1)
        tok = ("c", e, self.cnt[e])
        self._record(tok, reads, writes)
        self.ninst += 1
        return tok

    def dma(self, q, out, in_, reads=(), writes=(), is_output=False, fn=None):
        i = self.dnext[q]
        self.dnext[q] = (i + 1) % self.NDSEM
        sem = self.dsem[q][i]
        if self.dcnt[q][i] > 0:
            self._wait(q, ("d", sem, 16 * self.dcnt[q][i]))
        self._deps(q, reads, writes)
        if fn is None:
            inst = self.eng[q].dma_start(out=out, in_=in_)
        else:
            inst = fn(self.eng[q])
        self.dcnt[q][i] += 1
        inst.then_inc(sem, 16)
        tok = ("d", sem, 16 * self.dcnt[q][i])
        self._record(tok, reads, writes)
        if is_output:
            self.out_tokens.append(tok)
        self.ninst += 1
        return tok

    def finish(self):
        for q in self.dsem:
            for i, sem in enumerate(self.dsem[q]):
                if self.dcnt[q][i] > 0:
                    self._wait("sp", ("d", sem, 16 * self.dcnt[q][i]))
        for e in self.eng:
            if e != "sp" and self.cnt[e] > 0:
                self._wait("sp", ("c", e, self.cnt[e]))
        self.es.close()
        return self.nc


def run(kb_or_nc, in_maps):
    nc = kb_or_nc.finish() if isinstance(kb_or_nc, KB) else kb_or_nc
    res = run_bass_kernel_spmd(nc, in_maps, core_ids=list(range(NCORES)))
    return res.results


def ident(kb, name="ident"):
    t = kb.sb(name, [128, 128])
    kb.op("pool", lambda e: e.memset(t[:], 1.0), writes=[name])
    kb.op("pool", lambda e: e.affine_select(out=t[:], in_=t[:], pattern=[[1, 128]], compare_op=ALU.is_equal,
                                             fill=0.0, base=0, channel_multiplier=-1), reads=[name], writes=[name])
    return t


def tri_mask(kb, name, mode):
    t = kb.sb(name, [128, 128])
    kb.op("pool", lambda e: e.memset(t[:], 1.0), writes=[name])
    if mode == "ones":
        return t
    if mode == "le":
        pat, cm, base, cmp = [[1, 128]], -1, 0, ALU.is_ge
    elif mode == "lt":
        pat, cm, base, cmp = [[1, 128]], -1, 0, ALU.is_gt
    elif mode == "ge":
        pat, cm, base, cmp = [[-1, 128]], 1, 0, ALU.is_ge
    else:
        pat, cm, base, cmp = [[-1, 128]], 1, 0, ALU.is_gt
    kb.op("pool", lambda e: e.affine_select(out=t[:], in_=t[:], pattern=pat, compare_op=cmp, fill=0.0,
                                             base=base, channel_multiplier=cm), reads=[name], writes=[name])
    return t


def build_mod():
    kb = KB()
    NCOL = 3072
    condT = kb.dram_in("condT", [128, 16, 3])
    w = kb.dram_in("w", [D, NCOL])
    bias = kb.dram_in("bias", [3, NCOL])
    out = kb.dram_out("out", [3, NCOL])
    ct = kb.sb("ct", [128, 16, 3])
    sg = kb.sb("sg", [128, 16, 3])
    bt = kb.sb("bt", [3, NCOL])
    ot = kb.sb("ot", [3, NCOL])
    kb.dma("sp", ct[:], condT, writes=["ct"])
    kb.dma("sp", bt[:], bias, writes=["bt"])
    kb.op("act", lambda e: e.activation(out=sg[:], in_=ct[:], func=AF.Sigmoid), reads=["ct"], writes=["sg"])
    kb.op("dve", lambda e: e.tensor_tensor(out=ct[:], in0=ct[:], in1=sg[:], op=ALU.mult), reads=["ct", "sg"], writes=["ct"])
    wv = w.rearrange("(kc p) n -> p kc n", p=128)
    wb = [kb.sb(f"wb{i}", [128, 16, 512]) for i in range(2)]
    pp = [kb.ps(f"pp{i}", [3, 512]) for i in range(2)]
    for nb in range(NCOL // 512):
        wt = wb[nb % 2]
        wk = f"wb{nb % 2}"
        for h in range(2):
            kb.dma("sp", wt[:, h * 8:(h + 1) * 8, :], wv[:, h * 8:(h + 1) * 8, nb * 512:(nb + 1) * 512], writes=[wk + f"h{h}"])
        p = pp[nb % 2]
        pk = f"pp{nb % 2}"
        for kc in range(16):
            kb.op("pe", lambda e, kc=kc: e.matmul(p[:], lhsT=ct[:, kc, :], rhs=wt[:, kc, :], start=(kc == 0), stop=(kc == 15)),
                  reads=["ct", wk + f"h{kc // 8}"], writes=[pk])
        kb.op("dve", lambda e: e.tensor_tensor(out=ot[:, nb * 512:(nb + 1) * 512], in0=p[:], in1=bt[:, nb * 512:(nb + 1) * 512], op=ALU.add),
              reads=[pk, "bt"], writes=["ot"])
    kb.dma("sp", out, ot[:], reads=["ot"], is_output=True)
    return kb


def run_mod(c, c_ctx, ada_w, ada_b):
    cond = np.concatenate([c, c_ctx[None, :]], axis=0).astype(np.float32)
    condT = np.ascontiguousarray(cond.reshape(3, 16, 128).transpose(2, 1, 0))
    maps = []
    for core in range(NCORES):
        l, q = divmod(core, 4)
        sl = slice(q * 3072, (q + 1) * 3072)
        maps.append({"condT": condT, "w": np.ascontiguousarray(ada_w[l][:, sl]),
                     "bias": np.ascontiguousarray(np.broadcast_to(ada_b[l][sl], (3, 3072)))})
    res = run(build_mod(), maps)
    mod = np.zeros((DEPTH, 3, 6 * D), np.float32)
    for core in range(NCORES):
        l, q = divmod(core, 4)
        mod[l][:, q * 3072:(q + 1) * 3072] = res[core]["out"]
    return mod


NT = 9
ROWS = NT * 128


def rms_rstd(kb, x, xk, junk, rstd, key, n=D, eps=EPS):
    kb.op("act", lambda e: e.activation(out=junk, in_=x, func=AF.Square, accum_out=rstd), reads=[xk], writes=["junk", key])
    kb.op("act", lambda e: e.activation(out=rstd, in_=rstd, func=AF.Sqrt, scale=1.0 / n, bias=eps), reads=[key], writes=[key])
    kb.op("dve", lambda e: e.reciprocal(out=rstd, in_=rstd), reads=[key], writes=[key])


def transpose_rows(kb, idt, src, srck, dstT, dstk, pst, pstk, nchunks=16, evac="dve"):
    for c0 in range(0, nchunks, 4):
        n = min(4, nchunks - c0)
        bank = (c0 // 4) % len(pst)
        for i in range(n):
            kb.op("pe", lambda e, i=i: e.transpose(out=pst[bank][:, i, :], in_=src[:, (c0 + i) * 128:(c0 + i + 1) * 128], identity=idt[:]),
                  reads=[srck, "ident"], writes=[pstk[bank]])
        kb.op(evac, lambda e: e.tensor_copy(out=dstT[:, c0:c0 + n, :], in_=pst[bank][:, 0:n, :]), reads=[pstk[bank]], writes=[dstk])


def build_rows(do_combine, mode, NOUT=N_IN):
    kb = KB()
    xin = kb.dram_in("xin", [ROWS, D])
    gvec = kb.dram_in("g", [1, D])
    if do_combine:
        parts = kb.dram_in("parts", [NCORES, ROWS, D])
        m5 = kb.dram_in("m5", [NT, D])
        xout = kb.dram_out("xout", [ROWS, D])
    if mode == "inproj":
        modrows = kb.dram_in("modrows", [NT, 2, D])
        w = kb.dram_in("w", [D, NOUT])
        out = kb.dram_out("out", [ROWS, NOUT])
    else:
        out = kb.dram_out("out", [ROWS, D])

    idt = ident(kb)
    g_bc = kb.sb("g_bc", [128, D])
    kb.dma("sp", g_bc[:], gvec[0, :].partition_broadcast(128), writes=["g_bc"])
    xt = [kb.sb(f"xt{i}", [128, D]) for i in range(2)]
    junk = kb.sb("junk", [128, D])
    rstd = kb.sb("rstd", [128, 2])
    if mode == "inproj":
        sc = kb.sb("sc", [128, D])
        sh = kb.sb("sh", [128, D])
        hT = kb.sb("hT", [128, NT, 16, 128])
        pst = [kb.ps(f"pst{i}", [128, 4, 128]) for i in range(2)]
        pstk = ["pst0", "pst1"]
    if do_combine:
        pt = [kb.sb(f"pt{i}", [128, D]) for i in range(2)]
        m5b = kb.sb("m5b", [128, D])

    for t in range(NT):
        x = xt[t % 2]
        xk = f"xt{t % 2}"
        rows = slice(t * 128, (t + 1) * 128)
        kb.dma("sp", x[:], xin[rows, :], writes=[xk])
        if do_combine:
            acc = junk
            for c in range(NCORES):
                p = pt[c % 2]
                pk = f"pt{c % 2}"
                kb.dma("sp", p[:], parts[c, rows, :], writes=[pk])
                if c == 0:
                    kb.op("pool", lambda e, p=p: e.tensor_copy(out=acc[:], in_=p[:]), reads=[pk], writes=["junk"])
                else:
                    kb.op("pool", lambda e, p=p: e.tensor_tensor(out=acc[:], in0=acc[:], in1=p[:], op=ALU.add), reads=[pk, "junk"], writes=["junk"])
            kb.dma("sp", m5b[:], m5[t, :].partition_broadcast(128), writes=["m5b"])
            kb.op("dve", lambda e: e.tensor_tensor(out=acc[:], in0=acc[:], in1=m5b[:], op=ALU.mult), reads=["junk", "m5b"], writes=["junk"])
            kb.op("dve", lambda e, x=x: e.tensor_tensor(out=x[:], in0=x[:], in1=acc[:], op=ALU.add), reads=["junk", xk], writes=[xk])
            kb.dma("pool", xout[rows, :], x[:], reads=[xk], is_output=True)
        rk = "rstd"
        rms_rstd(kb, x[:], xk, junk[:], rstd[:, 0:1], rk)
        if mode == "inproj":
            kb.dma("sp", sh[:], modrows[t, 0, :].partition_broadcast(128), writes=["sh"])
            kb.dma("sp", sc[:], modrows[t, 1, :].partition_broadcast(128), writes=["sc"])
            kb.op("dve", lambda e: e.scalar_tensor_tensor(out=sc[:], in0=sc[:], scalar=1.0, in1=g_bc[:], op0=ALU.add, op1=ALU.mult),
                  reads=["sc", "g_bc"], writes=["sc"])
            kb.op("dve", lambda e, x=x: e.scalar_tensor_tensor(out=x[:], in0=x[:], scalar=rstd[:, 0:1], in1=sc[:], op0=ALU.mult, op1=ALU.mult),
                  reads=[xk, rk, "sc"], writes=[xk])
            kb.op("dve", lambda e, x=x: e.tensor_tensor(out=x[:], in0=x[:], in1=sh[:], op=ALU.add), reads=[xk, "sh"], writes=[xk])
            transpose_rows(kb, idt, x, xk, hT[:, t], f"hT{t}", pst, pstk)
        else:
            kb.op("dve", lambda e, x=x: e.scalar_tensor_tensor(out=x[:], in0=x[:], scalar=rstd[:, 0:1], in1=g_bc[:], op0=ALU.mult, op1=ALU.mult),
                  reads=[xk, rk, "g_bc"], writes=[xk])
            kb.dma("pool", out[rows, :], x[:], reads=[xk], is_output=True)

    if mode == "inproj":
        NB = 256
        wv = w.rearrange("(kc p) n -> p kc n", p=128)
        wb = [kb.sb(f"wb{i}", [128, 16, NB]) for i in range(2)]
        pp = [kb.ps(f"pp{i}", [128, NB]) for i in range(4)]
        ot = [kb.sb(f"ot{i}", [128, NB]) for i in range(4)]
        nblocks = (NOUT + NB - 1) // NB
        cnt = 0
        for nb in range(nblocks):
            n0 = nb * NB
            nw = min(NB, NOUT - n0)
            wt = wb[nb % 2]
            wk = f"wb{nb % 2}"
            kb.dma("sp", wt[:, :, 0:nw], wv[:, :, n0:n0 + nw], writes=[wk])
            for t in range(NT):
                p = pp[cnt % 4]
                pk = f"pp{cnt % 4}"
                o = ot[cnt % 4]
                ok = f"ot{cnt % 4}"
                cnt += 1
                for kc in range(16):
                    kb.op("pe", lambda e, kc=kc, p=p, wt=wt: e.matmul(p[:, 0:nw], lhsT=hT[:, t, kc, :], rhs=wt[:, kc, 0:nw], start=(kc == 0), stop=(kc == 15)),
                          reads=[f"hT{t}", wk], writes=[pk])
                ev = "act" if cnt % 2 else "dve"
                if ev == "act":
                    kb.op("act", lambda e, p=p, o=o: e.copy(out=o[:, 0:nw], in_=p[:, 0:nw]), reads=[pk], writes=[ok])
                else:
                    kb.op("dve", lambda e, p=p, o=o: e.tensor_copy(out=o[:, 0:nw], in_=p[:, 0:nw]), reads=[pk], writes=[ok])
                kb.dma("pool", out[t * 128:(t + 1) * 128, n0:n0 + nw], o[:, 0:nw], reads=[ok], is_output=True)
    return kb


TOT = B * SEQ + B * CTX


def rows_to_cores(a):
    n = a.shape[1]
    pad = np.zeros((NCORES * ROWS, n), a.dtype)
    pad[:TOT] = a
    return [np.ascontiguousarray(pad[c * ROWS:(c + 1) * ROWS]) for c in range(NCORES)]


def cores_to_rows(lst):
    return np.concatenate(lst, axis=0)[:TOT]


def tile_group(g):
    r = g * 128
    if r < SEQ:
        return 0
    if r < 2 * SEQ:
        return 1
    return 2


def mod_rows_for(modl, idxs):
    outs = []
    for c in range(NCORES):
        a = np.zeros((NT, len(idxs), D), np.float32)
        for t in range(NT):
            g = c * NT + t
            if g * 128 < TOT:
                j = tile_group(g)
                for ii, m in enumerate(idxs):
                    a[t, ii] = modl[j, m * D:(m + 1) * D]
        outs.append(a)
    return outs


class Dplr:
    def __init__(self, kb, dk, dvp, mode, has_delta, tag, consts):
        self.kb, self.dk, self.dvp, self.mode, self.hd, self.tag = kb, dk, dvp, mode, has_delta, tag
        self.c = consts
        t = tag
        sb = lambda n, s: kb.sb(f"{t}_{n}", s)
        self.r, self.kap, self.a, self.kt = sb("r", [128, dk]), sb("kap", [128, dk]), sb("a", [128, dk]), sb("kt", [128, dk])
        self.v, self.lw = sb("v", [128, dvp]), sb("lw", [128, dk])
        self.M = sb("M", [dk, dvp])
        self.ecw, self.encw, self.ecwx, self.el = sb("ecw", [128, dk]), sb("encw", [128, dk]), sb("ecwx", [128, dk]), sb("el", [128, dk])
        self.r0, self.k0 = sb("r0", [128, dk]), sb("k0", [128, dk])
        self.ap, self.ktp = sb("ap", [128, dk]), sb("ktp", [128, dk])
        self.aL, self.ktL = sb("aL", [128, dk]), sb("ktL", [128, dk])
        self.kr0T = sb("kr0T", [dk, 2, 128])
        self.krT = sb("krT", [dk, 2, 128])
        self.apT, self.ktpT = sb("apT", [dk, 128]), sb("ktpT", [dk, 128])
        self.AR, self.BR = sb("AR", [128, 256]), sb("BR", [128, 256])
        self.E2 = sb("E2", [128, 256])
        self.Mm = [sb(f"Mm{i}", [128, 128]) for i in range(2)]
        self.Nm = [sb(f"Nm{i}", [128, 128]) for i in range(2)]
        self.P = sb("P", [128, 128])
        self.negG, self.U = sb("negG", [128, dvp]), sb("U", [128, dvp])
        self.ecl = sb("ecl", [dk, 1])
        self.cwc = sb("cwc", [128, 1])

    def k(self, n):
        return f"{self.tag}_{n}"

    def init_state(self):
        self.kb.op("pool", lambda e: e.memset(self.M[:], 0.0), writes=[self.k("M")])

    def step(self, d, bank, y_cb, stop=99):
        kb, dk, dvp, k, c = self.kb, self.dk, self.dvp, self.k, self.c
        TI, TS = (c["le"], c["lt"]) if d == "f" else (c["ge"], c["gt"])
        TIk, TSk = ("m_le", "m_lt") if d == "f" else ("m_ge", "m_gt")
        mask2, mask2k = (c["mask2f"], "mask2f") if d == "f" else (c["mask2b"], "mask2b")
        tri2, tri2k = (c["tri2f"], "tri2f") if d == "f" else (c["tri2b"], "tri2b")
        b0, b0k = bank["b0"]
        b1, b1k = bank["b1"]
        b2, b2k = bank["b2"]
        b3, b3k = bank["b3"]
        b4, b4k = bank["b4"]
        b5, b5k = bank["b5"]
        b6, b6k = bank["b6"]
        b7, b7k = bank["b7"]
        MUL, ADD, SUB = ALU.mult, ALU.add, ALU.subtract
        kb.op("pe", lambda e: e.matmul(b0[:, 0:dk], lhsT=TI[:], rhs=self.lw[:], start=True, stop=True), reads=[TIk, k("lw")], writes=[b0k])
        kb.op("pe", lambda e: e.matmul(b0[:, dk:2 * dk], lhsT=c["ones"][:], rhs=self.lw[:], start=True, stop=True), reads=["m_ones", k("lw")], writes=[b0k])
        kb.op("pe", lambda e: e.matmul(b0[0:dk, 2 * dk:2 * dk + 1], lhsT=self.lw[:], rhs=c["ones"][:, 0:1], start=True, stop=True), reads=["m_ones", k("lw")], writes=[b0k])
        cw, cwl = b0[:, 0:dk], b0[:, dk:2 * dk]
        kb.op("act", lambda e: e.activation(out=self.ecw[:], in_=cw, func=AF.Exp), reads=[b0k], writes=[k("ecw")])
        kb.op("act", lambda e: e.activation(out=self.ecl[:], in_=b0[0:dk, 2 * dk:2 * dk + 1], func=AF.Exp), reads=[b0k], writes=[k("ecl")])
        kb.op("dve", lambda e: e.tensor_tensor(out=self.ecwx[:], in0=cw, in1=self.lw[:], op=SUB), reads=[b0k, k("lw")], writes=[k("ecwx")])
        kb.op("act", lambda e: e.activation(out=self.ecwx[:], in_=self.ecwx[:], func=AF.Exp), reads=[k("ecwx")], writes=[k("ecwx")])
        kb.op("dve", lambda e: e.tensor_copy(out=self.el[:], in_=cw), reads=[b0k], writes=[k("el")])
        kb.op("dve", lambda e: e.tensor_tensor(out=self.el[:], in0=cwl, in1=self.el[:], op=SUB), reads=[b0k, k("el")], writes=[k("el")])
        kb.op("act", lambda e: e.activation(out=self.el[:], in_=self.el[:], func=AF.Exp), reads=[k("el")], writes=[k("el")])
        kb.op("dve", lambda e: e.tensor_tensor(out=self.r0[:], in0=self.r[:], in1=self.ecw[:], op=MUL), reads=[k("r"), k("ecw")], writes=[k("r0")])
        kb.op("dve", lambda e: e.tensor_tensor(out=self.k0[:], in0=self.kap[:], in1=self.ecwx[:], op=MUL), reads=[k("kap"), k("ecwx")], writes=[k("k0")])
        kb.op("pool", lambda e: e.tensor_tensor(out=self.ktL[:], in0=self.kt[:], in1=self.el[:], op=MUL), reads=[k("kt"), k("el")], writes=[k("ktL")])
        if self.hd:
            kb.op("pool", lambda e: e.tensor_tensor(out=self.aL[:], in0=self.a[:], in1=self.el[:], op=MUL), reads=[k("a"), k("el")], writes=[k("aL")])
        if self.mode == "V":
            kb.op("act", lambda e: e.activation(out=self.encw[:], in_=cw, func=AF.Exp, scale=-1.0), reads=[b0k], writes=[k("encw")])
            kb.op("dve", lambda e: e.tensor_tensor(out=self.ktp[:], in0=self.kt[:], in1=self.encw[:], op=MUL), reads=[k("kt"), k("encw")], writes=[k("ktp")])
            if self.hd:
                kb.op("dve", lambda e: e.tensor_tensor(out=self.ap[:], in0=self.a[:], in1=self.encw[:], op=MUL), reads=[k("a"), k("encw")], writes=[k("ap")])
            ktp_src, ktp_k, ap_src, ap_k = self.ktp, k("ktp"), self.ap, k("ap")
        else:
            kb.op("dve", lambda e: e.tensor_copy(out=self.cwc[:], in_=b0[:, 0:1]), reads=[b0k], writes=[k("cwc")])
            ktp_src, ktp_k, ap_src, ap_k = self.kt, k("kt"), self.a, k("a")
        if stop < 3:
            return
        idt = c["ident"]

        def tr(slot, src, srck):
            kb.op("pe", lambda e: e.transpose(out=b1[0:dk, slot * 128:(slot + 1) * 128], in_=src[:], identity=idt[:]), reads=[srck, "ident"], writes=[b1k])
        tr(0, self.k0, k("k0"))
        tr(1, self.r0, k("r0"))
        tr(2, ktp_src, ktp_k)
        if self.hd:
            tr(3, ap_src, ap_k)
        kb.op("dve", lambda e: e.tensor_copy(out=self.kr0T[:].rearrange("p a b -> p (a b)"), in_=b1[0:dk, 0:256]), reads=[b1k], writes=[k("kr0T")])
        kb.op("dve", lambda e: e.tensor_copy(out=self.ktpT[:], in_=b1[0:dk, 256:384]), reads=[b1k], writes=[k("ktpT")])
        if self.hd:
            kb.op("dve", lambda e: e.tensor_copy(out=self.apT[:], in_=b1[0:dk, 384:512]), reads=[b1k], writes=[k("apT")])
        if self.mode == "S":
            tr(0, self.kap, k("kap"))
            tr(1, self.r, k("r"))
            kb.op("dve", lambda e: e.tensor_copy(out=self.krT[:].rearrange("p a b -> p (a b)"), in_=b1[0:dk, 0:256]), reads=[b1k], writes=[k("krT")])
            rhs2, rhs2k = self.krT, k("krT")
        else:
            rhs2, rhs2k = self.kr0T, k("kr0T")
        rhs2f = rhs2[:].rearrange("p a b -> p (a b)")
        if stop < 4:
            return
        if self.hd:
            kb.op("pe", lambda e: e.matmul(b2[:, 0:256], lhsT=self.apT[:], rhs=rhs2f, start=True, stop=True), reads=[k("apT"), rhs2k], writes=[b2k])
        kb.op("pe", lambda e: e.matmul(b2[:, 256:512], lhsT=self.ktpT[:], rhs=rhs2f, start=True, stop=True), reads=[k("ktpT"), rhs2k], writes=[b2k])
        if self.mode == "S":
            kb.op("pe", lambda e: e.matmul(b3[:, 0:256], lhsT=self.lw[:], rhs=tri2[:], start=True, stop=True), reads=[k("lw"), tri2k], writes=[b3k])
            kb.op("dve", lambda e: e.tensor_scalar(out=self.E2[:], in0=b3[:, 0:256], scalar1=self.cwc[:, 0:1], scalar2=0.0, op0=SUB, op1=ALU.min),
                  reads=[b3k, k("cwc")], writes=[k("E2")])
            kb.op("act", lambda e: e.activation(out=self.E2[:], in_=self.E2[:], func=AF.Exp), reads=[k("E2")], writes=[k("E2")])
            kb.op("pool", lambda e: e.tensor_tensor(out=self.E2[:], in0=self.E2[:], in1=mask2[:], op=MUL), reads=[k("E2"), mask2k], writes=[k("E2")])
            mm2, mm2k = self.E2, k("E2")
        else:
            mm2, mm2k = mask2, mask2k
        if self.hd:
            kb.op("dve", lambda e: e.tensor_tensor(out=self.AR[:], in0=b2[:, 0:256], in1=mm2[:], op=MUL), reads=[b2k, mm2k], writes=[k("AR")])
        kb.op("dve", lambda e: e.tensor_tensor(out=self.BR[:], in0=b2[:, 256:512], in1=mm2[:], op=MUL), reads=[b2k, mm2k], writes=[k("BR")])
        Mk = k("M")
        if stop < 5:
            return
        if self.hd:
            M0, N0 = self.Mm[0], self.Nm[0]
            kb.op("dve", lambda e: e.tensor_scalar(out=M0[:], in0=self.AR[:, 0:128], scalar1=-1.0, scalar2=None, op0=MUL), reads=[k("AR")], writes=[k("Mm0")])
            kb.op("pe", lambda e: e.transpose(out=b4[:, 0:128], in_=M0[:], identity=idt[:]), reads=[k("Mm0"), "ident"], writes=[b4k])
            kb.op("act", lambda e: e.copy(out=N0[:], in_=b4[:, 0:128]), reads=[b4k], writes=[k("Nm0")])
            kb.op("pool", lambda e: e.tensor_tensor(out=self.P[:], in0=M0[:], in1=idt[:], op=ADD), reads=[k("Mm0"), "ident"], writes=[k("P")])
            cur = 0
            for lvl in range(6):
                Mc, Nc, Mn, Nn = self.Mm[cur], self.Nm[cur], self.Mm[1 - cur], self.Nm[1 - cur]
                Mck, Nck, Mnk, Nnk = k(f"Mm{cur}"), k(f"Nm{cur}"), k(f"Mm{1 - cur}"), k(f"Nm{1 - cur}")
                last = lvl == 5
                if not last:
                    kb.op("pe", lambda e, Mc=Mc, Nc=Nc: e.matmul(b4[:, 0:128], lhsT=Nc[:], rhs=Mc[:], start=True, stop=True), reads=[Mck, Nck], writes=[b4k])
                kb.op("pe", lambda e, Mc=Mc, Nc=Nc: e.matmul(b4[:, 128:256], lhsT=Mc[:], rhs=Nc[:], start=True, stop=True), reads=[Mck, Nck], writes=[b4k])
                if not last:
                    kb.op("dve", lambda e, Mn=Mn: e.tensor_copy(out=Mn[:], in_=b4[:, 0:128]), reads=[b4k], writes=[Mnk])
                kb.op("act", lambda e, Nn=Nn: e.copy(out=Nn[:], in_=b4[:, 128:256]), reads=[b4k], writes=[Nnk])
                kb.op("pe", lambda e, Nn=Nn: e.matmul(b4[:, 256:384], lhsT=Nn[:], rhs=self.P[:], start=True, stop=True), reads=[Nnk, k("P")], writes=[b4k])
                kb.op("dve", lambda e: e.tensor_tensor(out=self.P[:], in0=b4[:, 256:384], in1=self.P[:], op=ADD), reads=[b4k, k("P")], writes=[k("P")])
                cur = 1 - cur
            kb.op("pe", lambda e: e.matmul(b5[:, 0:dvp], lhsT=self.kr0T[:, 0, :], rhs=self.M[:], start=True, stop=False), reads=[k("kr0T"), Mk], writes=[b5k])
            kb.op("pe", lambda e: e.matmul(b5[:, 0:dvp], lhsT=self.BR[:, 0:128], rhs=self.v[:], start=False, stop=True), reads=[k("BR"), k("v")], writes=[b5k])
            kb.op("dve", lambda e: e.tensor_scalar(out=self.negG[:], in0=b5[:, 0:dvp], scalar1=-1.0, scalar2=None, op0=MUL), reads=[b5k], writes=[k("negG")])
            kb.op("pe", lambda e: e.matmul(b5[:, 256:256 + dvp], lhsT=self.P[:], rhs=self.negG[:], start=True, stop=True), reads=[k("P"), k("negG")], writes=[b5k])
            kb.op("act", lambda e: e.copy(out=self.U[:], in_=b5[:, 256:256 + dvp]), reads=[b5k], writes=[k("U")])
        if stop < 7:
            return
        kb.op("pe", lambda e: e.matmul(b6[:, 0:dvp], lhsT=self.kr0T[:, 1, :], rhs=self.M[:], start=True, stop=False), reads=[k("kr0T"), Mk], writes=[b6k])
        if self.hd:
            kb.op("pe", lambda e: e.matmul(b6[:, 0:dvp], lhsT=self.AR[:, 128:256], rhs=self.U[:], start=False, stop=False), reads=[k("AR"), k("U")], writes=[b6k])
        kb.op("pe", lambda e: e.matmul(b6[:, 0:dvp], lhsT=self.BR[:, 128:256], rhs=self.v[:], start=False, stop=True), reads=[k("BR"), k("v")], writes=[b6k])
        y_cb(b6[:, 0:dvp], b6k)
        if stop < 8:
            return
        if self.hd:
            kb.op("pe", lambda e: e.matmul(b7[0:dk, 0:dvp], lhsT=self.aL[:], rhs=self.U[:], start=True, stop=False), reads=[k("aL"), k("U")], writes=[b7k])
        kb.op("pe", lambda e: e.matmul(b7[0:dk, 0:dvp], lhsT=self.ktL[:], rhs=self.v[:], start=(not self.hd), stop=True), reads=[k("ktL"), k("v")], writes=[b7k])
        kb.op("dve", lambda e: e.scalar_tensor_tensor(out=self.M[:], in0=self.M[:], scalar=self.ecl[:, 0:1], in1=b7[0:dk, 0:dvp], op0=MUL, op1=ADD),
              reads=[Mk, k("ecl"), b7k], writes=[Mk])


def dplr_consts(kb):
    c = {"ident": ident(kb)}
    for m in ("le", "lt", "ge", "gt", "ones"):
        c[m] = tri_mask(kb, "m_" + m, m)
    for d, (ms, mi) in (("f", ("lt", "le")), ("b", ("gt", "ge"))):
        t = kb.sb("mask2" + d, [128, 256])
        kb.op("pool", lambda e, t=t, ms=ms: e.tensor_copy(out=t[:, 0:128], in_=c[ms][:]), reads=["m_" + ms], writes=["mask2" + d])
        kb.op("pool", lambda e, t=t, mi=mi: e.tensor_copy(out=t[:, 128:256], in_=c[mi][:]), reads=["m_" + mi], writes=["mask2" + d])
        c["mask2" + d] = t
        c["tri2" + d] = t
    return c


def dplr_banks(kb):
    return {f"b{i}": (kb.ps(f"bank{i}", [128, 512]), f"bank{i}") for i in range(8)}


def build_dplr_test(T, dk, dvp, mode, has_delta):
    kb = KB()
    nch = T // 128
    ins = {n: kb.dram_in(n, [T, dk]) for n in ("r", "kap", "a", "kt", "lw")}
    ins["v"] = kb.dram_in("v", [T, dvp])
    outs = {d: kb.dram_out("y" + d, [T, dvp]) for d in "fb"}
    c = dplr_consts(kb)
    banks = dplr_banks(kb)
    sc = Dplr(kb, dk, dvp, mode, has_delta, "s", c)
    yo = kb.sb("yo", [128, dvp])
    for d in "fb":
        sc.init_state()
        order = range(nch) if d == "f" else range(nch - 1, -1, -1)
        for ci in order:
            rows = slice(ci * 128, (ci + 1) * 128)
            for n in ("r", "kap", "a", "kt", "lw", "v"):
                kb.dma("sp", getattr(sc, n)[:], ins[n][rows, :], writes=[sc.k(n)])

            def cb(yp, ypk):
                kb.op("dve", lambda e: e.tensor_copy(out=yo[:], in_=yp), reads=[ypk], writes=["yo"])
                kb.dma("pool", outs[d][rows, :], yo[:], reads=["yo"], is_output=True)
            sc.step(d, banks, cb)
    return kb


def TT(kb, e, out, in0, in1, op, reads, writes):
    return kb.op(e, lambda g: g.tensor_tensor(out=out, in0=in0, in1=in1, op=op), reads=reads, writes=writes)


def TS(kb, e, out, in0, s1, s2, op0, op1, reads, writes):
    if op1 is None:
        return kb.op(e, lambda g: g.tensor_scalar(out=out, in0=in0, scalar1=s1, scalar2=None, op0=op0), reads=reads, writes=writes)
    return kb.op(e, lambda g: g.tensor_scalar(out=out, in0=in0, scalar1=s1, scalar2=s2, op0=op0, op1=op1), reads=reads, writes=writes)


def STT(kb, out, in0, scalar, in1, op0, op1, reads, writes):
    return kb.op("dve", lambda g: g.scalar_tensor_tensor(out=out, in0=in0, scalar=scalar, in1=in1, op0=op0, op1=op1), reads=reads, writes=writes)


def ACT(kb, out, in_, func, reads, writes, **kw):
    return kb.op("act", lambda g: g.activation(out=out, in_=in_, func=func, **kw), reads=reads, writes=writes)


def CP(kb, e, out, in_, reads, writes):
    if e == "act":
        return kb.op("act", lambda g: g.copy(out=out, in_=in_), reads=reads, writes=writes)
    return kb.op(e, lambda g: g.tensor_copy(out=out, in_=in_), reads=reads, writes=writes)


def sumsq_rs(kb, x, xk, junk, junkk, out, outk, n, eps, mean=True):
    ACT(kb, junk, x, AF.Square, [xk], [junkk, outk], accum_out=out)
    ACT(kb, out, out, AF.Sqrt, [outk], [outk], scale=(1.0 / n if mean else 1.0), bias=eps)
    kb.op("dve", lambda g: g.reciprocal(out=out, in_=out), reads=[outk], writes=[outk])


def layernorm_rs(kb, x, xk, st, stk, n, eps):
    kb.op("dve", lambda g: g.bn_stats(out=st[:, 2:8], in_=x), reads=[xk], writes=[stk])
    kb.op("dve", lambda g: g.bn_aggr(out=st[:, 0:2], in_=st[:, 2:8]), reads=[stk], writes=[stk])
    ACT(kb, st[:, 1:2], st[:, 1:2], AF.Sqrt, [stk], [stk], bias=eps, scale=1.0)
    kb.op("dve", lambda g: g.reciprocal(out=st[:, 1:2], in_=st[:, 1:2]), reads=[stk], writes=[stk])


def shared_yacc(kb):
    if not hasattr(kb, "_yacc"):
        kb._yacc = kb.sb("yacc", [128, (CTX + SEQ) // 128, 128])
    return kb._yacc


LSEQ = CTX + SEQ
NCH = LSEQ // 128
FWD_ORDER = list(range(NCH))
BWD_ORDER = [1, 0] + list(range(NCH - 1, 1, -1))


def emit_rwkv(kb, c, banks):
    MUL, ADD, SUB = ALU.mult, ALU.add, ALU.subtract
    X = {n: kb.dram_in("rw_" + n, [LSEQ, 640]) for n in ("cur", "prev", "next")}
    cst_d = kb.dram_in("rw_cst", [128, 2 * 640 + 128 * 9])
    wup_d = kb.dram_in("rw_wup", [2, 64, 128])
    aup_d = kb.dram_in("rw_aup", [2, 64, 128])
    gup_d = kb.dram_in("rw_gup", [128, 128])
    out = kb.dram_out("rw_out", [LSEQ, 128])
    cst = kb.sb("rw_cst_s", [128, 2 * 640 + 128 * 9])
    kb.dma("sp", cst[:], cst_d, writes=["rw_cst"])
    mu0, mu1 = cst[:, 0:640], cst[:, 640:1280]
    o = 1280
    k_k, k_a, r_k, ln_w, ln_b = (cst[:, o + i * 128:o + (i + 1) * 128] for i in range(5))
    w0 = [cst[:, o + (5 + d) * 128:o + (6 + d) * 128] for d in range(2)]
    a0 = [cst[:, o + (7 + d) * 128:o + (8 + d) * 128] for d in range(2)]
    wup = kb.sb("rw_wup_s", [64, 2, 128])
    aup = kb.sb("rw_aup_s", [64, 2, 128])
    gup = kb.sb("rw_gup_s", [128, 128])
    kb.dma("sp", wup[:], wup_d.rearrange("d r n -> r d n"), writes=["rw_wup"])
    kb.dma("sp", aup[:], aup_d.rearrange("d r n -> r d n"), writes=["rw_aup"])
    kb.dma("sp", gup[:], gup_d, writes=["rw_gup"])
    cur, prv, nxt = kb.sb("rw_cur_s", [128, 640]), kb.sb("rw_prv", [128, 640]), kb.sb("rw_nxt", [128, 640])
    kkp, kk = kb.sb("rw_kkp", [128, 128]), kb.sb("rw_kk", [128, 128])
    junk = kb.sb("rw_junk", [128, 128])
    st = kb.sb("rw_st", [128, 8])
    twd = kb.sb("rw_twd", [128, 128])
    twdT = kb.sb("rw_twdT", [64, 2, 128])
    sg = kb.sb("rw_sg", [128, 128])
    sgT = kb.sb("rw_sgT", [128, 128])
    lwt, at, ktt, alt = kb.sb("rw_lw", [128, 128]), kb.sb("rw_a", [128, 128]), kb.sb("rw_kt", [128, 128]), kb.sb("rw_al", [128, 128])
    gt = kb.sb("rw_g", [128, 128])
    bon = kb.sb("rw_bon", [128, 4])
    yacc = shared_yacc(kb)
    yn = kb.sb("rw_yn", [128, 128])
    sc = [Dplr(kb, 64, 64, "V", True, f"rw{h}", c) for h in range(2)]
    b1, b1k = banks["b1"]
    b3, b3k = banks["b3"]
    idt = c["ident"]

    def prep_common(ci):
        rows = slice(ci * 128, (ci + 1) * 128)
        kb.dma("sp", cur[:], X["cur"][rows, :], writes=["rw_cur"])
        kb.dma("sp", prv[:], X["prev"][rows, :], writes=["rw_prv"])
        kb.dma("sp", nxt[:], X["next"][rows, :], writes=["rw_nxt"])
        TT(kb, "dve", prv[:], prv[:], cur[:], SUB, ["rw_prv", "rw_cur"], ["rw_prv"])
        TT(kb, "pool", nxt[:], nxt[:], cur[:], SUB, ["rw_nxt", "rw_cur"], ["rw_nxt"])
        TT(kb, "dve", prv[:], prv[:], mu0, MUL, ["rw_prv", "rw_cst"], ["rw_prv"])
        TT(kb, "pool", nxt[:], nxt[:], mu1, MUL, ["rw_nxt", "rw_cst"], ["rw_nxt"])
        TT(kb, "dve", cur[:], cur[:], prv[:], ADD, ["rw_prv", "rw_cur"], ["rw_cur"])
        TT(kb, "dve", cur[:], cur[:], nxt[:], ADD, ["rw_nxt", "rw_cur"], ["rw_cur"])
        TT(kb, "dve", kkp[:], cur[:, 128:256], k_k, MUL, ["rw_cur", "rw_cst"], ["rw_kkp"])
        for h in range(2):
            ACT(kb, junk[:, 0:64], kkp[:, h * 64:(h + 1) * 64], AF.Square, ["rw_kkp"], ["rw_junk", "rw_st"], accum_out=st[:, h:h + 1])
        ACT(kb, st[:, 0:2], st[:, 0:2], AF.Sqrt, ["rw_st"], ["rw_st"], bias=1e-6, scale=1.0)
        kb.op("dve", lambda g: g.reciprocal(out=st[:, 0:2], in_=st[:, 0:2]), reads=["rw_st"], writes=["rw_st"])
        for h in range(2):
            TS(kb, "dve", kk[:, h * 64:(h + 1) * 64], kkp[:, h * 64:(h + 1) * 64], st[:, h:h + 1], None, MUL, None, ["rw_kkp", "rw_st"], ["rw_kk"])
        ACT(kb, twd[:, 0:64], cur[:, 384:448], AF.Tanh, ["rw_cur"], ["rw_twd"])
        CP(kb, "pool", twd[:, 64:128], cur[:, 448:512], ["rw_cur"], ["rw_twd"])
        for i in range(2):
            kb.op("pe", lambda g, i=i: g.transpose(out=b1[0:64, i * 128:(i + 1) * 128], in_=twd[:, i * 64:(i + 1) * 64], identity=idt[:]),
                  reads=["rw_twd", "ident"], writes=[b1k])
        CP(kb, "dve", twdT[:].rearrange("p a b -> p (a b)"), b1[0:64, 0:256], [b1k], ["rw_twdT"])

    def prep_dir(d):
        kb.op("pe", lambda g: g.matmul(b3[:, 0:128], lhsT=twdT[:, 0, :], rhs=wup[:, d, :], start=True, stop=True), reads=["rw_twdT", "rw_wup"], writes=[b3k])
        kb.op("pe", lambda g: g.matmul(b3[:, 128:256], lhsT=twdT[:, 1, :], rhs=aup[:, d, :], start=True, stop=True), reads=["rw_twdT", "rw_aup"], writes=[b3k])
        TT(kb, "dve", lwt[:], b3[:, 0:128], w0[d], ADD, [b3k, "rw_cst"], ["rw_lw"])
        TT(kb, "dve", at[:], b3[:, 128:256], a0[d], ADD, [b3k, "rw_cst"], ["rw_a"])
        ACT(kb, lwt[:], lwt[:], AF.Sigmoid, ["rw_lw"], ["rw_lw"])
        ACT(kb, at[:], at[:], AF.Sigmoid, ["rw_a"], ["rw_a"])
        TS(kb, "pool", lwt[:], lwt[:], -math.exp(-0.5), None, MUL, None, ["rw_lw"], ["rw_lw"])
        STT(kb, ktt[:], at[:], -1.0, k_a, ADD, MUL, ["rw_a", "rw_cst"], ["rw_kt"])
        STT(kb, ktt[:], ktt[:], 1.0, cur[:, 128:256], ADD, MUL, ["rw_kt", "rw_cur"], ["rw_kt"])

    def run_dir(d, order):
        for s in sc:
            s.init_state()
        for ci in order:
            prep_common(ci)
            prep_dir(d)
            TT(kb, "pool", alt[:], kk[:], at[:], MUL, ["rw_kk", "rw_a"], ["rw_al"])
            for h in range(2):
                s = sc[h]
                hs = slice(h * 64, (h + 1) * 64)
                CP(kb, "pool", s.r[:], cur[:, hs], ["rw_cur"], [s.k("r")])
                CP(kb, "pool", s.v[:], cur[:, 256 + h * 64:256 + (h + 1) * 64], ["rw_cur"], [s.k("v")])
                CP(kb, "pool", s.kap[:], kk[:, hs], ["rw_kk"], [s.k("kap")])
                CP(kb, "pool", s.a[:], alt[:, hs], ["rw_al"], [s.k("a")])
                CP(kb, "pool", s.kt[:], ktt[:, hs], ["rw_kt"], [s.k("kt")])
                CP(kb, "pool", s.lw[:], lwt[:, hs], ["rw_lw"], [s.k("lw")])

                def cb(yp, ypk, h=h, ci=ci):
                    dst = yacc[:, ci, h * 64:(h + 1) * 64]
                    if d == 0:
                        CP(kb, "act", dst, yp, [ypk], ["yacc"])
                    else:
                        TT(kb, "dve", dst, yp, dst, ADD, [ypk, "yacc"], ["yacc"])
                s.step("f" if d == 0 else "b", banks, cb)

    run_dir(0, FWD_ORDER)
    run_dir(1, BWD_ORDER)
    for ci in range(NCH):
        prep_common(ci)
        ACT(kb, sg[:], cur[:, 512:640], AF.Sigmoid, ["rw_cur"], ["rw_sg"])
        kb.op("pe", lambda g: g.transpose(out=b1[:, 0:128], in_=sg[:], identity=idt[:]), reads=["rw_sg", "ident"], writes=[b1k])
        CP(kb, "dve", sgT[:], b1[:, 0:128], [b1k], ["rw_sgT"])
        kb.op("pe", lambda g: g.matmul(b1[:, 128:256], lhsT=sgT[:], rhs=gup[:], start=True, stop=True), reads=["rw_sgT", "rw_gup"], writes=[b1k])
        CP(kb, "act", gt[:], b1[:, 128:256], [b1k], ["rw_g"])
        for d in range(2):
            prep_dir(d)
            TT(kb, "dve", junk[:], cur[:, 0:128], ktt[:], MUL, ["rw_cur", "rw_kt"], ["rw_junk"])
            TT(kb, "dve", junk[:], junk[:], r_k, MUL, ["rw_junk", "rw_cst"], ["rw_junk"])
            kb.op("dve", lambda g, d=d: g.tensor_reduce(out=bon[:, 2 * d:2 * d + 2], in_=junk[:].rearrange("p (h n) -> p h n", h=2), axis=AX.X, op=ADD),
                  reads=["rw_junk"], writes=["rw_bon"])
        TT(kb, "dve", bon[:, 0:2], bon[:, 0:2], bon[:, 2:4], ADD, ["rw_bon"], ["rw_bon"])
        for h in range(2):
            hs = slice(h * 64, (h + 1) * 64)
            layernorm_rs(kb, yacc[:, ci, hs], "yacc", st, "rw_st", 64, 64e-5)
            TS(kb, "dve", yn[:, hs], yacc[:, ci, hs], st[:, 0:1], st[:, 1:2], SUB, MUL, ["yacc", "rw_st"], ["rw_yn"])
        TT(kb, "dve", yn[:], yn[:], ln_w, MUL, ["rw_yn", "rw_cst"], ["rw_yn"])
        TT(kb, "dve", yn[:], yn[:], ln_b, ADD, ["rw_yn", "rw_cst"], ["rw_yn"])
        for h in range(2):
            hs = slice(h * 64, (h + 1) * 64)
            STT(kb, yn[:, hs], cur[:, 256 + h * 64:256 + (h + 1) * 64], bon[:, h:h + 1], yn[:, hs], MUL, ADD, ["rw_cur", "rw_bon", "rw_yn"], ["rw_yn"])
        TT(kb, "dve", yn[:], yn[:], gt[:], MUL, ["rw_yn", "rw_g"], ["rw_yn"])
        kb.dma("pool", out[ci * 128:(ci + 1) * 128, :], yn[:], reads=["rw_yn"], is_output=True)


def seq_rows(pl_all, b):
    lat = pl_all[b * SEQ:(b + 1) * SEQ]
    cx = pl_all[2 * SEQ + b * CTX:2 * SEQ + (b + 1) * CTX]
    return cx, lat


def shifted(cx, lat, sh):
    def s(x):
        o = np.zeros_like(x)
        if sh < 0:
            o[1:] = x[:-1]
        else:
            o[:-1] = x[1:]
        return o
    return np.concatenate([s(cx), s(lat)], 0)


def rep(v):
    return np.ascontiguousarray(np.broadcast_to(np.asarray(v, np.float32).reshape(1, -1), (128, np.asarray(v).size)))


def rwkv_inputs(pl_all, p, b, j):
    cx, lat = seq_rows(pl_all[:, 0:RW_COLS], b)
    cs = slice(j * 128, (j + 1) * 128)
    cols = np.r_[np.arange(j * 128, (j + 1) * 128), GW + np.arange(j * 128, (j + 1) * 128), 2 * GW + np.arange(j * 128, (j + 1) * 128),
                 np.arange(3 * GW, 3 * GW + 256)]
    m = {}
    m["rw_cur"] = np.ascontiguousarray(np.concatenate([cx, lat], 0)[:, cols])
    m["rw_prev"] = np.ascontiguousarray(shifted(cx, lat, -1)[:, cols])
    m["rw_next"] = np.ascontiguousarray(shifted(cx, lat, +1)[:, cols])
    cst = [rep(p["rw_mu"][0][cols]), rep(p["rw_mu"][1][cols]), rep(p["rw_k_k"][cs]), rep(p["rw_k_a"][cs]),
           rep(p["rw_r_k"].reshape(-1)[cs]), rep(p["rw_ln_w"][cs]), rep(p["rw_ln_b"][cs]),
           rep(p["rw_w0"][0][cs]), rep(p["rw_w0"][1][cs]), rep(p["rw_a0"][0][cs]), rep(p["rw_a0"][1][cs])]
    m["rw_cst"] = np.ascontiguousarray(np.concatenate(cst, 1))
    m["rw_wup"] = np.ascontiguousarray(p["rw_w_up"][:, :, cs])
    m["rw_aup"] = np.ascontiguousarray(p["rw_a_up"][:, :, cs])
    m["rw_gup"] = np.ascontiguousarray(p["rw_g_up"][:, cs])
    return m


def emit_mlstm(kb, c, banks):
    MUL, ADD, SUB = ALU.mult, ALU.add, ALU.subtract
    X = kb.dram_in("ml_x", [LSEQ, 516])
    cst_d = kb.dram_in("ml_cst", [128, 128 + 4])
    out = kb.dram_out("ml_out", [LSEQ, 128])
    cst = kb.sb("ml_cst_s", [128, 132])
    kb.dma("sp", cst[:], cst_d, writes=["ml_cst"])
    x = kb.sb("ml_xs", [128, 516])
    gs = kb.sb("ml_gs", [128, 4])
    st = kb.sb("ml_st", [128, 8])
    yacc = shared_yacc(kb)
    yn = kb.sb("ml_yn", [128, 128])
    sgo = kb.sb("ml_sgo", [128, 128])
    s = Dplr(kb, 128, 129, "S", False, "ml", c)
    kb.op("pool", lambda g: g.memset(s.v[:, 128:129], 1.0), writes=[s.k("v")])
    ones = c["ones"]

    def run_dir(d, order):
        s.init_state()
        for ci in order:
            rows = slice(ci * 128, (ci + 1) * 128)
            kb.dma("sp", x[:], X[rows, :], writes=["ml_x"])
            ACT(kb, gs[:, 0:1], x[:, 512 + d:513 + d], AF.Exp, ["ml_x", "ml_cst"], ["ml_gs"], bias=cst[:, 128 + d:129 + d], scale=1.0)
            ACT(kb, gs[:, 1:2], x[:, 514 + d:515 + d], AF.Sigmoid, ["ml_x", "ml_cst"], ["ml_gs"], bias=cst[:, 130 + d:131 + d], scale=1.0)
            ACT(kb, gs[:, 1:2], gs[:, 1:2], AF.Ln, ["ml_gs"], ["ml_gs"])
            CP(kb, "pool", s.r[:], x[:, 0:128], ["ml_x"], [s.k("r")])
            TS(kb, "dve", s.kt[:], x[:, 128:256], gs[:, 0:1], 128.0 ** -0.5, MUL, MUL, ["ml_x", "ml_gs"], [s.k("kt")])
            CP(kb, "pool", s.v[:, 0:128], x[:, 256:384], ["ml_x"], [s.k("v")])
            TS(kb, "dve", s.lw[:], ones[:], gs[:, 1:2], None, MUL, None, ["m_ones", "ml_gs"], [s.k("lw")])

            def cb(yp, ypk, ci=ci):
                TS(kb, "dve", st[:, 1:2], yp[:, 128:129], -1.0, None, MUL, None, [ypk], ["ml_st"])
                TT(kb, "dve", st[:, 0:1], yp[:, 128:129], st[:, 1:2], ALU.max, [ypk, "ml_st"], ["ml_st"])
                TS(kb, "dve", st[:, 0:1], st[:, 0:1], 1.0, None, ALU.max, None, ["ml_st"], ["ml_st"])
                kb.op("dve", lambda g: g.reciprocal(out=st[:, 0:1], in_=st[:, 0:1]), reads=["ml_st"], writes=["ml_st"])
                dst = yacc[:, ci, :]
                if d == 0:
                    TS(kb, "dve", dst, yp[:, 0:128], st[:, 0:1], None, MUL, None, [ypk, "ml_st"], ["yacc"])
                else:
                    STT(kb, dst, yp[:, 0:128], st[:, 0:1], dst, MUL, ADD, [ypk, "ml_st", "yacc"], ["yacc"])
            s.step("f" if d == 0 else "b", banks, cb)

    run_dir(0, FWD_ORDER)
    run_dir(1, BWD_ORDER)
    for ci in range(NCH):
        rows = slice(ci * 128, (ci + 1) * 128)
        kb.dma("sp", x[:], X[rows, :], writes=["ml_x"])
        layernorm_rs(kb, yacc[:, ci, :], "yacc", st, "ml_st", 128, EPS)
        TS(kb, "dve", yn[:], yacc[:, ci, :], st[:, 0:1], st[:, 1:2], SUB, MUL, ["yacc", "ml_st"], ["ml_yn"])
        TT(kb, "dve", yn[:], yn[:], cst[:, 0:128], MUL, ["ml_yn", "ml_cst"], ["ml_yn"])
        ACT(kb, sgo[:], x[:, 384:512], AF.Sigmoid, ["ml_x"], ["ml_sgo"])
        TT(kb, "dve", yn[:], yn[:], sgo[:], MUL, ["ml_yn", "ml_sgo"], ["ml_yn"])
        kb.dma("pool", out[rows, :], yn[:], reads=["ml_yn"], is_output=True)


def mlstm_inputs(pl_all, p, b, j):
    cx, lat = seq_rows(pl_all[:, RW_COLS:RW_COLS + ML_COLS], b)
    cols = np.r_[np.arange(j * 128, (j + 1) * 128), GW + np.arange(j * 128, (j + 1) * 128), 2 * GW + np.arange(j * 128, (j + 1) * 128),
                 3 * GW + np.arange(j * 128, (j + 1) * 128), 4 * GW + np.array([j, 4 + j, 8 + j, 12 + j])]
    m = {"ml_x": np.ascontiguousarray(np.concatenate([cx, lat], 0)[:, cols])}
    cst = [rep(p["ml_norm_g"][j * 128:(j + 1) * 128]), rep([p["ml_ib"][0][j], p["ml_ib"][1][j], p["ml_fb"][0][j], p["ml_fb"][1][j]])]
    m["ml_cst"] = np.ascontiguousarray(np.concatenate(cst, 1))
    return m


def emit_gdn(kb, c, banks):
    MUL, ADD, SUB = ALU.mult, ALU.add, ALU.subtract
    X = {n: kb.dram_in("gd_" + n, [LSEQ, 384]) for n in ("cur", "prev", "next")}
    G = kb.dram_in("gd_g", [LSEQ, 132])
    cst_d = kb.dram_in("gd_cst", [128, 3 * 384 + 128 + 4])
    out = kb.dram_out("gd_out", [LSEQ, 128])
    cst = kb.sb("gd_cst_s", [128, 3 * 384 + 132])
    kb.dma("sp", cst[:], cst_d, writes=["gd_cst"])
    nega = kb.sb("gd_nega", [128, 2])
    ACT(kb, nega[:], cst[:, 1280:1282], AF.Exp, ["gd_cst"], ["gd_nega"])
    TS(kb, "dve", nega[:], nega[:], -1.0, None, MUL, None, ["gd_nega"], ["gd_nega"])
    cur, prv, nxt = kb.sb("gd_cur_s", [128, 384]), kb.sb("gd_prv", [128, 384]), kb.sb("gd_nxt", [128, 384])
    gg = kb.sb("gd_gs", [128, 132])
    sg = kb.sb("gd_sg", [128, 384])
    junk = kb.sb("gd_junk", [128, 128])
    st = kb.sb("gd_st", [128, 8])
    gs = kb.sb("gd_gsc", [128, 4])
    yacc = shared_yacc(kb)
    yn = kb.sb("gd_yn", [128, 128])
    s = Dplr(kb, 128, 128, "S", True, "gd", c)
    ones = c["ones"]

    def run_dir(d, order):
        s.init_state()
        for ci in order:
            rows = slice(ci * 128, (ci + 1) * 128)
            kb.dma("sp", cur[:], X["cur"][rows, :], writes=["gd_cur"])
            kb.dma("sp", prv[:], X["prev"][rows, :], writes=["gd_prv"])
            kb.dma("sp", nxt[:], X["next"][rows, :], writes=["gd_nxt"])
            kb.dma("sp", gg[:], G[rows, :], writes=["gd_g"])
            TT(kb, "dve", cur[:], cur[:], cst[:, 384:768], MUL, ["gd_cur", "gd_cst"], ["gd_cur"])
            TT(kb, "pool", prv[:], prv[:], cst[:, 0:384], MUL, ["gd_prv", "gd_cst"], ["gd_prv"])
            TT(kb, "pool", nxt[:], nxt[:], cst[:, 768:1152], MUL, ["gd_nxt", "gd_cst"], ["gd_nxt"])
            TT(kb, "dve", cur[:], cur[:], prv[:], ADD, ["gd_cur", "gd_prv"], ["gd_cur"])
            TT(kb, "dve", cur[:], cur[:], nxt[:], ADD, ["gd_cur", "gd_nxt"], ["gd_cur"])
            ACT(kb, sg[:], cur[:], AF.Sigmoid, ["gd_cur"], ["gd_sg"])
            TT(kb, "dve", cur[:], cur[:], sg[:], MUL, ["gd_cur", "gd_sg"], ["gd_cur"])
            for i in range(2):
                ACT(kb, junk[:], cur[:, i * 128:(i + 1) * 128], AF.Square, ["gd_cur"], ["gd_junk", "gd_st"], accum_out=st[:, i:i + 1])
            ACT(kb, st[:, 0:2], st[:, 0:2], AF.Sqrt, ["gd_st"], ["gd_st"], bias=1e-6, scale=1.0)
            kb.op("dve", lambda g: g.reciprocal(out=st[:, 0:2], in_=st[:, 0:2]), reads=["gd_st"], writes=["gd_st"])
            TS(kb, "dve", s.r[:], cur[:, 0:128], st[:, 0:1], 128.0 ** -0.5, MUL, MUL, ["gd_cur", "gd_st"], [s.k("r")])
            TS(kb, "dve", s.kap[:], cur[:, 128:256], st[:, 1:2], None, MUL, None, ["gd_cur", "gd_st"], [s.k("kap")])
            CP(kb, "pool", s.v[:], cur[:, 256:384], ["gd_cur"], [s.k("v")])
            ACT(kb, gs[:, 0:1], gg[:, 128 + d:129 + d], AF.Exp, ["gd_g", "gd_cst"], ["gd_gsc"], bias=cst[:, 1282 + d:1283 + d], scale=1.0)
            ACT(kb, gs[:, 0:1], gs[:, 0:1], AF.Ln, ["gd_gsc"], ["gd_gsc"], bias=1.0, scale=1.0)
            TT(kb, "dve", gs[:, 0:1], gs[:, 0:1], nega[:, d:d + 1], MUL, ["gd_gsc", "gd_nega"], ["gd_gsc"])
            ACT(kb, gs[:, 1:2], gg[:, 130 + d:131 + d], AF.Sigmoid, ["gd_g"], ["gd_gsc"])
            ACT(kb, gs[:, 2:3], gs[:, 0:1], AF.Exp, ["gd_gsc"], ["gd_gsc"])
            TT(kb, "dve", gs[:, 2:3], gs[:, 2:3], gs[:, 1:2], MUL, ["gd_gsc"], ["gd_gsc"])
            TS(kb, "dve", s.lw[:], ones[:], gs[:, 0:1], None, MUL, None, ["m_ones", "gd_gsc"], [s.k("lw")])
            TS(kb, "dve", s.kt[:], s.kap[:], gs[:, 1:2], None, MUL, None, [s.k("kap"), "gd_gsc"], [s.k("kt")])
            TS(kb, "dve", s.a[:], s.kap[:], gs[:, 2:3], None, MUL, None, [s.k("kap"), "gd_gsc"], [s.k("a")])

            def cb(yp, ypk, ci=ci):
                dst = yacc[:, ci, :]
                if d == 0:
                    CP(kb, "act", dst, yp, [ypk], ["yacc"])
                else:
                    TT(kb, "dve", dst, yp, dst, ADD, [ypk, "yacc"], ["yacc"])
            s.step("f" if d == 0 else "b", banks, cb)

    run_dir(0, FWD_ORDER)
    run_dir(1, BWD_ORDER)
    for ci in range(NCH):
        rows = slice(ci * 128, (ci + 1) * 128)
        kb.dma("sp", gg[:], G[rows, :], writes=["gd_g"])
        sumsq_rs(kb, yacc[:, ci, :], "yacc", junk[:], "gd_junk", st[:, 0:1], "gd_st", 128, EPS)
        STT(kb, yn[:], yacc[:, ci, :], st[:, 0:1], cst[:, 1152:1280], MUL, MUL, ["yacc", "gd_st", "gd_cst"], ["gd_yn"])
        ACT(kb, sg[:, 0:128], gg[:, 0:128], AF.Sigmoid, ["gd_g"], ["gd_sg"])
        TT(kb, "dve", sg[:, 0:128], sg[:, 0:128], gg[:, 0:128], MUL, ["gd_sg", "gd_g"], ["gd_sg"])
        TT(kb, "dve", yn[:], yn[:], sg[:, 0:128], MUL, ["gd_yn", "gd_sg"], ["gd_yn"])
        kb.dma("pool", out[rows, :], yn[:], reads=["gd_yn"], is_output=True)


def gdn_inputs(pl_all, p, b, j):
    o = RW_COLS + ML_COLS
    cx, lat = seq_rows(pl_all[:, o:o + GD_COLS], b)
    cols = np.r_[np.arange(j * 128, (j + 1) * 128), GW + np.arange(j * 128, (j + 1) * 128), 2 * GW + np.arange(j * 128, (j + 1) * 128)]
    gcols = np.r_[3 * GW + np.arange(j * 128, (j + 1) * 128), 4 * GW + np.array([j, 4 + j, 8 + j, 12 + j])]
    m = {}
    m["gd_cur"] = np.ascontiguousarray(np.concatenate([cx, lat], 0)[:, cols])
    m["gd_prev"] = np.ascontiguousarray(shifted(cx, lat, -1)[:, cols])
    m["gd_next"] = np.ascontiguousarray(shifted(cx, lat, +1)[:, cols])
    m["gd_g"] = np.ascontiguousarray(np.concatenate([cx, lat], 0)[:, gcols])
    cst = [rep(p["gd_conv"][0][cols]), rep(p["gd_conv"][1][cols]), rep(p["gd_conv"][2][cols]), rep(p["gd_norm_g"]),
           rep([p["gd_a_log"][0][j], p["gd_a_log"][1][j], p["gd_dt_bias"][0][j], p["gd_dt_bias"][1][j]])]
    m["gd_cst"] = np.ascontiguousarray(np.concatenate(cst, 1))
    return m


def emit_attn(kb, c, banks, need_ctx=True):
    MUL, ADD, SUB = ALU.mult, ALU.add, ALU.subtract
    Q, Kd, V = kb.dram_in("at_q", [LSEQ, 128]), kb.dram_in("at_k", [LSEQ, 128]), kb.dram_in("at_v", [LSEQ, 128])
    COS, SIN = kb.dram_in("at_cos", [LSEQ, 128]), kb.dram_in("at_sin", [LSEQ, 128])
    cst_d = kb.dram_in("at_cst", [128, 256])
    out = kb.dram_out("at_out", [LSEQ, 128])
    cst = kb.sb("at_cst_s", [128, 256])
    kb.dma("sp", cst[:], cst_d, writes=["at_cst"])
    qT, kT = kb.sb("at_qT", [128, LSEQ]), kb.sb("at_kT", [128, LSEQ])
    va = kb.sb("at_va", [128, NCH, 129])
    kb.op("pool", lambda g: g.memset(va[:], 1.0), writes=["at_va"])
    x = kb.sb("at_x", [128, 2, 128])
    xn = kb.sb("at_xn", [128, 2, 128])
    rot = kb.sb("at_rot", [128, 2, 128])
    cs = kb.sb("at_cs", [128, 2, 128])
    junk = kb.sb("at_junk", [128, 128])
    st = kb.sb("at_st", [128, 4])
    idt = c["ident"]
    b2, b2k = banks["b2"]
    for ci in range(NCH):
        rows = slice(ci * 128, (ci + 1) * 128)
        kb.dma("sp", x[:, 0, :], Q[rows, :], writes=["at_x"])
        kb.dma("sp", x[:, 1, :], Kd[rows, :], writes=["at_x"])
        kb.dma("sp", va[:, ci, 0:128], V[rows, :], writes=["at_va"])
        kb.dma("sp", cs[:, 0, :], COS[rows, :], writes=["at_cs"])
        kb.dma("sp", cs[:, 1, :], SIN[rows, :], writes=["at_cs"])
        for i in range(2):
            sumsq_rs(kb, x[:, i, :], "at_x", junk[:], "at_junk", st[:, i:i + 1], "at_st", 128, EPS)
            STT(kb, xn[:, i, :], x[:, i, :], st[:, i:i + 1], cst[:, i * 128:(i + 1) * 128], MUL, MUL, ["at_x", "at_st", "at_cst"], ["at_xn"])
            xv = xn[:, i, :].rearrange("p (h t n) -> p h t n", h=2, t=2)
            rv = rot[:, i, :].rearrange("p (h t n) -> p h t n", h=2, t=2)
            CP(kb, "pool", rv[:, :, 0, :], xv[:, :, 1, :], ["at_xn"], ["at_rot"])
            CP(kb, "pool", rv[:, :, 1, :], xv[:, :, 0, :], ["at_xn"], ["at_rot"])
            TT(kb, "dve", xn[:, i, :], xn[:, i, :], cs[:, 0, :], MUL, ["at_xn", "at_cs"], ["at_xn"])
            TT(kb, "pool", rot[:, i, :], rot[:, i, :], cs[:, 1, :], MUL, ["at_rot", "at_cs"], ["at_rot"])
            TT(kb, "dve", xn[:, i, :], xn[:, i, :], rot[:, i, :], ADD, ["at_xn", "at_rot"], ["at_xn"])
            kb.op("pe", lambda g, i=i: g.transpose(out=b2[:, i * 128:(i + 1) * 128], in_=xn[:, i, :], identity=idt[:]), reads=["at_xn", "ident"], writes=[b2k])
        CP(kb, "dve", qT[:, rows], b2[:, 0:128], [b2k], ["at_qT"])
        CP(kb, "dve", kT[:, rows], b2[:, 128:256], [b2k], ["at_kT"])
    bS = [banks["b0"], banks["b1"]]
    bO = [banks["b4"], banks["b5"], banks["b6"], banks["b7"]]
    pT = [kb.sb(f"at_pT{i}", [128, 512]) for i in range(2)]
    ot = kb.sb("at_ot", [128, 128])
    blocks = []
    if need_ctx:
        blocks.append((0, 256, [0, 1]))
    for qb in range(SEQ // 512):
        blocks.append((CTX + qb * 512, 512, list(range(NCH))))
    n = 0
    for q0, qn, kts in blocks:
        nq = qn // 128
        for idx, kt in enumerate(kts):
            (bs, bsk), p, pk = bS[n % 2], pT[n % 2], f"at_pT{n % 2}"
            n += 1
            kb.op("pe", lambda g, bs=bs, kt=kt: g.matmul(bs[:, 0:qn], lhsT=kT[:, kt * 128:(kt + 1) * 128], rhs=qT[:, q0:q0 + qn], start=True, stop=True),
                  reads=["at_kT", "at_qT"], writes=[bsk])
            ACT(kb, p[:, 0:qn], bs[:, 0:qn], AF.Exp, [bsk], [pk], scale=128.0 ** -0.5)
            for qs in range(nq):
                bo, bok = bO[qs]
                kb.op("pe", lambda g, bo=bo, p=p, qs=qs, kt=kt, idx=idx: g.matmul(bo[:, 0:129], lhsT=p[:, qs * 128:(qs + 1) * 128], rhs=va[:, kt, :],
                                                                          start=(idx == 0), stop=(idx == len(kts) - 1)),
                      reads=[pk, "at_va"], writes=[bok])
        for qs in range(nq):
            bo, bok = bO[qs]
            kb.op("dve", lambda g, bo=bo: g.reciprocal(out=st[:, 2:3], in_=bo[:, 128:129]), reads=[bok], writes=["at_st2"])
            TS(kb, "dve", ot[:], bo[:, 0:128], st[:, 2:3], None, MUL, None, [bok, "at_st2"], ["at_ot"])
            kb.dma("pool", out[q0 + qs * 128:q0 + (qs + 1) * 128, :], ot[:], reads=["at_ot"], is_output=True)


def rope_tables():
    rows = SEQ // 64
    row = np.repeat(np.arange(rows), 64).astype(np.float32)
    col = np.tile(np.arange(64), rows).astype(np.float32)
    inv = (10000.0 ** (-np.arange(0, 64, 2, dtype=np.float32) / 64)).astype(np.float32)
    ar, ac = row[:, None] * inv[None, :], col[:, None] * inv[None, :]
    cos = np.concatenate([np.cos(ar), np.cos(ar), np.cos(ac), np.cos(ac)], 1)
    sin = np.concatenate([-np.sin(ar), np.sin(ar), -np.sin(ac), np.sin(ac)], 1)
    cos = np.concatenate([np.ones((CTX, 128)), cos], 0).astype(np.float32)
    sin = np.concatenate([np.zeros((CTX, 128)), sin], 0).astype(np.float32)
    return np.ascontiguousarray(cos), np.ascontiguousarray(sin)


def attn_inputs(pl_all, p, b, j):
    o = RW_COLS + ML_COLS + GD_COLS
    cx, lat = seq_rows(pl_all[:, o:o + AT_COLS], b)
    a = np.concatenate([cx, lat], 0)
    kv = j // 2
    cos, sin = rope_tables()
    m = {"at_q": np.ascontiguousarray(a[:, j * 128:(j + 1) * 128]), "at_k": np.ascontiguousarray(a[:, 512 + kv * 128:512 + (kv + 1) * 128]),
         "at_v": np.ascontiguousarray(a[:, 768 + kv * 128:768 + (kv + 1) * 128]), "at_cos": cos, "at_sin": sin,
         "at_cst": np.ascontiguousarray(np.concatenate([rep(p["at_q_norm"]), rep(p["at_k_norm"])], 1))}
    return m


def build_outproj():
    MUL, ADD, SUB = ALU.mult, ALU.add, ALU.subtract
    kb = KB()
    mix = kb.dram_in("mix", [ROWS, D])
    xin = kb.dram_in("xin", [ROWS, D])
    w = kb.dram_in("w", [D, D])
    mods = kb.dram_in("mods", [NT, 3, D])
    gvec = kb.dram_in("g", [1, D])
    wr = kb.dram_in("wr", [D, NE])
    xmid = kb.dram_out("xmid", [ROWS, D])
    h2o = kb.dram_out("h2", [ROWS, D])
    affo = kb.dram_out("aff", [ROWS, NE])
    idt = ident(kb)
    g_bc = kb.sb("g_bc", [128, D])
    kb.dma("sp", g_bc[:], gvec[0, :].partition_broadcast(128), writes=["g_bc"])
    wrs = kb.sb("wrs", [128, 16, NE])
    kb.dma("sp", wrs[:], wr.rearrange("(kc p) n -> p kc n", p=128), writes=["wrs"])
    GT = 5
    mixT = kb.sb("mixT", [128, GT, 16, 128])
    xm = kb.sb("xm", [128, GT, D])
    xt = kb.sb("xt", [128, D])
    sc, sh = kb.sb("sc", [128, D]), kb.sb("sh", [128, D])
    junk = kb.sb("junk", [128, D])
    h2T = kb.sb("h2T", [128, 16, 128])
    rstd = kb.sb("rstd", [128, 4])
    lg = kb.sb("lg", [128, NE])
    pst = [kb.ps(f"pst{i}", [128, 4, 128]) for i in range(2)]
    pstk = ["pst0", "pst1"]
    NB = 256
    wv = w.rearrange("(kc p) n -> p kc n", p=128)
    wb = [kb.sb(f"wb{i}", [128, 16, NB]) for i in range(2)]
    pp = [kb.ps(f"pp{i}", [128, NB]) for i in range(4)]
    m2b = [kb.sb(f"m2b{i}", [128, NB]) for i in range(4)]
    pr = kb.ps("pr", [128, NE])
    cnt = 0
    wcnt = 0
    for g0 in range(0, NT, GT):
        tiles = list(range(g0, min(NT, g0 + GT)))
        for t in tiles:
            lt = t - g0
            rows = slice(t * 128, (t + 1) * 128)
            kb.dma("sp", xt[:], mix[rows, :], writes=["xt"])
            kb.dma("sp", xm[:, lt, :], xin[rows, :], writes=[f"xm{lt}"])
            transpose_rows(kb, idt, xt, "xt", mixT[:, lt], f"mixT{lt}", pst, pstk)
        for nb in range(D // NB):
            wt, wk = wb[wcnt % 2], f"wb{wcnt % 2}"
            wcnt += 1
            kb.dma("sp", wt[:], wv[:, :, nb * NB:(nb + 1) * NB], writes=[wk])
            for t in tiles:
                lt = t - g0
                p, pk, mb, mk = pp[cnt % 4], f"pp{cnt % 4}", m2b[cnt % 4], f"m2b{cnt % 4}"
                cnt += 1
                kb.dma("sp", mb[:], mods[t, 0, nb * NB:(nb + 1) * NB].partition_broadcast(128), writes=[mk])
                for kc in range(16):
                    kb.op("pe", lambda e, kc=kc, p=p, wt=wt, lt=lt: e.matmul(p[:], lhsT=mixT[:, lt, kc, :], rhs=wt[:, kc, :], start=(kc == 0), stop=(kc == 15)),
                          reads=[f"mixT{lt}", wk], writes=[pk])
                TT(kb, "dve", mb[:], p[:], mb[:], MUL, [pk, mk], [mk])
                TT(kb, "pool", xm[:, lt, nb * NB:(nb + 1) * NB], xm[:, lt, nb * NB:(nb + 1) * NB], mb[:], ADD, [mk, f"xm{lt}"], [f"xm{lt}"])
        for t in tiles:
            lt = t - g0
            rows = slice(t * 128, (t + 1) * 128)
            x, xk = xm[:, lt, :], f"xm{lt}"
            kb.dma("pool", xmid[rows, :], x, reads=[xk], is_output=True)
            rms_rstd(kb, x, xk, junk[:], rstd[:, 0:1], "rstd")
            kb.dma("sp", sh[:], mods[t, 1, :].partition_broadcast(128), writes=["sh"])
            kb.dma("sp", sc[:], mods[t, 2, :].partition_broadcast(128), writes=["sc"])
            STT(kb, sc[:], sc[:], 1.0, g_bc[:], ADD, MUL, ["sc", "g_bc"], ["sc"])
            STT(kb, xt[:], x, rstd[:, 0:1], sc[:], MUL, MUL, [xk, "rstd", "sc"], ["xt"])
            TT(kb, "dve", xt[:], xt[:], sh[:], ADD, ["xt", "sh"], ["xt"])
            kb.dma("pool", h2o[rows, :], xt[:], reads=["xt"], is_output=True)
            transpose_rows(kb, idt, xt, "xt", h2T, "h2T", pst, pstk)
            for kc in range(16):
                kb.op("pe", lambda e, kc=kc: e.matmul(pr[:], lhsT=h2T[:, kc, :], rhs=wrs[:, kc, :], start=(kc == 0), stop=(kc == 15)),
                      reads=["h2T", "wrs"], writes=["pr"])
            kb.op("dve", lambda e: e.tensor_reduce(out=rstd[:, 1:2], in_=pr[:], axis=AX.X, op=ALU.max, negate=True), reads=["pr"], writes=["rmax"])
            ACT(kb, lg[:], pr[:], AF.Exp, ["pr", "rmax"], ["lg", "rsum"], bias=rstd[:, 1:2], scale=1.0, accum_out=rstd[:, 2:3])
            kb.op("dve", lambda e: e.reciprocal(out=rstd[:, 2:3], in_=rstd[:, 2:3]), reads=["rsum"], writes=["rsum"])
            TS(kb, "dve", lg[:], lg[:], rstd[:, 2:3], None, MUL, None, ["lg", "rsum"], ["lg"])
            kb.dma("pool", affo[rows, :], lg[:], reads=["lg"], is_output=True)
    return kb


CAP_L = 2 * SEQ // NE
CAP_C = 2 * CTX // NE


def build_experts(has_ctx):
    MUL, ADD, SUB = ALU.mult, ALU.add, ALU.subtract
    kb = KB()
    affT = kb.dram_in("affT", [4, SEQ])
    h2l = [kb.dram_in(f"h2l{b}", [SEQ, D]) for b in range(B)]
    pl_ = [kb.dram_out(f"part_l{b}", [SEQ, D]) for b in range(B)]
    if has_ctx:
        affTc = kb.dram_in("affTc", [4, CTX])
        h2c = [kb.dram_in(f"h2c{b}", [CTX, D]) for b in range(B)]
        pc_ = [kb.dram_out(f"part_c{b}", [CTX, D]) for b in range(B)]
    w1 = kb.dram_in("w1", [2, D, D])
    w3 = kb.dram_in("w3", [2, D, D])
    w2 = kb.dram_in("w2", [2, D, D])
    idt = ident(kb)
    NTOK = CAP_L + (CAP_C if has_ctx else 0)
    NTT = 4 + (1 if has_ctx else 0)
    arena = kb.sb("arena", [128, 5 * D])
    xsT = arena[:, 0:16 * NTOK].rearrange("p (k n) -> p k n", k=16)
    ysb = arena[:, 0:NTT * D].rearrange("p (t n) -> p t n", t=NTT)
    AK = "arena"
    hidT = kb.sb("hidT", [128, 16, NTOK])
    xg = kb.sb("xg", [128, D])
    sgm = kb.sb("sgm", [128, 512])
    kb.op("pool", lambda e: e.memset(xg[:], 0.0), writes=["xg"])
    for b in range(B):
        for t in range(SEQ // 128):
            kb.dma("sp", pl_[b][t * 128:(t + 1) * 128, :], xg[:], reads=["xg"], writes=[f"part_l{b}"], is_output=True)
        if has_ctx:
            for t in range(CTX // 128):
                kb.dma("sp", pc_[b][t * 128:(t + 1) * 128, :], xg[:], reads=["xg"], writes=[f"part_c{b}"], is_output=True)
    pt = kb.ps("pt", [128, 64])

    def topk(src, n, cap, tag):
        wk = kb.sb(f"wk{tag}", [4, n])
        vals = kb.sb(f"vals{tag}", [4, cap])
        idxs = kb.sb(f"idxs{tag}", [4, cap], U32)
        idxf = kb.sb(f"idxf{tag}", [4, cap])
        kb.dma("sp", wk[:], src, writes=[f"wk{tag}"])
        for it in range(cap // 8):
            sl = slice(it * 8, (it + 1) * 8)
            kb.op("dve", lambda e, sl=sl: e.max(out=vals[:, sl], in_=wk[:]), reads=[f"wk{tag}"], writes=[f"vals{tag}"])
            kb.op("dve", lambda e, sl=sl: e.max_index(out=idxs[:, sl], in_max=vals[:, sl], in_values=wk[:]), reads=[f"wk{tag}", f"vals{tag}"], writes=[f"idxs{tag}"])
            kb.op("dve", lambda e, sl=sl: e.match_replace(out=wk[:], in_to_replace=vals[:, sl], in_values=wk[:], imm_value=-1.0),
                  reads=[f"vals{tag}"], writes=[f"wk{tag}"])
        CP(kb, "dve", idxf[:], idxs[:], [f"idxs{tag}"], [f"idxf{tag}"])
        nblk = (cap + 127) // 128
        pw = min(cap, 128)
        idxT = kb.sb(f"idxT{tag}", [128, nblk * 4], I32)
        gT = kb.sb(f"gT{tag}", [128, nblk * 4])
        for srcv, srck, dst, dstk in ((idxf, f"idxf{tag}", idxT, f"idxT{tag}"), (vals, f"vals{tag}", gT, f"gT{tag}")):
            for blk in range(nblk):
                kb.op("pe", lambda e, srcv=srcv, blk=blk: e.transpose(out=pt[0:pw, blk * 4:(blk + 1) * 4], in_=srcv[:, blk * 128:blk * 128 + pw], identity=idt[0:4, 0:4]),
                      reads=[srck, "ident"], writes=["pt"])
            CP(kb, "dve", dst[0:pw, :], pt[0:pw, 0:nblk * 4], ["pt"], [dstk])
        return idxT, gT

    idxT, gT = topk(affT, SEQ, CAP_L, "L")
    if has_ctx:
        idxTc, gTc = topk(affTc, CTX, CAP_C, "C")
    pst = [kb.ps(f"pst{i}", [128, 4, 128]) for i in range(2)]
    pstk = ["pst0", "pst1"]
    ph = [kb.ps(f"ph{i}", [128, 512]) for i in range(2)]
    phc = kb.ps("phc", [128, 2, 32])
    py = [kb.ps(f"py{i}", [128, 512]) for i in range(2)]
    w13 = [kb.sb(f"w13_{i}", [128, 2, 16, 128]) for i in range(2)]
    w2b = [kb.sb(f"w2b{i}", [128, 16, 256]) for i in range(2)]
    wc = 0
    w2c = 0
    yc = 0
    for el in range(2):
        w1v = w1[el].rearrange("(kc p) n -> p kc n", p=128)
        w3v = w3[el].rearrange("(kc p) n -> p kc n", p=128)
        w2v = w2[el].rearrange("(fc p) n -> p fc n", p=128)
        for b in range(B):
            r = el * 2 + b
            for blk in range(4):
                col = blk * 4 + r
                kb.dma("pool", None, None, reads=["idxTL"], writes=["xg"],
                       fn=lambda e, col=col: e.indirect_dma_start(out=xg[:], out_offset=None, in_=h2l[b][:, :],
                                                                  in_offset=bass.IndirectOffsetOnAxis(ap=idxT[:, col:col + 1], axis=0)))
                transpose_rows(kb, idt, xg, "xg", xsT[:, :, blk * 128:(blk + 1) * 128], AK, pst, pstk)
            if has_ctx:
                kb.dma("pool", None, None, reads=["idxTC"], writes=["xg"],
                       fn=lambda e: e.indirect_dma_start(out=xg[0:CAP_C, :], out_offset=None, in_=h2c[b][:, :],
                                                         in_offset=bass.IndirectOffsetOnAxis(ap=idxTc[0:CAP_C, r:r + 1], axis=0)))
                for c0 in range(0, 16, 4):
                    bank = (c0 // 4) % 2
                    for i in range(4):
                        kb.op("pe", lambda e, i=i: e.transpose(out=pst[bank][:, i, 0:CAP_C], in_=xg[0:CAP_C, (c0 + i) * 128:(c0 + i + 1) * 128], identity=idt[0:CAP_C, 0:CAP_C]),
                              reads=["xg", "ident"], writes=[pstk[bank]])
                    CP(kb, "dve", xsT[:, c0:c0 + 4, CAP_L:NTOK], pst[bank][:, :, 0:CAP_C], [pstk[bank]], [AK])
            for fb in range(16):
                wt, wk_ = w13[wc % 2], f"w13_{wc % 2}"
                wc += 1
                kb.dma("sp", wt[:, 0], w1v[:, :, fb * 128:(fb + 1) * 128], writes=[wk_ + "a"])
                kb.dma("sp", wt[:, 1], w3v[:, :, fb * 128:(fb + 1) * 128], writes=[wk_ + "b"])
                for i in range(2):
                    for kc in range(16):
                        kb.op("pe", lambda e, i=i, kc=kc, wt=wt: e.matmul(ph[i][:], lhsT=wt[:, i, kc, :], rhs=xsT[:, kc, 0:CAP_L], start=(kc == 0), stop=(kc == 15)),
                              reads=[wk_ + "ab"[i], AK], writes=[f"ph{i}"])
                ACT(kb, sgm[:], ph[0][:], AF.Sigmoid, ["ph0"], ["sgm"])
                TT(kb, "dve", sgm[:], ph[0][:], sgm[:], MUL, ["ph0", "sgm"], ["sgm"])
                TT(kb, "dve", hidT[:, fb, 0:CAP_L], ph[1][:], sgm[:], MUL, ["ph1", "sgm"], ["hidT"])
                if has_ctx:
                    for i in range(2):
                        for kc in range(16):
                            kb.op("pe", lambda e, i=i, kc=kc, wt=wt: e.matmul(phc[:, i, :], lhsT=wt[:, i, kc, :], rhs=xsT[:, kc, CAP_L:NTOK], start=(kc == 0), stop=(kc == 15)),
                                  reads=[wk_ + "ab"[i], AK], writes=["phc"])
                    ACT(kb, sgm[:, 0:CAP_C], phc[:, 0, :], AF.Sigmoid, ["phc"], ["sgm"])
                    TT(kb, "dve", sgm[:, 0:CAP_C], phc[:, 0, :], sgm[:, 0:CAP_C], MUL, ["phc", "sgm"], ["sgm"])
                    TT(kb, "dve", hidT[:, fb, CAP_L:NTOK], phc[:, 1, :], sgm[:, 0:CAP_C], MUL, ["phc", "sgm"], ["hidT"])
            for db in range(D // 256):
                wt, wk_ = w2b[w2c % 2], f"w2b{w2c % 2}"
                w2c += 1
                kb.dma("sp", wt[:], w2v[:, :, db * 256:(db + 1) * 256], writes=[wk_])
                for tt in range(NTT):
                    np_ = 128 if tt < 4 else CAP_C
                    t0 = tt * 128
                    p, pk = py[yc % 2], f"py{yc % 2}"
                    yc += 1
                    for fc in range(16):
                        kb.op("pe", lambda e, fc=fc, p=p, wt=wt: e.matmul(p[0:np_, 0:256], lhsT=hidT[:, fc, t0:t0 + np_], rhs=wt[:, fc, :], start=(fc == 0), stop=(fc == 15)),
                              reads=["hidT", wk_], writes=[pk])
                    gsc = gT[:, tt * 4 + r:tt * 4 + r + 1] if tt < 4 else gTc[0:CAP_C, r:r + 1]
                    gk = "gTL" if tt < 4 else "gTC"
                    TS(kb, "dve", ysb[0:np_, tt, db * 256:(db + 1) * 256], p[0:np_, 0:256], gsc, None, MUL, None, [pk, gk], [AK])
            for tt in range(NTT):
                if tt < 4:
                    col = tt * 4 + r
                    kb.dma("pool", None, None, reads=[AK, "idxTL"], writes=[f"part_l{b}"], is_output=True,
                           fn=lambda e, col=col, tt=tt: e.indirect_dma_start(out=pl_[b][:, :], out_offset=bass.IndirectOffsetOnAxis(ap=idxT[:, col:col + 1], axis=0),
                                                                              in_=ysb[:, tt, :], in_offset=None, compute_op=ALU.add))
                else:
                    kb.dma("pool", None, None, reads=[AK, "idxTC"], writes=[f"part_c{b}"], is_output=True,
                           fn=lambda e, tt=tt: e.indirect_dma_start(out=pc_[b][:, :], out_offset=bass.IndirectOffsetOnAxis(ap=idxTc[0:CAP_C, r:r + 1], axis=0),
                                                                    in_=ysb[0:CAP_C, tt, :], in_offset=None, compute_op=ALU.add))
    return kb


def expert_inputs(aff, h2, wts, core, has_ctx):
    m = {}
    e0 = 2 * core
    rows = []
    rows_c = []
    for el in range(2):
        for b in range(B):
            rows.append(aff[b * SEQ:(b + 1) * SEQ, e0 + el])
            rows_c.append(aff[2 * SEQ + b * CTX:2 * SEQ + (b + 1) * CTX, e0 + el])
    m["affT"] = np.ascontiguousarray(np.stack(rows, 0))
    for b in range(B):
        m[f"h2l{b}"] = np.ascontiguousarray(h2[b * SEQ:(b + 1) * SEQ])
    if has_ctx:
        m["affTc"] = np.ascontiguousarray(np.stack(rows_c, 0))
        for b in range(B):
            m[f"h2c{b}"] = np.ascontiguousarray(h2[2 * SEQ + b * CTX:2 * SEQ + (b + 1) * CTX])
    for n, w in zip(("w1", "w3", "w2"), wts):
        m[n] = np.ascontiguousarray(w[e0:e0 + 2])
    return m


def expert_parts(res, has_ctx):
    outs = []
    for r in res:
        a = [r["part_l0"], r["part_l1"]]
        if has_ctx:
            a += [r["part_c0"], r["part_c1"]]
        else:
            a += [np.zeros((CTX, D), np.float32)] * 2
        outs.append(np.concatenate(a, 0))
    return outs


def build_mixers():
    kb = KB()
    c = dplr_consts(kb)
    banks = dplr_banks(kb)
    emit_attn(kb, c, banks)
    emit_mlstm(kb, c, banks)
    emit_gdn(kb, c, banks)
    emit_rwkv(kb, c, banks)
    return kb


def mixer_inputs(pl_all, p, core):
    b, j = core // 4, core % 4
    m = {}
    m.update(rwkv_inputs(pl_all, p, b, j))
    m.update(mlstm_inputs(pl_all, p, b, j))
    m.update(gdn_inputs(pl_all, p, b, j))
    m.update(attn_inputs(pl_all, p, b, j))
    return m


def mixer_outputs(res):
    mix = np.zeros((TOT, D), np.float32)
    for core in range(NCORES):
        b, j = core // 4, core % 4
        for gi, nm in enumerate(("rw_out", "ml_out", "gd_out", "at_out")):
            o = res[core][nm]
            cs = slice(gi * GW + j * 128, gi * GW + (j + 1) * 128)
            mix[2 * SEQ + b * CTX:2 * SEQ + (b + 1) * CTX, cs] = o[:CTX]
            mix[b * SEQ:(b + 1) * SEQ, cs] = o[CTX:]
    return mix


_PROGS = {}


def _prog(name, fn):
    if name not in _PROGS:
        _PROGS[name] = fn().finish()
    return _PROGS[name]


LAYER_PARAMS = ['rw_mu', 'rw_w0', 'rw_w_up', 'rw_a0', 'rw_a_up', 'rw_g_up', 'rw_k_k', 'rw_k_a', 'rw_r_k', 'rw_ln_w', 'rw_ln_b',
                'ml_ib', 'ml_fb', 'ml_norm_g', 'gd_conv', 'gd_a_log', 'gd_dt_bias', 'gd_norm_g', 'at_q_norm', 'at_k_norm']


def kernel(**inp):
    inp = {k: np.asarray(v, np.float32) for k, v in inp.items()}
    mod = run_mod(inp["c"], inp["c_ctx"], inp["ada_w"], inp["ada_b"])
    x = np.concatenate([inp["x"].reshape(-1, D), inp["ctx"].reshape(-1, D)], 0)
    parts = None
    m5 = None
    for l in range(DEPTH):
        p = {n: inp[n][l] for n in LAYER_PARAMS}
        xs = rows_to_cores(x)
        mr = mod_rows_for(mod[l], [0, 1])
        g1 = np.ascontiguousarray(inp["norm1_g"][l][None, :])
        w_in = np.ascontiguousarray(inp["w_in"][l])
        if parts is None:
            nc = _prog("rows0", lambda: build_rows(False, "inproj"))
            maps = [{"xin": xs[c], "g": g1, "modrows": mr[c], "w": w_in} for c in range(NCORES)]
        else:
            nc = _prog("rows1", lambda: build_rows(True, "inproj"))
            maps = [{"xin": xs[c], "g": g1, "modrows": mr[c], "w": w_in, "parts": parts[c], "m5": m5[c]} for c in range(NCORES)]
        res = run(nc, maps)
        pl_all = cores_to_rows([r["out"] for r in res])
        if parts is not None:
            x = cores_to_rows([r["xout"] for r in res])
            xs = rows_to_cores(x)
        nc = _prog("mixers", build_mixers)
        res = run(nc, [mixer_inputs(pl_all, p, c) for c in range(NCORES)])
        mix = mixer_outputs(res)
        del pl_all
        nc = _prog("outproj", build_outproj)
        ms = rows_to_cores(mix)
        mr2 = mod_rows_for(mod[l], [2, 3, 4])
        g2 = np.ascontiguousarray(inp["norm2_g"][l][None, :])
        w_out = np.ascontiguousarray(inp["w_out"][l])
        wr = np.ascontiguousarray(inp["w_router"][l])
        res = run(nc, [{"mix": ms[c], "xin": xs[c], "w": w_out, "mods": mr2[c], "g": g2, "wr": wr} for c in range(NCORES)])
        x = cores_to_rows([r["xmid"] for r in res])
        h2 = cores_to_rows([r["h2"] for r in res])
        aff = cores_to_rows([r["aff"] for r in res])
        has_ctx = l < DEPTH - 1
        nc = _prog("experts%d" % has_ctx, lambda: build_experts(has_ctx))
        wts = (inp["w_exp1"][l], inp["w_exp3"][l], inp["w_exp2"][l])
        res = run(nc, [expert_inputs(aff, h2, wts, c, has_ctx) for c in range(NCORES)])
        pfull = expert_parts(res, has_ctx)
        pc = [rows_to_cores(a) for a in pfull]
        parts = [np.ascontiguousarray(np.stack([pc[e][c] for e in range(NCORES)], 0)) for c in range(NCORES)]
        m5 = [np.ascontiguousarray(a[:, 0, :]) for a in mod_rows_for(mod[l], [5])]
        del pfull, pc
    nc = _prog("final", lambda: build_rows(True, "final"))
    xs = rows_to_cores(x)
    gf = np.ascontiguousarray(inp["final_g"][None, :])
    res = run(nc, [{"xin": xs[c], "g": gf, "parts": parts[c], "m5": m5[c]} for c in range(NCORES)])
    out = cores_to_rows([r["out"] for r in res])[:B * SEQ]
    return np.ascontiguousarray(out.reshape(B, SEQ, D).astype(np.float32))
```

```python
import math
from contextlib import ExitStack

import numpy as np
import concourse.bass as bass
import concourse.mybir as mybir
from concourse.bass_utils import run_bass_kernel_spmd

F32 = mybir.dt.float32
U32 = mybir.dt.uint32
I32 = mybir.dt.int32
AF = mybir.ActivationFunctionType
ALU = mybir.AluOpType
AX = mybir.AxisListType

NCORES = 8
D = 2048
B = 2
SEQ = 4096
CTX = 256
DEPTH = 2
GW = 512
RW_COLS = 3 * GW + 64 + 64 + 128
ML_COLS = 4 * GW + 16
GD_COLS = 4 * GW + 16
AT_COLS = 1024
N_IN = RW_COLS + ML_COLS + GD_COLS + AT_COLS
NE = 16
EPS = 1e-6


class KB:
    NDSEM = 8

    def __init__(self):
        self.nc = bass.Bass("TRN2", target_bir_lowering=False)
        self.es = ExitStack()
        nc = self.nc
        self.eng = {"pe": nc.tensor, "dve": nc.vector, "act": nc.scalar, "pool": nc.gpsimd, "sp": nc.sync}
        self.sem = {e: self.es.enter_context(nc.semaphore("s_" + e)) for e in self.eng}
        self.cnt = {e: 0 for e in self.eng}
        self.waited = {e: {} for e in self.eng}
        self.dsem = {}
        self.dcnt = {}
        self.dnext = {}
        for q in ("sp", "pool", "act"):
            self.dsem[q] = [self.es.enter_context(nc.semaphore(f"d_{q}{i}")) for i in range(self.NDSEM)]
            self.dcnt[q] = [0] * self.NDSEM
            self.dnext[q] = 0
        self.last_w = {}
        self.readers = {}
        self.excl = set()
        self.ninst = 0
        self.out_tokens = []

    def sb(self, name, shape, dt=F32):
        return self.es.enter_context(self.nc.sbuf_tensor(name, list(shape), dt))

    def ps(self, name, shape, dt=F32):
        self.excl.add(name)
        return self.es.enter_context(self.nc.psum_tensor(name, list(shape), dt))

    def dram_in(self, name, shape, dt=F32):
        return self.nc.dram_tensor(name, list(shape), dt, kind="ExternalInput").ap()

    def dram_out(self, name, shape, dt=F32):
        return self.nc.dram_tensor(name, list(shape), dt, kind="ExternalOutput").ap()

    def _wait(self, e, tok):
        if tok is None:
            return
        kind, key, val = tok
        w = self.waited[e]
        k = (kind, key if kind == "c" else id(key))
        if w.get(k, 0) >= val:
            return
        w[k] = val
        sem = self.sem[key] if kind == "c" else key
        self.eng[e].wait_ge(sem, val)

    def _deps(self, e, reads, writes):
        deps = []
        for k in reads:
            t = self.last_w.get(k)
            if t is not None:
                deps.append(t)
        for k in writes:
            t = self.last_w.get(k)
            if t is not None:
                deps.append(t)
            deps.extend(self.readers.get(k, ()))
        for t in deps:
            if e == "pe" and t[0] == "c" and t[1] == "pe":
                continue
            self._wait(e, t)

    def _record(self, tok, reads, writes):
        for k in reads:
            self.readers.setdefault(k, []).append(tok)
        for k in writes:
            self.last_w[k] = tok
            self.readers[k] = []

    def _x(self, reads, writes):
        ex = [k for k in reads if k in self.excl]
        if ex:
            reads = [k for k in reads if k not in self.excl]
            writes = list(writes) + ex
        return reads, writes

    def run_streams(self, fns):
        import threading
        n = len(fns)
        cv = threading.Condition()
        st = {"turn": 0, "alive": [True] * n, "err": None}
        self._stream = (cv, st, n)
        tl = threading.local()
        self._tl = tl

        def nxt(i):
            for k in range(1, n + 1):
                j = (i + k) % n
                if st["alive"][j]:
                    return j
            return i

        def worker(i):
            tl.sid = i
            try:
                with cv:
                    while st["turn"] != i:
                        cv.wait()
                fns[i]()
            except BaseException as ex:
                st["err"] = ex
            finally:
                with cv:
                    st["alive"][i] = False
                    st["turn"] = nxt(i)
                    cv.notify_all()
        ths = [threading.Thread(target=worker, args=(i,)) for i in range(n)]
        for t in ths:
            t.start()
        for t in ths:
            t.join()
        self._stream = None
        if st["err"] is not None:
            raise st["err"]

    def _yield_turn(self):
        if getattr(self, "_stream", None) is None:
            return
        cv, st, n = self._stream
        i = self._tl.sid
        with cv:
            j = i
            for k in range(1, n + 1):
                c = (i + k) % n
                if st["alive"][c]:
                    j = c
                    break
            if j != i:
                st["turn"] = j
                cv.notify_all()
                while st["turn"] != i:
                    cv.wait()

    def op(self, e, fn, reads=(), writes=()):
        self._yield_turn()
        reads, writes = self._x(reads, writes)
        self._deps(e, reads, writes)
        inst = fn(self.eng[e])
        self.cnt[e] += 1
        inst.then_inc(self.sem[e], 1)
        tok = ("c", e, self.cnt[e])
        self._record(tok, reads, writes)
        self.ninst += 1
        return tok

    def dma(self, q, out, in_, reads=(), writes=(), is_output=False, fn=None):
        self._yield_turn()
        i = self.dnext[q]
        self.dnext[q] = (i + 1) % self.NDSEM
        sem = self.dsem[q][i]
        if self.dcnt[q][i] > 0:
            self._wait(q, ("d", sem, 16 * self.dcnt[q][i]))
        self._deps(q, reads, writes)
        if fn is None:
            inst = self.eng[q].dma_start(out=out, in_=in_)
        else:
            inst = fn(self.eng[q])
        self.dcnt[q][i] += 1
        inst.then_inc(sem, 16)
        tok = ("d", sem, 16 * self.dcnt[q][i])
        self._record(tok, reads, writes)
        if is_output:
            self.out_tokens.append(tok)
        self.ninst += 1
        return tok

    def finish(self):
        for q in self.dsem:
            for i, sem in enumerate(self.dsem[q]):
                if self.dcnt[q][i] > 0:
                    self._wait("sp", ("d", sem, 16 * self.dcnt[q][i]))
        for e in self.eng:
            if e != "sp" and self.cnt[e] > 0:
                self._wait("sp", ("c", e, self.cnt[e]))
        self.es.close()
        return self.nc


def run(kb_or_nc, in_maps):
    nc = kb_or_nc.finish() if isinstance(kb_or_nc, KB) else kb_or_nc
    res = run_bass_kernel_spmd(nc, in_maps, core_ids=list(range(NCORES)))
    return res.results


def ident(kb, name="ident"):
    t = kb.sb(name, [128, 128])
    kb.op("pool", lambda e: e.memset(t[:], 1.0), writes=[name])
    kb.op("pool", lambda e: e.affine_select(out=t[:], in_=t[:], pattern=[[1, 128]], compare_op=ALU.is_equal,
                                             fill=0.0, base=0, channel_multiplier=-1), reads=[name], writes=[name])
    return t


def tri_mask(kb, name, mode):
    t = kb.sb(name, [128, 128])
    kb.op("pool", lambda e: e.memset(t[:], 1.0), writes=[name])
    if mode == "ones":
        return t
    if mode == "le":
        pat, cm, base, cmp = [[1, 128]], -1, 0, ALU.is_ge
    elif mode == "lt":
        pat, cm, base, cmp = [[1, 128]], -1, 0, ALU.is_gt
    elif mode == "ge":
        pat, cm, base, cmp = [[-1, 128]], 1, 0, ALU.is_ge
    else:
        pat, cm, base, cmp = [[-1, 128]], 1, 0, ALU.is_gt
    kb.op("pool", lambda e: e.affine_select(out=t[:], in_=t[:], pattern=pat, compare_op=cmp, fill=0.0,
                                             base=base, channel_multiplier=cm), reads=[name], writes=[name])
    return t


def build_mod():
    kb = KB()
    NCOL = 3072
    condT = kb.dram_in("condT", [128, 16, 3])
    w = kb.dram_in("w", [D, NCOL])
    bias = kb.dram_in("bias", [3, NCOL])
    out = kb.dram_out("out", [3, NCOL])
    ct = kb.sb("ct", [128, 16, 3])
    sg = kb.sb("sg", [128, 16, 3])
    bt = kb.sb("bt", [3, NCOL])
    ot = kb.sb("ot", [3, NCOL])
    kb.dma("sp", ct[:], condT, writes=["ct"])
    kb.dma("sp", bt[:], bias, writes=["bt"])
    kb.op("act", lambda e: e.activation(out=sg[:], in_=ct[:], func=AF.Sigmoid), reads=["ct"], writes=["sg"])
    kb.op("dve", lambda e: e.tensor_tensor(out=ct[:], in0=ct[:], in1=sg[:], op=ALU.mult), reads=["ct", "sg"], writes=["ct"])
    wv = w.rearrange("(kc p) n -> p kc n", p=128)
    wb = [kb.sb(f"wb{i}", [128, 16, 512]) for i in range(2)]
    pp = [kb.ps(f"pp{i}", [3, 512]) for i in range(2)]
    for nb in range(NCOL // 512):
        wt = wb[nb % 2]
        wk = f"wb{nb % 2}"
        for h in range(2):
            kb.dma("sp", wt[:, h * 8:(h + 1) * 8, :], wv[:, h * 8:(h + 1) * 8, nb * 512:(nb + 1) * 512], writes=[wk + f"h{h}"])
        p = pp[nb % 2]
        pk = f"pp{nb % 2}"
        for kc in range(16):
            kb.op("pe", lambda e, kc=kc: e.matmul(p[:], lhsT=ct[:, kc, :], rhs=wt[:, kc, :], start=(kc == 0), stop=(kc == 15)),
                  reads=["ct", wk + f"h{kc // 8}"], writes=[pk])
        kb.op("dve", lambda e: e.tensor_tensor(out=ot[:, nb * 512:(nb + 1) * 512], in0=p[:], in1=bt[:, nb * 512:(nb + 1) * 512], op=ALU.add),
              reads=[pk, "bt"], writes=["ot"])
    kb.dma("sp", out, ot[:], reads=["ot"], is_output=True)
    return kb


def run_mod(c, c_ctx, ada_w, ada_b):
    cond = np.concatenate([c, c_ctx[None, :]], axis=0).astype(np.float32)
    condT = np.ascontiguousarray(cond.reshape(3, 16, 128).transpose(2, 1, 0))
    maps = []
    for core in range(NCORES):
        l, q = divmod(core, 4)
        sl = slice(q * 3072, (q + 1) * 3072)
        maps.append({"condT": condT, "w": np.ascontiguousarray(ada_w[l][:, sl]),
                     "bias": np.ascontiguousarray(np.broadcast_to(ada_b[l][sl], (3, 3072)))})
    res = run(build_mod(), maps)
    mod = np.zeros((DEPTH, 3, 6 * D), np.float32)
    for core in range(NCORES):
        l, q = divmod(core, 4)
        mod[l][:, q * 3072:(q + 1) * 3072] = res[core]["out"]
    return mod


NT = 9
ROWS = NT * 128


def rms_rstd(kb, x, xk, junk, rstd, key, n=D, eps=EPS):
    kb.op("act", lambda e: e.activation(out=junk, in_=x, func=AF.Square, accum_out=rstd), reads=[xk], writes=["junk", key])
    kb.op("act", lambda e: e.activation(out=rstd, in_=rstd, func=AF.Sqrt, scale=1.0 / n, bias=eps), reads=[key], writes=[key])
    kb.op("dve", lambda e: e.reciprocal(out=rstd, in_=rstd), reads=[key], writes=[key])


def transpose_rows(kb, idt, src, srck, dstT, dstk, pst, pstk, nchunks=16, evac="dve"):
    for c0 in range(0, nchunks, 4):
        n = min(4, nchunks - c0)
        bank = (c0 // 4) % len(pst)
        for i in range(n):
            kb.op("pe", lambda e, i=i: e.transpose(out=pst[bank][:, i, :], in_=src[:, (c0 + i) * 128:(c0 + i + 1) * 128], identity=idt[:]),
                  reads=[srck, "ident"], writes=[pstk[bank]])
        kb.op(evac, lambda e: e.tensor_copy(out=dstT[:, c0:c0 + n, :], in_=pst[bank][:, 0:n, :]), reads=[pstk[bank]], writes=[dstk])


def build_rows(do_combine, mode, NOUT=N_IN):
    kb = KB()
    xin = kb.dram_in("xin", [ROWS, D])
    gvec = kb.dram_in("g", [1, D])
    if do_combine:
        parts = kb.dram_in("parts", [NCORES, ROWS, D])
        m5 = kb.dram_in("m5", [NT, D])
        xout = kb.dram_out("xout", [ROWS, D])
    if mode == "inproj":
        modrows = kb.dram_in("modrows", [NT, 2, D])
        w = kb.dram_in("w", [D, NOUT])
        out = kb.dram_out("out", [ROWS, NOUT])
    else:
        out = kb.dram_out("out", [ROWS, D])

    idt = ident(kb)
    g_bc = kb.sb("g_bc", [128, D])
    kb.dma("sp", g_bc[:], gvec[0, :].partition_broadcast(128), writes=["g_bc"])
    xt = [kb.sb(f"xt{i}", [128, D]) for i in range(2)]
    junk = kb.sb("junk", [128, D])
    rstd = kb.sb("rstd", [128, 2])
    if mode == "inproj":
        sc = kb.sb("sc", [128, D])
        sh = kb.sb("sh", [128, D])
        hT = kb.sb("hT", [128, NT, 16, 128])
        pst = [kb.ps(f"pst{i}", [128, 4, 128]) for i in range(2)]
        pstk = ["pst0", "pst1"]
    if do_combine:
        pt = [kb.sb(f"pt{i}", [128, D]) for i in range(2)]
        m5b = kb.sb("m5b", [128, D])

    for t in range(NT):
        x = xt[t % 2]
        xk = f"xt{t % 2}"
        rows = slice(t * 128, (t + 1) * 128)
        kb.dma("sp", x[:], xin[rows, :], writes=[xk])
        if do_combine:
            acc = junk
            for c in range(NCORES):
                p = pt[c % 2]
                pk = f"pt{c % 2}"
                kb.dma("sp", p[:], parts[c, rows, :], writes=[pk])
                if c == 0:
                    kb.op("pool", lambda e, p=p: e.tensor_copy(out=acc[:], in_=p[:]), reads=[pk], writes=["junk"])
                else:
                    kb.op("pool", lambda e, p=p: e.tensor_tensor(out=acc[:], in0=acc[:], in1=p[:], op=ALU.add), reads=[pk, "junk"], writes=["junk"])
            kb.dma("sp", m5b[:], m5[t, :].partition_broadcast(128), writes=["m5b"])
            kb.op("dve", lambda e: e.tensor_tensor(out=acc[:], in0=acc[:], in1=m5b[:], op=ALU.mult), reads=["junk", "m5b"], writes=["junk"])
            kb.op("dve", lambda e, x=x: e.tensor_tensor(out=x[:], in0=x[:], in1=acc[:], op=ALU.add), reads=["junk", xk], writes=[xk])
            kb.dma("pool", xout[rows, :], x[:], reads=[xk], is_output=True)
        rk = "rstd"
        rms_rstd(kb, x[:], xk, junk[:], rstd[:, 0:1], rk)
        if mode == "inproj":
            kb.dma("sp", sh[:], modrows[t, 0, :].partition_broadcast(128), writes=["sh"])
            kb.dma("sp", sc[:], modrows[t, 1, :].partition_broadcast(128), writes=["sc"])
            kb.op("dve", lambda e: e.scalar_tensor_tensor(out=sc[:], in0=sc[:], scalar=1.0, in1=g_bc[:], op0=ALU.add, op1=ALU.mult),
                  reads=["sc", "g_bc"], writes=["sc"])
            kb.op("dve", lambda e, x=x: e.scalar_tensor_tensor(out=x[:], in0=x[:], scalar=rstd[:, 0:1], in1=sc[:], op0=ALU.mult, op1=ALU.mult),
                  reads=[xk, rk, "sc"], writes=[xk])
            kb.op("dve", lambda e, x=x: e.tensor_tensor(out=x[:], in0=x[:], in1=sh[:], op=ALU.add), reads=[xk, "sh"], writes=[xk])
            transpose_rows(kb, idt, x, xk, hT[:, t], f"hT{t}", pst, pstk)
        else:
            kb.op("dve", lambda e, x=x: e.scalar_tensor_tensor(out=x[:], in0=x[:], scalar=rstd[:, 0:1], in1=g_bc[:], op0=ALU.mult, op1=ALU.mult),
                  reads=[xk, rk, "g_bc"], writes=[xk])
            kb.dma("pool", out[rows, :], x[:], reads=[xk], is_output=True)

    if mode == "inproj":
        NB = 256
        wv = w.rearrange("(kc p) n -> p kc n", p=128)
        wb = [kb.sb(f"wb{i}", [128, 16, NB]) for i in range(2)]
        pp = [kb.ps(f"pp{i}", [128, NB]) for i in range(4)]
        ot = [kb.sb(f"ot{i}", [128, NB]) for i in range(4)]
        nblocks = (NOUT + NB - 1) // NB
        cnt = 0
        for nb in range(nblocks):
            n0 = nb * NB
            nw = min(NB, NOUT - n0)
            wt = wb[nb % 2]
            wk = f"wb{nb % 2}"
            kb.dma("sp", wt[:, :, 0:nw], wv[:, :, n0:n0 + nw], writes=[wk])
            for t in range(NT):
                p = pp[cnt % 4]
                pk = f"pp{cnt % 4}"
                o = ot[cnt % 4]
                ok = f"ot{cnt % 4}"
                cnt += 1
                for kc in range(16):
                    kb.op("pe", lambda e, kc=kc, p=p, wt=wt: e.matmul(p[:, 0:nw], lhsT=hT[:, t, kc, :], rhs=wt[:, kc, 0:nw], start=(kc == 0), stop=(kc == 15)),
                          reads=[f"hT{t}", wk], writes=[pk])
                ev = "act" if cnt % 2 else "dve"
                if ev == "act":
                    kb.op("act", lambda e, p=p, o=o: e.copy(out=o[:, 0:nw], in_=p[:, 0:nw]), reads=[pk], writes=[ok])
                else:
                    kb.op("dve", lambda e, p=p, o=o: e.tensor_copy(out=o[:, 0:nw], in_=p[:, 0:nw]), reads=[pk], writes=[ok])
                kb.dma("pool", out[t * 128:(t + 1) * 128, n0:n0 + nw], o[:, 0:nw], reads=[ok], is_output=True)
    return kb


TOT = B * SEQ + B * CTX


def rows_to_cores(a):
    n = a.shape[1]
    pad = np.zeros((NCORES * ROWS, n), a.dtype)
    pad[:TOT] = a
    return [np.ascontiguousarray(pad[c * ROWS:(c + 1) * ROWS]) for c in range(NCORES)]


def cores_to_rows(lst):
    return np.concatenate(lst, axis=0)[:TOT]


def tile_group(g):
    r = g * 128
    if r < SEQ:
        return 0
    if r < 2 * SEQ:
        return 1
    return 2


def mod_rows_for(modl, idxs):
    outs = []
    for c in range(NCORES):
        a = np.zeros((NT, len(idxs), D), np.float32)
        for t in range(NT):
            g = c * NT + t
            if g * 128 < TOT:
                j = tile_group(g)
                for ii, m in enumerate(idxs):
                    a[t, ii] = modl[j, m * D:(m + 1) * D]
        outs.append(a)
    return outs


class Dplr:
    def __init__(self, kb, dk, dvp, mode, has_delta, tag, consts):
        self.kb, self.dk, self.dvp, self.mode, self.hd, self.tag = kb, dk, dvp, mode, has_delta, tag
        self.c = consts
        t = tag
        sb = lambda n, s: kb.sb(f"{t}_{n}", s)
        self.r, self.kap, self.a, self.kt = sb("r", [128, dk]), sb("kap", [128, dk]), sb("a", [128, dk]), sb("kt", [128, dk])
        self.v, self.lw = sb("v", [128, dvp]), sb("lw", [128, dk])
        self.M = sb("M", [dk, dvp])
        self.ecw, self.encw, self.ecwx, self.el = sb("ecw", [128, dk]), sb("encw", [128, dk]), sb("ecwx", [128, dk]), sb("el", [128, dk])
        self.r0, self.k0 = sb("r0", [128, dk]), sb("k0", [128, dk])
        self.ap, self.ktp = sb("ap", [128, dk]), sb("ktp", [128, dk])
        self.aL, self.ktL = sb("aL", [128, dk]), sb("ktL", [128, dk])
        self.kr0T = sb("kr0T", [dk, 2, 128])
        self.krT = sb("krT", [dk, 2, 128])
        self.apT, self.ktpT = sb("apT", [dk, 128]), sb("ktpT", [dk, 128])
        self.AR, self.BR = sb("AR", [128, 256]), sb("BR", [128, 256])
        self.E2 = sb("E2", [128, 256])
        self.Mm = [sb(f"Mm{i}", [128, 128]) for i in range(2)]
        self.Nm = [sb(f"Nm{i}", [128, 128]) for i in range(2)]
        self.P = sb("P", [128, 128])
        self.negG, self.U = sb("negG", [128, dvp]), sb("U", [128, dvp])
        self.ecl = sb("ecl", [dk, 1])
        self.cwc = sb("cwc", [128, 1])

    def k(self, n):
        return f"{self.tag}_{n}"

    def init_state(self):
        self.kb.op("pool", lambda e: e.memset(self.M[:], 0.0), writes=[self.k("M")])

    def step(self, d, bank, y_cb, stop=99):
        kb, dk, dvp, k, c = self.kb, self.dk, self.dvp, self.k, self.c
        TI, TS = (c["le"], c["lt"]) if d == "f" else (c["ge"], c["gt"])
        TIk, TSk = ("m_le", "m_lt") if d == "f" else ("m_ge", "m_gt")
        mask2, mask2k = (c["mask2f"], "mask2f") if d == "f" else (c["mask2b"], "mask2b")
        tri2, tri2k = (c["tri2f"], "tri2f") if d == "f" else (c["tri2b"], "tri2b")
        b0, b0k = bank["b0"]
        b1, b1k = bank["b1"]
        b2, b2k = bank["b2"]
        b3, b3k = bank["b3"]
        b4, b4k = bank["b4"]
        b5, b5k = bank["b5"]
        b6, b6k = bank["b6"]
        b7, b7k = bank["b7"]
        MUL, ADD, SUB = ALU.mult, ALU.add, ALU.subtract
        kb.op("pe", lambda e: e.matmul(b0[:, 0:dk], lhsT=TI[:], rhs=self.lw[:], start=True, stop=True), reads=[TIk, k("lw")], writes=[b0k])
        kb.op("pe", lambda e: e.matmul(b0[:, dk:2 * dk], lhsT=c["ones"][:], rhs=self.lw[:], start=True, stop=True), reads=["m_ones", k("lw")], writes=[b0k])
        kb.op("pe", lambda e: e.matmul(b0[0:dk, 2 * dk:2 * dk + 1], lhsT=self.lw[:], rhs=c["ones"][:, 0:1], start=True, stop=True), reads=["m_ones", k("lw")], writes=[b0k])
        cw, cwl = b0[:, 0:dk], b0[:, dk:2 * dk]
        kb.op("act", lambda e: e.activation(out=self.ecw[:], in_=cw, func=AF.Exp), reads=[b0k], writes=[k("ecw")])
        kb.op("act", lambda e: e.activation(out=self.ecl[:], in_=b0[0:dk, 2 * dk:2 * dk + 1], func=AF.Exp), reads=[b0k], writes=[k("ecl")])
        kb.op("dve", lambda e: e.tensor_tensor(out=self.ecwx[:], in0=cw, in1=self.lw[:], op=SUB), reads=[b0k, k("lw")], writes=[k("ecwx")])
        kb.op("act", lambda e: e.activation(out=self.ecwx[:], in_=self.ecwx[:], func=AF.Exp), reads=[k("ecwx")], writes=[k("ecwx")])
        kb.op("dve", lambda e: e.tensor_copy(out=self.el[:], in_=cw), reads=[b0k], writes=[k("el")])
        kb.op("dve", lambda e: e.tensor_tensor(out=self.el[:], in0=cwl, in1=self.el[:], op=SUB), reads=[b0k, k("el")], writes=[k("el")])
        kb.op("act", lambda e: e.activation(out=self.el[:], in_=self.el[:], func=AF.Exp), reads=[k("el")], writes=[k("el")])
        kb.op("dve", lambda e: e.tensor_tensor(out=self.r0[:], in0=self.r[:], in1=self.ecw[:], op=MUL), reads=[k("r"), k("ecw")], writes=[k("r0")])
        kb.op("dve", lambda e: e.tensor_tensor(out=self.k0[:], in0=self.kap[:], in1=self.ecwx[:], op=MUL), reads=[k("kap"), k("ecwx")], writes=[k("k0")])
        kb.op("pool", lambda e: e.tensor_tensor(out=self.ktL[:], in0=self.kt[:], in1=self.el[:], op=MUL), reads=[k("kt"), k("el")], writes=[k("ktL")])
        if self.hd:
            kb.op("pool", lambda e: e.tensor_tensor(out=self.aL[:], in0=self.a[:], in1=self.el[:], op=MUL), reads=[k("a"), k("el")], writes=[k("aL")])
        if self.mode == "V":
            kb.op("act", lambda e: e.activation(out=self.encw[:], in_=cw, func=AF.Exp, scale=-1.0), reads=[b0k], writes=[k("encw")])
            kb.op("dve", lambda e: e.tensor_tensor(out=self.ktp[:], in0=self.kt[:], in1=self.encw[:], op=MUL), reads=[k("kt"), k("encw")], writes=[k("ktp")])
            if self.hd:
                kb.op("dve", lambda e: e.tensor_tensor(out=self.ap[:], in0=self.a[:], in1=self.encw[:], op=MUL), reads=[k("a"), k("encw")], writes=[k("ap")])
            ktp_src, ktp_k, ap_src, ap_k = self.ktp, k("ktp"), self.ap, k("ap")
        else:
            kb.op("dve", lambda e: e.tensor_copy(out=self.cwc[:], in_=b0[:, 0:1]), reads=[b0k], writes=[k("cwc")])
            ktp_src, ktp_k, ap_src, ap_k = self.kt, k("kt"), self.a, k("a")
        if stop < 3:
            return
        idt = c["ident"]

        def tr(slot, src, srck):
            kb.op("pe", lambda e: e.transpose(out=b1[0:dk, slot * 128:(slot + 1) * 128], in_=src[:], identity=idt[:]), reads=[srck, "ident"], writes=[b1k])
        tr(0, self.k0, k("k0"))
        tr(1, self.r0, k("r0"))
        tr(2, ktp_src, ktp_k)
        if self.hd:
            tr(3, ap_src, ap_k)
        kb.op("dve", lambda e: e.tensor_copy(out=self.kr0T[:].rearrange("p a b -> p (a b)"), in_=b1[0:dk, 0:256]), reads=[b1k], writes=[k("kr0T")])
        kb.op("dve", lambda e: e.tensor_copy(out=self.ktpT[:], in_=b1[0:dk, 256:384]), reads=[b1k], writes=[k("ktpT")])
        if self.hd:
            kb.op("dve", lambda e: e.tensor_copy(out=self.apT[:], in_=b1[0:dk, 384:512]), reads=[b1k], writes=[k("apT")])
        if self.mode == "S":
            tr(0, self.kap, k("kap"))
            tr(1, self.r, k("r"))
            kb.op("dve", lambda e: e.tensor_copy(out=self.krT[:].rearrange("p a b -> p (a b)"), in_=b1[0:dk, 0:256]), reads=[b1k], writes=[k("krT")])
            rhs2, rhs2k = self.krT, k("krT")
        else:
            rhs2, rhs2k = self.kr0T, k("kr0T")
        rhs2f = rhs2[:].rearrange("p a b -> p (a b)")
        if stop < 4:
            return
        if self.hd:
            kb.op("pe", lambda e: e.matmul(b2[:, 0:256], lhsT=self.apT[:], rhs=rhs2f, start=True, stop=True), reads=[k("apT"), rhs2k], writes=[b2k])
        kb.op("pe", lambda e: e.matmul(b2[:, 256:512], lhsT=self.ktpT[:], rhs=rhs2f, start=True, stop=True), reads=[k("ktpT"), rhs2k], writes=[b2k])
        if self.mode == "S":
            kb.op("pe", lambda e: e.matmul(b3[:, 0:256], lhsT=self.lw[:], rhs=tri2[:], start=True, stop=True), reads=[k("lw"), tri2k], writes=[b3k])
            kb.op("dve", lambda e: e.tensor_scalar(out=self.E2[:], in0=b3[:, 0:256], scalar1=self.cwc[:, 0:1], scalar2=0.0, op0=SUB, op1=ALU.min),
                  reads=[b3k, k("cwc")], writes=[k("E2")])
            kb.op("act", lambda e: e.activation(out=self.E2[:], in_=self.E2[:], func=AF.Exp), reads=[k("E2")], writes=[k("E2")])
            kb.op("pool", lambda e: e.tensor_tensor(out=self.E2[:], in0=self.E2[:], in1=mask2[:], op=MUL), reads=[k("E2"), mask2k], writes=[k("E2")])
            mm2, mm2k = self.E2, k("E2")
        else:
            mm2, mm2k = mask2, mask2k
        if self.hd:
            kb.op("dve", lambda e: e.tensor_tensor(out=self.AR[:], in0=b2[:, 0:256], in1=mm2[:], op=MUL), reads=[b2k, mm2k], writes=[k("AR")])
        kb.op("dve", lambda e: e.tensor_tensor(out=self.BR[:], in0=b2[:, 256:512], in1=mm2[:], op=MUL), reads=[b2k, mm2k], writes=[k("BR")])
        Mk = k("M")
        if stop < 5:
            return
        if self.hd:
            M0, N0 = self.Mm[0], self.Nm[0]
            kb.op("dve", lambda e: e.tensor_scalar(out=M0[:], in0=self.AR[:, 0:128], scalar1=-1.0, scalar2=None, op0=MUL), reads=[k("AR")], writes=[k("Mm0")])
            kb.op("pe", lambda e: e.transpose(out=b4[:, 0:128], in_=M0[:], identity=idt[:]), reads=[k("Mm0"), "ident"], writes=[b4k])
            kb.op("act", lambda e: e.copy(out=N0[:], in_=b4[:, 0:128]), reads=[b4k], writes=[k("Nm0")])
            kb.op("pool", lambda e: e.tensor_tensor(out=self.P[:], in0=M0[:], in1=idt[:], op=ADD), reads=[k("Mm0"), "ident"], writes=[k("P")])
            cur = 0
            for lvl in range(6):
                Mc, Nc, Mn, Nn = self.Mm[cur], self.Nm[cur], self.Mm[1 - cur], self.Nm[1 - cur]
                Mck, Nck, Mnk, Nnk = k(f"Mm{cur}"), k(f"Nm{cur}"), k(f"Mm{1 - cur}"), k(f"Nm{1 - cur}")
                last = lvl == 5
                kb.op("pe", lambda e, Mc=Mc, Nc=Nc: e.matmul(b4[:, 0:128], lhsT=Nc[:], rhs=Mc[:], start=True, stop=True), reads=[Mck, Nck], writes=[b4k])
                kb.op("pe", lambda e, Mc=Mc, Nc=Nc: e.matmul(b4[:, 128:256], lhsT=Mc[:], rhs=Nc[:], start=True, stop=True), reads=[Mck, Nck], writes=[b4k])
                if lvl > 0:
                    kb.op("pe", lambda e, Nc=Nc: e.matmul(b4[:, 256:384], lhsT=Nc[:], rhs=self.P[:], start=True, stop=True), reads=[Nck, k("P")], writes=[b4k])
                kb.op("dve", lambda e, Mn=Mn: e.tensor_copy(out=Mn[:], in_=b4[:, 0:128]), reads=[b4k], writes=[Mnk])
                kb.op("act", lambda e, Nn=Nn: e.copy(out=Nn[:], in_=b4[:, 128:256]), reads=[b4k], writes=[Nnk])
                if lvl > 0:
                    kb.op("dve", lambda e: e.tensor_tensor(out=self.P[:], in0=b4[:, 256:384], in1=self.P[:], op=ADD), reads=[b4k, k("P")], writes=[k("P")])
                cur = 1 - cur
            Nc, Nck = self.Nm[cur], k(f"Nm{cur}")
            kb.op("pe", lambda e, Nc=Nc: e.matmul(b4[:, 256:384], lhsT=Nc[:], rhs=self.P[:], start=True, stop=True), reads=[Nck, k("P")], writes=[b4k])
            kb.op("dve", lambda e: e.tensor_tensor(out=self.P[:], in0=b4[:, 256:384], in1=self.P[:], op=ADD), reads=[b4k, k("P")], writes=[k("P")])
            kb.op("pe", lambda e: e.matmul(b5[:, 0:dvp], lhsT=self.kr0T[:, 0, :], rhs=self.M[:], start=True, stop=False), reads=[k("kr0T"), Mk], writes=[b5k])
            kb.op("pe", lambda e: e.matmul(b5[:, 0:dvp], lhsT=self.BR[:, 0:128], rhs=self.v[:], start=False, stop=True), reads=[k("BR"), k("v")], writes=[b5k])
            kb.op("dve", lambda e: e.tensor_scalar(out=self.negG[:], in0=b5[:, 0:dvp], scalar1=-1.0, scalar2=None, op0=MUL), reads=[b5k], writes=[k("negG")])
            kb.op("pe", lambda e: e.matmul(b5[:, 256:256 + dvp], lhsT=self.P[:], rhs=self.negG[:], start=True, stop=True), reads=[k("P"), k("negG")], writes=[b5k])
            kb.op("act", lambda e: e.copy(out=self.U[:], in_=b5[:, 256:256 + dvp]), reads=[b5k], writes=[k("U")])
        if stop < 7:
            return
        kb.op("pe", lambda e: e.matmul(b6[:, 0:dvp], lhsT=self.kr0T[:, 1, :], rhs=self.M[:], start=True, stop=False), reads=[k("kr0T"), Mk], writes=[b6k])
        if self.hd:
            kb.op("pe", lambda e: e.matmul(b6[:, 0:dvp], lhsT=self.AR[:, 128:256], rhs=self.U[:], start=False, stop=False), reads=[k("AR"), k("U")], writes=[b6k])
        kb.op("pe", lambda e: e.matmul(b6[:, 0:dvp], lhsT=self.BR[:, 128:256], rhs=self.v[:], start=False, stop=True), reads=[k("BR"), k("v")], writes=[b6k])
        y_cb(b6[:, 0:dvp], b6k)
        if stop < 8:
            return
        if self.hd:
            kb.op("pe", lambda e: e.matmul(b7[0:dk, 0:dvp], lhsT=self.aL[:], rhs=self.U[:], start=True, stop=False), reads=[k("aL"), k("U")], writes=[b7k])
        kb.op("pe", lambda e: e.matmul(b7[0:dk, 0:dvp], lhsT=self.ktL[:], rhs=self.v[:], start=(not self.hd), stop=True), reads=[k("ktL"), k("v")], writes=[b7k])
        kb.op("dve", lambda e: e.scalar_tensor_tensor(out=self.M[:], in0=self.M[:], scalar=self.ecl[:, 0:1], in1=b7[0:dk, 0:dvp], op0=MUL, op1=ADD),
              reads=[Mk, k("ecl"), b7k], writes=[Mk])


def dplr_consts(kb):
    c = {"ident": ident(kb)}
    for m in ("le", "lt", "ge", "gt", "ones"):
        c[m] = tri_mask(kb, "m_" + m, m)
    for d, (ms, mi) in (("f", ("lt", "le")), ("b", ("gt", "ge"))):
        t = kb.sb("mask2" + d, [128, 256])
        kb.op("pool", lambda e, t=t, ms=ms: e.tensor_copy(out=t[:, 0:128], in_=c[ms][:]), reads=["m_" + ms], writes=["mask2" + d])
        kb.op("pool", lambda e, t=t, mi=mi: e.tensor_copy(out=t[:, 128:256], in_=c[mi][:]), reads=["m_" + mi], writes=["mask2" + d])
        c["mask2" + d] = t
        c["tri2" + d] = t
    return c


def dplr_banks(kb):
    return {f"b{i}": (kb.ps(f"bank{i}", [128, 512]), f"bank{i}") for i in range(8)}


def dplr_banks2(kb):
    sets = []
    for s_ in range(2):
        t = [(kb.ps(f"bank{s_}_{i}", [128, 512]), f"bank{s_}_{i}") for i in range(4)]
        sets.append({f"b{i}": t[i % 4] for i in range(8)})
    return sets


def build_dplr_test(T, dk, dvp, mode, has_delta):
    kb = KB()
    nch = T // 128
    ins = {n: kb.dram_in(n, [T, dk]) for n in ("r", "kap", "a", "kt", "lw")}
    ins["v"] = kb.dram_in("v", [T, dvp])
    outs = {d: kb.dram_out("y" + d, [T, dvp]) for d in "fb"}
    c = dplr_consts(kb)
    banks = dplr_banks(kb)
    sc = Dplr(kb, dk, dvp, mode, has_delta, "s", c)
    yo = kb.sb("yo", [128, dvp])
    for d in "fb":
        sc.init_state()
        order = range(nch) if d == "f" else range(nch - 1, -1, -1)
        for ci in order:
            rows = slice(ci * 128, (ci + 1) * 128)
            for n in ("r", "kap", "a", "kt", "lw", "v"):
                kb.dma("sp", getattr(sc, n)[:], ins[n][rows, :], writes=[sc.k(n)])

            def cb(yp, ypk):
                kb.op("dve", lambda e: e.tensor_copy(out=yo[:], in_=yp), reads=[ypk], writes=["yo"])
                kb.dma("pool", outs[d][rows, :], yo[:], reads=["yo"], is_output=True)
            sc.step(d, banks, cb)
    return kb


def TT(kb, e, out, in0, in1, op, reads, writes):
    return kb.op(e, lambda g: g.tensor_tensor(out=out, in0=in0, in1=in1, op=op), reads=reads, writes=writes)


def TS(kb, e, out, in0, s1, s2, op0, op1, reads, writes):
    if op1 is None:
        return kb.op(e, lambda g: g.tensor_scalar(out=out, in0=in0, scalar1=s1, scalar2=None, op0=op0), reads=reads, writes=writes)
    return kb.op(e, lambda g: g.tensor_scalar(out=out, in0=in0, scalar1=s1, scalar2=s2, op0=op0, op1=op1), reads=reads, writes=writes)


def STT(kb, out, in0, scalar, in1, op0, op1, reads, writes):
    return kb.op("dve", lambda g: g.scalar_tensor_tensor(out=out, in0=in0, scalar=scalar, in1=in1, op0=op0, op1=op1), reads=reads, writes=writes)


def ACT(kb, out, in_, func, reads, writes, **kw):
    return kb.op("act", lambda g: g.activation(out=out, in_=in_, func=func, **kw), reads=reads, writes=writes)


def CP(kb, e, out, in_, reads, writes):
    if e == "act":
        return kb.op("act", lambda g: g.copy(out=out, in_=in_), reads=reads, writes=writes)
    return kb.op(e, lambda g: g.tensor_copy(out=out, in_=in_), reads=reads, writes=writes)


def sumsq_rs(kb, x, xk, junk, junkk, out, outk, n, eps, mean=True):
    ACT(kb, junk, x, AF.Square, [xk], [junkk, outk], accum_out=out)
    ACT(kb, out, out, AF.Sqrt, [outk], [outk], scale=(1.0 / n if mean else 1.0), bias=eps)
    kb.op("dve", lambda g: g.reciprocal(out=out, in_=out), reads=[outk], writes=[outk])


def layernorm_rs(kb, x, xk, st, stk, n, eps):
    kb.op("dve", lambda g: g.bn_stats(out=st[:, 2:8], in_=x), reads=[xk], writes=[stk])
    kb.op("dve", lambda g: g.bn_aggr(out=st[:, 0:2], in_=st[:, 2:8]), reads=[stk], writes=[stk])
    ACT(kb, st[:, 1:2], st[:, 1:2], AF.Sqrt, [stk], [stk], bias=eps, scale=1.0)
    kb.op("dve", lambda g: g.reciprocal(out=st[:, 1:2], in_=st[:, 1:2]), reads=[stk], writes=[stk])


def shared_yacc(kb, tag="yacc"):
    if not hasattr(kb, "_yacc"):
        kb._yacc = {}
    if tag not in kb._yacc:
        kb._yacc[tag] = kb.sb(tag, [128, (CTX + SEQ) // 128, 128])
    return kb._yacc[tag]


LSEQ = CTX + SEQ
NCH = LSEQ // 128
FWD_ORDER = list(range(NCH))
BWD_ORDER = [1, 0] + list(range(NCH - 1, 1, -1))


def emit_rwkv(kb, c, banks):
    MUL, ADD, SUB = ALU.mult, ALU.add, ALU.subtract
    X = {n: kb.dram_in("rw_" + n, [LSEQ, 640]) for n in ("cur", "prev", "next")}
    cst_d = kb.dram_in("rw_cst", [128, 2 * 640 + 128 * 9])
    wup_d = kb.dram_in("rw_wup", [2, 64, 128])
    aup_d = kb.dram_in("rw_aup", [2, 64, 128])
    gup_d = kb.dram_in("rw_gup", [128, 128])
    out = kb.dram_out("rw_out", [LSEQ, 128])
    cst = kb.sb("rw_cst_s", [128, 2 * 640 + 128 * 9])
    kb.dma("sp", cst[:], cst_d, writes=["rw_cst"])
    mu0, mu1 = cst[:, 0:640], cst[:, 640:1280]
    o = 1280
    k_k, k_a, r_k, ln_w, ln_b = (cst[:, o + i * 128:o + (i + 1) * 128] for i in range(5))
    w0 = [cst[:, o + (5 + d) * 128:o + (6 + d) * 128] for d in range(2)]
    a0 = [cst[:, o + (7 + d) * 128:o + (8 + d) * 128] for d in range(2)]
    wup = kb.sb("rw_wup_s", [64, 2, 128])
    aup = kb.sb("rw_aup_s", [64, 2, 128])
    gup = kb.sb("rw_gup_s", [128, 128])
    kb.dma("sp", wup[:], wup_d.rearrange("d r n -> r d n"), writes=["rw_wup"])
    kb.dma("sp", aup[:], aup_d.rearrange("d r n -> r d n"), writes=["rw_aup"])
    kb.dma("sp", gup[:], gup_d, writes=["rw_gup"])
    cur, prv, nxt = kb.sb("rw_cur_s", [128, 640]), kb.sb("rw_prv", [128, 640]), kb.sb("rw_nxt", [128, 640])
    kkp, kk = kb.sb("rw_kkp", [128, 128]), kb.sb("rw_kk", [128, 128])
    junk = kb.sb("rw_junk", [128, 128])
    st = kb.sb("rw_st", [128, 8])
    twd = kb.sb("rw_twd", [128, 128])
    twdT = kb.sb("rw_twdT", [64, 2, 128])
    sg = kb.sb("rw_sg", [128, 128])
    sgT = kb.sb("rw_sgT", [128, 128])
    lwt, at, ktt, alt = kb.sb("rw_lw", [128, 128]), kb.sb("rw_a", [128, 128]), kb.sb("rw_kt", [128, 128]), kb.sb("rw_al", [128, 128])
    gt = kb.sb("rw_g", [128, 128])
    bon = kb.sb("rw_bon", [128, 4])
    yacc = shared_yacc(kb, "yaccR")
    yn = kb.sb("rw_yn", [128, 128])
    sc = [Dplr(kb, 64, 64, "V", True, f"rw{h}", c) for h in range(2)]
    b1, b1k = banks["b1"]
    b3, b3k = banks["b3"]
    idt = c["ident"]

    def prep_common(ci):
        rows = slice(ci * 128, (ci + 1) * 128)
        kb.dma("sp", cur[:], X["cur"][rows, :], writes=["rw_cur"])
        kb.dma("sp", prv[:], X["prev"][rows, :], writes=["rw_prv"])
        kb.dma("sp", nxt[:], X["next"][rows, :], writes=["rw_nxt"])
        TT(kb, "dve", prv[:], prv[:], cur[:], SUB, ["rw_prv", "rw_cur"], ["rw_prv"])
        TT(kb, "pool", nxt[:], nxt[:], cur[:], SUB, ["rw_nxt", "rw_cur"], ["rw_nxt"])
        TT(kb, "dve", prv[:], prv[:], mu0, MUL, ["rw_prv", "rw_cst"], ["rw_prv"])
        TT(kb, "pool", nxt[:], nxt[:], mu1, MUL, ["rw_nxt", "rw_cst"], ["rw_nxt"])
        TT(kb, "dve", cur[:], cur[:], prv[:], ADD, ["rw_prv", "rw_cur"], ["rw_cur"])
        TT(kb, "dve", cur[:], cur[:], nxt[:], ADD, ["rw_nxt", "rw_cur"], ["rw_cur"])
        TT(kb, "dve", kkp[:], cur[:, 128:256], k_k, MUL, ["rw_cur", "rw_cst"], ["rw_kkp"])
        for h in range(2):
            ACT(kb, junk[:, 0:64], kkp[:, h * 64:(h + 1) * 64], AF.Square, ["rw_kkp"], ["rw_junk", "rw_st"], accum_out=st[:, h:h + 1])
        ACT(kb, st[:, 0:2], st[:, 0:2], AF.Sqrt, ["rw_st"], ["rw_st"], bias=1e-6, scale=1.0)
        kb.op("dve", lambda g: g.reciprocal(out=st[:, 0:2], in_=st[:, 0:2]), reads=["rw_st"], writes=["rw_st"])
        for h in range(2):
            TS(kb, "dve", kk[:, h * 64:(h + 1) * 64], kkp[:, h * 64:(h + 1) * 64], st[:, h:h + 1], None, MUL, None, ["rw_kkp", "rw_st"], ["rw_kk"])
        ACT(kb, twd[:, 0:64], cur[:, 384:448], AF.Tanh, ["rw_cur"], ["rw_twd"])
        CP(kb, "pool", twd[:, 64:128], cur[:, 448:512], ["rw_cur"], ["rw_twd"])
        for i in range(2):
            kb.op("pe", lambda g, i=i: g.transpose(out=b1[0:64, i * 128:(i + 1) * 128], in_=twd[:, i * 64:(i + 1) * 64], identity=idt[:]),
                  reads=["rw_twd", "ident"], writes=[b1k])
        CP(kb, "dve", twdT[:].rearrange("p a b -> p (a b)"), b1[0:64, 0:256], [b1k], ["rw_twdT"])

    def prep_dir(d):
        kb.op("pe", lambda g: g.matmul(b3[:, 0:128], lhsT=twdT[:, 0, :], rhs=wup[:, d, :], start=True, stop=True), reads=["rw_twdT", "rw_wup"], writes=[b3k])
        kb.op("pe", lambda g: g.matmul(b3[:, 128:256], lhsT=twdT[:, 1, :], rhs=aup[:, d, :], start=True, stop=True), reads=["rw_twdT", "rw_aup"], writes=[b3k])
        TT(kb, "dve", lwt[:], b3[:, 0:128], w0[d], ADD, [b3k, "rw_cst"], ["rw_lw"])
        TT(kb, "dve", at[:], b3[:, 128:256], a0[d], ADD, [b3k, "rw_cst"], ["rw_a"])
        ACT(kb, lwt[:], lwt[:], AF.Sigmoid, ["rw_lw"], ["rw_lw"])
        ACT(kb, at[:], at[:], AF.Sigmoid, ["rw_a"], ["rw_a"])
        TS(kb, "pool", lwt[:], lwt[:], -math.exp(-0.5), None, MUL, None, ["rw_lw"], ["rw_lw"])
        STT(kb, ktt[:], at[:], -1.0, k_a, ADD, MUL, ["rw_a", "rw_cst"], ["rw_kt"])
        STT(kb, ktt[:], ktt[:], 1.0, cur[:, 128:256], ADD, MUL, ["rw_kt", "rw_cur"], ["rw_kt"])

    def run_dir(d, order):
        for s in sc:
            s.init_state()
        for ci in order:
            prep_common(ci)
            prep_dir(d)
            TT(kb, "pool", alt[:], kk[:], at[:], MUL, ["rw_kk", "rw_a"], ["rw_al"])
            for h in range(2):
                s = sc[h]
                hs = slice(h * 64, (h + 1) * 64)
                CP(kb, "pool", s.r[:], cur[:, hs], ["rw_cur"], [s.k("r")])
                CP(kb, "pool", s.v[:], cur[:, 256 + h * 64:256 + (h + 1) * 64], ["rw_cur"], [s.k("v")])
                CP(kb, "pool", s.kap[:], kk[:, hs], ["rw_kk"], [s.k("kap")])
                CP(kb, "pool", s.a[:], alt[:, hs], ["rw_al"], [s.k("a")])
                CP(kb, "pool", s.kt[:], ktt[:, hs], ["rw_kt"], [s.k("kt")])
                CP(kb, "pool", s.lw[:], lwt[:, hs], ["rw_lw"], [s.k("lw")])

                def cb(yp, ypk, h=h, ci=ci):
                    dst = yacc[:, ci, h * 64:(h + 1) * 64]
                    if d == 0:
                        CP(kb, "act", dst, yp, [ypk], ["yaccR"])
                    else:
                        TT(kb, "dve", dst, yp, dst, ADD, [ypk, "yaccR"], ["yaccR"])
                s.step("f" if d == 0 else "b", banks, cb)

    run_dir(0, FWD_ORDER)
    run_dir(1, BWD_ORDER)
    for ci in range(NCH):
        prep_common(ci)
        ACT(kb, sg[:], cur[:, 512:640], AF.Sigmoid, ["rw_cur"], ["rw_sg"])
        kb.op("pe", lambda g: g.transpose(out=b1[:, 0:128], in_=sg[:], identity=idt[:]), reads=["rw_sg", "ident"], writes=[b1k])
        CP(kb, "dve", sgT[:], b1[:, 0:128], [b1k], ["rw_sgT"])
        kb.op("pe", lambda g: g.matmul(b1[:, 128:256], lhsT=sgT[:], rhs=gup[:], start=True, stop=True), reads=["rw_sgT", "rw_gup"], writes=[b1k])
        CP(kb, "act", gt[:], b1[:, 128:256], [b1k], ["rw_g"])
        for d in range(2):
            prep_dir(d)
            TT(kb, "dve", junk[:], cur[:, 0:128], ktt[:], MUL, ["rw_cur", "rw_kt"], ["rw_junk"])
            TT(kb, "dve", junk[:], junk[:], r_k, MUL, ["rw_junk", "rw_cst"], ["rw_junk"])
            kb.op("dve", lambda g, d=d: g.tensor_reduce(out=bon[:, 2 * d:2 * d + 2], in_=junk[:].rearrange("p (h n) -> p h n", h=2), axis=AX.X, op=ADD),
                  reads=["rw_junk"], writes=["rw_bon"])
        TT(kb, "dve", bon[:, 0:2], bon[:, 0:2], bon[:, 2:4], ADD, ["rw_bon"], ["rw_bon"])
        for h in range(2):
            hs = slice(h * 64, (h + 1) * 64)
            layernorm_rs(kb, yacc[:, ci, hs], "yaccR", st, "rw_st", 64, 64e-5)
            TS(kb, "dve", yn[:, hs], yacc[:, ci, hs], st[:, 0:1], st[:, 1:2], SUB, MUL, ["yaccR", "rw_st"], ["rw_yn"])
        TT(kb, "dve", yn[:], yn[:], ln_w, MUL, ["rw_yn", "rw_cst"], ["rw_yn"])
        TT(kb, "dve", yn[:], yn[:], ln_b, ADD, ["rw_yn", "rw_cst"], ["rw_yn"])
        for h in range(2):
            hs = slice(h * 64, (h + 1) * 64)
            STT(kb, yn[:, hs], cur[:, 256 + h * 64:256 + (h + 1) * 64], bon[:, h:h + 1], yn[:, hs], MUL, ADD, ["rw_cur", "rw_bon", "rw_yn"], ["rw_yn"])
        TT(kb, "dve", yn[:], yn[:], gt[:], MUL, ["rw_yn", "rw_g"], ["rw_yn"])
        kb.dma("pool", out[ci * 128:(ci + 1) * 128, :], yn[:], reads=["rw_yn"], is_output=True)


def seq_rows(pl_all, b):
    lat = pl_all[b * SEQ:(b + 1) * SEQ]
    cx = pl_all[2 * SEQ + b * CTX:2 * SEQ + (b + 1) * CTX]
    return cx, lat


def shifted(cx, lat, sh):
    def s(x):
        o = np.zeros_like(x)
        if sh < 0:
            o[1:] = x[:-1]
        else:
            o[:-1] = x[1:]
        return o
    return np.concatenate([s(cx), s(lat)], 0)


def rep(v):
    return np.ascontiguousarray(np.broadcast_to(np.asarray(v, np.float32).reshape(1, -1), (128, np.asarray(v).size)))


def rwkv_inputs(pl_all, p, b, j):
    cx, lat = seq_rows(pl_all[:, 0:RW_COLS], b)
    cs = slice(j * 128, (j + 1) * 128)
    cols = np.r_[np.arange(j * 128, (j + 1) * 128), GW + np.arange(j * 128, (j + 1) * 128), 2 * GW + np.arange(j * 128, (j + 1) * 128),
                 np.arange(3 * GW, 3 * GW + 256)]
    m = {}
    m["rw_cur"] = np.ascontiguousarray(np.concatenate([cx, lat], 0)[:, cols])
    m["rw_prev"] = np.ascontiguousarray(shifted(cx, lat, -1)[:, cols])
    m["rw_next"] = np.ascontiguousarray(shifted(cx, lat, +1)[:, cols])
    cst = [rep(p["rw_mu"][0][cols]), rep(p["rw_mu"][1][cols]), rep(p["rw_k_k"][cs]), rep(p["rw_k_a"][cs]),
           rep(p["rw_r_k"].reshape(-1)[cs]), rep(p["rw_ln_w"][cs]), rep(p["rw_ln_b"][cs]),
           rep(p["rw_w0"][0][cs]), rep(p["rw_w0"][1][cs]), rep(p["rw_a0"][0][cs]), rep(p["rw_a0"][1][cs])]
    m["rw_cst"] = np.ascontiguousarray(np.concatenate(cst, 1))
    m["rw_wup"] = np.ascontiguousarray(p["rw_w_up"][:, :, cs])
    m["rw_aup"] = np.ascontiguousarray(p["rw_a_up"][:, :, cs])
    m["rw_gup"] = np.ascontiguousarray(p["rw_g_up"][:, cs])
    return m


def emit_mlstm(kb, c, banks):
    MUL, ADD, SUB = ALU.mult, ALU.add, ALU.subtract
    X = kb.dram_in("ml_x", [LSEQ, 516])
    cst_d = kb.dram_in("ml_cst", [128, 128 + 4])
    out = kb.dram_out("ml_out", [LSEQ, 128])
    cst = kb.sb("ml_cst_s", [128, 132])
    kb.dma("sp", cst[:], cst_d, writes=["ml_cst"])
    x = kb.sb("ml_xs", [128, 516])
    gs = kb.sb("ml_gs", [128, 4])
    st = kb.sb("ml_st", [128, 8])
    yacc = shared_yacc(kb)
    yn = kb.sb("ml_yn", [128, 128])
    sgo = kb.sb("ml_sgo", [128, 128])
    s = Dplr(kb, 128, 129, "S", False, "ml", c)
    kb.op("pool", lambda g: g.memset(s.v[:, 128:129], 1.0), writes=[s.k("v")])
    ones = c["ones"]

    def run_dir(d, order):
        s.init_state()
        for ci in order:
            rows = slice(ci * 128, (ci + 1) * 128)
            kb.dma("sp", x[:], X[rows, :], writes=["ml_x"])
            ACT(kb, gs[:, 0:1], x[:, 512 + d:513 + d], AF.Exp, ["ml_x", "ml_cst"], ["ml_gs"], bias=cst[:, 128 + d:129 + d], scale=1.0)
            ACT(kb, gs[:, 1:2], x[:, 514 + d:515 + d], AF.Sigmoid, ["ml_x", "ml_cst"], ["ml_gs"], bias=cst[:, 130 + d:131 + d], scale=1.0)
            ACT(kb, gs[:, 1:2], gs[:, 1:2], AF.Ln, ["ml_gs"], ["ml_gs"])
            CP(kb, "pool", s.r[:], x[:, 0:128], ["ml_x"], [s.k("r")])
            TS(kb, "dve", s.kt[:], x[:, 128:256], gs[:, 0:1], 128.0 ** -0.5, MUL, MUL, ["ml_x", "ml_gs"], [s.k("kt")])
            CP(kb, "pool", s.v[:, 0:128], x[:, 256:384], ["ml_x"], [s.k("v")])
            TS(kb, "dve", s.lw[:], ones[:], gs[:, 1:2], None, MUL, None, ["m_ones", "ml_gs"], [s.k("lw")])

            def cb(yp, ypk, ci=ci):
                TS(kb, "dve", st[:, 1:2], yp[:, 128:129], -1.0, None, MUL, None, [ypk], ["ml_st"])
                TT(kb, "dve", st[:, 0:1], yp[:, 128:129], st[:, 1:2], ALU.max, [ypk, "ml_st"], ["ml_st"])
                TS(kb, "dve", st[:, 0:1], st[:, 0:1], 1.0, None, ALU.max, None, ["ml_st"], ["ml_st"])
                kb.op("dve", lambda g: g.reciprocal(out=st[:, 0:1], in_=st[:, 0:1]), reads=["ml_st"], writes=["ml_st"])
                dst = yacc[:, ci, :]
                if d == 0:
                    TS(kb, "dve", dst, yp[:, 0:128], st[:, 0:1], None, MUL, None, [ypk, "ml_st"], ["yacc"])
                else:
                    STT(kb, dst, yp[:, 0:128], st[:, 0:1], dst, MUL, ADD, [ypk, "ml_st", "yacc"], ["yacc"])
            s.step("f" if d == 0 else "b", banks, cb)

    run_dir(0, FWD_ORDER)
    run_dir(1, BWD_ORDER)
    for ci in range(NCH):
        rows = slice(ci * 128, (ci + 1) * 128)
        kb.dma("sp", x[:], X[rows, :], writes=["ml_x"])
        layernorm_rs(kb, yacc[:, ci, :], "yacc", st, "ml_st", 128, EPS)
        TS(kb, "dve", yn[:], yacc[:, ci, :], st[:, 0:1], st[:, 1:2], SUB, MUL, ["yacc", "ml_st"], ["ml_yn"])
        TT(kb, "dve", yn[:], yn[:], cst[:, 0:128], MUL, ["ml_yn", "ml_cst"], ["ml_yn"])
        ACT(kb, sgo[:], x[:, 384:512], AF.Sigmoid, ["ml_x"], ["ml_sgo"])
        TT(kb, "dve", yn[:], yn[:], sgo[:], MUL, ["ml_yn", "ml_sgo"], ["ml_yn"])
        kb.dma("pool", out[rows, :], yn[:], reads=["ml_yn"], is_output=True)


def mlstm_inputs(pl_all, p, b, j):
    cx, lat = seq_rows(pl_all[:, RW_COLS:RW_COLS + ML_COLS], b)
    cols = np.r_[np.arange(j * 128, (j + 1) * 128), GW + np.arange(j * 128, (j + 1) * 128), 2 * GW + np.arange(j * 128, (j + 1) * 128),
                 3 * GW + np.arange(j * 128, (j + 1) * 128), 4 * GW + np.array([j, 4 + j, 8 + j, 12 + j])]
    m = {"ml_x": np.ascontiguousarray(np.concatenate([cx, lat], 0)[:, cols])}
    cst = [rep(p["ml_norm_g"][j * 128:(j + 1) * 128]), rep([p["ml_ib"][0][j], p["ml_ib"][1][j], p["ml_fb"][0][j], p["ml_fb"][1][j]])]
    m["ml_cst"] = np.ascontiguousarray(np.concatenate(cst, 1))
    return m


def emit_gdn(kb, c, banks):
    MUL, ADD, SUB = ALU.mult, ALU.add, ALU.subtract
    X = {n: kb.dram_in("gd_" + n, [LSEQ, 384]) for n in ("cur", "prev", "next")}
    G = kb.dram_in("gd_g", [LSEQ, 132])
    cst_d = kb.dram_in("gd_cst", [128, 3 * 384 + 128 + 4])
    out = kb.dram_out("gd_out", [LSEQ, 128])
    cst = kb.sb("gd_cst_s", [128, 3 * 384 + 132])
    kb.dma("sp", cst[:], cst_d, writes=["gd_cst"])
    nega = kb.sb("gd_nega", [128, 2])
    ACT(kb, nega[:], cst[:, 1280:1282], AF.Exp, ["gd_cst"], ["gd_nega"])
    TS(kb, "dve", nega[:], nega[:], -1.0, None, MUL, None, ["gd_nega"], ["gd_nega"])
    cur, prv, nxt = kb.sb("gd_cur_s", [128, 384]), kb.sb("gd_prv", [128, 384]), kb.sb("gd_nxt", [128, 384])
    gg = kb.sb("gd_gs", [128, 132])
    sg = kb.sb("gd_sg", [128, 384])
    junk = kb.sb("gd_junk", [128, 128])
    st = kb.sb("gd_st", [128, 8])
    gs = kb.sb("gd_gsc", [128, 4])
    yacc = shared_yacc(kb)
    yn = kb.sb("gd_yn", [128, 128])
    s = Dplr(kb, 128, 128, "S", True, "gd", c)
    ones = c["ones"]

    def run_dir(d, order):
        s.init_state()
        for ci in order:
            rows = slice(ci * 128, (ci + 1) * 128)
            kb.dma("sp", cur[:], X["cur"][rows, :], writes=["gd_cur"])
            kb.dma("sp", prv[:], X["prev"][rows, :], writes=["gd_prv"])
            kb.dma("sp", nxt[:], X["next"][rows, :], writes=["gd_nxt"])
            kb.dma("sp", gg[:], G[rows, :], writes=["gd_g"])
            TT(kb, "dve", cur[:], cur[:], cst[:, 384:768], MUL, ["gd_cur", "gd_cst"], ["gd_cur"])
            TT(kb, "pool", prv[:], prv[:], cst[:, 0:384], MUL, ["gd_prv", "gd_cst"], ["gd_prv"])
            TT(kb, "pool", nxt[:], nxt[:], cst[:, 768:1152], MUL, ["gd_nxt", "gd_cst"], ["gd_nxt"])
            TT(kb, "dve", cur[:], cur[:], prv[:], ADD, ["gd_cur", "gd_prv"], ["gd_cur"])
            TT(kb, "dve", cur[:], cur[:], nxt[:], ADD, ["gd_cur", "gd_nxt"], ["gd_cur"])
            ACT(kb, sg[:], cur[:], AF.Sigmoid, ["gd_cur"], ["gd_sg"])
            TT(kb, "dve", cur[:], cur[:], sg[:], MUL, ["gd_cur", "gd_sg"], ["gd_cur"])
            for i in range(2):
                ACT(kb, junk[:], cur[:, i * 128:(i + 1) * 128], AF.Square, ["gd_cur"], ["gd_junk", "gd_st"], accum_out=st[:, i:i + 1])
            ACT(kb, st[:, 0:2], st[:, 0:2], AF.Sqrt, ["gd_st"], ["gd_st"], bias=1e-6, scale=1.0)
            kb.op("dve", lambda g: g.reciprocal(out=st[:, 0:2], in_=st[:, 0:2]), reads=["gd_st"], writes=["gd_st"])
            TS(kb, "dve", s.r[:], cur[:, 0:128], st[:, 0:1], 128.0 ** -0.5, MUL, MUL, ["gd_cur", "gd_st"], [s.k("r")])
            TS(kb, "dve", s.kap[:], cur[:, 128:256], st[:, 1:2], None, MUL, None, ["gd_cur", "gd_st"], [s.k("kap")])
            CP(kb, "pool", s.v[:], cur[:, 256:384], ["gd_cur"], [s.k("v")])
            ACT(kb, gs[:, 0:1], gg[:, 128 + d:129 + d], AF.Exp, ["gd_g", "gd_cst"], ["gd_gsc"], bias=cst[:, 1282 + d:1283 + d], scale=1.0)
            ACT(kb, gs[:, 0:1], gs[:, 0:1], AF.Ln, ["gd_gsc"], ["gd_gsc"], bias=1.0, scale=1.0)
            TT(kb, "dve", gs[:, 0:1], gs[:, 0:1], nega[:, d:d + 1], MUL, ["gd_gsc", "gd_nega"], ["gd_gsc"])
            ACT(kb, gs[:, 1:2], gg[:, 130 + d:131 + d], AF.Sigmoid, ["gd_g"], ["gd_gsc"])
            ACT(kb, gs[:, 2:3], gs[:, 0:1], AF.Exp, ["gd_gsc"], ["gd_gsc"])
            TT(kb, "dve", gs[:, 2:3], gs[:, 2:3], gs[:, 1:2], MUL, ["gd_gsc"], ["gd_gsc"])
            TS(kb, "dve", s.lw[:], ones[:], gs[:, 0:1], None, MUL, None, ["m_ones", "gd_gsc"], [s.k("lw")])
            TS(kb, "dve", s.kt[:], s.kap[:], gs[:, 1:2], None, MUL, None, [s.k("kap"), "gd_gsc"], [s.k("kt")])
            TS(kb, "dve", s.a[:], s.kap[:], gs[:, 2:3], None, MUL, None, [s.k("kap"), "gd_gsc"], [s.k("a")])

            def cb(yp, ypk, ci=ci):
                dst = yacc[:, ci, :]
                if d == 0:
                    CP(kb, "act", dst, yp, [ypk], ["yacc"])
                else:
                    TT(kb, "dve", dst, yp, dst, ADD, [ypk, "yacc"], ["yacc"])
            s.step("f" if d == 0 else "b", banks, cb)

    run_dir(0, FWD_ORDER)
    run_dir(1, BWD_ORDER)
    for ci in range(NCH):
        rows = slice(ci * 128, (ci + 1) * 128)
        kb.dma("sp", gg[:], G[rows, :], writes=["gd_g"])
        sumsq_rs(kb, yacc[:, ci, :], "yacc", junk[:], "gd_junk", st[:, 0:1], "gd_st", 128, EPS)
        STT(kb, yn[:], yacc[:, ci, :], st[:, 0:1], cst[:, 1152:1280], MUL, MUL, ["yacc", "gd_st", "gd_cst"], ["gd_yn"])
        ACT(kb, sg[:, 0:128], gg[:, 0:128], AF.Sigmoid, ["gd_g"], ["gd_sg"])
        TT(kb, "dve", sg[:, 0:128], sg[:, 0:128], gg[:, 0:128], MUL, ["gd_sg", "gd_g"], ["gd_sg"])
        TT(kb, "dve", yn[:], yn[:], sg[:, 0:128], MUL, ["gd_yn", "gd_sg"], ["gd_yn"])
        kb.dma("pool", out[rows, :], yn[:], reads=["gd_yn"], is_output=True)


def gdn_inputs(pl_all, p, b, j):
    o = RW_COLS + ML_COLS
    cx, lat = seq_rows(pl_all[:, o:o + GD_COLS], b)
    cols = np.r_[np.arange(j * 128, (j + 1) * 128), GW + np.arange(j * 128, (j + 1) * 128), 2 * GW + np.arange(j * 128, (j + 1) * 128)]
    gcols = np.r_[3 * GW + np.arange(j * 128, (j + 1) * 128), 4 * GW + np.array([j, 4 + j, 8 + j, 12 + j])]
    m = {}
    m["gd_cur"] = np.ascontiguousarray(np.concatenate([cx, lat], 0)[:, cols])
    m["gd_prev"] = np.ascontiguousarray(shifted(cx, lat, -1)[:, cols])
    m["gd_next"] = np.ascontiguousarray(shifted(cx, lat, +1)[:, cols])
    m["gd_g"] = np.ascontiguousarray(np.concatenate([cx, lat], 0)[:, gcols])
    cst = [rep(p["gd_conv"][0][cols]), rep(p["gd_conv"][1][cols]), rep(p["gd_conv"][2][cols]), rep(p["gd_norm_g"]),
           rep([p["gd_a_log"][0][j], p["gd_a_log"][1][j], p["gd_dt_bias"][0][j], p["gd_dt_bias"][1][j]])]
    m["gd_cst"] = np.ascontiguousarray(np.concatenate(cst, 1))
    return m


def emit_attn(kb, c, banks, need_ctx=True):
    MUL, ADD, SUB = ALU.mult, ALU.add, ALU.subtract
    Q, Kd, V = kb.dram_in("at_q", [LSEQ, 128]), kb.dram_in("at_k", [LSEQ, 128]), kb.dram_in("at_v", [LSEQ, 128])
    COS, SIN = kb.dram_in("at_cos", [LSEQ, 128]), kb.dram_in("at_sin", [LSEQ, 128])
    cst_d = kb.dram_in("at_cst", [128, 256])
    out = kb.dram_out("at_out", [LSEQ, 128])
    cst = kb.sb("at_cst_s", [128, 256])
    kb.dma("sp", cst[:], cst_d, writes=["at_cst"])
    qT, kT = kb.sb("at_qT", [128, LSEQ]), kb.sb("at_kT", [128, LSEQ])
    va = kb.sb("at_va", [128, NCH, 129])
    kb.op("pool", lambda g: g.memset(va[:], 1.0), writes=["at_va"])
    x = kb.sb("at_x", [128, 2, 128])
    xn = kb.sb("at_xn", [128, 2, 128])
    rot = kb.sb("at_rot", [128, 2, 128])
    cs = kb.sb("at_cs", [128, 2, 128])
    junk = kb.sb("at_junk", [128, 128])
    st = kb.sb("at_st", [128, 4])
    idt = c["ident"]
    b2, b2k = banks["b1"]
    for ci in range(NCH):
        rows = slice(ci * 128, (ci + 1) * 128)
        kb.dma("sp", x[:, 0, :], Q[rows, :], writes=["at_x"])
        kb.dma("sp", x[:, 1, :], Kd[rows, :], writes=["at_x"])
        kb.dma("sp", va[:, ci, 0:128], V[rows, :], writes=["at_va"])
        kb.dma("sp", cs[:, 0, :], COS[rows, :], writes=["at_cs"])
        kb.dma("sp", cs[:, 1, :], SIN[rows, :], writes=["at_cs"])
        for i in range(2):
            sumsq_rs(kb, x[:, i, :], "at_x", junk[:], "at_junk", st[:, i:i + 1], "at_st", 128, EPS)
            STT(kb, xn[:, i, :], x[:, i, :], st[:, i:i + 1], cst[:, i * 128:(i + 1) * 128], MUL, MUL, ["at_x", "at_st", "at_cst"], ["at_xn"])
            xv = xn[:, i, :].rearrange("p (h t n) -> p h t n", h=2, t=2)
            rv = rot[:, i, :].rearrange("p (h t n) -> p h t n", h=2, t=2)
            CP(kb, "pool", rv[:, :, 0, :], xv[:, :, 1, :], ["at_xn"], ["at_rot"])
            CP(kb, "pool", rv[:, :, 1, :], xv[:, :, 0, :], ["at_xn"], ["at_rot"])
            TT(kb, "dve", xn[:, i, :], xn[:, i, :], cs[:, 0, :], MUL, ["at_xn", "at_cs"], ["at_xn"])
            TT(kb, "pool", rot[:, i, :], rot[:, i, :], cs[:, 1, :], MUL, ["at_rot", "at_cs"], ["at_rot"])
            TT(kb, "dve", xn[:, i, :], xn[:, i, :], rot[:, i, :], ADD, ["at_xn", "at_rot"], ["at_xn"])
            kb.op("pe", lambda g, i=i: g.transpose(out=b2[:, i * 128:(i + 1) * 128], in_=xn[:, i, :], identity=idt[:]), reads=["at_xn", "ident"], writes=[b2k])
        CP(kb, "dve", qT[:, rows], b2[:, 0:128], [b2k], ["at_qT"])
        CP(kb, "dve", kT[:, rows], b2[:, 128:256], [b2k], ["at_kT"])
    bS = [banks["b0"], banks["b1"]]
    bO = [banks["b2"], banks["b3"]]
    pT = [kb.sb(f"at_pT{i}", [128, 256]) for i in range(2)]
    ot = kb.sb("at_ot", [128, 128])
    blocks = []
    if need_ctx:
        blocks.append((0, 256, [0, 1]))
    for qb in range(SEQ // 256):
        blocks.append((CTX + qb * 256, 256, list(range(NCH))))
    n = 0
    for q0, qn, kts in blocks:
        nq = qn // 128
        for idx, kt in enumerate(kts):
            (bs, bsk), p, pk = bS[n % 2], pT[n % 2], f"at_pT{n % 2}"
            n += 1
            kb.op("pe", lambda g, bs=bs, kt=kt: g.matmul(bs[:, 0:qn], lhsT=kT[:, kt * 128:(kt + 1) * 128], rhs=qT[:, q0:q0 + qn], start=True, stop=True),
                  reads=["at_kT", "at_qT"], writes=[bsk])
            ACT(kb, p[:, 0:qn], bs[:, 0:qn], AF.Exp, [bsk], [pk], scale=128.0 ** -0.5)
            for qs in range(nq):
                bo, bok = bO[qs]
                kb.op("pe", lambda g, bo=bo, p=p, qs=qs, kt=kt, idx=idx: g.matmul(bo[:, 0:129], lhsT=p[:, qs * 128:(qs + 1) * 128], rhs=va[:, kt, :],
                                                                          start=(idx == 0), stop=(idx == len(kts) - 1)),
                      reads=[pk, "at_va"], writes=[bok])
        for qs in range(nq):
            bo, bok = bO[qs]
            kb.op("dve", lambda g, bo=bo: g.reciprocal(out=st[:, 2:3], in_=bo[:, 128:129]), reads=[bok], writes=["at_st2"])
            TS(kb, "dve", ot[:], bo[:, 0:128], st[:, 2:3], None, MUL, None, [bok, "at_st2"], ["at_ot"])
            kb.dma("pool", out[q0 + qs * 128:q0 + (qs + 1) * 128, :], ot[:], reads=["at_ot"], is_output=True)


def rope_tables():
    rows = SEQ // 64
    row = np.repeat(np.arange(rows), 64).astype(np.float32)
    col = np.tile(np.arange(64), rows).astype(np.float32)
    inv = (10000.0 ** (-np.arange(0, 64, 2, dtype=np.float32) / 64)).astype(np.float32)
    ar, ac = row[:, None] * inv[None, :], col[:, None] * inv[None, :]
    cos = np.concatenate([np.cos(ar), np.cos(ar), np.cos(ac), np.cos(ac)], 1)
    sin = np.concatenate([-np.sin(ar), np.sin(ar), -np.sin(ac), np.sin(ac)], 1)
    cos = np.concatenate([np.ones((CTX, 128)), cos], 0).astype(np.float32)
    sin = np.concatenate([np.zeros((CTX, 128)), sin], 0).astype(np.float32)
    return np.ascontiguousarray(cos), np.ascontiguousarray(sin)


def attn_inputs(pl_all, p, b, j):
    o = RW_COLS + ML_COLS + GD_COLS
    cx, lat = seq_rows(pl_all[:, o:o + AT_COLS], b)
    a = np.concatenate([cx, lat], 0)
    kv = j // 2
    cos, sin = rope_tables()
    m = {"at_q": np.ascontiguousarray(a[:, j * 128:(j + 1) * 128]), "at_k": np.ascontiguousarray(a[:, 512 + kv * 128:512 + (kv + 1) * 128]),
         "at_v": np.ascontiguousarray(a[:, 768 + kv * 128:768 + (kv + 1) * 128]), "at_cos": cos, "at_sin": sin,
         "at_cst": np.ascontiguousarray(np.concatenate([rep(p["at_q_norm"]), rep(p["at_k_norm"])], 1))}
    return m


def build_outproj():
    MUL, ADD, SUB = ALU.mult, ALU.add, ALU.subtract
    kb = KB()
    mix = kb.dram_in("mix", [ROWS, D])
    xin = kb.dram_in("xin", [ROWS, D])
    w = kb.dram_in("w", [D, D])
    mods = kb.dram_in("mods", [NT, 3, D])
    gvec = kb.dram_in("g", [1, D])
    wr = kb.dram_in("wr", [D, NE])
    xmid = kb.dram_out("xmid", [ROWS, D])
    h2o = kb.dram_out("h2", [ROWS, D])
    affo = kb.dram_out("aff", [ROWS, NE])
    idt = ident(kb)
    g_bc = kb.sb("g_bc", [128, D])
    kb.dma("sp", g_bc[:], gvec[0, :].partition_broadcast(128), writes=["g_bc"])
    wrs = kb.sb("wrs", [128, 16, NE])
    kb.dma("sp", wrs[:], wr.rearrange("(kc p) n -> p kc n", p=128), writes=["wrs"])
    GT = 5
    mixT = kb.sb("mixT", [128, GT, 16, 128])
    xm = kb.sb("xm", [128, GT, D])
    xt = kb.sb("xt", [128, D])
    sc, sh = kb.sb("sc", [128, D]), kb.sb("sh", [128, D])
    junk = kb.sb("junk", [128, D])
    h2T = kb.sb("h2T", [128, 16, 128])
    rstd = kb.sb("rstd", [128, 4])
    lg = kb.sb("lg", [128, NE])
    pst = [kb.ps(f"pst{i}", [128, 4, 128]) for i in range(2)]
    pstk = ["pst0", "pst1"]
    NB = 256
    wv = w.rearrange("(kc p) n -> p kc n", p=128)
    wb = [kb.sb(f"wb{i}", [128, 16, NB]) for i in range(2)]
    pp = [kb.ps(f"pp{i}", [128, NB]) for i in range(4)]
    m2b = [kb.sb(f"m2b{i}", [128, NB]) for i in range(4)]
    pr = kb.ps("pr", [128, NE])
    cnt = 0
    wcnt = 0
    for g0 in range(0, NT, GT):
        tiles = list(range(g0, min(NT, g0 + GT)))
        for t in tiles:
            lt = t - g0
            rows = slice(t * 128, (t + 1) * 128)
            kb.dma("sp", xt[:], mix[rows, :], writes=["xt"])
            kb.dma("sp", xm[:, lt, :], xin[rows, :], writes=[f"xm{lt}"])
            transpose_rows(kb, idt, xt, "xt", mixT[:, lt], f"mixT{lt}", pst, pstk)
        for nb in range(D // NB):
            wt, wk = wb[wcnt % 2], f"wb{wcnt % 2}"
            wcnt += 1
            kb.dma("sp", wt[:], wv[:, :, nb * NB:(nb + 1) * NB], writes=[wk])
            for t in tiles:
                lt = t - g0
                p, pk, mb, mk = pp[cnt % 4], f"pp{cnt % 4}", m2b[cnt % 4], f"m2b{cnt % 4}"
                cnt += 1
                kb.dma("sp", mb[:], mods[t, 0, nb * NB:(nb + 1) * NB].partition_broadcast(128), writes=[mk])
                for kc in range(16):
                    kb.op("pe", lambda e, kc=kc, p=p, wt=wt, lt=lt: e.matmul(p[:], lhsT=mixT[:, lt, kc, :], rhs=wt[:, kc, :], start=(kc == 0), stop=(kc == 15)),
                          reads=[f"mixT{lt}", wk], writes=[pk])
                TT(kb, "dve", mb[:], p[:], mb[:], MUL, [pk, mk], [mk])
                TT(kb, "pool", xm[:, lt, nb * NB:(nb + 1) * NB], xm[:, lt, nb * NB:(nb + 1) * NB], mb[:], ADD, [mk, f"xm{lt}"], [f"xm{lt}"])
        for t in tiles:
            lt = t - g0
            rows = slice(t * 128, (t + 1) * 128)
            x, xk = xm[:, lt, :], f"xm{lt}"
            kb.dma("pool", xmid[rows, :], x, reads=[xk], is_output=True)
            rms_rstd(kb, x, xk, junk[:], rstd[:, 0:1], "rstd")
            kb.dma("sp", sh[:], mods[t, 1, :].partition_broadcast(128), writes=["sh"])
            kb.dma("sp", sc[:], mods[t, 2, :].partition_broadcast(128), writes=["sc"])
            STT(kb, sc[:], sc[:], 1.0, g_bc[:], ADD, MUL, ["sc", "g_bc"], ["sc"])
            STT(kb, xt[:], x, rstd[:, 0:1], sc[:], MUL, MUL, [xk, "rstd", "sc"], ["xt"])
            TT(kb, "dve", xt[:], xt[:], sh[:], ADD, ["xt", "sh"], ["xt"])
            kb.dma("pool", h2o[rows, :], xt[:], reads=["xt"], is_output=True)
            transpose_rows(kb, idt, xt, "xt", h2T, "h2T", pst, pstk)
            for kc in range(16):
                kb.op("pe", lambda e, kc=kc: e.matmul(pr[:], lhsT=h2T[:, kc, :], rhs=wrs[:, kc, :], start=(kc == 0), stop=(kc == 15)),
                      reads=["h2T", "wrs"], writes=["pr"])
            kb.op("dve", lambda e: e.tensor_reduce(out=rstd[:, 1:2], in_=pr[:], axis=AX.X, op=ALU.max, negate=True), reads=["pr"], writes=["rmax"])
            ACT(kb, lg[:], pr[:], AF.Exp, ["pr", "rmax"], ["lg", "rsum"], bias=rstd[:, 1:2], scale=1.0, accum_out=rstd[:, 2:3])
            kb.op("dve", lambda e: e.reciprocal(out=rstd[:, 2:3], in_=rstd[:, 2:3]), reads=["rsum"], writes=["rsum"])
            TS(kb, "dve", lg[:], lg[:], rstd[:, 2:3], None, MUL, None, ["lg", "rsum"], ["lg"])
            kb.dma("pool", affo[rows, :], lg[:], reads=["lg"], is_output=True)
    return kb


CAP_L = 2 * SEQ // NE
CAP_C = 2 * CTX // NE


def build_experts(has_ctx):
    MUL, ADD, SUB = ALU.mult, ALU.add, ALU.subtract
    kb = KB()
    affT = kb.dram_in("affT", [4, SEQ])
    h2l = [kb.dram_in(f"h2l{b}", [SEQ, D]) for b in range(B)]
    pl_ = [kb.dram_out(f"part_l{b}", [SEQ, D]) for b in range(B)]
    if has_ctx:
        affTc = kb.dram_in("affTc", [4, CTX])
        h2c = [kb.dram_in(f"h2c{b}", [CTX, D]) for b in range(B)]
        pc_ = [kb.dram_out(f"part_c{b}", [CTX, D]) for b in range(B)]
    w1 = kb.dram_in("w1", [2, D, D])
    w3 = kb.dram_in("w3", [2, D, D])
    w2 = kb.dram_in("w2", [2, D, D])
    idt = ident(kb)
    NTOK = CAP_L + (CAP_C if has_ctx else 0)
    NTT = 4 + (1 if has_ctx else 0)
    arena = kb.sb("arena", [128, 5 * D])
    xsT = arena[:, 0:16 * NTOK].rearrange("p (k n) -> p k n", k=16)
    ysb = arena[:, 0:NTT * D].rearrange("p (t n) -> p t n", t=NTT)
    AK = "arena"
    hidT = kb.sb("hidT", [128, 16, NTOK])
    xg = kb.sb("xg", [128, D])
    sgm = kb.sb("sgm", [128, 512])
    kb.op("pool", lambda e: e.memset(xg[:], 0.0), writes=["xg"])
    for b in range(B):
        for t in range(SEQ // 128):
            kb.dma("sp", pl_[b][t * 128:(t + 1) * 128, :], xg[:], reads=["xg"], writes=[f"part_l{b}"], is_output=True)
        if has_ctx:
            for t in range(CTX // 128):
                kb.dma("sp", pc_[b][t * 128:(t + 1) * 128, :], xg[:], reads=["xg"], writes=[f"part_c{b}"], is_output=True)
    pt = kb.ps("pt", [128, 64])

    def topk(src, n, cap, tag):
        wk = kb.sb(f"wk{tag}", [4, n])
        vals = kb.sb(f"vals{tag}", [4, cap])
        idxs = kb.sb(f"idxs{tag}", [4, cap], U32)
        idxf = kb.sb(f"idxf{tag}", [4, cap])
        kb.dma("sp", wk[:], src, writes=[f"wk{tag}"])
        for it in range(cap // 8):
            sl = slice(it * 8, (it + 1) * 8)
            kb.op("dve", lambda e, sl=sl: e.max(out=vals[:, sl], in_=wk[:]), reads=[f"wk{tag}"], writes=[f"vals{tag}"])
            kb.op("dve", lambda e, sl=sl: e.max_index(out=idxs[:, sl], in_max=vals[:, sl], in_values=wk[:]), reads=[f"wk{tag}", f"vals{tag}"], writes=[f"idxs{tag}"])
            kb.op("dve", lambda e, sl=sl: e.match_replace(out=wk[:], in_to_replace=vals[:, sl], in_values=wk[:], imm_value=-1.0),
                  reads=[f"vals{tag}"], writes=[f"wk{tag}"])
        CP(kb, "dve", idxf[:], idxs[:], [f"idxs{tag}"], [f"idxf{tag}"])
        nblk = (cap + 127) // 128
        pw = min(cap, 128)
        idxT = kb.sb(f"idxT{tag}", [128, nblk * 4], I32)
        gT = kb.sb(f"gT{tag}", [128, nblk * 4])
        for srcv, srck, dst, dstk in ((idxf, f"idxf{tag}", idxT, f"idxT{tag}"), (vals, f"vals{tag}", gT, f"gT{tag}")):
            for blk in range(nblk):
                kb.op("pe", lambda e, srcv=srcv, blk=blk: e.transpose(out=pt[0:pw, blk * 4:(blk + 1) * 4], in_=srcv[:, blk * 128:blk * 128 + pw], identity=idt[0:4, 0:4]),
                      reads=[srck, "ident"], writes=["pt"])
            CP(kb, "dve", dst[0:pw, :], pt[0:pw, 0:nblk * 4], ["pt"], [dstk])
        return idxT, gT

    idxT, gT = topk(affT, SEQ, CAP_L, "L")
    if has_ctx:
        idxTc, gTc = topk(affTc, CTX, CAP_C, "C")
    pst = [kb.ps(f"pst{i}", [128, 4, 128]) for i in range(2)]
    pstk = ["pst0", "pst1"]
    ph = [kb.ps(f"ph{i}", [128, 512]) for i in range(2)]
    phc = kb.ps("phc", [128, 2, 32])
    py = [kb.ps(f"py{i}", [128, 512]) for i in range(2)]
    w13 = [kb.sb(f"w13_{i}", [128, 2, 16, 128]) for i in range(2)]
    w2b = [kb.sb(f"w2b{i}", [128, 16, 256]) for i in range(2)]
    wc = 0
    w2c = 0
    yc = 0
    for el in range(2):
        w1v = w1[el].rearrange("(kc p) n -> p kc n", p=128)
        w3v = w3[el].rearrange("(kc p) n -> p kc n", p=128)
        w2v = w2[el].rearrange("(fc p) n -> p fc n", p=128)
        for b in range(B):
            r = el * 2 + b
            for blk in range(4):
                col = blk * 4 + r
                kb.dma("pool", None, None, reads=["idxTL"], writes=["xg"],
                       fn=lambda e, col=col: e.indirect_dma_start(out=xg[:], out_offset=None, in_=h2l[b][:, :],
                                                                  in_offset=bass.IndirectOffsetOnAxis(ap=idxT[:, col:col + 1], axis=0)))
                transpose_rows(kb, idt, xg, "xg", xsT[:, :, blk * 128:(blk + 1) * 128], AK, pst, pstk)
            if has_ctx:
                kb.dma("pool", None, None, reads=["idxTC"], writes=["xg"],
                       fn=lambda e: e.indirect_dma_start(out=xg[0:CAP_C, :], out_offset=None, in_=h2c[b][:, :],
                                                         in_offset=bass.IndirectOffsetOnAxis(ap=idxTc[0:CAP_C, r:r + 1], axis=0)))
                for c0 in range(0, 16, 4):
                    bank = (c0 // 4) % 2
                    for i in range(4):
                        kb.op("pe", lambda e, i=i: e.transpose(out=pst[bank][:, i, 0:CAP_C], in_=xg[0:CAP_C, (c0 + i) * 128:(c0 + i + 1) * 128], identity=idt[0:CAP_C, 0:CAP_C]),
                              reads=["xg", "ident"], writes=[pstk[bank]])
                    CP(kb, "dve", xsT[:, c0:c0 + 4, CAP_L:NTOK], pst[bank][:, :, 0:CAP_C], [pstk[bank]], [AK])
            for fb in range(16):
                wt, wk_ = w13[wc % 2], f"w13_{wc % 2}"
                wc += 1
                kb.dma("sp", wt[:, 0], w1v[:, :, fb * 128:(fb + 1) * 128], writes=[wk_ + "a"])
                kb.dma("sp", wt[:, 1], w3v[:, :, fb * 128:(fb + 1) * 128], writes=[wk_ + "b"])
                for i in range(2):
                    for kc in range(16):
                        kb.op("pe", lambda e, i=i, kc=kc, wt=wt: e.matmul(ph[i][:], lhsT=wt[:, i, kc, :], rhs=xsT[:, kc, 0:CAP_L], start=(kc == 0), stop=(kc == 15)),
                              reads=[wk_ + "ab"[i], AK], writes=[f"ph{i}"])
                ACT(kb, sgm[:], ph[0][:], AF.Sigmoid, ["ph0"], ["sgm"])
                TT(kb, "dve", sgm[:], ph[0][:], sgm[:], MUL, ["ph0", "sgm"], ["sgm"])
                TT(kb, "dve", hidT[:, fb, 0:CAP_L], ph[1][:], sgm[:], MUL, ["ph1", "sgm"], ["hidT"])
                if has_ctx:
                    for i in range(2):
                        for kc in range(16):
                            kb.op("pe", lambda e, i=i, kc=kc, wt=wt: e.matmul(phc[:, i, :], lhsT=wt[:, i, kc, :], rhs=xsT[:, kc, CAP_L:NTOK], start=(kc == 0), stop=(kc == 15)),
                                  reads=[wk_ + "ab"[i], AK], writes=["phc"])
                    ACT(kb, sgm[:, 0:CAP_C], phc[:, 0, :], AF.Sigmoid, ["phc"], ["sgm"])
                    TT(kb, "dve", sgm[:, 0:CAP_C], phc[:, 0, :], sgm[:, 0:CAP_C], MUL, ["phc", "sgm"], ["sgm"])
                    TT(kb, "dve", hidT[:, fb, CAP_L:NTOK], phc[:, 1, :], sgm[:, 0:CAP_C], MUL, ["phc", "sgm"], ["hidT"])
            for db in range(D // 256):
                wt, wk_ = w2b[w2c % 2], f"w2b{w2c % 2}"
                w2c += 1
                kb.dma("sp", wt[:], w2v[:, :, db * 256:(db + 1) * 256], writes=[wk_])
                for tt in range(NTT):
                    np_ = 128 if tt < 4 else CAP_C
                    t0 = tt * 128
                    p, pk = py[yc % 2], f"py{yc % 2}"
                    yc += 1
                    for fc in range(16):
                        kb.op("pe", lambda e, fc=fc, p=p, wt=wt: e.matmul(p[0:np_, 0:256], lhsT=hidT[:, fc, t0:t0 + np_], rhs=wt[:, fc, :], start=(fc == 0), stop=(fc == 15)),
                              reads=["hidT", wk_], writes=[pk])
                    gsc = gT[:, tt * 4 + r:tt * 4 + r + 1] if tt < 4 else gTc[0:CAP_C, r:r + 1]
                    gk = "gTL" if tt < 4 else "gTC"
                    TS(kb, "dve", ysb[0:np_, tt, db * 256:(db + 1) * 256], p[0:np_, 0:256], gsc, None, MUL, None, [pk, gk], [AK])
            for tt in range(NTT):
                if tt < 4:
                    col = tt * 4 + r
                    kb.dma("pool", None, None, reads=[AK, "idxTL"], writes=[f"part_l{b}"], is_output=True,
                           fn=lambda e, col=col, tt=tt: e.indirect_dma_start(out=pl_[b][:, :], out_offset=bass.IndirectOffsetOnAxis(ap=idxT[:, col:col + 1], axis=0),
                                                                              in_=ysb[:, tt, :], in_offset=None, compute_op=ALU.add))
                else:
                    kb.dma("pool", None, None, reads=[AK, "idxTC"], writes=[f"part_c{b}"], is_output=True,
                           fn=lambda e, tt=tt: e.indirect_dma_start(out=pc_[b][:, :], out_offset=bass.IndirectOffsetOnAxis(ap=idxTc[0:CAP_C, r:r + 1], axis=0),
                                                                    in_=ysb[0:CAP_C, tt, :], in_offset=None, compute_op=ALU.add))
    return kb


def expert_inputs(aff, h2, wts, core, has_ctx):
    m = {}
    e0 = 2 * core
    rows = []
    rows_c = []
    for el in range(2):
        for b in range(B):
            rows.append(aff[b * SEQ:(b + 1) * SEQ, e0 + el])
            rows_c.append(aff[2 * SEQ + b * CTX:2 * SEQ + (b + 1) * CTX, e0 + el])
    m["affT"] = np.ascontiguousarray(np.stack(rows, 0))
    for b in range(B):
        m[f"h2l{b}"] = np.ascontiguousarray(h2[b * SEQ:(b + 1) * SEQ])
    if has_ctx:
        m["affTc"] = np.ascontiguousarray(np.stack(rows_c, 0))
        for b in range(B):
            m[f"h2c{b}"] = np.ascontiguousarray(h2[2 * SEQ + b * CTX:2 * SEQ + (b + 1) * CTX])
    for n, w in zip(("w1", "w3", "w2"), wts):
        m[n] = np.ascontiguousarray(w[e0:e0 + 2])
    return m


def expert_parts(res, has_ctx):
    outs = []
    for r in res:
        a = [r["part_l0"], r["part_l1"]]
        if has_ctx:
            a += [r["part_c0"], r["part_c1"]]
        else:
            a += [np.zeros((CTX, D), np.float32)] * 2
        outs.append(np.concatenate(a, 0))
    return outs


def build_mixers():
    kb = KB()
    c = dplr_consts(kb)
    bA, bB = dplr_banks2(kb)

    def sA():
        emit_rwkv(kb, c, bA)

    def sB():
        emit_attn(kb, c, bB)
        emit_mlstm(kb, c, bB)
        emit_gdn(kb, c, bB)
    kb.run_streams([sA, sB])
    return kb


def mixer_inputs(pl_all, p, core):
    b, j = core // 4, core % 4
    m = {}
    m.update(rwkv_inputs(pl_all, p, b, j))
    m.update(mlstm_inputs(pl_all, p, b, j))
    m.update(gdn_inputs(pl_all, p, b, j))
    m.update(attn_inputs(pl_all, p, b, j))
    return m


def mixer_outputs(res):
    mix = np.zeros((TOT, D), np.float32)
    for core in range(NCORES):
        b, j = core // 4, core % 4
        for gi, nm in enumerate(("rw_out", "ml_out", "gd_out", "at_out")):
            o = res[core][nm]
            cs = slice(gi * GW + j * 128, gi * GW + (j + 1) * 128)
            mix[2 * SEQ + b * CTX:2 * SEQ + (b + 1) * CTX, cs] = o[:CTX]
            mix[b * SEQ:(b + 1) * SEQ, cs] = o[CTX:]
    return mix


_PROGS = {}


def _prog(name, fn):
    if name not in _PROGS:
        _PROGS[name] = fn().finish()
    return _PROGS[name]


LAYER_PARAMS = ['rw_mu', 'rw_w0', 'rw_w_up', 'rw_a0', 'rw_a_up', 'rw_g_up', 'rw_k_k', 'rw_k_a', 'rw_r_k', 'rw_ln_w', 'rw_ln_b',
                'ml_ib', 'ml_fb', 'ml_norm_g', 'gd_conv', 'gd_a_log', 'gd_dt_bias', 'gd_norm_g', 'at_q_norm', 'at_k_norm']


def kernel(**inp):
    inp = {k: np.asarray(v, np.float32) for k, v in inp.items()}
    mod = run_mod(inp["c"], inp["c_ctx"], inp["ada_w"], inp["ada_b"])
    x = np.concatenate([inp["x"].reshape(-1, D), inp["ctx"].reshape(-1, D)], 0)
    parts = None
    m5 = None
    for l in range(DEPTH):
        p = {n: inp[n][l] for n in LAYER_PARAMS}
        xs = rows_to_cores(x)
        mr = mod_rows_for(mod[l], [0, 1])
        g1 = np.ascontiguousarray(inp["norm1_g"][l][None, :])
        w_in = np.ascontiguousarray(inp["w_in"][l])
        if parts is None:
            nc = _prog("rows0", lambda: build_rows(False, "inproj"))
            maps = [{"xin": xs[c], "g": g1, "modrows": mr[c], "w": w_in} for c in range(NCORES)]
        else:
            nc = _prog("rows1", lambda: build_rows(True, "inproj"))
            maps = [{"xin": xs[c], "g": g1, "modrows": mr[c], "w": w_in, "parts": parts[c], "m5": m5[c]} for c in range(NCORES)]
        res = run(nc, maps)
        pl_all = cores_to_rows([r["out"] for r in res])
        if parts is not None:
            x = cores_to_rows([r["xout"] for r in res])
            xs = rows_to_cores(x)
        nc = _prog("mixers", build_mixers)
        res = run(nc, [mixer_inputs(pl_all, p, c) for c in range(NCORES)])
        mix = mixer_outputs(res)
        del pl_all
        nc = _prog("outproj", build_outproj)
        ms = rows_to_cores(mix)
        mr2 = mod_rows_for(mod[l], [2, 3, 4])
        g2 = np.ascontiguousarray(inp["norm2_g"][l][None, :])
        w_out = np.ascontiguousarray(inp["w_out"][l])
        wr = np.ascontiguousarray(inp["w_router"][l])
        res = run(nc, [{"mix": ms[c], "xin": xs[c], "w": w_out, "mods": mr2[c], "g": g2, "wr": wr} for c in range(NCORES)])
        x = cores_to_rows([r["xmid"] for r in res])
        h2 = cores_to_rows([r["h2"] for r in res])
        aff = cores_to_rows([r["aff"] for r in res])
        has_ctx = l < DEPTH - 1
        nc = _prog("experts%d" % has_ctx, lambda: build_experts(has_ctx))
        wts = (inp["w_exp1"][l], inp["w_exp3"][l], inp["w_exp2"][l])
        res = run(nc, [expert_inputs(aff, h2, wts, c, has_ctx) for c in range(NCORES)])
        pfull = expert_parts(res, has_ctx)
        pc = [rows_to_cores(a) for a in pfull]
        parts = [np.ascontiguousarray(np.stack([pc[e][c] for e in range(NCORES)], 0)) for c in range(NCORES)]
        m5 = [np.ascontiguousarray(a[:, 0, :]) for a in mod_rows_for(mod[l], [5])]
        del pfull, pc
    nc = _prog("final", lambda: build_rows(True, "final"))
    xs = rows_to_cores(x)
    gf = np.ascontiguousarray(inp["final_g"][None, :])
    res = run(nc, [{"xin": xs[c], "g": gf, "parts": parts[c], "m5": m5[c]} for c in range(NCORES)])
    out = cores_to_rows([r["out"] for r in res])[:B * SEQ]
    return np.ascontiguousarray(out.reshape(B, SEQ, D).astype(np.float32))
```

```python
import math
from contextlib import ExitStack

import numpy as np
import concourse.bass as bass
import concourse.mybir as mybir
from concourse.bass_utils import run_bass_kernel_spmd

F32 = mybir.dt.float32
U32 = mybir.dt.uint32
I32 = mybir.dt.int32
AF = mybir.ActivationFunctionType
ALU = mybir.AluOpType
AX = mybir.AxisListType

NCORES = 8
D = 2048
B = 2
SEQ = 4096
CTX = 256
DEPTH = 2
GW = 512
RW_COLS = 3 * GW + 64 + 64 + 128
ML_COLS = 4 * GW + 16
GD_COLS = 4 * GW + 16
AT_COLS = 1024
N_IN = RW_COLS + ML_COLS + GD_COLS + AT_COLS
NE = 16
EPS = 1e-6


class KB:
    NDSEM = 8

    def __init__(self):
        self.nc = bass.Bass("TRN2", target_bir_lowering=False)
        self.es = ExitStack()
        nc = self.nc
        self.eng = {"pe": nc.tensor, "dve": nc.vector, "act": nc.scalar, "pool": nc.gpsimd, "sp": nc.sync}
        self.sem = {e: self.es.enter_context(nc.semaphore("s_" + e)) for e in self.eng}
        self.cnt = {e: 0 for e in self.eng}
        self.waited = {e: {} for e in self.eng}
        self.dsem = {}
        self.dcnt = {}
        self.dnext = {}
        for q in ("sp", "pool", "act"):
            self.dsem[q] = [self.es.enter_context(nc.semaphore(f"d_{q}{i}")) for i in range(self.NDSEM)]
            self.dcnt[q] = [0] * self.NDSEM
            self.dnext[q] = 0
        self.last_w = {}
        self.readers = {}
        self.excl = set()
        self.ninst = 0
        self.out_tokens = []

    def sb(self, name, shape, dt=F32):
        return self.es.enter_context(self.nc.sbuf_tensor(name, list(shape), dt))

    def ps(self, name, shape, dt=F32):
        self.excl.add(name)
        return self.es.enter_context(self.nc.psum_tensor(name, list(shape), dt))

    def dram_in(self, name, shape, dt=F32):
        return self.nc.dram_tensor(name, list(shape), dt, kind="ExternalInput").ap()

    def dram_out(self, name, shape, dt=F32):
        return self.nc.dram_tensor(name, list(shape), dt, kind="ExternalOutput").ap()

    def _wait(self, e, tok):
        if tok is None:
            return
        kind, key, val = tok
        w = self.waited[e]
        k = (kind, key if kind == "c" else id(key))
        if w.get(k, 0) >= val:
            return
        w[k] = val
        sem = self.sem[key] if kind == "c" else key
        self.eng[e].wait_ge(sem, val)

    def _deps(self, e, reads, writes):
        deps = []
        for k in reads:
            t = self.last_w.get(k)
            if t is not None:
                deps.append(t)
        for k in writes:
            t = self.last_w.get(k)
            if t is not None:
                deps.append(t)
            deps.extend(self.readers.get(k, ()))
        for t in deps:
            if e == "pe" and t[0] == "c" and t[1] == "pe":
                continue
            self._wait(e, t)

    def _record(self, tok, reads, writes):
        for k in reads:
            self.readers.setdefault(k, []).append(tok)
        for k in writes:
            self.last_w[k] = tok
            self.readers[k] = []

    def _x(self, reads, writes):
        ex = [k for k in reads if k in self.excl]
        if ex:
            reads = [k for k in reads if k not in self.excl]
            writes = list(writes) + ex
        return reads, writes

    def run_streams(self, fns):
        import threading
        n = len(fns)
        cv = threading.Condition()
        st = {"turn": 0, "alive": [True] * n, "err": None}
        self._stream = (cv, st, n)
        tl = threading.local()
        self._tl = tl

        def nxt(i):
            for k in range(1, n + 1):
                j = (i + k) % n
                if st["alive"][j]:
                    return j
            return i

        def worker(i):
            tl.sid = i
            try:
                with cv:
                    while st["turn"] != i:
                        cv.wait()
                fns[i]()
            except BaseException as ex:
                st["err"] = ex
            finally:
                with cv:
                    st["alive"][i] = False
                    st["turn"] = nxt(i)
                    cv.notify_all()
        ths = [threading.Thread(target=worker, args=(i,)) for i in range(n)]
        for t in ths:
            t.start()
        for t in ths:
            t.join()
        self._stream = None
        if st["err"] is not None:
            raise st["err"]

    def _yield_turn(self):
        if getattr(self, "_stream", None) is None:
            return
        cv, st, n = self._stream
        i = self._tl.sid
        with cv:
            j = i
            for k in range(1, n + 1):
                c = (i + k) % n
                if st["alive"][c]:
                    j = c
                    break
            if j != i:
                st["turn"] = j
                cv.notify_all()
                while st["turn"] != i:
                    cv.wait()

    def spin(self, cond):
        n = 0
        while not cond():
            self._yield_turn()
            n += 1
            assert n < 10_000_000, "stream handshake never satisfied"

    def op(self, e, fn, reads=(), writes=()):
        self._yield_turn()
        reads, writes = self._x(reads, writes)
        self._deps(e, reads, writes)
        inst = fn(self.eng[e])
        self.cnt[e] += 1
        inst.then_inc(self.sem[e], 1)
        tok = ("c", e, self.cnt[e])
        self._record(tok, reads, writes)
        self.ninst += 1
        return tok

    def dma(self, q, out, in_, reads=(), writes=(), is_output=False, fn=None):
        self._yield_turn()
        i = self.dnext[q]
        self.dnext[q] = (i + 1) % self.NDSEM
        sem = self.dsem[q][i]
        if self.dcnt[q][i] > 0:
            self._wait(q, ("d", sem, 16 * self.dcnt[q][i]))
        self._deps(q, reads, writes)
        if fn is None:
            inst = self.eng[q].dma_start(out=out, in_=in_)
        else:
            inst = fn(self.eng[q])
        self.dcnt[q][i] += 1
        inst.then_inc(sem, 16)
        tok = ("d", sem, 16 * self.dcnt[q][i])
        self._record(tok, reads, writes)
        if is_output:
            self.out_tokens.append(tok)
        self.ninst += 1
        return tok

    def finish(self):
        for q in self.dsem:
            for i, sem in enumerate(self.dsem[q]):
                if self.dcnt[q][i] > 0:
                    self._wait("sp", ("d", sem, 16 * self.dcnt[q][i]))
        for e in self.eng:
            if e != "sp" and self.cnt[e] > 0:
                self._wait("sp", ("c", e, self.cnt[e]))
        self.es.close()
        return self.nc


def run(kb_or_nc, in_maps):
    nc = kb_or_nc.finish() if isinstance(kb_or_nc, KB) else kb_or_nc
    res = run_bass_kernel_spmd(nc, in_maps, core_ids=list(range(NCORES)))
    return res.results


def ident(kb, name="ident"):
    t = kb.sb(name, [128, 128])
    kb.op("pool", lambda e: e.memset(t[:], 1.0), writes=[name])
    kb.op("pool", lambda e: e.affine_select(out=t[:], in_=t[:], pattern=[[1, 128]], compare_op=ALU.is_equal,
                                             fill=0.0, base=0, channel_multiplier=-1), reads=[name], writes=[name])
    return t


def tri_mask(kb, name, mode):
    t = kb.sb(name, [128, 128])
    kb.op("pool", lambda e: e.memset(t[:], 1.0), writes=[name])
    if mode == "ones":
        return t
    if mode == "le":
        pat, cm, base, cmp = [[1, 128]], -1, 0, ALU.is_ge
    elif mode == "lt":
        pat, cm, base, cmp = [[1, 128]], -1, 0, ALU.is_gt
    elif mode == "ge":
        pat, cm, base, cmp = [[-1, 128]], 1, 0, ALU.is_ge
    else:
        pat, cm, base, cmp = [[-1, 128]], 1, 0, ALU.is_gt
    kb.op("pool", lambda e: e.affine_select(out=t[:], in_=t[:], pattern=pat, compare_op=cmp, fill=0.0,
                                             base=base, channel_multiplier=cm), reads=[name], writes=[name])
    return t


def build_mod():
    kb = KB()
    NCOL = 3072
    condT = kb.dram_in("condT", [128, 16, 3])
    w = kb.dram_in("w", [D, NCOL])
    bias = kb.dram_in("bias", [3, NCOL])
    out = kb.dram_out("out", [3, NCOL])
    ct = kb.sb("ct", [128, 16, 3])
    sg = kb.sb("sg", [128, 16, 3])
    bt = kb.sb("bt", [3, NCOL])
    ot = kb.sb("ot", [3, NCOL])
    kb.dma("sp", ct[:], condT, writes=["ct"])
    kb.dma("sp", bt[:], bias, writes=["bt"])
    kb.op("act", lambda e: e.activation(out=sg[:], in_=ct[:], func=AF.Sigmoid), reads=["ct"], writes=["sg"])
    kb.op("dve", lambda e: e.tensor_tensor(out=ct[:], in0=ct[:], in1=sg[:], op=ALU.mult), reads=["ct", "sg"], writes=["ct"])
    wv = w.rearrange("(kc p) n -> p kc n", p=128)
    wb = [kb.sb(f"wb{i}", [128, 16, 512]) for i in range(2)]
    pp = [kb.ps(f"pp{i}", [3, 512]) for i in range(2)]
    for nb in range(NCOL // 512):
        wt = wb[nb % 2]
        wk = f"wb{nb % 2}"
        for h in range(2):
            kb.dma("sp", wt[:, h * 8:(h + 1) * 8, :], wv[:, h * 8:(h + 1) * 8, nb * 512:(nb + 1) * 512], writes=[wk + f"h{h}"])
        p = pp[nb % 2]
        pk = f"pp{nb % 2}"
        for kc in range(16):
            kb.op("pe", lambda e, kc=kc: e.matmul(p[:], lhsT=ct[:, kc, :], rhs=wt[:, kc, :], start=(kc == 0), stop=(kc == 15)),
                  reads=["ct", wk + f"h{kc // 8}"], writes=[pk])
        kb.op("dve", lambda e: e.tensor_tensor(out=ot[:, nb * 512:(nb + 1) * 512], in0=p[:], in1=bt[:, nb * 512:(nb + 1) * 512], op=ALU.add),
              reads=[pk, "bt"], writes=["ot"])
    kb.dma("sp", out, ot[:], reads=["ot"], is_output=True)
    return kb


def run_mod(c, c_ctx, ada_w, ada_b):
    cond = np.concatenate([c, c_ctx[None, :]], axis=0).astype(np.float32)
    condT = np.ascontiguousarray(cond.reshape(3, 16, 128).transpose(2, 1, 0))
    maps = []
    for core in range(NCORES):
        l, q = divmod(core, 4)
        sl = slice(q * 3072, (q + 1) * 3072)
        maps.append({"condT": condT, "w": np.ascontiguousarray(ada_w[l][:, sl]),
                     "bias": np.ascontiguousarray(np.broadcast_to(ada_b[l][sl], (3, 3072)))})
    res = run(build_mod(), maps)
    mod = np.zeros((DEPTH, 3, 6 * D), np.float32)
    for core in range(NCORES):
        l, q = divmod(core, 4)
        mod[l][:, q * 3072:(q + 1) * 3072] = res[core]["out"]
    return mod


NT = 9
ROWS = NT * 128


def rms_rstd(kb, x, xk, junk, rstd, key, n=D, eps=EPS):
    kb.op("act", lambda e: e.activation(out=junk, in_=x, func=AF.Square, accum_out=rstd), reads=[xk], writes=["junk", key])
    kb.op("act", lambda e: e.activation(out=rstd, in_=rstd, func=AF.Sqrt, scale=1.0 / n, bias=eps), reads=[key], writes=[key])
    kb.op("dve", lambda e: e.reciprocal(out=rstd, in_=rstd), reads=[key], writes=[key])


def transpose_rows(kb, idt, src, srck, dstT, dstk, pst, pstk, nchunks=16, evac="dve"):
    for c0 in range(0, nchunks, 4):
        n = min(4, nchunks - c0)
        bank = (c0 // 4) % len(pst)
        for i in range(n):
            kb.op("pe", lambda e, i=i: e.transpose(out=pst[bank][:, i, :], in_=src[:, (c0 + i) * 128:(c0 + i + 1) * 128], identity=idt[:]),
                  reads=[srck, "ident"], writes=[pstk[bank]])
        kb.op(evac, lambda e: e.tensor_copy(out=dstT[:, c0:c0 + n, :], in_=pst[bank][:, 0:n, :]), reads=[pstk[bank]], writes=[dstk])


def build_rows(do_combine, mode, NOUT=N_IN):
    kb = KB()
    xin = kb.dram_in("xin", [ROWS, D])
    gvec = kb.dram_in("g", [1, D])
    if do_combine:
        parts = kb.dram_in("parts", [NCORES, ROWS, D])
        m5 = kb.dram_in("m5", [NT, D])
        xout = kb.dram_out("xout", [ROWS, D])
    if mode == "inproj":
        modrows = kb.dram_in("modrows", [NT, 2, D])
        w = kb.dram_in("w", [D, NOUT])
        out = kb.dram_out("out", [ROWS, NOUT])
    else:
        out = kb.dram_out("out", [ROWS, D])

    idt = ident(kb)
    g_bc = kb.sb("g_bc", [128, D])
    kb.dma("sp", g_bc[:], gvec[0, :].partition_broadcast(128), writes=["g_bc"])
    xt = [kb.sb(f"xt{i}", [128, D]) for i in range(2)]
    junk = kb.sb("junk", [128, D])
    rstd = kb.sb("rstd", [128, 2])
    if mode == "inproj":
        sc = kb.sb("sc", [128, D])
        sh = kb.sb("sh", [128, D])
        hT = kb.sb("hT", [128, NT, 16, 128])
        pst = [kb.ps(f"pst{i}", [128, 4, 128]) for i in range(2)]
        pstk = ["pst0", "pst1"]
    if do_combine:
        pt = [kb.sb(f"pt{i}", [128, D]) for i in range(2)]
        m5b = kb.sb("m5b", [128, D])

    for t in range(NT):
        x = xt[t % 2]
        xk = f"xt{t % 2}"
        rows = slice(t * 128, (t + 1) * 128)
        kb.dma("sp", x[:], xin[rows, :], writes=[xk])
        if do_combine:
            acc = junk
            for c in range(NCORES):
                p = pt[c % 2]
                pk = f"pt{c % 2}"
                kb.dma("sp", p[:], parts[c, rows, :], writes=[pk])
                if c == 0:
                    kb.op("pool", lambda e, p=p: e.tensor_copy(out=acc[:], in_=p[:]), reads=[pk], writes=["junk"])
                else:
                    kb.op("pool", lambda e, p=p: e.tensor_tensor(out=acc[:], in0=acc[:], in1=p[:], op=ALU.add), reads=[pk, "junk"], writes=["junk"])
            kb.dma("sp", m5b[:], m5[t, :].partition_broadcast(128), writes=["m5b"])
            kb.op("dve", lambda e: e.tensor_tensor(out=acc[:], in0=acc[:], in1=m5b[:], op=ALU.mult), reads=["junk", "m5b"], writes=["junk"])
            kb.op("dve", lambda e, x=x: e.tensor_tensor(out=x[:], in0=x[:], in1=acc[:], op=ALU.add), reads=["junk", xk], writes=[xk])
            kb.dma("pool", xout[rows, :], x[:], reads=[xk], is_output=True)
        rk = "rstd"
        rms_rstd(kb, x[:], xk, junk[:], rstd[:, 0:1], rk)
        if mode == "inproj":
            kb.dma("sp", sh[:], modrows[t, 0, :].partition_broadcast(128), writes=["sh"])
            kb.dma("sp", sc[:], modrows[t, 1, :].partition_broadcast(128), writes=["sc"])
            kb.op("dve", lambda e: e.scalar_tensor_tensor(out=sc[:], in0=sc[:], scalar=1.0, in1=g_bc[:], op0=ALU.add, op1=ALU.mult),
                  reads=["sc", "g_bc"], writes=["sc"])
            kb.op("dve", lambda e, x=x: e.scalar_tensor_tensor(out=x[:], in0=x[:], scalar=rstd[:, 0:1], in1=sc[:], op0=ALU.mult, op1=ALU.mult),
                  reads=[xk, rk, "sc"], writes=[xk])
            kb.op("dve", lambda e, x=x: e.tensor_tensor(out=x[:], in0=x[:], in1=sh[:], op=ALU.add), reads=[xk, "sh"], writes=[xk])
            transpose_rows(kb, idt, x, xk, hT[:, t], f"hT{t}", pst, pstk)
        else:
            kb.op("dve", lambda e, x=x: e.scalar_tensor_tensor(out=x[:], in0=x[:], scalar=rstd[:, 0:1], in1=g_bc[:], op0=ALU.mult, op1=ALU.mult),
                  reads=[xk, rk, "g_bc"], writes=[xk])
            kb.dma("pool", out[rows, :], x[:], reads=[xk], is_output=True)

    if mode == "inproj":
        NB = 256
        wv = w.rearrange("(kc p) n -> p kc n", p=128)
        wb = [kb.sb(f"wb{i}", [128, 16, NB]) for i in range(2)]
        pp = [kb.ps(f"pp{i}", [128, NB]) for i in range(4)]
        ot = [kb.sb(f"ot{i}", [128, NB]) for i in range(4)]
        nblocks = (NOUT + NB - 1) // NB
        cnt = 0
        for nb in range(nblocks):
            n0 = nb * NB
            nw = min(NB, NOUT - n0)
            wt = wb[nb % 2]
            wk = f"wb{nb % 2}"
            kb.dma("sp", wt[:, :, 0:nw], wv[:, :, n0:n0 + nw], writes=[wk])
            for t in range(NT):
                p = pp[cnt % 4]
                pk = f"pp{cnt % 4}"
                o = ot[cnt % 4]
                ok = f"ot{cnt % 4}"
                cnt += 1
                for kc in range(16):
                    kb.op("pe", lambda e, kc=kc, p=p, wt=wt: e.matmul(p[:, 0:nw], lhsT=hT[:, t, kc, :], rhs=wt[:, kc, 0:nw], start=(kc == 0), stop=(kc == 15)),
                          reads=[f"hT{t}", wk], writes=[pk])
                ev = "act" if cnt % 2 else "dve"
                if ev == "act":
                    kb.op("act", lambda e, p=p, o=o: e.copy(out=o[:, 0:nw], in_=p[:, 0:nw]), reads=[pk], writes=[ok])
                else:
                    kb.op("dve", lambda e, p=p, o=o: e.tensor_copy(out=o[:, 0:nw], in_=p[:, 0:nw]), reads=[pk], writes=[ok])
                kb.dma("pool", out[t * 128:(t + 1) * 128, n0:n0 + nw], o[:, 0:nw], reads=[ok], is_output=True)
    return kb


TOT = B * SEQ + B * CTX


def rows_to_cores(a):
    n = a.shape[1]
    pad = np.zeros((NCORES * ROWS, n), a.dtype)
    pad[:TOT] = a
    return [np.ascontiguousarray(pad[c * ROWS:(c + 1) * ROWS]) for c in range(NCORES)]


def cores_to_rows(lst):
    return np.concatenate(lst, axis=0)[:TOT]


def tile_group(g):
    r = g * 128
    if r < SEQ:
        return 0
    if r < 2 * SEQ:
        return 1
    return 2


def mod_rows_for(modl, idxs):
    outs = []
    for c in range(NCORES):
        a = np.zeros((NT, len(idxs), D), np.float32)
        for t in range(NT):
            g = c * NT + t
            if g * 128 < TOT:
                j = tile_group(g)
                for ii, m in enumerate(idxs):
                    a[t, ii] = modl[j, m * D:(m + 1) * D]
        outs.append(a)
    return outs


class Dplr:
    def __init__(self, kb, dk, dvp, mode, has_delta, tag, consts):
        self.kb, self.dk, self.dvp, self.mode, self.hd, self.tag = kb, dk, dvp, mode, has_delta, tag
        self.c = consts
        t = tag
        sb = lambda n, s: kb.sb(f"{t}_{n}", s)
        self.r, self.kap, self.a, self.kt = sb("r", [128, dk]), sb("kap", [128, dk]), sb("a", [128, dk]), sb("kt", [128, dk])
        self.v, self.lw = sb("v", [128, dvp]), sb("lw", [128, dk])
        self.M = sb("M", [dk, dvp])
        self.ecw, self.encw, self.ecwx, self.el = sb("ecw", [128, dk]), sb("encw", [128, dk]), sb("ecwx", [128, dk]), sb("el", [128, dk])
        self.r0, self.k0 = sb("r0", [128, dk]), sb("k0", [128, dk])
        self.ap, self.ktp = sb("ap", [128, dk]), sb("ktp", [128, dk])
        self.aL, self.ktL = sb("aL", [128, dk]), sb("ktL", [128, dk])
        self.kr0T = sb("kr0T", [dk, 2, 128])
        self.krT = sb("krT", [dk, 2, 128])
        self.apT, self.ktpT = sb("apT", [dk, 128]), sb("ktpT", [dk, 128])
        self.AR, self.BR = sb("AR", [128, 256]), sb("BR", [128, 256])
        self.E2 = sb("E2", [128, 256])
        self.Mm = [sb(f"Mm{i}", [128, 128]) for i in range(2)]
        self.Nm = [sb(f"Nm{i}", [128, 128]) for i in range(2)]
        self.P = sb("P", [128, 128])
        self.negG, self.U = sb("negG", [128, dvp]), sb("U", [128, dvp])
        self.ecl = sb("ecl", [dk, 1])
        self.cwc = sb("cwc", [128, 1])

    def k(self, n):
        return f"{self.tag}_{n}"

    def init_state(self):
        self.kb.op("pool", lambda e: e.memset(self.M[:], 0.0), writes=[self.k("M")])

    def step(self, d, bank, y_cb, stop=99):
        kb, dk, dvp, k, c = self.kb, self.dk, self.dvp, self.k, self.c
        TI, TS = (c["le"], c["lt"]) if d == "f" else (c["ge"], c["gt"])
        TIk, TSk = ("m_le", "m_lt") if d == "f" else ("m_ge", "m_gt")
        mask2, mask2k = (c["mask2f"], "mask2f") if d == "f" else (c["mask2b"], "mask2b")
        tri2, tri2k = (c["tri2f"], "tri2f") if d == "f" else (c["tri2b"], "tri2b")
        b0, b0k = bank["b0"]
        b1, b1k = bank["b1"]
        b2, b2k = bank["b2"]
        b3, b3k = bank["b3"]
        b4, b4k = bank["b4"]
        b5, b5k = bank["b5"]
        b6, b6k = bank["b6"]
        b7, b7k = bank["b7"]
        MUL, ADD, SUB = ALU.mult, ALU.add, ALU.subtract
        kb.op("pe", lambda e: e.matmul(b0[:, 0:dk], lhsT=TI[:], rhs=self.lw[:], start=True, stop=True), reads=[TIk, k("lw")], writes=[b0k])
        kb.op("pe", lambda e: e.matmul(b0[:, dk:2 * dk], lhsT=c["ones"][:], rhs=self.lw[:], start=True, stop=True), reads=["m_ones", k("lw")], writes=[b0k])
        kb.op("pe", lambda e: e.matmul(b0[0:dk, 2 * dk:2 * dk + 1], lhsT=self.lw[:], rhs=c["ones"][:, 0:1], start=True, stop=True), reads=["m_ones", k("lw")], writes=[b0k])
        cw, cwl = b0[:, 0:dk], b0[:, dk:2 * dk]
        kb.op("act", lambda e: e.activation(out=self.ecw[:], in_=cw, func=AF.Exp), reads=[b0k], writes=[k("ecw")])
        kb.op("act", lambda e: e.activation(out=self.ecl[:], in_=b0[0:dk, 2 * dk:2 * dk + 1], func=AF.Exp), reads=[b0k], writes=[k("ecl")])
        kb.op("dve", lambda e: e.tensor_tensor(out=self.ecwx[:], in0=cw, in1=self.lw[:], op=SUB), reads=[b0k, k("lw")], writes=[k("ecwx")])
        kb.op("act", lambda e: e.activation(out=self.ecwx[:], in_=self.ecwx[:], func=AF.Exp), reads=[k("ecwx")], writes=[k("ecwx")])
        kb.op("dve", lambda e: e.tensor_copy(out=self.el[:], in_=cw), reads=[b0k], writes=[k("el")])
        kb.op("dve", lambda e: e.tensor_tensor(out=self.el[:], in0=cwl, in1=self.el[:], op=SUB), reads=[b0k, k("el")], writes=[k("el")])
        kb.op("act", lambda e: e.activation(out=self.el[:], in_=self.el[:], func=AF.Exp), reads=[k("el")], writes=[k("el")])
        kb.op("dve", lambda e: e.tensor_tensor(out=self.r0[:], in0=self.r[:], in1=self.ecw[:], op=MUL), reads=[k("r"), k("ecw")], writes=[k("r0")])
        kb.op("dve", lambda e: e.tensor_tensor(out=self.k0[:], in0=self.kap[:], in1=self.ecwx[:], op=MUL), reads=[k("kap"), k("ecwx")], writes=[k("k0")])
        kb.op("pool", lambda e: e.tensor_tensor(out=self.ktL[:], in0=self.kt[:], in1=self.el[:], op=MUL), reads=[k("kt"), k("el")], writes=[k("ktL")])
        if self.hd:
            kb.op("pool", lambda e: e.tensor_tensor(out=self.aL[:], in0=self.a[:], in1=self.el[:], op=MUL), reads=[k("a"), k("el")], writes=[k("aL")])
        if self.mode == "V":
            kb.op("act", lambda e: e.activation(out=self.encw[:], in_=cw, func=AF.Exp, scale=-1.0), reads=[b0k], writes=[k("encw")])
            kb.op("dve", lambda e: e.tensor_tensor(out=self.ktp[:], in0=self.kt[:], in1=self.encw[:], op=MUL), reads=[k("kt"), k("encw")], writes=[k("ktp")])
            if self.hd:
                kb.op("dve", lambda e: e.tensor_tensor(out=self.ap[:], in0=self.a[:], in1=self.encw[:], op=MUL), reads=[k("a"), k("encw")], writes=[k("ap")])
            ktp_src, ktp_k, ap_src, ap_k = self.ktp, k("ktp"), self.ap, k("ap")
        else:
            kb.op("dve", lambda e: e.tensor_copy(out=self.cwc[:], in_=b0[:, 0:1]), reads=[b0k], writes=[k("cwc")])
            ktp_src, ktp_k, ap_src, ap_k = self.kt, k("kt"), self.a, k("a")
        if stop < 3:
            return
        idt = c["ident"]

        def tr(slot, src, srck):
            kb.op("pe", lambda e: e.transpose(out=b1[0:dk, slot * 128:(slot + 1) * 128], in_=src[:], identity=idt[:]), reads=[srck, "ident"], writes=[b1k])
        tr(0, self.k0, k("k0"))
        tr(1, self.r0, k("r0"))
        tr(2, ktp_src, ktp_k)
        if self.hd:
            tr(3, ap_src, ap_k)
        kb.op("dve", lambda e: e.tensor_copy(out=self.kr0T[:].rearrange("p a b -> p (a b)"), in_=b1[0:dk, 0:256]), reads=[b1k], writes=[k("kr0T")])
        kb.op("dve", lambda e: e.tensor_copy(out=self.ktpT[:], in_=b1[0:dk, 256:384]), reads=[b1k], writes=[k("ktpT")])
        if self.hd:
            kb.op("dve", lambda e: e.tensor_copy(out=self.apT[:], in_=b1[0:dk, 384:512]), reads=[b1k], writes=[k("apT")])
        if self.mode == "S":
            tr(0, self.kap, k("kap"))
            tr(1, self.r, k("r"))
            kb.op("dve", lambda e: e.tensor_copy(out=self.krT[:].rearrange("p a b -> p (a b)"), in_=b1[0:dk, 0:256]), reads=[b1k], writes=[k("krT")])
            rhs2, rhs2k = self.krT, k("krT")
        else:
            rhs2, rhs2k = self.kr0T, k("kr0T")
        rhs2f = rhs2[:].rearrange("p a b -> p (a b)")
        if stop < 4:
            return
        if self.hd:
            kb.op("pe", lambda e: e.matmul(b2[:, 0:256], lhsT=self.apT[:], rhs=rhs2f, start=True, stop=True), reads=[k("apT"), rhs2k], writes=[b2k])
        kb.op("pe", lambda e: e.matmul(b2[:, 256:512], lhsT=self.ktpT[:], rhs=rhs2f, start=True, stop=True), reads=[k("ktpT"), rhs2k], writes=[b2k])
        if self.mode == "S":
            kb.op("pe", lambda e: e.matmul(b3[:, 0:256], lhsT=self.lw[:], rhs=tri2[:], start=True, stop=True), reads=[k("lw"), tri2k], writes=[b3k])
            kb.op("dve", lambda e: e.tensor_scalar(out=self.E2[:], in0=b3[:, 0:256], scalar1=self.cwc[:, 0:1], scalar2=0.0, op0=SUB, op1=ALU.min),
                  reads=[b3k, k("cwc")], writes=[k("E2")])
            kb.op("act", lambda e: e.activation(out=self.E2[:], in_=self.E2[:], func=AF.Exp), reads=[k("E2")], writes=[k("E2")])
            kb.op("pool", lambda e: e.tensor_tensor(out=self.E2[:], in0=self.E2[:], in1=mask2[:], op=MUL), reads=[k("E2"), mask2k], writes=[k("E2")])
            mm2, mm2k = self.E2, k("E2")
        else:
            mm2, mm2k = mask2, mask2k
        if self.hd:
            kb.op("dve", lambda e: e.tensor_tensor(out=self.AR[:], in0=b2[:, 0:256], in1=mm2[:], op=MUL), reads=[b2k, mm2k], writes=[k("AR")])
        kb.op("dve", lambda e: e.tensor_tensor(out=self.BR[:], in0=b2[:, 256:512], in1=mm2[:], op=MUL), reads=[b2k, mm2k], writes=[k("BR")])
        Mk = k("M")
        if stop < 5:
            return
        if self.hd:
            M0, N0 = self.Mm[0], self.Nm[0]
            kb.op("dve", lambda e: e.tensor_scalar(out=M0[:], in0=self.AR[:, 0:128], scalar1=-1.0, scalar2=None, op0=MUL), reads=[k("AR")], writes=[k("Mm0")])
            kb.op("pe", lambda e: e.transpose(out=b4[:, 0:128], in_=M0[:], identity=idt[:]), reads=[k("Mm0"), "ident"], writes=[b4k])
            kb.op("act", lambda e: e.copy(out=N0[:], in_=b4[:, 0:128]), reads=[b4k], writes=[k("Nm0")])
            kb.op("pool", lambda e: e.tensor_tensor(out=self.P[:], in0=M0[:], in1=idt[:], op=ADD), reads=[k("Mm0"), "ident"], writes=[k("P")])
            cur = 0
            for lvl in range(6):
                Mc, Nc, Mn, Nn = self.Mm[cur], self.Nm[cur], self.Mm[1 - cur], self.Nm[1 - cur]
                Mck, Nck, Mnk, Nnk = k(f"Mm{cur}"), k(f"Nm{cur}"), k(f"Mm{1 - cur}"), k(f"Nm{1 - cur}")
                last = lvl == 5
                kb.op("pe", lambda e, Mc=Mc, Nc=Nc: e.matmul(b4[:, 0:128], lhsT=Nc[:], rhs=Mc[:], start=True, stop=True), reads=[Mck, Nck], writes=[b4k])
                kb.op("pe", lambda e, Mc=Mc, Nc=Nc: e.matmul(b4[:, 128:256], lhsT=Mc[:], rhs=Nc[:], start=True, stop=True), reads=[Mck, Nck], writes=[b4k])
                if lvl > 0:
                    kb.op("pe", lambda e, Nc=Nc: e.matmul(b4[:, 256:384], lhsT=Nc[:], rhs=self.P[:], start=True, stop=True), reads=[Nck, k("P")], writes=[b4k])
                kb.op("dve", lambda e, Mn=Mn: e.tensor_copy(out=Mn[:], in_=b4[:, 0:128]), reads=[b4k], writes=[Mnk])
                kb.op("act", lambda e, Nn=Nn: e.copy(out=Nn[:], in_=b4[:, 128:256]), reads=[b4k], writes=[Nnk])
                if lvl > 0:
                    kb.op("dve", lambda e: e.tensor_tensor(out=self.P[:], in0=b4[:, 256:384], in1=self.P[:], op=ADD), reads=[b4k, k("P")], writes=[k("P")])
                cur = 1 - cur
            Nc, Nck = self.Nm[cur], k(f"Nm{cur}")
            kb.op("pe", lambda e, Nc=Nc: e.matmul(b4[:, 256:384], lhsT=Nc[:], rhs=self.P[:], start=True, stop=True), reads=[Nck, k("P")], writes=[b4k])
            kb.op("dve", lambda e: e.tensor_tensor(out=self.P[:], in0=b4[:, 256:384], in1=self.P[:], op=ADD), reads=[b4k, k("P")], writes=[k("P")])
            kb.op("pe", lambda e: e.matmul(b5[:, 0:dvp], lhsT=self.kr0T[:, 0, :], rhs=self.M[:], start=True, stop=False), reads=[k("kr0T"), Mk], writes=[b5k])
            kb.op("pe", lambda e: e.matmul(b5[:, 0:dvp], lhsT=self.BR[:, 0:128], rhs=self.v[:], start=False, stop=True), reads=[k("BR"), k("v")], writes=[b5k])
            kb.op("dve", lambda e: e.tensor_scalar(out=self.negG[:], in0=b5[:, 0:dvp], scalar1=-1.0, scalar2=None, op0=MUL), reads=[b5k], writes=[k("negG")])
            kb.op("pe", lambda e: e.matmul(b5[:, 256:256 + dvp], lhsT=self.P[:], rhs=self.negG[:], start=True, stop=True), reads=[k("P"), k("negG")], writes=[b5k])
            kb.op("act", lambda e: e.copy(out=self.U[:], in_=b5[:, 256:256 + dvp]), reads=[b5k], writes=[k("U")])
        if stop < 7:
            return
        kb.op("pe", lambda e: e.matmul(b6[:, 0:dvp], lhsT=self.kr0T[:, 1, :], rhs=self.M[:], start=True, stop=False), reads=[k("kr0T"), Mk], writes=[b6k])
        if self.hd:
            kb.op("pe", lambda e: e.matmul(b6[:, 0:dvp], lhsT=self.AR[:, 128:256], rhs=self.U[:], start=False, stop=False), reads=[k("AR"), k("U")], writes=[b6k])
        kb.op("pe", lambda e: e.matmul(b6[:, 0:dvp], lhsT=self.BR[:, 128:256], rhs=self.v[:], start=False, stop=True), reads=[k("BR"), k("v")], writes=[b6k])
        y_cb(b6[:, 0:dvp], b6k)
        if stop < 8:
            return
        if self.hd:
            kb.op("pe", lambda e: e.matmul(b7[0:dk, 0:dvp], lhsT=self.aL[:], rhs=self.U[:], start=True, stop=False), reads=[k("aL"), k("U")], writes=[b7k])
        kb.op("pe", lambda e: e.matmul(b7[0:dk, 0:dvp], lhsT=self.ktL[:], rhs=self.v[:], start=(not self.hd), stop=True), reads=[k("ktL"), k("v")], writes=[b7k])
        kb.op("dve", lambda e: e.scalar_tensor_tensor(out=self.M[:], in0=self.M[:], scalar=self.ecl[:, 0:1], in1=b7[0:dk, 0:dvp], op0=MUL, op1=ADD),
              reads=[Mk, k("ecl"), b7k], writes=[Mk])


def dplr_consts(kb):
    c = {"ident": ident(kb)}
    for m in ("le", "lt", "ge", "gt", "ones"):
        c[m] = tri_mask(kb, "m_" + m, m)
    for d, (ms, mi) in (("f", ("lt", "le")), ("b", ("gt", "ge"))):
        t = kb.sb("mask2" + d, [128, 256])
        kb.op("pool", lambda e, t=t, ms=ms: e.tensor_copy(out=t[:, 0:128], in_=c[ms][:]), reads=["m_" + ms], writes=["mask2" + d])
        kb.op("pool", lambda e, t=t, mi=mi: e.tensor_copy(out=t[:, 128:256], in_=c[mi][:]), reads=["m_" + mi], writes=["mask2" + d])
        c["mask2" + d] = t
        c["tri2" + d] = t
    return c


def dplr_banks(kb):
    return {f"b{i}": (kb.ps(f"bank{i}", [128, 512]), f"bank{i}") for i in range(8)}


def dplr_banks2(kb):
    sets = []
    for s_ in range(2):
        t = [(kb.ps(f"bank{s_}_{i}", [128, 512]), f"bank{s_}_{i}") for i in range(4)]
        sets.append({f"b{i}": t[i % 4] for i in range(8)})
    return sets


def build_dplr_test(T, dk, dvp, mode, has_delta):
    kb = KB()
    nch = T // 128
    ins = {n: kb.dram_in(n, [T, dk]) for n in ("r", "kap", "a", "kt", "lw")}
    ins["v"] = kb.dram_in("v", [T, dvp])
    outs = {d: kb.dram_out("y" + d, [T, dvp]) for d in "fb"}
    c = dplr_consts(kb)
    banks = dplr_banks(kb)
    sc = Dplr(kb, dk, dvp, mode, has_delta, "s", c)
    yo = kb.sb("yo", [128, dvp])
    for d in "fb":
        sc.init_state()
        order = range(nch) if d == "f" else range(nch - 1, -1, -1)
        for ci in order:
            rows = slice(ci * 128, (ci + 1) * 128)
            for n in ("r", "kap", "a", "kt", "lw", "v"):
                kb.dma("sp", getattr(sc, n)[:], ins[n][rows, :], writes=[sc.k(n)])

            def cb(yp, ypk):
                kb.op("dve", lambda e: e.tensor_copy(out=yo[:], in_=yp), reads=[ypk], writes=["yo"])
                kb.dma("pool", outs[d][rows, :], yo[:], reads=["yo"], is_output=True)
            sc.step(d, banks, cb)
    return kb


def TT(kb, e, out, in0, in1, op, reads, writes):
    return kb.op(e, lambda g: g.tensor_tensor(out=out, in0=in0, in1=in1, op=op), reads=reads, writes=writes)


def TS(kb, e, out, in0, s1, s2, op0, op1, reads, writes):
    if op1 is None:
        return kb.op(e, lambda g: g.tensor_scalar(out=out, in0=in0, scalar1=s1, scalar2=None, op0=op0), reads=reads, writes=writes)
    return kb.op(e, lambda g: g.tensor_scalar(out=out, in0=in0, scalar1=s1, scalar2=s2, op0=op0, op1=op1), reads=reads, writes=writes)


def STT(kb, out, in0, scalar, in1, op0, op1, reads, writes):
    return kb.op("dve", lambda g: g.scalar_tensor_tensor(out=out, in0=in0, scalar=scalar, in1=in1, op0=op0, op1=op1), reads=reads, writes=writes)


def ACT(kb, out, in_, func, reads, writes, **kw):
    return kb.op("act", lambda g: g.activation(out=out, in_=in_, func=func, **kw), reads=reads, writes=writes)


def CP(kb, e, out, in_, reads, writes):
    if e == "act":
        return kb.op("act", lambda g: g.copy(out=out, in_=in_), reads=reads, writes=writes)
    return kb.op(e, lambda g: g.tensor_copy(out=out, in_=in_), reads=reads, writes=writes)


def sumsq_rs(kb, x, xk, junk, junkk, out, outk, n, eps, mean=True):
    ACT(kb, junk, x, AF.Square, [xk], [junkk, outk], accum_out=out)
    ACT(kb, out, out, AF.Sqrt, [outk], [outk], scale=(1.0 / n if mean else 1.0), bias=eps)
    kb.op("dve", lambda g: g.reciprocal(out=out, in_=out), reads=[outk], writes=[outk])


def layernorm_rs(kb, x, xk, st, stk, n, eps):
    kb.op("dve", lambda g: g.bn_stats(out=st[:, 2:8], in_=x), reads=[xk], writes=[stk])
    kb.op("dve", lambda g: g.bn_aggr(out=st[:, 0:2], in_=st[:, 2:8]), reads=[stk], writes=[stk])
    ACT(kb, st[:, 1:2], st[:, 1:2], AF.Sqrt, [stk], [stk], bias=eps, scale=1.0)
    kb.op("dve", lambda g: g.reciprocal(out=st[:, 1:2], in_=st[:, 1:2]), reads=[stk], writes=[stk])


def shared_yacc(kb, tag="yacc"):
    if not hasattr(kb, "_yacc"):
        kb._yacc = {}
    if tag not in kb._yacc:
        kb._yacc[tag] = kb.sb(tag, [128, (CTX + SEQ) // 128, 128])
    return kb._yacc[tag]


LSEQ = CTX + SEQ
NCH = LSEQ // 128
FWD_ORDER = list(range(NCH))
BWD_ORDER = [1, 0] + list(range(NCH - 1, 1, -1))


def emit_rwkv(kb, c, banks, banks1=None, sync=None):
    MUL, ADD, SUB = ALU.mult, ALU.add, ALU.subtract
    X = {n: kb.dram_in("rw_" + n, [LSEQ, 640]) for n in ("cur", "prev", "next")}
    cst_d = kb.dram_in("rw_cst", [128, 2 * 640 + 128 * 9])
    wup_d = kb.dram_in("rw_wup", [2, 64, 128])
    aup_d = kb.dram_in("rw_aup", [2, 64, 128])
    gup_d = kb.dram_in("rw_gup", [128, 128])
    out = kb.dram_out("rw_out", [LSEQ, 128])
    cst = kb.sb("rw_cst_s", [128, 2 * 640 + 128 * 9])
    kb.dma("sp", cst[:], cst_d, writes=["rw_cst"])
    mu0, mu1 = cst[:, 0:640], cst[:, 640:1280]
    o = 1280
    k_k, k_a, r_k, ln_w, ln_b = (cst[:, o + i * 128:o + (i + 1) * 128] for i in range(5))
    w0 = [cst[:, o + (5 + d) * 128:o + (6 + d) * 128] for d in range(2)]
    a0 = [cst[:, o + (7 + d) * 128:o + (8 + d) * 128] for d in range(2)]
    wup = kb.sb("rw_wup_s", [64, 2, 128])
    aup = kb.sb("rw_aup_s", [64, 2, 128])
    gup = kb.sb("rw_gup_s", [128, 128])
    kb.dma("sp", wup[:], wup_d.rearrange("d r n -> r d n"), writes=["rw_wup"])
    kb.dma("sp", aup[:], aup_d.rearrange("d r n -> r d n"), writes=["rw_aup"])
    kb.dma("sp", gup[:], gup_d, writes=["rw_gup"])
    cur, prv, nxt = kb.sb("rw_cur_s", [128, 640]), kb.sb("rw_prv", [128, 640]), kb.sb("rw_nxt", [128, 640])
    kkp, kk = kb.sb("rw_kkp", [128, 128]), kb.sb("rw_kk", [128, 128])
    junk = kb.sb("rw_junk", [128, 128])
    st = kb.sb("rw_st", [128, 8])
    twd = kb.sb("rw_twd", [128, 128])
    twdT = kb.sb("rw_twdT", [64, 2, 128])
    sg = kb.sb("rw_sg", [128, 128])
    sgT = kb.sb("rw_sgT", [128, 128])
    lwt, at, ktt, alt = kb.sb("rw_lw", [128, 128]), kb.sb("rw_a", [128, 128]), kb.sb("rw_kt", [128, 128]), kb.sb("rw_al", [128, 128])
    gt = kb.sb("rw_g", [128, 128])
    bon = kb.sb("rw_bon", [128, 4])
    yacc = shared_yacc(kb, "yaccR")
    yn = kb.sb("rw_yn", [128, 128])
    sc = [Dplr(kb, 64, 64, "V", True, f"rw{h}", c) for h in range(2)]
    b1, b1k = banks["b1"]
    b3, b3k = banks["b3"]
    idt = c["ident"]

    def prep_common(ci):
        rows = slice(ci * 128, (ci + 1) * 128)
        kb.dma("sp", cur[:], X["cur"][rows, :], writes=["rw_cur"])
        kb.dma("sp", prv[:], X["prev"][rows, :], writes=["rw_prv"])
        kb.dma("sp", nxt[:], X["next"][rows, :], writes=["rw_nxt"])
        TT(kb, "dve", prv[:], prv[:], cur[:], SUB, ["rw_prv", "rw_cur"], ["rw_prv"])
        TT(kb, "pool", nxt[:], nxt[:], cur[:], SUB, ["rw_nxt", "rw_cur"], ["rw_nxt"])
        TT(kb, "dve", prv[:], prv[:], mu0, MUL, ["rw_prv", "rw_cst"], ["rw_prv"])
        TT(kb, "pool", nxt[:], nxt[:], mu1, MUL, ["rw_nxt", "rw_cst"], ["rw_nxt"])
        TT(kb, "dve", cur[:], cur[:], prv[:], ADD, ["rw_prv", "rw_cur"], ["rw_cur"])
        TT(kb, "dve", cur[:], cur[:], nxt[:], ADD, ["rw_nxt", "rw_cur"], ["rw_cur"])
        TT(kb, "dve", kkp[:], cur[:, 128:256], k_k, MUL, ["rw_cur", "rw_cst"], ["rw_kkp"])
        for h in range(2):
            ACT(kb, junk[:, 0:64], kkp[:, h * 64:(h + 1) * 64], AF.Square, ["rw_kkp"], ["rw_junk", "rw_st"], accum_out=st[:, h:h + 1])
        ACT(kb, st[:, 0:2], st[:, 0:2], AF.Sqrt, ["rw_st"], ["rw_st"], bias=1e-6, scale=1.0)
        kb.op("dve", lambda g: g.reciprocal(out=st[:, 0:2], in_=st[:, 0:2]), reads=["rw_st"], writes=["rw_st"])
        for h in range(2):
            TS(kb, "dve", kk[:, h * 64:(h + 1) * 64], kkp[:, h * 64:(h + 1) * 64], st[:, h:h + 1], None, MUL, None, ["rw_kkp", "rw_st"], ["rw_kk"])
        ACT(kb, twd[:, 0:64], cur[:, 384:448], AF.Tanh, ["rw_cur"], ["rw_twd"])
        CP(kb, "pool", twd[:, 64:128], cur[:, 448:512], ["rw_cur"], ["rw_twd"])
        for i in range(2):
            kb.op("pe", lambda g, i=i: g.transpose(out=b1[0:64, i * 128:(i + 1) * 128], in_=twd[:, i * 64:(i + 1) * 64], identity=idt[:]),
                  reads=["rw_twd", "ident"], writes=[b1k])
        CP(kb, "dve", twdT[:].rearrange("p a b -> p (a b)"), b1[0:64, 0:256], [b1k], ["rw_twdT"])

    def prep_dir(d):
        kb.op("pe", lambda g: g.matmul(b3[:, 0:128], lhsT=twdT[:, 0, :], rhs=wup[:, d, :], start=True, stop=True), reads=["rw_twdT", "rw_wup"], writes=[b3k])
        kb.op("pe", lambda g: g.matmul(b3[:, 128:256], lhsT=twdT[:, 1, :], rhs=aup[:, d, :], start=True, stop=True), reads=["rw_twdT", "rw_aup"], writes=[b3k])
        TT(kb, "dve", lwt[:], b3[:, 0:128], w0[d], ADD, [b3k, "rw_cst"], ["rw_lw"])
        TT(kb, "dve", at[:], b3[:, 128:256], a0[d], ADD, [b3k, "rw_cst"], ["rw_a"])
        ACT(kb, lwt[:], lwt[:], AF.Sigmoid, ["rw_lw"], ["rw_lw"])
        ACT(kb, at[:], at[:], AF.Sigmoid, ["rw_a"], ["rw_a"])
        TS(kb, "pool", lwt[:], lwt[:], -math.exp(-0.5), None, MUL, None, ["rw_lw"], ["rw_lw"])
        STT(kb, ktt[:], at[:], -1.0, k_a, ADD, MUL, ["rw_a", "rw_cst"], ["rw_kt"])
        STT(kb, ktt[:], ktt[:], 1.0, cur[:, 128:256], ADD, MUL, ["rw_kt", "rw_cur"], ["rw_kt"])

    def mkcb(h, ci, d):
        def cb(yp, ypk):
            dst = yacc[:, ci, h * 64:(h + 1) * 64]
            if d == 0:
                CP(kb, "act", dst, yp, [ypk], ["yaccR"])
            else:
                TT(kb, "dve", dst, yp, dst, ADD, [ypk, "yaccR"], ["yaccR"])
        return cb

    if sync is not None:
        sync.update(sc=sc, mkcb=mkcb, ready=-1, done=-1, nsteps=2 * NCH)
    step_no = [0]

    def run_dir(d, order):
        for hh, s in enumerate(sc):
            if sync is None or hh == 0:
                s.init_state()
        for ci in order:
            if sync is not None:
                kb.spin(lambda: sync["done"] >= step_no[0] - 1)
            prep_common(ci)
            prep_dir(d)
            TT(kb, "pool", alt[:], kk[:], at[:], MUL, ["rw_kk", "rw_a"], ["rw_al"])
            for h in range(2):
                s = sc[h]
                hs = slice(h * 64, (h + 1) * 64)
                CP(kb, "pool", s.r[:], cur[:, hs], ["rw_cur"], [s.k("r")])
                CP(kb, "pool", s.v[:], cur[:, 256 + h * 64:256 + (h + 1) * 64], ["rw_cur"], [s.k("v")])
                CP(kb, "pool", s.kap[:], kk[:, hs], ["rw_kk"], [s.k("kap")])
                CP(kb, "pool", s.a[:], alt[:, hs], ["rw_al"], [s.k("a")])
                CP(kb, "pool", s.kt[:], ktt[:, hs], ["rw_kt"], [s.k("kt")])
                CP(kb, "pool", s.lw[:], lwt[:, hs], ["rw_lw"], [s.k("lw")])

                if sync is None:
                    s.step("f" if d == 0 else "b", banks, mkcb(h, ci, d))
            if sync is not None:
                sync["job"] = (d, ci)
                sync["ready"] = step_no[0]
                sc[0].step("f" if d == 0 else "b", banks, mkcb(0, ci, d))
                step_no[0] += 1

    run_dir(0, FWD_ORDER)
    run_dir(1, BWD_ORDER)
    if sync is not None:
        kb.spin(lambda: sync["done"] >= 2 * NCH - 1)
    for ci in range(NCH):
        prep_common(ci)
        ACT(kb, sg[:], cur[:, 512:640], AF.Sigmoid, ["rw_cur"], ["rw_sg"])
        kb.op("pe", lambda g: g.transpose(out=b1[:, 0:128], in_=sg[:], identity=idt[:]), reads=["rw_sg", "ident"], writes=[b1k])
        CP(kb, "dve", sgT[:], b1[:, 0:128], [b1k], ["rw_sgT"])
        kb.op("pe", lambda g: g.matmul(b1[:, 128:256], lhsT=sgT[:], rhs=gup[:], start=True, stop=True), reads=["rw_sgT", "rw_gup"], writes=[b1k])
        CP(kb, "act", gt[:], b1[:, 128:256], [b1k], ["rw_g"])
        for d in range(2):
            prep_dir(d)
            TT(kb, "dve", junk[:], cur[:, 0:128], ktt[:], MUL, ["rw_cur", "rw_kt"], ["rw_junk"])
            TT(kb, "dve", junk[:], junk[:], r_k, MUL, ["rw_junk", "rw_cst"], ["rw_junk"])
            kb.op("dve", lambda g, d=d: g.tensor_reduce(out=bon[:, 2 * d:2 * d + 2], in_=junk[:].rearrange("p (h n) -> p h n", h=2), axis=AX.X, op=ADD),
                  reads=["rw_junk"], writes=["rw_bon"])
        TT(kb, "dve", bon[:, 0:2], bon[:, 0:2], bon[:, 2:4], ADD, ["rw_bon"], ["rw_bon"])
        for h in range(2):
            hs = slice(h * 64, (h + 1) * 64)
            layernorm_rs(kb, yacc[:, ci, hs], "yaccR", st, "rw_st", 64, 64e-5)
            TS(kb, "dve", yn[:, hs], yacc[:, ci, hs], st[:, 0:1], st[:, 1:2], SUB, MUL, ["yaccR", "rw_st"], ["rw_yn"])
        TT(kb, "dve", yn[:], yn[:], ln_w, MUL, ["rw_yn", "rw_cst"], ["rw_yn"])
        TT(kb, "dve", yn[:], yn[:], ln_b, ADD, ["rw_yn", "rw_cst"], ["rw_yn"])
        for h in range(2):
            hs = slice(h * 64, (h + 1) * 64)
            STT(kb, yn[:, hs], cur[:, 256 + h * 64:256 + (h + 1) * 64], bon[:, h:h + 1], yn[:, hs], MUL, ADD, ["rw_cur", "rw_bon", "rw_yn"], ["rw_yn"])
        TT(kb, "dve", yn[:], yn[:], gt[:], MUL, ["rw_yn", "rw_g"], ["rw_yn"])
        kb.dma("pool", out[ci * 128:(ci + 1) * 128, :], yn[:], reads=["rw_yn"], is_output=True)


def emit_rwkv_head1(kb, banks1, sync):
    kb.spin(lambda: "sc" in sync)
    s = sync["sc"][1]
    for n in range(2 * NCH):
        kb.spin(lambda: sync["ready"] >= n)
        d, ci = sync["job"]
        if n == 0 or n == NCH:
            s.init_state()
        s.step("f" if d == 0 else "b", banks1, sync["mkcb"](1, ci, d))
        sync["done"] = n


def seq_rows(pl_all, b):
    lat = pl_all[b * SEQ:(b + 1) * SEQ]
    cx = pl_all[2 * SEQ + b * CTX:2 * SEQ + (b + 1) * CTX]
    return cx, lat


def shifted(cx, lat, sh):
    def s(x):
        o = np.zeros_like(x)
        if sh < 0:
            o[1:] = x[:-1]
        else:
            o[:-1] = x[1:]
        return o
    return np.concatenate([s(cx), s(lat)], 0)


def rep(v):
    return np.ascontiguousarray(np.broadcast_to(np.asarray(v, np.float32).reshape(1, -1), (128, np.asarray(v).size)))


def rwkv_inputs(pl_all, p, b, j):
    cx, lat = seq_rows(pl_all[:, 0:RW_COLS], b)
    cs = slice(j * 128, (j + 1) * 128)
    cols = np.r_[np.arange(j * 128, (j + 1) * 128), GW + np.arange(j * 128, (j + 1) * 128), 2 * GW + np.arange(j * 128, (j + 1) * 128),
                 np.arange(3 * GW, 3 * GW + 256)]
    m = {}
    m["rw_cur"] = np.ascontiguousarray(np.concatenate([cx, lat], 0)[:, cols])
    m["rw_prev"] = np.ascontiguousarray(shifted(cx, lat, -1)[:, cols])
    m["rw_next"] = np.ascontiguousarray(shifted(cx, lat, +1)[:, cols])
    cst = [rep(p["rw_mu"][0][cols]), rep(p["rw_mu"][1][cols]), rep(p["rw_k_k"][cs]), rep(p["rw_k_a"][cs]),
           rep(p["rw_r_k"].reshape(-1)[cs]), rep(p["rw_ln_w"][cs]), rep(p["rw_ln_b"][cs]),
           rep(p["rw_w0"][0][cs]), rep(p["rw_w0"][1][cs]), rep(p["rw_a0"][0][cs]), rep(p["rw_a0"][1][cs])]
    m["rw_cst"] = np.ascontiguousarray(np.concatenate(cst, 1))
    m["rw_wup"] = np.ascontiguousarray(p["rw_w_up"][:, :, cs])
    m["rw_aup"] = np.ascontiguousarray(p["rw_a_up"][:, :, cs])
    m["rw_gup"] = np.ascontiguousarray(p["rw_g_up"][:, cs])
    return m


def emit_mlstm(kb, c, banks):
    MUL, ADD, SUB = ALU.mult, ALU.add, ALU.subtract
    X = kb.dram_in("ml_x", [LSEQ, 516])
    cst_d = kb.dram_in("ml_cst", [128, 128 + 4])
    out = kb.dram_out("ml_out", [LSEQ, 128])
    cst = kb.sb("ml_cst_s", [128, 132])
    kb.dma("sp", cst[:], cst_d, writes=["ml_cst"])
    x = kb.sb("ml_xs", [128, 516])
    gs = kb.sb("ml_gs", [128, 4])
    st = kb.sb("ml_st", [128, 8])
    yacc = shared_yacc(kb)
    yn = kb.sb("ml_yn", [128, 128])
    sgo = kb.sb("ml_sgo", [128, 128])
    s = Dplr(kb, 128, 129, "S", False, "ml", c)
    kb.op("pool", lambda g: g.memset(s.v[:, 128:129], 1.0), writes=[s.k("v")])
    ones = c["ones"]

    def run_dir(d, order):
        s.init_state()
        for ci in order:
            rows = slice(ci * 128, (ci + 1) * 128)
            kb.dma("sp", x[:], X[rows, :], writes=["ml_x"])
            ACT(kb, gs[:, 0:1], x[:, 512 + d:513 + d], AF.Exp, ["ml_x", "ml_cst"], ["ml_gs"], bias=cst[:, 128 + d:129 + d], scale=1.0)
            ACT(kb, gs[:, 1:2], x[:, 514 + d:515 + d], AF.Sigmoid, ["ml_x", "ml_cst"], ["ml_gs"], bias=cst[:, 130 + d:131 + d], scale=1.0)
            ACT(kb, gs[:, 1:2], gs[:, 1:2], AF.Ln, ["ml_gs"], ["ml_gs"])
            CP(kb, "pool", s.r[:], x[:, 0:128], ["ml_x"], [s.k("r")])
            TS(kb, "dve", s.kt[:], x[:, 128:256], gs[:, 0:1], 128.0 ** -0.5, MUL, MUL, ["ml_x", "ml_gs"], [s.k("kt")])
            CP(kb, "pool", s.v[:, 0:128], x[:, 256:384], ["ml_x"], [s.k("v")])
            TS(kb, "dve", s.lw[:], ones[:], gs[:, 1:2], None, MUL, None, ["m_ones", "ml_gs"], [s.k("lw")])

            def cb(yp, ypk, ci=ci):
                TS(kb, "dve", st[:, 1:2], yp[:, 128:129], -1.0, None, MUL, None, [ypk], ["ml_st"])
                TT(kb, "dve", st[:, 0:1], yp[:, 128:129], st[:, 1:2], ALU.max, [ypk, "ml_st"], ["ml_st"])
                TS(kb, "dve", st[:, 0:1], st[:, 0:1], 1.0, None, ALU.max, None, ["ml_st"], ["ml_st"])
                kb.op("dve", lambda g: g.reciprocal(out=st[:, 0:1], in_=st[:, 0:1]), reads=["ml_st"], writes=["ml_st"])
                dst = yacc[:, ci, :]
                if d == 0:
                    TS(kb, "dve", dst, yp[:, 0:128], st[:, 0:1], None, MUL, None, [ypk, "ml_st"], ["yacc"])
                else:
                    STT(kb, dst, yp[:, 0:128], st[:, 0:1], dst, MUL, ADD, [ypk, "ml_st", "yacc"], ["yacc"])
            s.step("f" if d == 0 else "b", banks, cb)

    run_dir(0, FWD_ORDER)
    run_dir(1, BWD_ORDER)
    for ci in range(NCH):
        rows = slice(ci * 128, (ci + 1) * 128)
        kb.dma("sp", x[:], X[rows, :], writes=["ml_x"])
        layernorm_rs(kb, yacc[:, ci, :], "yacc", st, "ml_st", 128, EPS)
        TS(kb, "dve", yn[:], yacc[:, ci, :], st[:, 0:1], st[:, 1:2], SUB, MUL, ["yacc", "ml_st"], ["ml_yn"])
        TT(kb, "dve", yn[:], yn[:], cst[:, 0:128], MUL, ["ml_yn", "ml_cst"], ["ml_yn"])
        ACT(kb, sgo[:], x[:, 384:512], AF.Sigmoid, ["ml_x"], ["ml_sgo"])
        TT(kb, "dve", yn[:], yn[:], sgo[:], MUL, ["ml_yn", "ml_sgo"], ["ml_yn"])
        kb.dma("pool", out[rows, :], yn[:], reads=["ml_yn"], is_output=True)


def mlstm_inputs(pl_all, p, b, j):
    cx, lat = seq_rows(pl_all[:, RW_COLS:RW_COLS + ML_COLS], b)
    cols = np.r_[np.arange(j * 128, (j + 1) * 128), GW + np.arange(j * 128, (j + 1) * 128), 2 * GW + np.arange(j * 128, (j + 1) * 128),
                 3 * GW + np.arange(j * 128, (j + 1) * 128), 4 * GW + np.array([j, 4 + j, 8 + j, 12 + j])]
    m = {"ml_x": np.ascontiguousarray(np.concatenate([cx, lat], 0)[:, cols])}
    cst = [rep(p["ml_norm_g"][j * 128:(j + 1) * 128]), rep([p["ml_ib"][0][j], p["ml_ib"][1][j], p["ml_fb"][0][j], p["ml_fb"][1][j]])]
    m["ml_cst"] = np.ascontiguousarray(np.concatenate(cst, 1))
    return m


def emit_gdn(kb, c, banks):
    MUL, ADD, SUB = ALU.mult, ALU.add, ALU.subtract
    X = {n: kb.dram_in("gd_" + n, [LSEQ, 384]) for n in ("cur", "prev", "next")}
    G = kb.dram_in("gd_g", [LSEQ, 132])
    cst_d = kb.dram_in("gd_cst", [128, 3 * 384 + 128 + 4])
    out = kb.dram_out("gd_out", [LSEQ, 128])
    cst = kb.sb("gd_cst_s", [128, 3 * 384 + 132])
    kb.dma("sp", cst[:], cst_d, writes=["gd_cst"])
    nega = kb.sb("gd_nega", [128, 2])
    ACT(kb, nega[:], cst[:, 1280:1282], AF.Exp, ["gd_cst"], ["gd_nega"])
    TS(kb, "dve", nega[:], nega[:], -1.0, None, MUL, None, ["gd_nega"], ["gd_nega"])
    cur, prv, nxt = kb.sb("gd_cur_s", [128, 384]), kb.sb("gd_prv", [128, 384]), kb.sb("gd_nxt", [128, 384])
    gg = kb.sb("gd_gs", [128, 132])
    sg = kb.sb("gd_sg", [128, 384])
    junk = kb.sb("gd_junk", [128, 128])
    st = kb.sb("gd_st", [128, 8])
    gs = kb.sb("gd_gsc", [128, 4])
    yacc = shared_yacc(kb)
    yn = kb.sb("gd_yn", [128, 128])
    s = Dplr(kb, 128, 128, "S", True, "gd", c)
    ones = c["ones"]

    def run_dir(d, order):
        s.init_state()
        for ci in order:
            rows = slice(ci * 128, (ci + 1) * 128)
            kb.dma("sp", cur[:], X["cur"][rows, :], writes=["gd_cur"])
            kb.dma("sp", prv[:], X["prev"][rows, :], writes=["gd_prv"])
            kb.dma("sp", nxt[:], X["next"][rows, :], writes=["gd_nxt"])
            kb.dma("sp", gg[:], G[rows, :], writes=["gd_g"])
            TT(kb, "dve", cur[:], cur[:], cst[:, 384:768], MUL, ["gd_cur", "gd_cst"], ["gd_cur"])
            TT(kb, "pool", prv[:], prv[:], cst[:, 0:384], MUL, ["gd_prv", "gd_cst"], ["gd_prv"])
            TT(kb, "pool", nxt[:], nxt[:], cst[:, 768:1152], MUL, ["gd_nxt", "gd_cst"], ["gd_nxt"])
            TT(kb, "dve", cur[:], cur[:], prv[:], ADD, ["gd_cur", "gd_prv"], ["gd_cur"])
            TT(kb, "dve", cur[:], cur[:], nxt[:], ADD, ["gd_cur", "gd_nxt"], ["gd_cur"])
            ACT(kb, sg[:], cur[:], AF.Sigmoid, ["gd_cur"], ["gd_sg"])
            TT(kb, "dve", cur[:], cur[:], sg[:], MUL, ["gd_cur", "gd_sg"], ["gd_cur"])
            for i in range(2):
                ACT(kb, junk[:], cur[:, i * 128:(i + 1) * 128], AF.Square, ["gd_cur"], ["gd_junk", "gd_st"], accum_out=st[:, i:i + 1])
            ACT(kb, st[:, 0:2], st[:, 0:2], AF.Sqrt, ["gd_st"], ["gd_st"], bias=1e-6, scale=1.0)
            kb.op("dve", lambda g: g.reciprocal(out=st[:, 0:2], in_=st[:, 0:2]), reads=["gd_st"], writes=["gd_st"])
            TS(kb, "dve", s.r[:], cur[:, 0:128], st[:, 0:1], 128.0 ** -0.5, MUL, MUL, ["gd_cur", "gd_st"], [s.k("r")])
            TS(kb, "dve", s.kap[:], cur[:, 128:256], st[:, 1:2], None, MUL, None, ["gd_cur", "gd_st"], [s.k("kap")])
            CP(kb, "pool", s.v[:], cur[:, 256:384], ["gd_cur"], [s.k("v")])
            ACT(kb, gs[:, 0:1], gg[:, 128 + d:129 + d], AF.Exp, ["gd_g", "gd_cst"], ["gd_gsc"], bias=cst[:, 1282 + d:1283 + d], scale=1.0)
            ACT(kb, gs[:, 0:1], gs[:, 0:1], AF.Ln, ["gd_gsc"], ["gd_gsc"], bias=1.0, scale=1.0)
            TT(kb, "dve", gs[:, 0:1], gs[:, 0:1], nega[:, d:d + 1], MUL, ["gd_gsc", "gd_nega"], ["gd_gsc"])
            ACT(kb, gs[:, 1:2], gg[:, 130 + d:131 + d], AF.Sigmoid, ["gd_g"], ["gd_gsc"])
            ACT(kb, gs[:, 2:3], gs[:, 0:1], AF.Exp, ["gd_gsc"], ["gd_gsc"])
            TT(kb, "dve", gs[:, 2:3], gs[:, 2:3], gs[:, 1:2], MUL, ["gd_gsc"], ["gd_gsc"])
            TS(kb, "dve", s.lw[:], ones[:], gs[:, 0:1], None, MUL, None, ["m_ones", "gd_gsc"], [s.k("lw")])
            TS(kb, "dve", s.kt[:], s.kap[:], gs[:, 1:2], None, MUL, None, [s.k("kap"), "gd_gsc"], [s.k("kt")])
            TS(kb, "dve", s.a[:], s.kap[:], gs[:, 2:3], None, MUL, None, [s.k("kap"), "gd_gsc"], [s.k("a")])

            def cb(yp, ypk, ci=ci):
                dst = yacc[:, ci, :]
                if d == 0:
                    CP(kb, "act", dst, yp, [ypk], ["yacc"])
                else:
                    TT(kb, "dve", dst, yp, dst, ADD, [ypk, "yacc"], ["yacc"])
            s.step("f" if d == 0 else "b", banks, cb)

    run_dir(0, FWD_ORDER)
    run_dir(1, BWD_ORDER)
    for ci in range(NCH):
        rows = slice(ci * 128, (ci + 1) * 128)
        kb.dma("sp", gg[:], G[rows, :], writes=["gd_g"])
        sumsq_rs(kb, yacc[:, ci, :], "yacc", junk[:], "gd_junk", st[:, 0:1], "gd_st", 128, EPS)
        STT(kb, yn[:], yacc[:, ci, :], st[:, 0:1], cst[:, 1152:1280], MUL, MUL, ["yacc", "gd_st", "gd_cst"], ["gd_yn"])
        ACT(kb, sg[:, 0:128], gg[:, 0:128], AF.Sigmoid, ["gd_g"], ["gd_sg"])
        TT(kb, "dve", sg[:, 0:128], sg[:, 0:128], gg[:, 0:128], MUL, ["gd_sg", "gd_g"], ["gd_sg"])
        TT(kb, "dve", yn[:], yn[:], sg[:, 0:128], MUL, ["gd_yn", "gd_sg"], ["gd_yn"])
        kb.dma("pool", out[rows, :], yn[:], reads=["gd_yn"], is_output=True)


def gdn_inputs(pl_all, p, b, j):
    o = RW_COLS + ML_COLS
    cx, lat = seq_rows(pl_all[:, o:o + GD_COLS], b)
    cols = np.r_[np.arange(j * 128, (j + 1) * 128), GW + np.arange(j * 128, (j + 1) * 128), 2 * GW + np.arange(j * 128, (j + 1) * 128)]
    gcols = np.r_[3 * GW + np.arange(j * 128, (j + 1) * 128), 4 * GW + np.array([j, 4 + j, 8 + j, 12 + j])]
    m = {}
    m["gd_cur"] = np.ascontiguousarray(np.concatenate([cx, lat], 0)[:, cols])
    m["gd_prev"] = np.ascontiguousarray(shifted(cx, lat, -1)[:, cols])
    m["gd_next"] = np.ascontiguousarray(shifted(cx, lat, +1)[:, cols])
    m["gd_g"] = np.ascontiguousarray(np.concatenate([cx, lat], 0)[:, gcols])
    cst = [rep(p["gd_conv"][0][cols]), rep(p["gd_conv"][1][cols]), rep(p["gd_conv"][2][cols]), rep(p["gd_norm_g"]),
           rep([p["gd_a_log"][0][j], p["gd_a_log"][1][j], p["gd_dt_bias"][0][j], p["gd_dt_bias"][1][j]])]
    m["gd_cst"] = np.ascontiguousarray(np.concatenate(cst, 1))
    return m


def emit_attn(kb, c, banks, need_ctx=True):
    MUL, ADD, SUB = ALU.mult, ALU.add, ALU.subtract
    Q, Kd, V = kb.dram_in("at_q", [LSEQ, 128]), kb.dram_in("at_k", [LSEQ, 128]), kb.dram_in("at_v", [LSEQ, 128])
    COS, SIN = kb.dram_in("at_cos", [LSEQ, 128]), kb.dram_in("at_sin", [LSEQ, 128])
    cst_d = kb.dram_in("at_cst", [128, 256])
    out = kb.dram_out("at_out", [LSEQ, 128])
    cst = kb.sb("at_cst_s", [128, 256])
    kb.dma("sp", cst[:], cst_d, writes=["at_cst"])
    qT, kT = kb.sb("at_qT", [128, LSEQ]), kb.sb("at_kT", [128, LSEQ])
    va = kb.sb("at_va", [128, NCH, 129])
    kb.op("pool", lambda g: g.memset(va[:], 1.0), writes=["at_va"])
    x = kb.sb("at_x", [128, 2, 128])
    xn = kb.sb("at_xn", [128, 2, 128])
    rot = kb.sb("at_rot", [128, 2, 128])
    cs = kb.sb("at_cs", [128, 2, 128])
    junk = kb.sb("at_junk", [128, 128])
    st = kb.sb("at_st", [128, 4])
    idt = c["ident"]
    b2, b2k = banks["b1"]
    for ci in range(NCH):
        rows = slice(ci * 128, (ci + 1) * 128)
        kb.dma("sp", x[:, 0, :], Q[rows, :], writes=["at_x"])
        kb.dma("sp", x[:, 1, :], Kd[rows, :], writes=["at_x"])
        kb.dma("sp", va[:, ci, 0:128], V[rows, :], writes=["at_va"])
        kb.dma("sp", cs[:, 0, :], COS[rows, :], writes=["at_cs"])
        kb.dma("sp", cs[:, 1, :], SIN[rows, :], writes=["at_cs"])
        for i in range(2):
            sumsq_rs(kb, x[:, i, :], "at_x", junk[:], "at_junk", st[:, i:i + 1], "at_st", 128, EPS)
            STT(kb, xn[:, i, :], x[:, i, :], st[:, i:i + 1], cst[:, i * 128:(i + 1) * 128], MUL, MUL, ["at_x", "at_st", "at_cst"], ["at_xn"])
            xv = xn[:, i, :].rearrange("p (h t n) -> p h t n", h=2, t=2)
            rv = rot[:, i, :].rearrange("p (h t n) -> p h t n", h=2, t=2)
            CP(kb, "pool", rv[:, :, 0, :], xv[:, :, 1, :], ["at_xn"], ["at_rot"])
            CP(kb, "pool", rv[:, :, 1, :], xv[:, :, 0, :], ["at_xn"], ["at_rot"])
            TT(kb, "dve", xn[:, i, :], xn[:, i, :], cs[:, 0, :], MUL, ["at_xn", "at_cs"], ["at_xn"])
            TT(kb, "pool", rot[:, i, :], rot[:, i, :], cs[:, 1, :], MUL, ["at_rot", "at_cs"], ["at_rot"])
            TT(kb, "dve", xn[:, i, :], xn[:, i, :], rot[:, i, :], ADD, ["at_xn", "at_rot"], ["at_xn"])
            kb.op("pe", lambda g, i=i: g.transpose(out=b2[:, i * 128:(i + 1) * 128], in_=xn[:, i, :], identity=idt[:]), reads=["at_xn", "ident"], writes=[b2k])
        CP(kb, "dve", qT[:, rows], b2[:, 0:128], [b2k], ["at_qT"])
        CP(kb, "dve", kT[:, rows], b2[:, 128:256], [b2k], ["at_kT"])
    bS = [banks["b0"], banks["b1"]]
    bO = [banks["b2"], banks["b3"]]
    pT = [kb.sb(f"at_pT{i}", [128, 256]) for i in range(2)]
    ot = kb.sb("at_ot", [128, 128])
    blocks = []
    if need_ctx:
        blocks.append((0, 256, [0, 1]))
    for qb in range(SEQ // 256):
        blocks.append((CTX + qb * 256, 256, list(range(NCH))))
    n = 0
    for q0, qn, kts in blocks:
        nq = qn // 128
        for idx, kt in enumerate(kts):
            (bs, bsk), p, pk = bS[n % 2], pT[n % 2], f"at_pT{n % 2}"
            n += 1
            kb.op("pe", lambda g, bs=bs, kt=kt: g.matmul(bs[:, 0:qn], lhsT=kT[:, kt * 128:(kt + 1) * 128], rhs=qT[:, q0:q0 + qn], start=True, stop=True),
                  reads=["at_kT", "at_qT"], writes=[bsk])
            ACT(kb, p[:, 0:qn], bs[:, 0:qn], AF.Exp, [bsk], [pk], scale=128.0 ** -0.5)
            for qs in range(nq):
                bo, bok = bO[qs]
                kb.op("pe", lambda g, bo=bo, p=p, qs=qs, kt=kt, idx=idx: g.matmul(bo[:, 0:129], lhsT=p[:, qs * 128:(qs + 1) * 128], rhs=va[:, kt, :],
                                                                          start=(idx == 0), stop=(idx == len(kts) - 1)),
                      reads=[pk, "at_va"], writes=[bok])
        for qs in range(nq):
            bo, bok = bO[qs]
            kb.op("dve", lambda g, bo=bo: g.reciprocal(out=st[:, 2:3], in_=bo[:, 128:129]), reads=[bok], writes=["at_st2"])
            TS(kb, "dve", ot[:], bo[:, 0:128], st[:, 2:3], None, MUL, None, [bok, "at_st2"], ["at_ot"])
            kb.dma("pool", out[q0 + qs * 128:q0 + (qs + 1) * 128, :], ot[:], reads=["at_ot"], is_output=True)


def rope_tables():
    rows = SEQ // 64
    row = np.repeat(np.arange(rows), 64).astype(np.float32)
    col = np.tile(np.arange(64), rows).astype(np.float32)
    inv = (10000.0 ** (-np.arange(0, 64, 2, dtype=np.float32) / 64)).astype(np.float32)
    ar, ac = row[:, None] * inv[None, :], col[:, None] * inv[None, :]
    cos = np.concatenate([np.cos(ar), np.cos(ar), np.cos(ac), np.cos(ac)], 1)
    sin = np.concatenate([-np.sin(ar), np.sin(ar), -np.sin(ac), np.sin(ac)], 1)
    cos = np.concatenate([np.ones((CTX, 128)), cos], 0).astype(np.float32)
    sin = np.concatenate([np.zeros((CTX, 128)), sin], 0).astype(np.float32)
    return np.ascontiguousarray(cos), np.ascontiguousarray(sin)


def attn_inputs(pl_all, p, b, j):
    o = RW_COLS + ML_COLS + GD_COLS
    cx, lat = seq_rows(pl_all[:, o:o + AT_COLS], b)
    a = np.concatenate([cx, lat], 0)
    kv = j // 2
    cos, sin = rope_tables()
    m = {"at_q": np.ascontiguousarray(a[:, j * 128:(j + 1) * 128]), "at_k": np.ascontiguousarray(a[:, 512 + kv * 128:512 + (kv + 1) * 128]),
         "at_v": np.ascontiguousarray(a[:, 768 + kv * 128:768 + (kv + 1) * 128]), "at_cos": cos, "at_sin": sin,
         "at_cst": np.ascontiguousarray(np.concatenate([rep(p["at_q_norm"]), rep(p["at_k_norm"])], 1))}
    return m


def build_outproj():
    MUL, ADD, SUB = ALU.mult, ALU.add, ALU.subtract
    kb = KB()
    mix = kb.dram_in("mix", [ROWS, D])
    xin = kb.dram_in("xin", [ROWS, D])
    w = kb.dram_in("w", [D, D])
    mods = kb.dram_in("mods", [NT, 3, D])
    gvec = kb.dram_in("g", [1, D])
    wr = kb.dram_in("wr", [D, NE])
    xmid = kb.dram_out("xmid", [ROWS, D])
    h2o = kb.dram_out("h2", [ROWS, D])
    affo = kb.dram_out("aff", [ROWS, NE])
    idt = ident(kb)
    g_bc = kb.sb("g_bc", [128, D])
    kb.dma("sp", g_bc[:], gvec[0, :].partition_broadcast(128), writes=["g_bc"])
    wrs = kb.sb("wrs", [128, 16, NE])
    kb.dma("sp", wrs[:], wr.rearrange("(kc p) n -> p kc n", p=128), writes=["wrs"])
    GT = 5
    mixT = kb.sb("mixT", [128, GT, 16, 128])
    xm = kb.sb("xm", [128, GT, D])
    xt = kb.sb("xt", [128, D])
    sc, sh = kb.sb("sc", [128, D]), kb.sb("sh", [128, D])
    junk = kb.sb("junk", [128, D])
    h2T = kb.sb("h2T", [128, 16, 128])
    rstd = kb.sb("rstd", [128, 4])
    lg = kb.sb("lg", [128, NE])
    pst = [kb.ps(f"pst{i}", [128, 4, 128]) for i in range(2)]
    pstk = ["pst0", "pst1"]
    NB = 256
    wv = w.rearrange("(kc p) n -> p kc n", p=128)
    wb = [kb.sb(f"wb{i}", [128, 16, NB]) for i in range(2)]
    pp = [kb.ps(f"pp{i}", [128, NB]) for i in range(4)]
    m2b = [kb.sb(f"m2b{i}", [128, NB]) for i in range(4)]
    pr = kb.ps("pr", [128, NE])
    cnt = 0
    wcnt = 0
    for g0 in range(0, NT, GT):
        tiles = list(range(g0, min(NT, g0 + GT)))
        for t in tiles:
            lt = t - g0
            rows = slice(t * 128, (t + 1) * 128)
            kb.dma("sp", xt[:], mix[rows, :], writes=["xt"])
            kb.dma("sp", xm[:, lt, :], xin[rows, :], writes=[f"xm{lt}"])
            transpose_rows(kb, idt, xt, "xt", mixT[:, lt], f"mixT{lt}", pst, pstk)
        for nb in range(D // NB):
            wt, wk = wb[wcnt % 2], f"wb{wcnt % 2}"
            wcnt += 1
            kb.dma("sp", wt[:], wv[:, :, nb * NB:(nb + 1) * NB], writes=[wk])
            for t in tiles:
                lt = t - g0
                p, pk, mb, mk = pp[cnt % 4], f"pp{cnt % 4}", m2b[cnt % 4], f"m2b{cnt % 4}"
                cnt += 1
                kb.dma("sp", mb[:], mods[t, 0, nb * NB:(nb + 1) * NB].partition_broadcast(128), writes=[mk])
                for kc in range(16):
                    kb.op("pe", lambda e, kc=kc, p=p, wt=wt, lt=lt: e.matmul(p[:], lhsT=mixT[:, lt, kc, :], rhs=wt[:, kc, :], start=(kc == 0), stop=(kc == 15)),
                          reads=[f"mixT{lt}", wk], writes=[pk])
                TT(kb, "dve", mb[:], p[:], mb[:], MUL, [pk, mk], [mk])
                TT(kb, "pool", xm[:, lt, nb * NB:(nb + 1) * NB], xm[:, lt, nb * NB:(nb + 1) * NB], mb[:], ADD, [mk, f"xm{lt}"], [f"xm{lt}"])
        for t in tiles:
            lt = t - g0
            rows = slice(t * 128, (t + 1) * 128)
            x, xk = xm[:, lt, :], f"xm{lt}"
            kb.dma("pool", xmid[rows, :], x, reads=[xk], is_output=True)
            rms_rstd(kb, x, xk, junk[:], rstd[:, 0:1], "rstd")
            kb.dma("sp", sh[:], mods[t, 1, :].partition_broadcast(128), writes=["sh"])
            kb.dma("sp", sc[:], mods[t, 2, :].partition_broadcast(128), writes=["sc"])
            STT(kb, sc[:], sc[:], 1.0, g_bc[:], ADD, MUL, ["sc", "g_bc"], ["sc"])
            STT(kb, xt[:], x, rstd[:, 0:1], sc[:], MUL, MUL, [xk, "rstd", "sc"], ["xt"])
            TT(kb, "dve", xt[:], xt[:], sh[:], ADD, ["xt", "sh"], ["xt"])
            kb.dma("pool", h2o[rows, :], xt[:], reads=["xt"], is_output=True)
            transpose_rows(kb, idt, xt, "xt", h2T, "h2T", pst, pstk)
            for kc in range(16):
                kb.op("pe", lambda e, kc=kc: e.matmul(pr[:], lhsT=h2T[:, kc, :], rhs=wrs[:, kc, :], start=(kc == 0), stop=(kc == 15)),
                      reads=["h2T", "wrs"], writes=["pr"])
            kb.op("dve", lambda e: e.tensor_reduce(out=rstd[:, 1:2], in_=pr[:], axis=AX.X, op=ALU.max, negate=True), reads=["pr"], writes=["rmax"])
            ACT(kb, lg[:], pr[:], AF.Exp, ["pr", "rmax"], ["lg", "rsum"], bias=rstd[:, 1:2], scale=1.0, accum_out=rstd[:, 2:3])
            kb.op("dve", lambda e: e.reciprocal(out=rstd[:, 2:3], in_=rstd[:, 2:3]), reads=["rsum"], writes=["rsum"])
            TS(kb, "dve", lg[:], lg[:], rstd[:, 2:3], None, MUL, None, ["lg", "rsum"], ["lg"])
            kb.dma("pool", affo[rows, :], lg[:], reads=["lg"], is_output=True)
    return kb


CAP_L = 2 * SEQ // NE
CAP_C = 2 * CTX // NE


def build_experts(has_ctx):
    MUL, ADD, SUB = ALU.mult, ALU.add, ALU.subtract
    kb = KB()
    affT = kb.dram_in("affT", [4, SEQ])
    h2l = [kb.dram_in(f"h2l{b}", [SEQ, D]) for b in range(B)]
    pl_ = [kb.dram_out(f"part_l{b}", [SEQ, D]) for b in range(B)]
    if has_ctx:
        affTc = kb.dram_in("affTc", [4, CTX])
        h2c = [kb.dram_in(f"h2c{b}", [CTX, D]) for b in range(B)]
        pc_ = [kb.dram_out(f"part_c{b}", [CTX, D]) for b in range(B)]
    w1 = kb.dram_in("w1", [2, 16, 128, 16, 128])
    w3 = kb.dram_in("w3", [2, 16, 128, 16, 128])
    w2 = kb.dram_in("w2", [2, 8, 128, 16, 256])
    idt = ident(kb)
    NTOK = CAP_L + (CAP_C if has_ctx else 0)
    NTT = 4 + (1 if has_ctx else 0)
    xsT = kb.sb("xsT", [128, 16, NTOK])
    ysb = kb.sb("ysb", [128, NTT, D])
    XK, YK = "xsT", "ysb"
    hidT = kb.sb("hidT", [128, 16, NTOK])
    xgs = [kb.sb("xg0", [128, D])] * 2
    xg = xgs[0]
    sgm = kb.sb("sgm", [128, 512])
    kb.op("pool", lambda e: e.memset(xg[:], 0.0), writes=["xg0"])
    for b in range(B):
        for t in range(SEQ // 128):
            kb.dma("sp", pl_[b][t * 128:(t + 1) * 128, :], xg[:], reads=["xg0"], writes=[f"part_l{b}"], is_output=True)
        if has_ctx:
            for t in range(CTX // 128):
                kb.dma("sp", pc_[b][t * 128:(t + 1) * 128, :], xg[:], reads=["xg0"], writes=[f"part_c{b}"], is_output=True)
    pt = kb.ps("pt", [128, 64])

    def topk(src, n, cap, tag):
        wk = kb.sb(f"wk{tag}", [4, n])
        vals = kb.sb(f"vals{tag}", [4, cap])
        idxs = kb.sb(f"idxs{tag}", [4, cap], U32)
        idxf = kb.sb(f"idxf{tag}", [4, cap])
        kb.dma("sp", wk[:], src, writes=[f"wk{tag}"])
        for it in range(cap // 8):
            sl = slice(it * 8, (it + 1) * 8)
            kb.op("dve", lambda e, sl=sl: e.max(out=vals[:, sl], in_=wk[:]), reads=[f"wk{tag}"], writes=[f"vals{tag}"])
            kb.op("dve", lambda e, sl=sl: e.max_index(out=idxs[:, sl], in_max=vals[:, sl], in_values=wk[:]), reads=[f"wk{tag}", f"vals{tag}"], writes=[f"idxs{tag}"])
            kb.op("dve", lambda e, sl=sl: e.match_replace(out=wk[:], in_to_replace=vals[:, sl], in_values=wk[:], imm_value=-1.0),
                  reads=[f"vals{tag}"], writes=[f"wk{tag}"])
        CP(kb, "dve", idxf[:], idxs[:], [f"idxs{tag}"], [f"idxf{tag}"])
        nblk = (cap + 127) // 128
        pw = min(cap, 128)
        idxT = kb.sb(f"idxT{tag}", [128, nblk * 4], I32)
        gT = kb.sb(f"gT{tag}", [128, nblk * 4])
        for srcv, srck, dst, dstk in ((idxf, f"idxf{tag}", idxT, f"idxT{tag}"), (vals, f"vals{tag}", gT, f"gT{tag}")):
            for blk in range(nblk):
                kb.op("pe", lambda e, srcv=srcv, blk=blk: e.transpose(out=pt[0:pw, blk * 4:(blk + 1) * 4], in_=srcv[:, blk * 128:blk * 128 + pw], identity=idt[0:4, 0:4]),
                      reads=[srck, "ident"], writes=["pt"])
            CP(kb, "dve", dst[0:pw, :], pt[0:pw, 0:nblk * 4], ["pt"], [dstk])
        return idxT, gT

    idxT, gT = topk(affT, SEQ, CAP_L, "L")
    if has_ctx:
        idxTc, gTc = topk(affTc, CTX, CAP_C, "C")
    pst = [kb.ps(f"pst{i}", [128, 4, 128]) for i in range(2)]
    pstk = ["pst0", "pst1"]
    ph = [kb.ps(f"ph{i}", [128, 512]) for i in range(2)]
    phc = kb.ps("phc", [128, 2, 32])
    py = [kb.ps(f"py{i}", [128, 512]) for i in range(2)]
    w13 = [kb.sb(f"w13_{i}", [128, 2, 16, 128]) for i in range(2)]
    w2b = [kb.sb(f"w2b{i}", [128, 16, 256]) for i in range(2)]
    wc = 0
    w2c = 0
    yc = 0
    gcnt = 0
    for el in range(2):
        for b in range(B):
            r = el * 2 + b
            for blk in range(4):
                col = blk * 4 + r
                xg, xgk = xgs[gcnt % 2], "xg0"
                gcnt += 1
                kb.dma("pool", None, None, reads=["idxTL"], writes=[xgk],
                       fn=lambda e, col=col, xg=xg: e.indirect_dma_start(out=xg[:], out_offset=None, in_=h2l[b][:, :],
                                                                         in_offset=bass.IndirectOffsetOnAxis(ap=idxT[:, col:col + 1], axis=0)))
                transpose_rows(kb, idt, xg, xgk, xsT[:, :, blk * 128:(blk + 1) * 128], XK, pst, pstk)
            if has_ctx:
                xg, xgk = xgs[gcnt % 2], "xg0"
                gcnt += 1
                kb.dma("pool", None, None, reads=["idxTC"], writes=[xgk],
                       fn=lambda e, xg=xg: e.indirect_dma_start(out=xg[0:CAP_C, :], out_offset=None, in_=h2c[b][:, :],
                                                         in_offset=bass.IndirectOffsetOnAxis(ap=idxTc[0:CAP_C, r:r + 1], axis=0)))
                for c0 in range(0, 16, 4):
                    bank = (c0 // 4) % 2
                    for i in range(4):
                        kb.op("pe", lambda e, i=i, xg=xg: e.transpose(out=pst[bank][:, i, 0:CAP_C], in_=xg[0:CAP_C, (c0 + i) * 128:(c0 + i + 1) * 128], identity=idt[0:CAP_C, 0:CAP_C]),
                              reads=[xgk, "ident"], writes=[pstk[bank]])
                    CP(kb, "dve", xsT[:, c0:c0 + 4, CAP_L:NTOK], pst[bank][:, :, 0:CAP_C], [pstk[bank]], [XK])
            for fb in range(16):
                wt, wk_ = w13[wc % 2], f"w13_{wc % 2}"
                wc += 1
                kb.dma("sp", wt[:, 0], w1[el, fb], writes=[wk_ + "a"])
                kb.dma("sp", wt[:, 1], w3[el, fb], writes=[wk_ + "b"])
                for i in range(2):
                    for kc in range(16):
                        kb.op("pe", lambda e, i=i, kc=kc, wt=wt: e.matmul(ph[i][:], lhsT=wt[:, i, kc, :], rhs=xsT[:, kc, 0:CAP_L], start=(kc == 0), stop=(kc == 15)),
                              reads=[wk_ + "ab"[i], XK], writes=[f"ph{i}"])
                ACT(kb, sgm[:], ph[0][:], AF.Sigmoid, ["ph0"], ["sgm"])
                TT(kb, "dve", sgm[:], ph[0][:], sgm[:], MUL, ["ph0", "sgm"], ["sgm"])
                TT(kb, "dve", hidT[:, fb, 0:CAP_L], ph[1][:], sgm[:], MUL, ["ph1", "sgm"], ["hidT"])
                if has_ctx:
                    for i in range(2):
                        for kc in range(16):
                            kb.op("pe", lambda e, i=i, kc=kc, wt=wt: e.matmul(phc[:, i, :], lhsT=wt[:, i, kc, :], rhs=xsT[:, kc, CAP_L:NTOK], start=(kc == 0), stop=(kc == 15)),
                                  reads=[wk_ + "ab"[i], XK], writes=["phc"])
                    ACT(kb, sgm[:, 0:CAP_C], phc[:, 0, :], AF.Sigmoid, ["phc"], ["sgm"])
                    TT(kb, "dve", sgm[:, 0:CAP_C], phc[:, 0, :], sgm[:, 0:CAP_C], MUL, ["phc", "sgm"], ["sgm"])
                    TT(kb, "dve", hidT[:, fb, CAP_L:NTOK], phc[:, 1, :], sgm[:, 0:CAP_C], MUL, ["phc", "sgm"], ["hidT"])
            for db in range(D // 256):
                wt, wk_ = w2b[w2c % 2], f"w2b{w2c % 2}"
                w2c += 1
                kb.dma("sp", wt[:], w2[el, db], writes=[wk_])
                for tt in range(NTT):
                    np_ = 128 if tt < 4 else CAP_C
                    t0 = tt * 128
                    p, pk = py[yc % 2], f"py{yc % 2}"
                    yc += 1
                    for fc in range(16):
                        kb.op("pe", lambda e, fc=fc, p=p, wt=wt: e.matmul(p[0:np_, 0:256], lhsT=hidT[:, fc, t0:t0 + np_], rhs=wt[:, fc, :], start=(fc == 0), stop=(fc == 15)),
                              reads=["hidT", wk_], writes=[pk])
                    gsc = gT[:, tt * 4 + r:tt * 4 + r + 1] if tt < 4 else gTc[0:CAP_C, r:r + 1]
                    gk = "gTL" if tt < 4 else "gTC"
                    TS(kb, "dve", ysb[0:np_, tt, db * 256:(db + 1) * 256], p[0:np_, 0:256], gsc, None, MUL, None, [pk, gk], [YK])
            for tt in range(NTT):
                if tt < 4:
                    col = tt * 4 + r
                    kb.dma("pool", None, None, reads=[YK, "idxTL"], writes=[f"part_l{b}"], is_output=True,
                           fn=lambda e, col=col, tt=tt: e.indirect_dma_start(out=pl_[b][:, :], out_offset=bass.IndirectOffsetOnAxis(ap=idxT[:, col:col + 1], axis=0),
                                                                              in_=ysb[:, tt, :], in_offset=None, compute_op=ALU.add))
                else:
                    kb.dma("pool", None, None, reads=[YK, "idxTC"], writes=[f"part_c{b}"], is_output=True,
                           fn=lambda e, tt=tt: e.indirect_dma_start(out=pc_[b][:, :], out_offset=bass.IndirectOffsetOnAxis(ap=idxTc[0:CAP_C, r:r + 1], axis=0),
                                                                    in_=ysb[0:CAP_C, tt, :], in_offset=None, compute_op=ALU.add))
    return kb


def expert_inputs(aff, h2, wts, core, has_ctx):
    m = {}
    e0 = 2 * core
    rows = []
    rows_c = []
    for el in range(2):
        for b in range(B):
            rows.append(aff[b * SEQ:(b + 1) * SEQ, e0 + el])
            rows_c.append(aff[2 * SEQ + b * CTX:2 * SEQ + (b + 1) * CTX, e0 + el])
    m["affT"] = np.ascontiguousarray(np.stack(rows, 0))
    for b in range(B):
        m[f"h2l{b}"] = np.ascontiguousarray(h2[b * SEQ:(b + 1) * SEQ])
    if has_ctx:
        m["affTc"] = np.ascontiguousarray(np.stack(rows_c, 0))
        for b in range(B):
            m[f"h2c{b}"] = np.ascontiguousarray(h2[2 * SEQ + b * CTX:2 * SEQ + (b + 1) * CTX])
    for n, w in zip(("w1", "w3"), wts[:2]):
        m[n] = np.ascontiguousarray(w[e0:e0 + 2].reshape(2, 16, 128, 16, 128).transpose(0, 3, 2, 1, 4))
    m["w2"] = np.ascontiguousarray(wts[2][e0:e0 + 2].reshape(2, 16, 128, 8, 256).transpose(0, 3, 2, 1, 4))
    return m


def expert_parts(res, has_ctx):
    outs = []
    for r in res:
        a = [r["part_l0"], r["part_l1"]]
        if has_ctx:
            a += [r["part_c0"], r["part_c1"]]
        else:
            a += [np.zeros((CTX, D), np.float32)] * 2
        outs.append(np.concatenate(a, 0))
    return outs


def dplr_banks3(kb):
    a0 = [(kb.ps(f"bankA0_{i}", [128, 512]), f"bankA0_{i}") for i in range(2)]
    a1 = [(kb.ps(f"bankA1_{i}", [128, 512]), f"bankA1_{i}") for i in range(2)]
    bb = [(kb.ps(f"bankB_{i}", [128, 512]), f"bankB_{i}") for i in range(4)]
    return ({f"b{i}": a0[i % 2] for i in range(8)}, {f"b{i}": a1[i % 2] for i in range(8)}, {f"b{i}": bb[i % 4] for i in range(8)})


def build_mixers():
    kb = KB()
    c = dplr_consts(kb)
    bA0, bA1, bB = dplr_banks3(kb)
    sync = {}

    def sA0():
        emit_rwkv(kb, c, bA0, bA1, sync)

    def sA1():
        emit_rwkv_head1(kb, bA1, sync)

    def sB():
        emit_attn(kb, c, bB)
        emit_mlstm(kb, c, bB)
        emit_gdn(kb, c, bB)
    kb.run_streams([sA0, sA1, sB])
    return kb


def mixer_inputs(pl_all, p, core):
    b, j = core // 4, core % 4
    m = {}
    m.update(rwkv_inputs(pl_all, p, b, j))
    m.update(mlstm_inputs(pl_all, p, b, j))
    m.update(gdn_inputs(pl_all, p, b, j))
    m.update(attn_inputs(pl_all, p, b, j))
    return m


def mixer_outputs(res):
    mix = np.zeros((TOT, D), np.float32)
    for core in range(NCORES):
        b, j = core // 4, core % 4
        for gi, nm in enumerate(("rw_out", "ml_out", "gd_out", "at_out")):
            o = res[core][nm]
            cs = slice(gi * GW + j * 128, gi * GW + (j + 1) * 128)
            mix[2 * SEQ + b * CTX:2 * SEQ + (b + 1) * CTX, cs] = o[:CTX]
            mix[b * SEQ:(b + 1) * SEQ, cs] = o[CTX:]
    return mix


_PROGS = {}


def _prog(name, fn):
    if name not in _PROGS:
        _PROGS[name] = fn().finish()
    return _PROGS[name]


LAYER_PARAMS = ['rw_mu', 'rw_w0', 'rw_w_up', 'rw_a0', 'rw_a_up', 'rw_g_up', 'rw_k_k', 'rw_k_a', 'rw_r_k', 'rw_ln_w', 'rw_ln_b',
                'ml_ib', 'ml_fb', 'ml_norm_g', 'gd_conv', 'gd_a_log', 'gd_dt_bias', 'gd_norm_g', 'at_q_norm', 'at_k_norm']


def kernel(**inp):
    inp = {k: np.asarray(v, np.float32) for k, v in inp.items()}
    mod = run_mod(inp["c"], inp["c_ctx"], inp["ada_w"], inp["ada_b"])
    x = np.concatenate([inp["x"].reshape(-1, D), inp["ctx"].reshape(-1, D)], 0)
    parts = None
    m5 = None
    for l in range(DEPTH):
        p = {n: inp[n][l] for n in LAYER_PARAMS}
        xs = rows_to_cores(x)
        mr = mod_rows_for(mod[l], [0, 1])
        g1 = np.ascontiguousarray(inp["norm1_g"][l][None, :])
        w_in = np.ascontiguousarray(inp["w_in"][l])
        if parts is None:
            nc = _prog("rows0", lambda: build_rows(False, "inproj"))
            maps = [{"xin": xs[c], "g": g1, "modrows": mr[c], "w": w_in} for c in range(NCORES)]
        else:
            nc = _prog("rows1", lambda: build_rows(True, "inproj"))
            maps = [{"xin": xs[c], "g": g1, "modrows": mr[c], "w": w_in, "parts": parts[c], "m5": m5[c]} for c in range(NCORES)]
        res = run(nc, maps)
        pl_all = cores_to_rows([r["out"] for r in res])
        if parts is not None:
            x = cores_to_rows([r["xout"] for r in res])
            xs = rows_to_cores(x)
        nc = _prog("mixers", build_mixers)
        res = run(nc, [mixer_inputs(pl_all, p, c) for c in range(NCORES)])
        mix = mixer_outputs(res)
        del pl_all
        nc = _prog("outproj", build_outproj)
        ms = rows_to_cores(mix)
        mr2 = mod_rows_for(mod[l], [2, 3, 4])
        g2 = np.ascontiguousarray(inp["norm2_g"][l][None, :])
        w_out = np.ascontiguousarray(inp["w_out"][l])
        wr = np.ascontiguousarray(inp["w_router"][l])
        res = run(nc, [{"mix": ms[c], "xin": xs[c], "w": w_out, "mods": mr2[c], "g": g2, "wr": wr} for c in range(NCORES)])
        x = cores_to_rows([r["xmid"] for r in res])
        h2 = cores_to_rows([r["h2"] for r in res])
        aff = cores_to_rows([r["aff"] for r in res])
        has_ctx = l < DEPTH - 1
        nc = _prog("experts%d" % has_ctx, lambda: build_experts(has_ctx))
        wts = (inp["w_exp1"][l], inp["w_exp3"][l], inp["w_exp2"][l])
        res = run(nc, [expert_inputs(aff, h2, wts, c, has_ctx) for c in range(NCORES)])
        pfull = expert_parts(res, has_ctx)
        pc = [rows_to_cores(a) for a in pfull]
        parts = [np.ascontiguousarray(np.stack([pc[e][c] for e in range(NCORES)], 0)) for c in range(NCORES)]
        m5 = [np.ascontiguousarray(a[:, 0, :]) for a in mod_rows_for(mod[l], [5])]
        del pfull, pc
    nc = _prog("final", lambda: build_rows(True, "final"))
    xs = rows_to_cores(x)
    gf = np.ascontiguousarray(inp["final_g"][None, :])
    res = run(nc, [{"xin": xs[c], "g": gf, "parts": parts[c], "m5": m5[c]} for c in range(NCORES)])
    out = cores_to_rows([r["out"] for r in res])[:B * SEQ]
    return np.ascontiguousarray(out.reshape(B, SEQ, D).astype(np.float32))
```

```python
import math
from contextlib import ExitStack

import numpy as np
import concourse.bass as bass
import concourse.mybir as mybir
from concourse.bass_utils import run_bass_kernel_spmd

F32 = mybir.dt.float32
U32 = mybir.dt.uint32
I32 = mybir.dt.int32
AF = mybir.ActivationFunctionType
ALU = mybir.AluOpType
AX = mybir.AxisListType

NCORES = 8
D = 2048
B = 2
SEQ = 4096
CTX = 256
DEPTH = 2
GW = 512
RW_COLS = 3 * GW + 64 + 64 + 128
ML_COLS = 4 * GW + 16
GD_COLS = 4 * GW + 16
AT_COLS = 1024
N_IN = RW_COLS + ML_COLS + GD_COLS + AT_COLS
NE = 16
EPS = 1e-6


class KB:
    NDSEM = 8

    def __init__(self):
        self.nc = bass.Bass("TRN2", target_bir_lowering=False)
        self.es = ExitStack()
        nc = self.nc
        self.eng = {"pe": nc.tensor, "dve": nc.vector, "act": nc.scalar, "pool": nc.gpsimd, "sp": nc.sync}
        self.sem = {e: self.es.enter_context(nc.semaphore("s_" + e)) for e in self.eng}
        self.cnt = {e: 0 for e in self.eng}
        self.waited = {e: {} for e in self.eng}
        self.dsem = {}
        self.dcnt = {}
        self.dnext = {}
        for q in ("sp", "pool", "act"):
            self.dsem[q] = [self.es.enter_context(nc.semaphore(f"d_{q}{i}")) for i in range(self.NDSEM)]
            self.dcnt[q] = [0] * self.NDSEM
            self.dnext[q] = 0
        self.last_w = {}
        self.readers = {}
        self.excl = set()
        self.ninst = 0
        self.out_tokens = []

    def sb(self, name, shape, dt=F32):
        return self.es.enter_context(self.nc.sbuf_tensor(name, list(shape), dt))

    def ps(self, name, shape, dt=F32):
        self.excl.add(name)
        return self.es.enter_context(self.nc.psum_tensor(name, list(shape), dt))

    def dram_in(self, name, shape, dt=F32):
        return self.nc.dram_tensor(name, list(shape), dt, kind="ExternalInput").ap()

    def dram_out(self, name, shape, dt=F32):
        return self.nc.dram_tensor(name, list(shape), dt, kind="ExternalOutput").ap()

    def _wait(self, e, tok):
        if tok is None:
            return
        kind, key, val = tok
        w = self.waited[e]
        k = (kind, key if kind == "c" else id(key))
        if w.get(k, 0) >= val:
            return
        w[k] = val
        sem = self.sem[key] if kind == "c" else key
        self.eng[e].wait_ge(sem, val)

    def _deps(self, e, reads, writes):
        deps = []
        for k in reads:
            t = self.last_w.get(k)
            if t is not None:
                deps.append(t)
        for k in writes:
            t = self.last_w.get(k)
            if t is not None:
                deps.append(t)
            deps.extend(self.readers.get(k, ()))
        for t in deps:
            if e == "pe" and t[0] == "c" and t[1] == "pe":
                continue
            self._wait(e, t)

    def _record(self, tok, reads, writes):
        for k in reads:
            self.readers.setdefault(k, []).append(tok)
        for k in writes:
            self.last_w[k] = tok
            self.readers[k] = []

    def _x(self, reads, writes):
        ex = [k for k in reads if k in self.excl]
        if ex:
            reads = [k for k in reads if k not in self.excl]
            writes = list(writes) + ex
        return reads, writes

    def run_streams(self, fns):
        import threading
        n = len(fns)
        cv = threading.Condition()
        st = {"turn": 0, "alive": [True] * n, "err": None}
        self._stream = (cv, st, n)
        tl = threading.local()
        self._tl = tl

        def nxt(i):
            for k in range(1, n + 1):
                j = (i + k) % n
                if st["alive"][j]:
                    return j
            return i

        def worker(i):
            tl.sid = i
            try:
                with cv:
                    while st["turn"] != i:
                        cv.wait()
                fns[i]()
            except BaseException as ex:
                st["err"] = ex
            finally:
                with cv:
                    st["alive"][i] = False
                    st["turn"] = nxt(i)
                    cv.notify_all()
        ths = [threading.Thread(target=worker, args=(i,)) for i in range(n)]
        for t in ths:
            t.start()
        for t in ths:
            t.join()
        self._stream = None
        if st["err"] is not None:
            raise st["err"]

    def _yield_turn(self):
        if getattr(self, "_stream", None) is None:
            return
        cv, st, n = self._stream
        i = self._tl.sid
        with cv:
            j = i
            for k in range(1, n + 1):
                c = (i + k) % n
                if st["alive"][c]:
                    j = c
                    break
            if j != i:
                st["turn"] = j
                cv.notify_all()
                while st["turn"] != i:
                    cv.wait()

    def spin(self, cond):
        n = 0
        while not cond():
            self._yield_turn()
            n += 1
            assert n < 10_000_000, "stream handshake never satisfied"

    def op(self, e, fn, reads=(), writes=()):
        self._yield_turn()
        reads, writes = self._x(reads, writes)
        self._deps(e, reads, writes)
        inst = fn(self.eng[e])
        self.cnt[e] += 1
        inst.then_inc(self.sem[e], 1)
        tok = ("c", e, self.cnt[e])
        self._record(tok, reads, writes)
        self.ninst += 1
        return tok

    def dma(self, q, out, in_, reads=(), writes=(), is_output=False, fn=None):
        self._yield_turn()
        i = self.dnext[q]
        self.dnext[q] = (i + 1) % self.NDSEM
        sem = self.dsem[q][i]
        if self.dcnt[q][i] > 0:
            self._wait(q, ("d", sem, 16 * self.dcnt[q][i]))
        self._deps(q, reads, writes)
        if fn is None:
            inst = self.eng[q].dma_start(out=out, in_=in_)
        else:
            inst = fn(self.eng[q])
        self.dcnt[q][i] += 1
        inst.then_inc(sem, 16)
        tok = ("d", sem, 16 * self.dcnt[q][i])
        self._record(tok, reads, writes)
        if is_output:
            self.out_tokens.append(tok)
        self.ninst += 1
        return tok

    def finish(self):
        for q in self.dsem:
            for i, sem in enumerate(self.dsem[q]):
                if self.dcnt[q][i] > 0:
                    self._wait("sp", ("d", sem, 16 * self.dcnt[q][i]))
        for e in self.eng:
            if e != "sp" and self.cnt[e] > 0:
                self._wait("sp", ("c", e, self.cnt[e]))
        self.es.close()
        return self.nc


def run(kb_or_nc, in_maps):
    nc = kb_or_nc.finish() if isinstance(kb_or_nc, KB) else kb_or_nc
    res = run_bass_kernel_spmd(nc, in_maps, core_ids=list(range(NCORES)))
    return res.results


def ident(kb, name="ident"):
    t = kb.sb(name, [128, 128])
    kb.op("pool", lambda e: e.memset(t[:], 1.0), writes=[name])
    kb.op("pool", lambda e: e.affine_select(out=t[:], in_=t[:], pattern=[[1, 128]], compare_op=ALU.is_equal,
                                             fill=0.0, base=0, channel_multiplier=-1), reads=[name], writes=[name])
    return t


def tri_mask(kb, name, mode):
    t = kb.sb(name, [128, 128])
    kb.op("pool", lambda e: e.memset(t[:], 1.0), writes=[name])
    if mode == "ones":
        return t
    if mode == "le":
        pat, cm, base, cmp = [[1, 128]], -1, 0, ALU.is_ge
    elif mode == "lt":
        pat, cm, base, cmp = [[1, 128]], -1, 0, ALU.is_gt
    elif mode == "ge":
        pat, cm, base, cmp = [[-1, 128]], 1, 0, ALU.is_ge
    else:
        pat, cm, base, cmp = [[-1, 128]], 1, 0, ALU.is_gt
    kb.op("pool", lambda e: e.affine_select(out=t[:], in_=t[:], pattern=pat, compare_op=cmp, fill=0.0,
                                             base=base, channel_multiplier=cm), reads=[name], writes=[name])
    return t


def build_mod():
    kb = KB()
    NCOL = 3072
    condT = kb.dram_in("condT", [128, 16, 3])
    w = kb.dram_in("w", [D, NCOL])
    bias = kb.dram_in("bias", [3, NCOL])
    out = kb.dram_out("out", [3, NCOL])
    ct = kb.sb("ct", [128, 16, 3])
    sg = kb.sb("sg", [128, 16, 3])
    bt = kb.sb("bt", [3, NCOL])
    ot = kb.sb("ot", [3, NCOL])
    kb.dma("sp", ct[:], condT, writes=["ct"])
    kb.dma("sp", bt[:], bias, writes=["bt"])
    kb.op("act", lambda e: e.activation(out=sg[:], in_=ct[:], func=AF.Sigmoid), reads=["ct"], writes=["sg"])
    kb.op("dve", lambda e: e.tensor_tensor(out=ct[:], in0=ct[:], in1=sg[:], op=ALU.mult), reads=["ct", "sg"], writes=["ct"])
    wv = w.rearrange("(kc p) n -> p kc n", p=128)
    wb = [kb.sb(f"wb{i}", [128, 16, 512]) for i in range(2)]
    pp = [kb.ps(f"pp{i}", [3, 512]) for i in range(2)]
    for nb in range(NCOL // 512):
        wt = wb[nb % 2]
        wk = f"wb{nb % 2}"
        for h in range(2):
            kb.dma("sp", wt[:, h * 8:(h + 1) * 8, :], wv[:, h * 8:(h + 1) * 8, nb * 512:(nb + 1) * 512], writes=[wk + f"h{h}"])
        p = pp[nb % 2]
        pk = f"pp{nb % 2}"
        for kc in range(16):
            kb.op("pe", lambda e, kc=kc: e.matmul(p[:], lhsT=ct[:, kc, :], rhs=wt[:, kc, :], start=(kc == 0), stop=(kc == 15)),
                  reads=["ct", wk + f"h{kc // 8}"], writes=[pk])
        kb.op("dve", lambda e: e.tensor_tensor(out=ot[:, nb * 512:(nb + 1) * 512], in0=p[:], in1=bt[:, nb * 512:(nb + 1) * 512], op=ALU.add),
              reads=[pk, "bt"], writes=["ot"])
    kb.dma("sp", out, ot[:], reads=["ot"], is_output=True)
    return kb


def run_mod(c, c_ctx, ada_w, ada_b):
    cond = np.concatenate([c, c_ctx[None, :]], axis=0).astype(np.float32)
    condT = np.ascontiguousarray(cond.reshape(3, 16, 128).transpose(2, 1, 0))
    maps = []
    for core in range(NCORES):
        l, q = divmod(core, 4)
        sl = slice(q * 3072, (q + 1) * 3072)
        maps.append({"condT": condT, "w": np.ascontiguousarray(ada_w[l][:, sl]),
                     "bias": np.ascontiguousarray(np.broadcast_to(ada_b[l][sl], (3, 3072)))})
    res = run(build_mod(), maps)
    mod = np.zeros((DEPTH, 3, 6 * D), np.float32)
    for core in range(NCORES):
        l, q = divmod(core, 4)
        mod[l][:, q * 3072:(q + 1) * 3072] = res[core]["out"]
    return mod


NT = 9
ROWS = NT * 128


def rms_rstd(kb, x, xk, junk, rstd, key, n=D, eps=EPS):
    kb.op("act", lambda e: e.activation(out=junk, in_=x, func=AF.Square, accum_out=rstd), reads=[xk], writes=["junk", key])
    kb.op("act", lambda e: e.activation(out=rstd, in_=rstd, func=AF.Sqrt, scale=1.0 / n, bias=eps), reads=[key], writes=[key])
    kb.op("dve", lambda e: e.reciprocal(out=rstd, in_=rstd), reads=[key], writes=[key])


def transpose_rows(kb, idt, src, srck, dstT, dstk, pst, pstk, nchunks=16, evac="dve"):
    for c0 in range(0, nchunks, 4):
        n = min(4, nchunks - c0)
        bank = (c0 // 4) % len(pst)
        for i in range(n):
            kb.op("pe", lambda e, i=i: e.transpose(out=pst[bank][:, i, :], in_=src[:, (c0 + i) * 128:(c0 + i + 1) * 128], identity=idt[:]),
                  reads=[srck, "ident"], writes=[pstk[bank]])
        kb.op(evac, lambda e: e.tensor_copy(out=dstT[:, c0:c0 + n, :], in_=pst[bank][:, 0:n, :]), reads=[pstk[bank]], writes=[dstk])


def build_rows(do_combine, mode, NOUT=N_IN):
    kb = KB()
    xin = kb.dram_in("xin", [ROWS, D])
    gvec = kb.dram_in("g", [1, D])
    if do_combine:
        parts = kb.dram_in("parts", [NCORES, ROWS, D])
        m5 = kb.dram_in("m5", [NT, D])
        xout = kb.dram_out("xout", [ROWS, D])
    if mode == "inproj":
        modrows = kb.dram_in("modrows", [NT, 2, D])
        w = kb.dram_in("w", [D, NOUT])
        out = kb.dram_out("out", [ROWS, NOUT])
    else:
        out = kb.dram_out("out", [ROWS, D])

    idt = ident(kb)
    g_bc = kb.sb("g_bc", [128, D])
    kb.dma("sp", g_bc[:], gvec[0, :].partition_broadcast(128), writes=["g_bc"])
    xt = [kb.sb(f"xt{i}", [128, D]) for i in range(2)]
    junk = kb.sb("junk", [128, D])
    rstd = kb.sb("rstd", [128, 2])
    if mode == "inproj":
        sc = kb.sb("sc", [128, D])
        sh = kb.sb("sh", [128, D])
        hT = kb.sb("hT", [128, NT, 16, 128])
        pst = [kb.ps(f"pst{i}", [128, 4, 128]) for i in range(2)]
        pstk = ["pst0", "pst1"]
    if do_combine:
        pt = [kb.sb(f"pt{i}", [128, D]) for i in range(2)]
        m5b = kb.sb("m5b", [128, D])

    for t in range(NT):
        x = xt[t % 2]
        xk = f"xt{t % 2}"
        rows = slice(t * 128, (t + 1) * 128)
        kb.dma("sp", x[:], xin[rows, :], writes=[xk])
        if do_combine:
            acc = junk
            for c in range(NCORES):
                p = pt[c % 2]
                pk = f"pt{c % 2}"
                kb.dma("sp", p[:], parts[c, rows, :], writes=[pk])
                if c == 0:
                    kb.op("pool", lambda e, p=p: e.tensor_copy(out=acc[:], in_=p[:]), reads=[pk], writes=["junk"])
                else:
                    kb.op("pool", lambda e, p=p: e.tensor_tensor(out=acc[:], in0=acc[:], in1=p[:], op=ALU.add), reads=[pk, "junk"], writes=["junk"])
            kb.dma("sp", m5b[:], m5[t, :].partition_broadcast(128), writes=["m5b"])
            kb.op("dve", lambda e: e.tensor_tensor(out=acc[:], in0=acc[:], in1=m5b[:], op=ALU.mult), reads=["junk", "m5b"], writes=["junk"])
            kb.op("dve", lambda e, x=x: e.tensor_tensor(out=x[:], in0=x[:], in1=acc[:], op=ALU.add), reads=["junk", xk], writes=[xk])
            kb.dma("pool", xout[rows, :], x[:], reads=[xk], is_output=True)
        rk = "rstd"
        rms_rstd(kb, x[:], xk, junk[:], rstd[:, 0:1], rk)
        if mode == "inproj":
            kb.dma("sp", sh[:], modrows[t, 0, :].partition_broadcast(128), writes=["sh"])
            kb.dma("sp", sc[:], modrows[t, 1, :].partition_broadcast(128), writes=["sc"])
            kb.op("dve", lambda e: e.scalar_tensor_tensor(out=sc[:], in0=sc[:], scalar=1.0, in1=g_bc[:], op0=ALU.add, op1=ALU.mult),
                  reads=["sc", "g_bc"], writes=["sc"])
            kb.op("dve", lambda e, x=x: e.scalar_tensor_tensor(out=x[:], in0=x[:], scalar=rstd[:, 0:1], in1=sc[:], op0=ALU.mult, op1=ALU.mult),
                  reads=[xk, rk, "sc"], writes=[xk])
            kb.op("dve", lambda e, x=x: e.tensor_tensor(out=x[:], in0=x[:], in1=sh[:], op=ALU.add), reads=[xk, "sh"], writes=[xk])
            transpose_rows(kb, idt, x, xk, hT[:, t], f"hT{t}", pst, pstk)
        else:
            kb.op("dve", lambda e, x=x: e.scalar_tensor_tensor(out=x[:], in0=x[:], scalar=rstd[:, 0:1], in1=g_bc[:], op0=ALU.mult, op1=ALU.mult),
                  reads=[xk, rk, "g_bc"], writes=[xk])
            kb.dma("pool", out[rows, :], x[:], reads=[xk], is_output=True)

    if mode == "inproj":
        NB = 256
        wv = w.rearrange("(kc p) n -> p kc n", p=128)
        wb = [kb.sb(f"wb{i}", [128, 16, NB]) for i in range(2)]
        pp = [kb.ps(f"pp{i}", [128, NB]) for i in range(4)]
        ot = [kb.sb(f"ot{i}", [128, NB]) for i in range(4)]
        nblocks = (NOUT + NB - 1) // NB
        cnt = 0
        for nb in range(nblocks):
            n0 = nb * NB
            nw = min(NB, NOUT - n0)
            wt = wb[nb % 2]
            wk = f"wb{nb % 2}"
            kb.dma("sp", wt[:, :, 0:nw], wv[:, :, n0:n0 + nw], writes=[wk])
            for t in range(NT):
                p = pp[cnt % 4]
                pk = f"pp{cnt % 4}"
                o = ot[cnt % 4]
                ok = f"ot{cnt % 4}"
                cnt += 1
                for kc in range(16):
                    kb.op("pe", lambda e, kc=kc, p=p, wt=wt: e.matmul(p[:, 0:nw], lhsT=hT[:, t, kc, :], rhs=wt[:, kc, 0:nw], start=(kc == 0), stop=(kc == 15)),
                          reads=[f"hT{t}", wk], writes=[pk])
                ev = "act" if cnt % 2 else "dve"
                if ev == "act":
                    kb.op("act", lambda e, p=p, o=o: e.copy(out=o[:, 0:nw], in_=p[:, 0:nw]), reads=[pk], writes=[ok])
                else:
                    kb.op("dve", lambda e, p=p, o=o: e.tensor_copy(out=o[:, 0:nw], in_=p[:, 0:nw]), reads=[pk], writes=[ok])
                kb.dma("pool", out[t * 128:(t + 1) * 128, n0:n0 + nw], o[:, 0:nw], reads=[ok], is_output=True)
    return kb


TOT = B * SEQ + B * CTX


def rows_to_cores(a):
    n = a.shape[1]
    pad = np.zeros((NCORES * ROWS, n), a.dtype)
    pad[:TOT] = a
    return [np.ascontiguousarray(pad[c * ROWS:(c + 1) * ROWS]) for c in range(NCORES)]


def cores_to_rows(lst):
    return np.concatenate(lst, axis=0)[:TOT]


def tile_group(g):
    r = g * 128
    if r < SEQ:
        return 0
    if r < 2 * SEQ:
        return 1
    return 2


def mod_rows_for(modl, idxs):
    outs = []
    for c in range(NCORES):
        a = np.zeros((NT, len(idxs), D), np.float32)
        for t in range(NT):
            g = c * NT + t
            if g * 128 < TOT:
                j = tile_group(g)
                for ii, m in enumerate(idxs):
                    a[t, ii] = modl[j, m * D:(m + 1) * D]
        outs.append(a)
    return outs


class Dplr:
    def __init__(self, kb, dk, dvp, mode, has_delta, tag, consts):
        self.kb, self.dk, self.dvp, self.mode, self.hd, self.tag = kb, dk, dvp, mode, has_delta, tag
        self.c = consts
        t = tag
        sb = lambda n, s: kb.sb(f"{t}_{n}", s)
        self.r, self.kap, self.a, self.kt = sb("r", [128, dk]), sb("kap", [128, dk]), sb("a", [128, dk]), sb("kt", [128, dk])
        self.v, self.lw = sb("v", [128, dvp]), sb("lw", [128, dk])
        self.M = sb("M", [dk, dvp])
        self.ecw, self.encw, self.ecwx, self.el = sb("ecw", [128, dk]), sb("encw", [128, dk]), sb("ecwx", [128, dk]), sb("el", [128, dk])
        self.r0, self.k0 = sb("r0", [128, dk]), sb("k0", [128, dk])
        self.ap, self.ktp = sb("ap", [128, dk]), sb("ktp", [128, dk])
        self.aL, self.ktL = sb("aL", [128, dk]), sb("ktL", [128, dk])
        self.kr0T = sb("kr0T", [dk, 2, 128])
        self.krT = sb("krT", [dk, 2, 128])
        self.apT, self.ktpT = sb("apT", [dk, 128]), sb("ktpT", [dk, 128])
        self.AR, self.BR = sb("AR", [128, 256]), sb("BR", [128, 256])
        self.E2 = sb("E2", [128, 256])
        self.Mm = [sb(f"Mm{i}", [128, 128]) for i in range(2)]
        self.Nm = [sb(f"Nm{i}", [128, 128]) for i in range(2)]
        self.P = sb("P", [128, 128])
        self.negG, self.U = sb("negG", [128, dvp]), sb("U", [128, dvp])
        self.ecl = sb("ecl", [dk, 1])
        self.cwc = sb("cwc", [128, 1])

    def k(self, n):
        return f"{self.tag}_{n}"

    def init_state(self):
        self.kb.op("pool", lambda e: e.memset(self.M[:], 0.0), writes=[self.k("M")])

    def step(self, d, bank, y_cb, stop=99):
        kb, dk, dvp, k, c = self.kb, self.dk, self.dvp, self.k, self.c
        TI, TS = (c["le"], c["lt"]) if d == "f" else (c["ge"], c["gt"])
        TIk, TSk = ("m_le", "m_lt") if d == "f" else ("m_ge", "m_gt")
        mask2, mask2k = (c["mask2f"], "mask2f") if d == "f" else (c["mask2b"], "mask2b")
        tri2, tri2k = (c["tri2f"], "tri2f") if d == "f" else (c["tri2b"], "tri2b")
        b0, b0k = bank["b0"]
        b1, b1k = bank["b1"]
        b2, b2k = bank["b2"]
        b3, b3k = bank["b3"]
        b4, b4k = bank["b4"]
        b5, b5k = bank["b5"]
        b6, b6k = bank["b6"]
        b7, b7k = bank["b7"]
        MUL, ADD, SUB = ALU.mult, ALU.add, ALU.subtract
        kb.op("pe", lambda e: e.matmul(b0[:, 0:dk], lhsT=TI[:], rhs=self.lw[:], start=True, stop=True), reads=[TIk, k("lw")], writes=[b0k])
        kb.op("pe", lambda e: e.matmul(b0[:, dk:2 * dk], lhsT=c["ones"][:], rhs=self.lw[:], start=True, stop=True), reads=["m_ones", k("lw")], writes=[b0k])
        kb.op("pe", lambda e: e.matmul(b0[0:dk, 2 * dk:2 * dk + 1], lhsT=self.lw[:], rhs=c["ones"][:, 0:1], start=True, stop=True), reads=["m_ones", k("lw")], writes=[b0k])
        cw, cwl = b0[:, 0:dk], b0[:, dk:2 * dk]
        kb.op("act", lambda e: e.activation(out=self.ecw[:], in_=cw, func=AF.Exp), reads=[b0k], writes=[k("ecw")])
        kb.op("act", lambda e: e.activation(out=self.ecl[:], in_=b0[0:dk, 2 * dk:2 * dk + 1], func=AF.Exp), reads=[b0k], writes=[k("ecl")])
        kb.op("dve", lambda e: e.tensor_tensor(out=self.ecwx[:], in0=cw, in1=self.lw[:], op=SUB), reads=[b0k, k("lw")], writes=[k("ecwx")])
        kb.op("act", lambda e: e.activation(out=self.ecwx[:], in_=self.ecwx[:], func=AF.Exp), reads=[k("ecwx")], writes=[k("ecwx")])
        kb.op("dve", lambda e: e.tensor_copy(out=self.el[:], in_=cw), reads=[b0k], writes=[k("el")])
        kb.op("dve", lambda e: e.tensor_tensor(out=self.el[:], in0=cwl, in1=self.el[:], op=SUB), reads=[b0k, k("el")], writes=[k("el")])
        kb.op("act", lambda e: e.activation(out=self.el[:], in_=self.el[:], func=AF.Exp), reads=[k("el")], writes=[k("el")])
        kb.op("dve", lambda e: e.tensor_tensor(out=self.r0[:], in0=self.r[:], in1=self.ecw[:], op=MUL), reads=[k("r"), k("ecw")], writes=[k("r0")])
        kb.op("dve", lambda e: e.tensor_tensor(out=self.k0[:], in0=self.kap[:], in1=self.ecwx[:], op=MUL), reads=[k("kap"), k("ecwx")], writes=[k("k0")])
        kb.op("pool", lambda e: e.tensor_tensor(out=self.ktL[:], in0=self.kt[:], in1=self.el[:], op=MUL), reads=[k("kt"), k("el")], writes=[k("ktL")])
        if self.hd:
            kb.op("pool", lambda e: e.tensor_tensor(out=self.aL[:], in0=self.a[:], in1=self.el[:], op=MUL), reads=[k("a"), k("el")], writes=[k("aL")])
        if self.mode == "V":
            kb.op("act", lambda e: e.activation(out=self.encw[:], in_=cw, func=AF.Exp, scale=-1.0), reads=[b0k], writes=[k("encw")])
            kb.op("dve", lambda e: e.tensor_tensor(out=self.ktp[:], in0=self.kt[:], in1=self.encw[:], op=MUL), reads=[k("kt"), k("encw")], writes=[k("ktp")])
            if self.hd:
                kb.op("dve", lambda e: e.tensor_tensor(out=self.ap[:], in0=self.a[:], in1=self.encw[:], op=MUL), reads=[k("a"), k("encw")], writes=[k("ap")])
            ktp_src, ktp_k, ap_src, ap_k = self.ktp, k("ktp"), self.ap, k("ap")
        else:
            kb.op("dve", lambda e: e.tensor_copy(out=self.cwc[:], in_=b0[:, 0:1]), reads=[b0k], writes=[k("cwc")])
            ktp_src, ktp_k, ap_src, ap_k = self.kt, k("kt"), self.a, k("a")
        if stop < 3:
            return
        idt = c["ident"]

        def tr(slot, src, srck):
            kb.op("pe", lambda e: e.transpose(out=b1[0:dk, slot * 128:(slot + 1) * 128], in_=src[:], identity=idt[:]), reads=[srck, "ident"], writes=[b1k])
        tr(0, self.k0, k("k0"))
        tr(1, self.r0, k("r0"))
        tr(2, ktp_src, ktp_k)
        if self.hd:
            tr(3, ap_src, ap_k)
        kb.op("dve", lambda e: e.tensor_copy(out=self.kr0T[:].rearrange("p a b -> p (a b)"), in_=b1[0:dk, 0:256]), reads=[b1k], writes=[k("kr0T")])
        kb.op("dve", lambda e: e.tensor_copy(out=self.ktpT[:], in_=b1[0:dk, 256:384]), reads=[b1k], writes=[k("ktpT")])
        if self.hd:
            kb.op("dve", lambda e: e.tensor_copy(out=self.apT[:], in_=b1[0:dk, 384:512]), reads=[b1k], writes=[k("apT")])
        if self.mode == "S":
            tr(0, self.kap, k("kap"))
            tr(1, self.r, k("r"))
            kb.op("dve", lambda e: e.tensor_copy(out=self.krT[:].rearrange("p a b -> p (a b)"), in_=b1[0:dk, 0:256]), reads=[b1k], writes=[k("krT")])
            rhs2, rhs2k = self.krT, k("krT")
        else:
            rhs2, rhs2k = self.kr0T, k("kr0T")
        rhs2f = rhs2[:].rearrange("p a b -> p (a b)")
        if stop < 4:
            return
        if self.hd:
            kb.op("pe", lambda e: e.matmul(b2[:, 0:256], lhsT=self.apT[:], rhs=rhs2f, start=True, stop=True), reads=[k("apT"), rhs2k], writes=[b2k])
        kb.op("pe", lambda e: e.matmul(b2[:, 256:512], lhsT=self.ktpT[:], rhs=rhs2f, start=True, stop=True), reads=[k("ktpT"), rhs2k], writes=[b2k])
        if self.mode == "S":
            kb.op("pe", lambda e: e.matmul(b3[:, 0:256], lhsT=self.lw[:], rhs=tri2[:], start=True, stop=True), reads=[k("lw"), tri2k], writes=[b3k])
            kb.op("dve", lambda e: e.tensor_scalar(out=self.E2[:], in0=b3[:, 0:256], scalar1=self.cwc[:, 0:1], scalar2=0.0, op0=SUB, op1=ALU.min),
                  reads=[b3k, k("cwc")], writes=[k("E2")])
            kb.op("act", lambda e: e.activation(out=self.E2[:], in_=self.E2[:], func=AF.Exp), reads=[k("E2")], writes=[k("E2")])
            kb.op("pool", lambda e: e.tensor_tensor(out=self.E2[:], in0=self.E2[:], in1=mask2[:], op=MUL), reads=[k("E2"), mask2k], writes=[k("E2")])
            mm2, mm2k = self.E2, k("E2")
        else:
            mm2, mm2k = mask2, mask2k
        if self.hd:
            kb.op("dve", lambda e: e.tensor_tensor(out=self.AR[:], in0=b2[:, 0:256], in1=mm2[:], op=MUL), reads=[b2k, mm2k], writes=[k("AR")])
        kb.op("dve", lambda e: e.tensor_tensor(out=self.BR[:], in0=b2[:, 256:512], in1=mm2[:], op=MUL), reads=[b2k, mm2k], writes=[k("BR")])
        Mk = k("M")
        if stop < 5:
            return
        if self.hd:
            M0, N0 = self.Mm[0], self.Nm[0]
            kb.op("dve", lambda e: e.tensor_scalar(out=M0[:], in0=self.AR[:, 0:128], scalar1=-1.0, scalar2=None, op0=MUL), reads=[k("AR")], writes=[k("Mm0")])
            kb.op("pe", lambda e: e.transpose(out=b4[:, 0:128], in_=M0[:], identity=idt[:]), reads=[k("Mm0"), "ident"], writes=[b4k])
            kb.op("act", lambda e: e.copy(out=N0[:], in_=b4[:, 0:128]), reads=[b4k], writes=[k("Nm0")])
            kb.op("pool", lambda e: e.tensor_tensor(out=self.P[:], in0=M0[:], in1=idt[:], op=ADD), reads=[k("Mm0"), "ident"], writes=[k("P")])
            cur = 0
            for lvl in range(6):
                Mc, Nc, Mn, Nn = self.Mm[cur], self.Nm[cur], self.Mm[1 - cur], self.Nm[1 - cur]
                Mck, Nck, Mnk, Nnk = k(f"Mm{cur}"), k(f"Nm{cur}"), k(f"Mm{1 - cur}"), k(f"Nm{1 - cur}")
                last = lvl == 5
                kb.op("pe", lambda e, Mc=Mc, Nc=Nc: e.matmul(b4[:, 0:128], lhsT=Nc[:], rhs=Mc[:], start=True, stop=True), reads=[Mck, Nck], writes=[b4k])
                kb.op("pe", lambda e, Mc=Mc, Nc=Nc: e.matmul(b4[:, 128:256], lhsT=Mc[:], rhs=Nc[:], start=True, stop=True), reads=[Mck, Nck], writes=[b4k])
                if lvl > 0:
                    kb.op("pe", lambda e, Nc=Nc: e.matmul(b4[:, 256:384], lhsT=Nc[:], rhs=self.P[:], start=True, stop=True), reads=[Nck, k("P")], writes=[b4k])
                kb.op("dve", lambda e, Mn=Mn: e.tensor_copy(out=Mn[:], in_=b4[:, 0:128]), reads=[b4k], writes=[Mnk])
                kb.op("act", lambda e, Nn=Nn: e.copy(out=Nn[:], in_=b4[:, 128:256]), reads=[b4k], writes=[Nnk])
                if lvl > 0:
                    kb.op("dve", lambda e: e.tensor_tensor(out=self.P[:], in0=b4[:, 256:384], in1=self.P[:], op=ADD), reads=[b4k, k("P")], writes=[k("P")])
                cur = 1 - cur
            Nc, Nck = self.Nm[cur], k(f"Nm{cur}")
            kb.op("pe", lambda e, Nc=Nc: e.matmul(b4[:, 256:384], lhsT=Nc[:], rhs=self.P[:], start=True, stop=True), reads=[Nck, k("P")], writes=[b4k])
            kb.op("dve", lambda e: e.tensor_tensor(out=self.P[:], in0=b4[:, 256:384], in1=self.P[:], op=ADD), reads=[b4k, k("P")], writes=[k("P")])
            kb.op("pe", lambda e: e.matmul(b5[:, 0:dvp], lhsT=self.kr0T[:, 0, :], rhs=self.M[:], start=True, stop=False), reads=[k("kr0T"), Mk], writes=[b5k])
            kb.op("pe", lambda e: e.matmul(b5[:, 0:dvp], lhsT=self.BR[:, 0:128], rhs=self.v[:], start=False, stop=True), reads=[k("BR"), k("v")], writes=[b5k])
            kb.op("dve", lambda e: e.tensor_scalar(out=self.negG[:], in0=b5[:, 0:dvp], scalar1=-1.0, scalar2=None, op0=MUL), reads=[b5k], writes=[k("negG")])
            kb.op("pe", lambda e: e.matmul(b5[:, 256:256 + dvp], lhsT=self.P[:], rhs=self.negG[:], start=True, stop=True), reads=[k("P"), k("negG")], writes=[b5k])
            kb.op("act", lambda e: e.copy(out=self.U[:], in_=b5[:, 256:256 + dvp]), reads=[b5k], writes=[k("U")])
        if stop < 7:
            return
        kb.op("pe", lambda e: e.matmul(b6[:, 0:dvp], lhsT=self.kr0T[:, 1, :], rhs=self.M[:], start=True, stop=False), reads=[k("kr0T"), Mk], writes=[b6k])
        if self.hd:
            kb.op("pe", lambda e: e.matmul(b6[:, 0:dvp], lhsT=self.AR[:, 128:256], rhs=self.U[:], start=False, stop=False), reads=[k("AR"), k("U")], writes=[b6k])
        kb.op("pe", lambda e: e.matmul(b6[:, 0:dvp], lhsT=self.BR[:, 128:256], rhs=self.v[:], start=False, stop=True), reads=[k("BR"), k("v")], writes=[b6k])
        y_cb(b6[:, 0:dvp], b6k)
        if stop < 8:
            return
        if self.hd:
            kb.op("pe", lambda e: e.matmul(b7[0:dk, 0:dvp], lhsT=self.aL[:], rhs=self.U[:], start=True, stop=False), reads=[k("aL"), k("U")], writes=[b7k])
        kb.op("pe", lambda e: e.matmul(b7[0:dk, 0:dvp], lhsT=self.ktL[:], rhs=self.v[:], start=(not self.hd), stop=True), reads=[k("ktL"), k("v")], writes=[b7k])
        kb.op("dve", lambda e: e.scalar_tensor_tensor(out=self.M[:], in0=self.M[:], scalar=self.ecl[:, 0:1], in1=b7[0:dk, 0:dvp], op0=MUL, op1=ADD),
              reads=[Mk, k("ecl"), b7k], writes=[Mk])


def dplr_consts(kb):
    c = {"ident": ident(kb)}
    for m in ("le", "lt", "ge", "gt", "ones"):
        c[m] = tri_mask(kb, "m_" + m, m)
    for d, (ms, mi) in (("f", ("lt", "le")), ("b", ("gt", "ge"))):
        t = kb.sb("mask2" + d, [128, 256])
        kb.op("pool", lambda e, t=t, ms=ms: e.tensor_copy(out=t[:, 0:128], in_=c[ms][:]), reads=["m_" + ms], writes=["mask2" + d])
        kb.op("pool", lambda e, t=t, mi=mi: e.tensor_copy(out=t[:, 128:256], in_=c[mi][:]), reads=["m_" + mi], writes=["mask2" + d])
        c["mask2" + d] = t
        c["tri2" + d] = t
    return c


def dplr_banks(kb):
    return {f"b{i}": (kb.ps(f"bank{i}", [128, 512]), f"bank{i}") for i in range(8)}


def dplr_banks2(kb):
    sets = []
    for s_ in range(2):
        t = [(kb.ps(f"bank{s_}_{i}", [128, 512]), f"bank{s_}_{i}") for i in range(4)]
        sets.append({f"b{i}": t[i % 4] for i in range(8)})
    return sets


def build_dplr_test(T, dk, dvp, mode, has_delta):
    kb = KB()
    nch = T // 128
    ins = {n: kb.dram_in(n, [T, dk]) for n in ("r", "kap", "a", "kt", "lw")}
    ins["v"] = kb.dram_in("v", [T, dvp])
    outs = {d: kb.dram_out("y" + d, [T, dvp]) for d in "fb"}
    c = dplr_consts(kb)
    banks = dplr_banks(kb)
    sc = Dplr(kb, dk, dvp, mode, has_delta, "s", c)
    yo = kb.sb("yo", [128, dvp])
    for d in "fb":
        sc.init_state()
        order = range(nch) if d == "f" else range(nch - 1, -1, -1)
        for ci in order:
            rows = slice(ci * 128, (ci + 1) * 128)
            for n in ("r", "kap", "a", "kt", "lw", "v"):
                kb.dma("sp", getattr(sc, n)[:], ins[n][rows, :], writes=[sc.k(n)])

            def cb(yp, ypk):
                kb.op("dve", lambda e: e.tensor_copy(out=yo[:], in_=yp), reads=[ypk], writes=["yo"])
                kb.dma("pool", outs[d][rows, :], yo[:], reads=["yo"], is_output=True)
            sc.step(d, banks, cb)
    return kb


def TT(kb, e, out, in0, in1, op, reads, writes):
    return kb.op(e, lambda g: g.tensor_tensor(out=out, in0=in0, in1=in1, op=op), reads=reads, writes=writes)


def TS(kb, e, out, in0, s1, s2, op0, op1, reads, writes):
    if op1 is None:
        return kb.op(e, lambda g: g.tensor_scalar(out=out, in0=in0, scalar1=s1, scalar2=None, op0=op0), reads=reads, writes=writes)
    return kb.op(e, lambda g: g.tensor_scalar(out=out, in0=in0, scalar1=s1, scalar2=s2, op0=op0, op1=op1), reads=reads, writes=writes)


def STT(kb, out, in0, scalar, in1, op0, op1, reads, writes):
    return kb.op("dve", lambda g: g.scalar_tensor_tensor(out=out, in0=in0, scalar=scalar, in1=in1, op0=op0, op1=op1), reads=reads, writes=writes)


def ACT(kb, out, in_, func, reads, writes, **kw):
    return kb.op("act", lambda g: g.activation(out=out, in_=in_, func=func, **kw), reads=reads, writes=writes)


def CP(kb, e, out, in_, reads, writes):
    if e == "act":
        return kb.op("act", lambda g: g.copy(out=out, in_=in_), reads=reads, writes=writes)
    return kb.op(e, lambda g: g.tensor_copy(out=out, in_=in_), reads=reads, writes=writes)


def sumsq_rs(kb, x, xk, junk, junkk, out, outk, n, eps, mean=True):
    ACT(kb, junk, x, AF.Square, [xk], [junkk, outk], accum_out=out)
    ACT(kb, out, out, AF.Sqrt, [outk], [outk], scale=(1.0 / n if mean else 1.0), bias=eps)
    kb.op("dve", lambda g: g.reciprocal(out=out, in_=out), reads=[outk], writes=[outk])


def layernorm_rs(kb, x, xk, st, stk, n, eps):
    kb.op("dve", lambda g: g.bn_stats(out=st[:, 2:8], in_=x), reads=[xk], writes=[stk])
    kb.op("dve", lambda g: g.bn_aggr(out=st[:, 0:2], in_=st[:, 2:8]), reads=[stk], writes=[stk])
    ACT(kb, st[:, 1:2], st[:, 1:2], AF.Sqrt, [stk], [stk], bias=eps, scale=1.0)
    kb.op("dve", lambda g: g.reciprocal(out=st[:, 1:2], in_=st[:, 1:2]), reads=[stk], writes=[stk])


def shared_yacc(kb, tag="yacc"):
    if not hasattr(kb, "_yacc"):
        kb._yacc = {}
    if tag not in kb._yacc:
        kb._yacc[tag] = kb.sb(tag, [128, (CTX + SEQ) // 128, 128])
    return kb._yacc[tag]


LSEQ = CTX + SEQ
NCH = LSEQ // 128
FWD_ORDER = list(range(NCH))
BWD_ORDER = [1, 0] + list(range(NCH - 1, 1, -1))


def emit_rwkv(kb, c, banks, banks1=None, sync=None):
    MUL, ADD, SUB = ALU.mult, ALU.add, ALU.subtract
    X = {n: kb.dram_in("rw_" + n, [LSEQ, 640]) for n in ("cur", "prev", "next")}
    cst_d = kb.dram_in("rw_cst", [128, 2 * 640 + 128 * 9])
    wup_d = kb.dram_in("rw_wup", [2, 64, 128])
    aup_d = kb.dram_in("rw_aup", [2, 64, 128])
    gup_d = kb.dram_in("rw_gup", [128, 128])
    out = kb.dram_out("rw_out", [LSEQ, 128])
    cst = kb.sb("rw_cst_s", [128, 2 * 640 + 128 * 9])
    kb.dma("sp", cst[:], cst_d, writes=["rw_cst"])
    mu0, mu1 = cst[:, 0:640], cst[:, 640:1280]
    o = 1280
    k_k, k_a, r_k, ln_w, ln_b = (cst[:, o + i * 128:o + (i + 1) * 128] for i in range(5))
    w0 = [cst[:, o + (5 + d) * 128:o + (6 + d) * 128] for d in range(2)]
    a0 = [cst[:, o + (7 + d) * 128:o + (8 + d) * 128] for d in range(2)]
    wup = kb.sb("rw_wup_s", [64, 2, 128])
    aup = kb.sb("rw_aup_s", [64, 2, 128])
    gup = kb.sb("rw_gup_s", [128, 128])
    kb.dma("sp", wup[:], wup_d.rearrange("d r n -> r d n"), writes=["rw_wup"])
    kb.dma("sp", aup[:], aup_d.rearrange("d r n -> r d n"), writes=["rw_aup"])
    kb.dma("sp", gup[:], gup_d, writes=["rw_gup"])
    cur, prv, nxt = kb.sb("rw_cur_s", [128, 640]), kb.sb("rw_prv", [128, 640]), kb.sb("rw_nxt", [128, 640])
    kkp, kk = kb.sb("rw_kkp", [128, 128]), kb.sb("rw_kk", [128, 128])
    junk = kb.sb("rw_junk", [128, 128])
    st = kb.sb("rw_st", [128, 8])
    twd = kb.sb("rw_twd", [128, 128])
    twdT = kb.sb("rw_twdT", [64, 2, 128])
    sg = kb.sb("rw_sg", [128, 128])
    sgT = kb.sb("rw_sgT", [128, 128])
    lwt, at, ktt, alt = kb.sb("rw_lw", [128, 128]), kb.sb("rw_a", [128, 128]), kb.sb("rw_kt", [128, 128]), kb.sb("rw_al", [128, 128])
    gt = kb.sb("rw_g", [128, 128])
    bon = kb.sb("rw_bon", [128, 4])
    yacc = shared_yacc(kb, "yaccR")
    yn = kb.sb("rw_yn", [128, 128])
    sc = [Dplr(kb, 64, 64, "V", True, f"rw{h}", c) for h in range(2)]
    b1, b1k = banks["b1"]
    b3, b3k = banks["b3"]
    idt = c["ident"]

    def prep_common(ci):
        rows = slice(ci * 128, (ci + 1) * 128)
        kb.dma("sp", cur[:], X["cur"][rows, :], writes=["rw_cur"])
        kb.dma("sp", prv[:], X["prev"][rows, :], writes=["rw_prv"])
        kb.dma("sp", nxt[:], X["next"][rows, :], writes=["rw_nxt"])
        TT(kb, "dve", prv[:], prv[:], cur[:], SUB, ["rw_prv", "rw_cur"], ["rw_prv"])
        TT(kb, "pool", nxt[:], nxt[:], cur[:], SUB, ["rw_nxt", "rw_cur"], ["rw_nxt"])
        TT(kb, "dve", prv[:], prv[:], mu0, MUL, ["rw_prv", "rw_cst"], ["rw_prv"])
        TT(kb, "pool", nxt[:], nxt[:], mu1, MUL, ["rw_nxt", "rw_cst"], ["rw_nxt"])
        TT(kb, "dve", cur[:], cur[:], prv[:], ADD, ["rw_prv", "rw_cur"], ["rw_cur"])
        TT(kb, "dve", cur[:], cur[:], nxt[:], ADD, ["rw_nxt", "rw_cur"], ["rw_cur"])
        TT(kb, "dve", kkp[:], cur[:, 128:256], k_k, MUL, ["rw_cur", "rw_cst"], ["rw_kkp"])
        for h in range(2):
            ACT(kb, junk[:, 0:64], kkp[:, h * 64:(h + 1) * 64], AF.Square, ["rw_kkp"], ["rw_junk", "rw_st"], accum_out=st[:, h:h + 1])
        ACT(kb, st[:, 0:2], st[:, 0:2], AF.Sqrt, ["rw_st"], ["rw_st"], bias=1e-6, scale=1.0)
        kb.op("dve", lambda g: g.reciprocal(out=st[:, 0:2], in_=st[:, 0:2]), reads=["rw_st"], writes=["rw_st"])
        for h in range(2):
            TS(kb, "dve", kk[:, h * 64:(h + 1) * 64], kkp[:, h * 64:(h + 1) * 64], st[:, h:h + 1], None, MUL, None, ["rw_kkp", "rw_st"], ["rw_kk"])
        ACT(kb, twd[:, 0:64], cur[:, 384:448], AF.Tanh, ["rw_cur"], ["rw_twd"])
        CP(kb, "pool", twd[:, 64:128], cur[:, 448:512], ["rw_cur"], ["rw_twd"])
        for i in range(2):
            kb.op("pe", lambda g, i=i: g.transpose(out=b1[0:64, i * 128:(i + 1) * 128], in_=twd[:, i * 64:(i + 1) * 64], identity=idt[:]),
                  reads=["rw_twd", "ident"], writes=[b1k])
        CP(kb, "dve", twdT[:].rearrange("p a b -> p (a b)"), b1[0:64, 0:256], [b1k], ["rw_twdT"])

    def prep_dir(d):
        kb.op("pe", lambda g: g.matmul(b3[:, 0:128], lhsT=twdT[:, 0, :], rhs=wup[:, d, :], start=True, stop=True), reads=["rw_twdT", "rw_wup"], writes=[b3k])
        kb.op("pe", lambda g: g.matmul(b3[:, 128:256], lhsT=twdT[:, 1, :], rhs=aup[:, d, :], start=True, stop=True), reads=["rw_twdT", "rw_aup"], writes=[b3k])
        TT(kb, "dve", lwt[:], b3[:, 0:128], w0[d], ADD, [b3k, "rw_cst"], ["rw_lw"])
        TT(kb, "dve", at[:], b3[:, 128:256], a0[d], ADD, [b3k, "rw_cst"], ["rw_a"])
        ACT(kb, lwt[:], lwt[:], AF.Sigmoid, ["rw_lw"], ["rw_lw"])
        ACT(kb, at[:], at[:], AF.Sigmoid, ["rw_a"], ["rw_a"])
        TS(kb, "pool", lwt[:], lwt[:], -math.exp(-0.5), None, MUL, None, ["rw_lw"], ["rw_lw"])
        STT(kb, ktt[:], at[:], -1.0, k_a, ADD, MUL, ["rw_a", "rw_cst"], ["rw_kt"])
        STT(kb, ktt[:], ktt[:], 1.0, cur[:, 128:256], ADD, MUL, ["rw_kt", "rw_cur"], ["rw_kt"])

    def mkcb(h, ci, d):
        def cb(yp, ypk):
            dst = yacc[:, ci, h * 64:(h + 1) * 64]
            if d == 0:
                CP(kb, "act", dst, yp, [ypk], ["yaccR"])
            else:
                TT(kb, "dve", dst, yp, dst, ADD, [ypk, "yaccR"], ["yaccR"])
        return cb

    if sync is not None:
        sync.update(sc=sc, mkcb=mkcb, ready=-1, done=-1, nsteps=2 * NCH)
    step_no = [0]

    def run_dir(d, order):
        for hh, s in enumerate(sc):
            if sync is None or hh == 0:
                s.init_state()
        for ci in order:
            if sync is not None:
                kb.spin(lambda: sync["done"] >= step_no[0] - 1)
            prep_common(ci)
            prep_dir(d)
            TT(kb, "pool", alt[:], kk[:], at[:], MUL, ["rw_kk", "rw_a"], ["rw_al"])
            for h in range(2):
                s = sc[h]
                hs = slice(h * 64, (h + 1) * 64)
                CP(kb, "pool", s.r[:], cur[:, hs], ["rw_cur"], [s.k("r")])
                CP(kb, "pool", s.v[:], cur[:, 256 + h * 64:256 + (h + 1) * 64], ["rw_cur"], [s.k("v")])
                CP(kb, "pool", s.kap[:], kk[:, hs], ["rw_kk"], [s.k("kap")])
                CP(kb, "pool", s.a[:], alt[:, hs], ["rw_al"], [s.k("a")])
                CP(kb, "pool", s.kt[:], ktt[:, hs], ["rw_kt"], [s.k("kt")])
                CP(kb, "pool", s.lw[:], lwt[:, hs], ["rw_lw"], [s.k("lw")])

                if sync is None:
                    s.step("f" if d == 0 else "b", banks, mkcb(h, ci, d))
            if sync is not None:
                sync["job"] = (d, ci)
                sync["ready"] = step_no[0]
                sc[0].step("f" if d == 0 else "b", banks, mkcb(0, ci, d))
                step_no[0] += 1

    run_dir(0, FWD_ORDER)
    run_dir(1, BWD_ORDER)
    if sync is not None:
        kb.spin(lambda: sync["done"] >= 2 * NCH - 1)
    for ci in range(NCH):
        prep_common(ci)
        ACT(kb, sg[:], cur[:, 512:640], AF.Sigmoid, ["rw_cur"], ["rw_sg"])
        kb.op("pe", lambda g: g.transpose(out=b1[:, 0:128], in_=sg[:], identity=idt[:]), reads=["rw_sg", "ident"], writes=[b1k])
        CP(kb, "dve", sgT[:], b1[:, 0:128], [b1k], ["rw_sgT"])
        kb.op("pe", lambda g: g.matmul(b1[:, 128:256], lhsT=sgT[:], rhs=gup[:], start=True, stop=True), reads=["rw_sgT", "rw_gup"], writes=[b1k])
        CP(kb, "act", gt[:], b1[:, 128:256], [b1k], ["rw_g"])
        for d in range(2):
            prep_dir(d)
            TT(kb, "dve", junk[:], cur[:, 0:128], ktt[:], MUL, ["rw_cur", "rw_kt"], ["rw_junk"])
            TT(kb, "dve", junk[:], junk[:], r_k, MUL, ["rw_junk", "rw_cst"], ["rw_junk"])
            kb.op("dve", lambda g, d=d: g.tensor_reduce(out=bon[:, 2 * d:2 * d + 2], in_=junk[:].rearrange("p (h n) -> p h n", h=2), axis=AX.X, op=ADD),
                  reads=["rw_junk"], writes=["rw_bon"])
        TT(kb, "dve", bon[:, 0:2], bon[:, 0:2], bon[:, 2:4], ADD, ["rw_bon"], ["rw_bon"])
        for h in range(2):
            hs = slice(h * 64, (h + 1) * 64)
            layernorm_rs(kb, yacc[:, ci, hs], "yaccR", st, "rw_st", 64, 64e-5)
            TS(kb, "dve", yn[:, hs], yacc[:, ci, hs], st[:, 0:1], st[:, 1:2], SUB, MUL, ["yaccR", "rw_st"], ["rw_yn"])
        TT(kb, "dve", yn[:], yn[:], ln_w, MUL, ["rw_yn", "rw_cst"], ["rw_yn"])
        TT(kb, "dve", yn[:], yn[:], ln_b, ADD, ["rw_yn", "rw_cst"], ["rw_yn"])
        for h in range(2):
            hs = slice(h * 64, (h + 1) * 64)
            STT(kb, yn[:, hs], cur[:, 256 + h * 64:256 + (h + 1) * 64], bon[:, h:h + 1], yn[:, hs], MUL, ADD, ["rw_cur", "rw_bon", "rw_yn"], ["rw_yn"])
        TT(kb, "dve", yn[:], yn[:], gt[:], MUL, ["rw_yn", "rw_g"], ["rw_yn"])
        kb.dma("pool", out[ci * 128:(ci + 1) * 128, :], yn[:], reads=["rw_yn"], is_output=True)


def emit_rwkv_head1(kb, banks1, sync):
    kb.spin(lambda: "sc" in sync)
    s = sync["sc"][1]
    for n in range(2 * NCH):
        kb.spin(lambda: sync["ready"] >= n)
        d, ci = sync["job"]
        if n == 0 or n == NCH:
            s.init_state()
        s.step("f" if d == 0 else "b", banks1, sync["mkcb"](1, ci, d))
        sync["done"] = n


def seq_rows(pl_all, b):
    lat = pl_all[b * SEQ:(b + 1) * SEQ]
    cx = pl_all[2 * SEQ + b * CTX:2 * SEQ + (b + 1) * CTX]
    return cx, lat


def shifted(cx, lat, sh):
    def s(x):
        o = np.zeros_like(x)
        if sh < 0:
            o[1:] = x[:-1]
        else:
            o[:-1] = x[1:]
        return o
    return np.concatenate([s(cx), s(lat)], 0)


def rep(v):
    return np.ascontiguousarray(np.broadcast_to(np.asarray(v, np.float32).reshape(1, -1), (128, np.asarray(v).size)))


def rwkv_inputs(pl_all, p, b, j):
    cx, lat = seq_rows(pl_all[:, 0:RW_COLS], b)
    cs = slice(j * 128, (j + 1) * 128)
    cols = np.r_[np.arange(j * 128, (j + 1) * 128), GW + np.arange(j * 128, (j + 1) * 128), 2 * GW + np.arange(j * 128, (j + 1) * 128),
                 np.arange(3 * GW, 3 * GW + 256)]
    m = {}
    m["rw_cur"] = np.ascontiguousarray(np.concatenate([cx, lat], 0)[:, cols])
    m["rw_prev"] = np.ascontiguousarray(shifted(cx, lat, -1)[:, cols])
    m["rw_next"] = np.ascontiguousarray(shifted(cx, lat, +1)[:, cols])
    cst = [rep(p["rw_mu"][0][cols]), rep(p["rw_mu"][1][cols]), rep(p["rw_k_k"][cs]), rep(p["rw_k_a"][cs]),
           rep(p["rw_r_k"].reshape(-1)[cs]), rep(p["rw_ln_w"][cs]), rep(p["rw_ln_b"][cs]),
           rep(p["rw_w0"][0][cs]), rep(p["rw_w0"][1][cs]), rep(p["rw_a0"][0][cs]), rep(p["rw_a0"][1][cs])]
    m["rw_cst"] = np.ascontiguousarray(np.concatenate(cst, 1))
    m["rw_wup"] = np.ascontiguousarray(p["rw_w_up"][:, :, cs])
    m["rw_aup"] = np.ascontiguousarray(p["rw_a_up"][:, :, cs])
    m["rw_gup"] = np.ascontiguousarray(p["rw_g_up"][:, cs])
    return m


def emit_mlstm(kb, c, banks, yacc_ext=None):
    MUL, ADD, SUB = ALU.mult, ALU.add, ALU.subtract
    X = kb.dram_in("ml_x", [LSEQ, 516])
    cst_d = kb.dram_in("ml_cst", [128, 128 + 4])
    out = kb.dram_out("ml_out", [LSEQ, 128])
    cst = kb.sb("ml_cst_s", [128, 132])
    kb.dma("sp", cst[:], cst_d, writes=["ml_cst"])
    x = kb.sb("ml_xs", [128, 516])
    gs = kb.sb("ml_gs", [128, 4])
    st = kb.sb("ml_st", [128, 8])
    yacc, YK = (shared_yacc(kb), "yacc") if yacc_ext is None else yacc_ext
    yn = kb.sb("ml_yn", [128, 128])
    sgo = kb.sb("ml_sgo", [128, 128])
    s = Dplr(kb, 128, 129, "S", False, "ml", c)
    kb.op("pool", lambda g: g.memset(s.v[:, 128:129], 1.0), writes=[s.k("v")])
    ones = c["ones"]

    def run_dir(d, order):
        s.init_state()
        for ci in order:
            rows = slice(ci * 128, (ci + 1) * 128)
            kb.dma("sp", x[:], X[rows, :], writes=["ml_x"])
            ACT(kb, gs[:, 0:1], x[:, 512 + d:513 + d], AF.Exp, ["ml_x", "ml_cst"], ["ml_gs"], bias=cst[:, 128 + d:129 + d], scale=1.0)
            ACT(kb, gs[:, 1:2], x[:, 514 + d:515 + d], AF.Sigmoid, ["ml_x", "ml_cst"], ["ml_gs"], bias=cst[:, 130 + d:131 + d], scale=1.0)
            ACT(kb, gs[:, 1:2], gs[:, 1:2], AF.Ln, ["ml_gs"], ["ml_gs"])
            CP(kb, "pool", s.r[:], x[:, 0:128], ["ml_x"], [s.k("r")])
            TS(kb, "dve", s.kt[:], x[:, 128:256], gs[:, 0:1], 128.0 ** -0.5, MUL, MUL, ["ml_x", "ml_gs"], [s.k("kt")])
            CP(kb, "pool", s.v[:, 0:128], x[:, 256:384], ["ml_x"], [s.k("v")])
            TS(kb, "dve", s.lw[:], ones[:], gs[:, 1:2], None, MUL, None, ["m_ones", "ml_gs"], [s.k("lw")])

            def cb(yp, ypk, ci=ci):
                TS(kb, "dve", st[:, 1:2], yp[:, 128:129], -1.0, None, MUL, None, [ypk], ["ml_st"])
                TT(kb, "dve", st[:, 0:1], yp[:, 128:129], st[:, 1:2], ALU.max, [ypk, "ml_st"], ["ml_st"])
                TS(kb, "dve", st[:, 0:1], st[:, 0:1], 1.0, None, ALU.max, None, ["ml_st"], ["ml_st"])
                kb.op("dve", lambda g: g.reciprocal(out=st[:, 0:1], in_=st[:, 0:1]), reads=["ml_st"], writes=["ml_st"])
                dst = yacc[:, ci, :]
                if d == 0:
                    TS(kb, "dve", dst, yp[:, 0:128], st[:, 0:1], None, MUL, None, [ypk, "ml_st"], [YK])
                else:
                    STT(kb, dst, yp[:, 0:128], st[:, 0:1], dst, MUL, ADD, [ypk, "ml_st", YK], [YK])
            s.step("f" if d == 0 else "b", banks, cb)

    run_dir(0, FWD_ORDER)
    run_dir(1, BWD_ORDER)
    for ci in range(NCH):
        rows = slice(ci * 128, (ci + 1) * 128)
        kb.dma("sp", x[:], X[rows, :], writes=["ml_x"])
        layernorm_rs(kb, yacc[:, ci, :], YK, st, "ml_st", 128, EPS)
        TS(kb, "dve", yn[:], yacc[:, ci, :], st[:, 0:1], st[:, 1:2], SUB, MUL, [YK, "ml_st"], ["ml_yn"])
        TT(kb, "dve", yn[:], yn[:], cst[:, 0:128], MUL, ["ml_yn", "ml_cst"], ["ml_yn"])
        ACT(kb, sgo[:], x[:, 384:512], AF.Sigmoid, ["ml_x"], ["ml_sgo"])
        TT(kb, "dve", yn[:], yn[:], sgo[:], MUL, ["ml_yn", "ml_sgo"], ["ml_yn"])
        kb.dma("pool", out[rows, :], yn[:], reads=["ml_yn"], is_output=True)


def mlstm_inputs(pl_all, p, b, j):
    cx, lat = seq_rows(pl_all[:, RW_COLS:RW_COLS + ML_COLS], b)
    cols = np.r_[np.arange(j * 128, (j + 1) * 128), GW + np.arange(j * 128, (j + 1) * 128), 2 * GW + np.arange(j * 128, (j + 1) * 128),
                 3 * GW + np.arange(j * 128, (j + 1) * 128), 4 * GW + np.array([j, 4 + j, 8 + j, 12 + j])]
    m = {"ml_x": np.ascontiguousarray(np.concatenate([cx, lat], 0)[:, cols])}
    cst = [rep(p["ml_norm_g"][j * 128:(j + 1) * 128]), rep([p["ml_ib"][0][j], p["ml_ib"][1][j], p["ml_fb"][0][j], p["ml_fb"][1][j]])]
    m["ml_cst"] = np.ascontiguousarray(np.concatenate(cst, 1))
    return m


def emit_gdn(kb, c, banks):
    MUL, ADD, SUB = ALU.mult, ALU.add, ALU.subtract
    X = {n: kb.dram_in("gd_" + n, [LSEQ, 384]) for n in ("cur", "prev", "next")}
    G = kb.dram_in("gd_g", [LSEQ, 132])
    cst_d = kb.dram_in("gd_cst", [128, 3 * 384 + 128 + 4])
    out = kb.dram_out("gd_out", [LSEQ, 128])
    cst = kb.sb("gd_cst_s", [128, 3 * 384 + 132])
    kb.dma("sp", cst[:], cst_d, writes=["gd_cst"])
    nega = kb.sb("gd_nega", [128, 2])
    ACT(kb, nega[:], cst[:, 1280:1282], AF.Exp, ["gd_cst"], ["gd_nega"])
    TS(kb, "dve", nega[:], nega[:], -1.0, None, MUL, None, ["gd_nega"], ["gd_nega"])
    cur, prv, nxt = kb.sb("gd_cur_s", [128, 384]), kb.sb("gd_prv", [128, 384]), kb.sb("gd_nxt", [128, 384])
    gg = kb.sb("gd_gs", [128, 132])
    sg = kb.sb("gd_sg", [128, 384])
    junk = kb.sb("gd_junk", [128, 128])
    st = kb.sb("gd_st", [128, 8])
    gs = kb.sb("gd_gsc", [128, 4])
    yacc = shared_yacc(kb)
    yn = kb.sb("gd_yn", [128, 128])
    s = Dplr(kb, 128, 128, "S", True, "gd", c)
    ones = c["ones"]

    def run_dir(d, order):
        s.init_state()
        for ci in order:
            rows = slice(ci * 128, (ci + 1) * 128)
            kb.dma("sp", cur[:], X["cur"][rows, :], writes=["gd_cur"])
            kb.dma("sp", prv[:], X["prev"][rows, :], writes=["gd_prv"])
            kb.dma("sp", nxt[:], X["next"][rows, :], writes=["gd_nxt"])
            kb.dma("sp", gg[:], G[rows, :], writes=["gd_g"])
            TT(kb, "dve", cur[:], cur[:], cst[:, 384:768], MUL, ["gd_cur", "gd_cst"], ["gd_cur"])
            TT(kb, "pool", prv[:], prv[:], cst[:, 0:384], MUL, ["gd_prv", "gd_cst"], ["gd_prv"])
            TT(kb, "pool", nxt[:], nxt[:], cst[:, 768:1152], MUL, ["gd_nxt", "gd_cst"], ["gd_nxt"])
            TT(kb, "dve", cur[:], cur[:], prv[:], ADD, ["gd_cur", "gd_prv"], ["gd_cur"])
            TT(kb, "dve", cur[:], cur[:], nxt[:], ADD, ["gd_cur", "gd_nxt"], ["gd_cur"])
            ACT(kb, sg[:], cur[:], AF.Sigmoid, ["gd_cur"], ["gd_sg"])
            TT(kb, "dve", cur[:], cur[:], sg[:], MUL, ["gd_cur", "gd_sg"], ["gd_cur"])
            for i in range(2):
                ACT(kb, junk[:], cur[:, i * 128:(i + 1) * 128], AF.Square, ["gd_cur"], ["gd_junk", "gd_st"], accum_out=st[:, i:i + 1])
            ACT(kb, st[:, 0:2], st[:, 0:2], AF.Sqrt, ["gd_st"], ["gd_st"], bias=1e-6, scale=1.0)
            kb.op("dve", lambda g: g.reciprocal(out=st[:, 0:2], in_=st[:, 0:2]), reads=["gd_st"], writes=["gd_st"])
            TS(kb, "dve", s.r[:], cur[:, 0:128], st[:, 0:1], 128.0 ** -0.5, MUL, MUL, ["gd_cur", "gd_st"], [s.k("r")])
            TS(kb, "dve", s.kap[:], cur[:, 128:256], st[:, 1:2], None, MUL, None, ["gd_cur", "gd_st"], [s.k("kap")])
            CP(kb, "pool", s.v[:], cur[:, 256:384], ["gd_cur"], [s.k("v")])
            ACT(kb, gs[:, 0:1], gg[:, 128 + d:129 + d], AF.Exp, ["gd_g", "gd_cst"], ["gd_gsc"], bias=cst[:, 1282 + d:1283 + d], scale=1.0)
            ACT(kb, gs[:, 0:1], gs[:, 0:1], AF.Ln, ["gd_gsc"], ["gd_gsc"], bias=1.0, scale=1.0)
            TT(kb, "dve", gs[:, 0:1], gs[:, 0:1], nega[:, d:d + 1], MUL, ["gd_gsc", "gd_nega"], ["gd_gsc"])
            ACT(kb, gs[:, 1:2], gg[:, 130 + d:131 + d], AF.Sigmoid, ["gd_g"], ["gd_gsc"])
            ACT(kb, gs[:, 2:3], gs[:, 0:1], AF.Exp, ["gd_gsc"], ["gd_gsc"])
            TT(kb, "dve", gs[:, 2:3], gs[:, 2:3], gs[:, 1:2], MUL, ["gd_gsc"], ["gd_gsc"])
            TS(kb, "dve", s.lw[:], ones[:], gs[:, 0:1], None, MUL, None, ["m_ones", "gd_gsc"], [s.k("lw")])
            TS(kb, "dve", s.kt[:], s.kap[:], gs[:, 1:2], None, MUL, None, [s.k("kap"), "gd_gsc"], [s.k("kt")])
            TS(kb, "dve", s.a[:], s.kap[:], gs[:, 2:3], None, MUL, None, [s.k("kap"), "gd_gsc"], [s.k("a")])

            def cb(yp, ypk, ci=ci):
                dst = yacc[:, ci, :]
                if d == 0:
                    CP(kb, "act", dst, yp, [ypk], ["yacc"])
                else:
                    TT(kb, "dve", dst, yp, dst, ADD, [ypk, "yacc"], ["yacc"])
            s.step("f" if d == 0 else "b", banks, cb)

    run_dir(0, FWD_ORDER)
    run_dir(1, BWD_ORDER)
    for ci in range(NCH):
        rows = slice(ci * 128, (ci + 1) * 128)
        kb.dma("sp", gg[:], G[rows, :], writes=["gd_g"])
        sumsq_rs(kb, yacc[:, ci, :], "yacc", junk[:], "gd_junk", st[:, 0:1], "gd_st", 128, EPS)
        STT(kb, yn[:], yacc[:, ci, :], st[:, 0:1], cst[:, 1152:1280], MUL, MUL, ["yacc", "gd_st", "gd_cst"], ["gd_yn"])
        ACT(kb, sg[:, 0:128], gg[:, 0:128], AF.Sigmoid, ["gd_g"], ["gd_sg"])
        TT(kb, "dve", sg[:, 0:128], sg[:, 0:128], gg[:, 0:128], MUL, ["gd_sg", "gd_g"], ["gd_sg"])
        TT(kb, "dve", yn[:], yn[:], sg[:, 0:128], MUL, ["gd_yn", "gd_sg"], ["gd_yn"])
        kb.dma("pool", out[rows, :], yn[:], reads=["gd_yn"], is_output=True)


def gdn_inputs(pl_all, p, b, j):
    o = RW_COLS + ML_COLS
    cx, lat = seq_rows(pl_all[:, o:o + GD_COLS], b)
    cols = np.r_[np.arange(j * 128, (j + 1) * 128), GW + np.arange(j * 128, (j + 1) * 128), 2 * GW + np.arange(j * 128, (j + 1) * 128)]
    gcols = np.r_[3 * GW + np.arange(j * 128, (j + 1) * 128), 4 * GW + np.array([j, 4 + j, 8 + j, 12 + j])]
    m = {}
    m["gd_cur"] = np.ascontiguousarray(np.concatenate([cx, lat], 0)[:, cols])
    m["gd_prev"] = np.ascontiguousarray(shifted(cx, lat, -1)[:, cols])
    m["gd_next"] = np.ascontiguousarray(shifted(cx, lat, +1)[:, cols])
    m["gd_g"] = np.ascontiguousarray(np.concatenate([cx, lat], 0)[:, gcols])
    cst = [rep(p["gd_conv"][0][cols]), rep(p["gd_conv"][1][cols]), rep(p["gd_conv"][2][cols]), rep(p["gd_norm_g"]),
           rep([p["gd_a_log"][0][j], p["gd_a_log"][1][j], p["gd_dt_bias"][0][j], p["gd_dt_bias"][1][j]])]
    m["gd_cst"] = np.ascontiguousarray(np.concatenate(cst, 1))
    return m


def emit_attn(kb, c, banks, need_ctx=True):
    MUL, ADD, SUB = ALU.mult, ALU.add, ALU.subtract
    Q, Kd, V = kb.dram_in("at_q", [LSEQ, 128]), kb.dram_in("at_k", [LSEQ, 128]), kb.dram_in("at_v", [LSEQ, 128])
    COS, SIN = kb.dram_in("at_cos", [LSEQ, 128]), kb.dram_in("at_sin", [LSEQ, 128])
    cst_d = kb.dram_in("at_cst", [128, 256])
    out = kb.dram_out("at_out", [LSEQ, 128])
    cst = kb.sb("at_cst_s", [128, 256])
    kb.dma("sp", cst[:], cst_d, writes=["at_cst"])
    qT, kT = kb.sb("at_qT", [128, LSEQ]), kb.sb("at_kT", [128, LSEQ])
    va = kb.sb("at_va", [128, NCH, 129])
    kb._at_va = va
    kb.op("pool", lambda g: g.memset(va[:], 1.0), writes=["at_va"])
    x = kb.sb("at_x", [128, 2, 128])
    xn = kb.sb("at_xn", [128, 2, 128])
    rot = kb.sb("at_rot", [128, 2, 128])
    cs = kb.sb("at_cs", [128, 2, 128])
    junk = kb.sb("at_junk", [128, 128])
    st = kb.sb("at_st", [128, 4])
    idt = c["ident"]
    b2, b2k = banks["b1"]
    for ci in range(NCH):
        rows = slice(ci * 128, (ci + 1) * 128)
        kb.dma("sp", x[:, 0, :], Q[rows, :], writes=["at_x"])
        kb.dma("sp", x[:, 1, :], Kd[rows, :], writes=["at_x"])
        kb.dma("sp", va[:, ci, 0:128], V[rows, :], writes=["at_va"])
        kb.dma("sp", cs[:, 0, :], COS[rows, :], writes=["at_cs"])
        kb.dma("sp", cs[:, 1, :], SIN[rows, :], writes=["at_cs"])
        for i in range(2):
            sumsq_rs(kb, x[:, i, :], "at_x", junk[:], "at_junk", st[:, i:i + 1], "at_st", 128, EPS)
            STT(kb, xn[:, i, :], x[:, i, :], st[:, i:i + 1], cst[:, i * 128:(i + 1) * 128], MUL, MUL, ["at_x", "at_st", "at_cst"], ["at_xn"])
            xv = xn[:, i, :].rearrange("p (h t n) -> p h t n", h=2, t=2)
            rv = rot[:, i, :].rearrange("p (h t n) -> p h t n", h=2, t=2)
            CP(kb, "pool", rv[:, :, 0, :], xv[:, :, 1, :], ["at_xn"], ["at_rot"])
            CP(kb, "pool", rv[:, :, 1, :], xv[:, :, 0, :], ["at_xn"], ["at_rot"])
            TT(kb, "dve", xn[:, i, :], xn[:, i, :], cs[:, 0, :], MUL, ["at_xn", "at_cs"], ["at_xn"])
            TT(kb, "pool", rot[:, i, :], rot[:, i, :], cs[:, 1, :], MUL, ["at_rot", "at_cs"], ["at_rot"])
            TT(kb, "dve", xn[:, i, :], xn[:, i, :], rot[:, i, :], ADD, ["at_xn", "at_rot"], ["at_xn"])
            kb.op("pe", lambda g, i=i: g.transpose(out=b2[:, i * 128:(i + 1) * 128], in_=xn[:, i, :], identity=idt[:]), reads=["at_xn", "ident"], writes=[b2k])
        CP(kb, "dve", qT[:, rows], b2[:, 0:128], [b2k], ["at_qT"])
        CP(kb, "dve", kT[:, rows], b2[:, 128:256], [b2k], ["at_kT"])
    bS = [banks["b0"], banks["b1"]]
    bO = [banks["b2"], banks["b3"]]
    pT = [kb.sb(f"at_pT{i}", [128, 256]) for i in range(2)]
    ot = kb.sb("at_ot", [128, 128])
    blocks = []
    if need_ctx:
        blocks.append((0, 256, [0, 1]))
    for qb in range(SEQ // 256):
        blocks.append((CTX + qb * 256, 256, list(range(NCH))))
    n = 0
    for q0, qn, kts in blocks:
        nq = qn // 128
        for idx, kt in enumerate(kts):
            (bs, bsk), p, pk = bS[n % 2], pT[n % 2], f"at_pT{n % 2}"
            n += 1
            kb.op("pe", lambda g, bs=bs, kt=kt: g.matmul(bs[:, 0:qn], lhsT=kT[:, kt * 128:(kt + 1) * 128], rhs=qT[:, q0:q0 + qn], start=True, stop=True),
                  reads=["at_kT", "at_qT"], writes=[bsk])
            ACT(kb, p[:, 0:qn], bs[:, 0:qn], AF.Exp, [bsk], [pk], scale=128.0 ** -0.5)
            for qs in range(nq):
                bo, bok = bO[qs]
                kb.op("pe", lambda g, bo=bo, p=p, qs=qs, kt=kt, idx=idx: g.matmul(bo[:, 0:129], lhsT=p[:, qs * 128:(qs + 1) * 128], rhs=va[:, kt, :],
                                                                          start=(idx == 0), stop=(idx == len(kts) - 1)),
                      reads=[pk, "at_va"], writes=[bok])
        for qs in range(nq):
            bo, bok = bO[qs]
            kb.op("dve", lambda g, bo=bo: g.reciprocal(out=st[:, 2:3], in_=bo[:, 128:129]), reads=[bok], writes=["at_st2"])
            TS(kb, "dve", ot[:], bo[:, 0:128], st[:, 2:3], None, MUL, None, [bok, "at_st2"], ["at_ot"])
            kb.dma("pool", out[q0 + qs * 128:q0 + (qs + 1) * 128, :], ot[:], reads=["at_ot"], is_output=True)


def rope_tables():
    rows = SEQ // 64
    row = np.repeat(np.arange(rows), 64).astype(np.float32)
    col = np.tile(np.arange(64), rows).astype(np.float32)
    inv = (10000.0 ** (-np.arange(0, 64, 2, dtype=np.float32) / 64)).astype(np.float32)
    ar, ac = row[:, None] * inv[None, :], col[:, None] * inv[None, :]
    cos = np.concatenate([np.cos(ar), np.cos(ar), np.cos(ac), np.cos(ac)], 1)
    sin = np.concatenate([-np.sin(ar), np.sin(ar), -np.sin(ac), np.sin(ac)], 1)
    cos = np.concatenate([np.ones((CTX, 128)), cos], 0).astype(np.float32)
    sin = np.concatenate([np.zeros((CTX, 128)), sin], 0).astype(np.float32)
    return np.ascontiguousarray(cos), np.ascontiguousarray(sin)


def attn_inputs(pl_all, p, b, j):
    o = RW_COLS + ML_COLS + GD_COLS
    cx, lat = seq_rows(pl_all[:, o:o + AT_COLS], b)
    a = np.concatenate([cx, lat], 0)
    kv = j // 2
    cos, sin = rope_tables()
    m = {"at_q": np.ascontiguousarray(a[:, j * 128:(j + 1) * 128]), "at_k": np.ascontiguousarray(a[:, 512 + kv * 128:512 + (kv + 1) * 128]),
         "at_v": np.ascontiguousarray(a[:, 768 + kv * 128:768 + (kv + 1) * 128]), "at_cos": cos, "at_sin": sin,
         "at_cst": np.ascontiguousarray(np.concatenate([rep(p["at_q_norm"]), rep(p["at_k_norm"])], 1))}
    return m


def build_outproj():
    MUL, ADD, SUB = ALU.mult, ALU.add, ALU.subtract
    kb = KB()
    mix = kb.dram_in("mix", [ROWS, D])
    xin = kb.dram_in("xin", [ROWS, D])
    w = kb.dram_in("w", [D, D])
    mods = kb.dram_in("mods", [NT, 3, D])
    gvec = kb.dram_in("g", [1, D])
    wr = kb.dram_in("wr", [D, NE])
    xmid = kb.dram_out("xmid", [ROWS, D])
    h2o = kb.dram_out("h2", [ROWS, D])
    affo = kb.dram_out("aff", [ROWS, NE])
    idt = ident(kb)
    g_bc = kb.sb("g_bc", [128, D])
    kb.dma("sp", g_bc[:], gvec[0, :].partition_broadcast(128), writes=["g_bc"])
    wrs = kb.sb("wrs", [128, 16, NE])
    kb.dma("sp", wrs[:], wr.rearrange("(kc p) n -> p kc n", p=128), writes=["wrs"])
    GT = 5
    mixT = kb.sb("mixT", [128, GT, 16, 128])
    xm = kb.sb("xm", [128, GT, D])
    xt = kb.sb("xt", [128, D])
    sc, sh = kb.sb("sc", [128, D]), kb.sb("sh", [128, D])
    junk = kb.sb("junk", [128, D])
    h2T = kb.sb("h2T", [128, 16, 128])
    rstd = kb.sb("rstd", [128, 4])
    lg = kb.sb("lg", [128, NE])
    pst = [kb.ps(f"pst{i}", [128, 4, 128]) for i in range(2)]
    pstk = ["pst0", "pst1"]
    NB = 256
    wv = w.rearrange("(kc p) n -> p kc n", p=128)
    wb = [kb.sb(f"wb{i}", [128, 16, NB]) for i in range(2)]
    pp = [kb.ps(f"pp{i}", [128, NB]) for i in range(4)]
    m2b = [kb.sb(f"m2b{i}", [128, NB]) for i in range(4)]
    pr = kb.ps("pr", [128, NE])
    cnt = 0
    wcnt = 0
    for g0 in range(0, NT, GT):
        tiles = list(range(g0, min(NT, g0 + GT)))
        for t in tiles:
            lt = t - g0
            rows = slice(t * 128, (t + 1) * 128)
            kb.dma("sp", xt[:], mix[rows, :], writes=["xt"])
            kb.dma("sp", xm[:, lt, :], xin[rows, :], writes=[f"xm{lt}"])
            transpose_rows(kb, idt, xt, "xt", mixT[:, lt], f"mixT{lt}", pst, pstk)
        for nb in range(D // NB):
            wt, wk = wb[wcnt % 2], f"wb{wcnt % 2}"
            wcnt += 1
            kb.dma("sp", wt[:], wv[:, :, nb * NB:(nb + 1) * NB], writes=[wk])
            for t in tiles:
                lt = t - g0
                p, pk, mb, mk = pp[cnt % 4], f"pp{cnt % 4}", m2b[cnt % 4], f"m2b{cnt % 4}"
                cnt += 1
                kb.dma("sp", mb[:], mods[t, 0, nb * NB:(nb + 1) * NB].partition_broadcast(128), writes=[mk])
                for kc in range(16):
                    kb.op("pe", lambda e, kc=kc, p=p, wt=wt, lt=lt: e.matmul(p[:], lhsT=mixT[:, lt, kc, :], rhs=wt[:, kc, :], start=(kc == 0), stop=(kc == 15)),
                          reads=[f"mixT{lt}", wk], writes=[pk])
                TT(kb, "dve", mb[:], p[:], mb[:], MUL, [pk, mk], [mk])
                TT(kb, "pool", xm[:, lt, nb * NB:(nb + 1) * NB], xm[:, lt, nb * NB:(nb + 1) * NB], mb[:], ADD, [mk, f"xm{lt}"], [f"xm{lt}"])
        for t in tiles:
            lt = t - g0
            rows = slice(t * 128, (t + 1) * 128)
            x, xk = xm[:, lt, :], f"xm{lt}"
            kb.dma("pool", xmid[rows, :], x, reads=[xk], is_output=True)
            rms_rstd(kb, x, xk, junk[:], rstd[:, 0:1], "rstd")
            kb.dma("sp", sh[:], mods[t, 1, :].partition_broadcast(128), writes=["sh"])
            kb.dma("sp", sc[:], mods[t, 2, :].partition_broadcast(128), writes=["sc"])
            STT(kb, sc[:], sc[:], 1.0, g_bc[:], ADD, MUL, ["sc", "g_bc"], ["sc"])
            STT(kb, xt[:], x, rstd[:, 0:1], sc[:], MUL, MUL, [xk, "rstd", "sc"], ["xt"])
            TT(kb, "dve", xt[:], xt[:], sh[:], ADD, ["xt", "sh"], ["xt"])
            kb.dma("pool", h2o[rows, :], xt[:], reads=["xt"], is_output=True)
            transpose_rows(kb, idt, xt, "xt", h2T, "h2T", pst, pstk)
            for kc in range(16):
                kb.op("pe", lambda e, kc=kc: e.matmul(pr[:], lhsT=h2T[:, kc, :], rhs=wrs[:, kc, :], start=(kc == 0), stop=(kc == 15)),
                      reads=["h2T", "wrs"], writes=["pr"])
            kb.op("dve", lambda e: e.tensor_reduce(out=rstd[:, 1:2], in_=pr[:], axis=AX.X, op=ALU.max, negate=True), reads=["pr"], writes=["rmax"])
            ACT(kb, lg[:], pr[:], AF.Exp, ["pr", "rmax"], ["lg", "rsum"], bias=rstd[:, 1:2], scale=1.0, accum_out=rstd[:, 2:3])
            kb.op("dve", lambda e: e.reciprocal(out=rstd[:, 2:3], in_=rstd[:, 2:3]), reads=["rsum"], writes=["rsum"])
            TS(kb, "dve", lg[:], lg[:], rstd[:, 2:3], None, MUL, None, ["lg", "rsum"], ["lg"])
            kb.dma("pool", affo[rows, :], lg[:], reads=["lg"], is_output=True)
    return kb


CAP_L = 2 * SEQ // NE
CAP_C = 2 * CTX // NE


def build_experts(has_ctx):
    MUL, ADD, SUB = ALU.mult, ALU.add, ALU.subtract
    kb = KB()
    affT = kb.dram_in("affT", [4, SEQ])
    h2l = [kb.dram_in(f"h2l{b}", [SEQ, D]) for b in range(B)]
    pl_ = [kb.dram_out(f"part_l{b}", [SEQ, D]) for b in range(B)]
    if has_ctx:
        affTc = kb.dram_in("affTc", [4, CTX])
        h2c = [kb.dram_in(f"h2c{b}", [CTX, D]) for b in range(B)]
        pc_ = [kb.dram_out(f"part_c{b}", [CTX, D]) for b in range(B)]
    w1 = kb.dram_in("w1", [2, 16, 128, 16, 128])
    w3 = kb.dram_in("w3", [2, 16, 128, 16, 128])
    w2 = kb.dram_in("w2", [2, 8, 128, 16, 256])
    idt = ident(kb)
    NTOK = CAP_L + (CAP_C if has_ctx else 0)
    NTT = 4 + (1 if has_ctx else 0)
    xsT = kb.sb("xsT", [128, 16, NTOK])
    ysb = kb.sb("ysb", [128, NTT, D])
    XK, YK = "xsT", "ysb"
    hidT = kb.sb("hidT", [128, 16, NTOK])
    xgs = [kb.sb("xg0", [128, D])] * 2
    xg = xgs[0]
    sgm = kb.sb("sgm", [128, 512])
    kb.op("pool", lambda e: e.memset(xg[:], 0.0), writes=["xg0"])
    for b in range(B):
        for t in range(SEQ // 128):
            kb.dma("sp", pl_[b][t * 128:(t + 1) * 128, :], xg[:], reads=["xg0"], writes=[f"part_l{b}"], is_output=True)
        if has_ctx:
            for t in range(CTX // 128):
                kb.dma("sp", pc_[b][t * 128:(t + 1) * 128, :], xg[:], reads=["xg0"], writes=[f"part_c{b}"], is_output=True)
    pt = kb.ps("pt", [128, 64])

    def topk(src, n, cap, tag):
        wk = kb.sb(f"wk{tag}", [4, n])
        vals = kb.sb(f"vals{tag}", [4, cap])
        idxs = kb.sb(f"idxs{tag}", [4, cap], U32)
        idxf = kb.sb(f"idxf{tag}", [4, cap])
        kb.dma("sp", wk[:], src, writes=[f"wk{tag}"])
        for it in range(cap // 8):
            sl = slice(it * 8, (it + 1) * 8)
            kb.op("dve", lambda e, sl=sl: e.max(out=vals[:, sl], in_=wk[:]), reads=[f"wk{tag}"], writes=[f"vals{tag}"])
            kb.op("dve", lambda e, sl=sl: e.max_index(out=idxs[:, sl], in_max=vals[:, sl], in_values=wk[:]), reads=[f"wk{tag}", f"vals{tag}"], writes=[f"idxs{tag}"])
            kb.op("dve", lambda e, sl=sl: e.match_replace(out=wk[:], in_to_replace=vals[:, sl], in_values=wk[:], imm_value=-1.0),
                  reads=[f"vals{tag}"], writes=[f"wk{tag}"])
        CP(kb, "dve", idxf[:], idxs[:], [f"idxs{tag}"], [f"idxf{tag}"])
        nblk = (cap + 127) // 128
        pw = min(cap, 128)
        idxT = kb.sb(f"idxT{tag}", [128, nblk * 4], I32)
        gT = kb.sb(f"gT{tag}", [128, nblk * 4])
        for srcv, srck, dst, dstk in ((idxf, f"idxf{tag}", idxT, f"idxT{tag}"), (vals, f"vals{tag}", gT, f"gT{tag}")):
            for blk in range(nblk):
                kb.op("pe", lambda e, srcv=srcv, blk=blk: e.transpose(out=pt[0:pw, blk * 4:(blk + 1) * 4], in_=srcv[:, blk * 128:blk * 128 + pw], identity=idt[0:4, 0:4]),
                      reads=[srck, "ident"], writes=["pt"])
            CP(kb, "dve", dst[0:pw, :], pt[0:pw, 0:nblk * 4], ["pt"], [dstk])
        return idxT, gT

    idxT, gT = topk(affT, SEQ, CAP_L, "L")
    if has_ctx:
        idxTc, gTc = topk(affTc, CTX, CAP_C, "C")
    pst = [kb.ps(f"pst{i}", [128, 4, 128]) for i in range(2)]
    pstk = ["pst0", "pst1"]
    ph = [kb.ps(f"ph{i}", [128, 512]) for i in range(2)]
    phc = kb.ps("phc", [128, 2, 32])
    py = [kb.ps(f"py{i}", [128, 512]) for i in range(2)]
    w13 = [kb.sb(f"w13_{i}", [128, 2, 16, 128]) for i in range(2)]
    w2b = [kb.sb(f"w2b{i}", [128, 16, 256]) for i in range(2)]
    wc = 0
    w2c = 0
    yc = 0
    gcnt = 0
    for el in range(2):
        for b in range(B):
            r = el * 2 + b
            for blk in range(4):
                col = blk * 4 + r
                xg, xgk = xgs[gcnt % 2], "xg0"
                gcnt += 1
                kb.dma("pool", None, None, reads=["idxTL"], writes=[xgk],
                       fn=lambda e, col=col, xg=xg: e.indirect_dma_start(out=xg[:], out_offset=None, in_=h2l[b][:, :],
                                                                         in_offset=bass.IndirectOffsetOnAxis(ap=idxT[:, col:col + 1], axis=0)))
                transpose_rows(kb, idt, xg, xgk, xsT[:, :, blk * 128:(blk + 1) * 128], XK, pst, pstk)
            if has_ctx:
                xg, xgk = xgs[gcnt % 2], "xg0"
                gcnt += 1
                kb.dma("pool", None, None, reads=["idxTC"], writes=[xgk],
                       fn=lambda e, xg=xg: e.indirect_dma_start(out=xg[0:CAP_C, :], out_offset=None, in_=h2c[b][:, :],
                                                         in_offset=bass.IndirectOffsetOnAxis(ap=idxTc[0:CAP_C, r:r + 1], axis=0)))
                for c0 in range(0, 16, 4):
                    bank = (c0 // 4) % 2
                    for i in range(4):
                        kb.op("pe", lambda e, i=i, xg=xg: e.transpose(out=pst[bank][:, i, 0:CAP_C], in_=xg[0:CAP_C, (c0 + i) * 128:(c0 + i + 1) * 128], identity=idt[0:CAP_C, 0:CAP_C]),
                              reads=[xgk, "ident"], writes=[pstk[bank]])
                    CP(kb, "dve", xsT[:, c0:c0 + 4, CAP_L:NTOK], pst[bank][:, :, 0:CAP_C], [pstk[bank]], [XK])
            for fb in range(16):
                wt, wk_ = w13[wc % 2], f"w13_{wc % 2}"
                wc += 1
                kb.dma("sp", wt[:, 0], w1[el, fb], writes=[wk_ + "a"])
                kb.dma("sp", wt[:, 1], w3[el, fb], writes=[wk_ + "b"])
                for i in range(2):
                    for kc in range(16):
                        kb.op("pe", lambda e, i=i, kc=kc, wt=wt: e.matmul(ph[i][:], lhsT=wt[:, i, kc, :], rhs=xsT[:, kc, 0:CAP_L], start=(kc == 0), stop=(kc == 15)),
                              reads=[wk_ + "ab"[i], XK], writes=[f"ph{i}"])
                ACT(kb, sgm[:], ph[0][:], AF.Sigmoid, ["ph0"], ["sgm"])
                TT(kb, "dve", sgm[:], ph[0][:], sgm[:], MUL, ["ph0", "sgm"], ["sgm"])
                TT(kb, "dve", hidT[:, fb, 0:CAP_L], ph[1][:], sgm[:], MUL, ["ph1", "sgm"], ["hidT"])
                if has_ctx:
                    for i in range(2):
                        for kc in range(16):
                            kb.op("pe", lambda e, i=i, kc=kc, wt=wt: e.matmul(phc[:, i, :], lhsT=wt[:, i, kc, :], rhs=xsT[:, kc, CAP_L:NTOK], start=(kc == 0), stop=(kc == 15)),
                                  reads=[wk_ + "ab"[i], XK], writes=["phc"])
                    ACT(kb, sgm[:, 0:CAP_C], phc[:, 0, :], AF.Sigmoid, ["phc"], ["sgm"])
                    TT(kb, "dve", sgm[:, 0:CAP_C], phc[:, 0, :], sgm[:, 0:CAP_C], MUL, ["phc", "sgm"], ["sgm"])
                    TT(kb, "dve", hidT[:, fb, CAP_L:NTOK], phc[:, 1, :], sgm[:, 0:CAP_C], MUL, ["phc", "sgm"], ["hidT"])
            for db in range(D // 256):
                wt, wk_ = w2b[w2c % 2], f"w2b{w2c % 2}"
                w2c += 1
                kb.dma("sp", wt[:], w2[el, db], writes=[wk_])
                for tt in range(NTT):
                    np_ = 128 if tt < 4 else CAP_C
                    t0 = tt * 128
                    p, pk = py[yc % 2], f"py{yc % 2}"
                    yc += 1
                    for fc in range(16):
                        kb.op("pe", lambda e, fc=fc, p=p, wt=wt: e.matmul(p[0:np_, 0:256], lhsT=hidT[:, fc, t0:t0 + np_], rhs=wt[:, fc, :], start=(fc == 0), stop=(fc == 15)),
                              reads=["hidT", wk_], writes=[pk])
                    gsc = gT[:, tt * 4 + r:tt * 4 + r + 1] if tt < 4 else gTc[0:CAP_C, r:r + 1]
                    gk = "gTL" if tt < 4 else "gTC"
                    TS(kb, "dve", ysb[0:np_, tt, db * 256:(db + 1) * 256], p[0:np_, 0:256], gsc, None, MUL, None, [pk, gk], [YK])
            for tt in range(NTT):
                if tt < 4:
                    col = tt * 4 + r
                    kb.dma("pool", None, None, reads=[YK, "idxTL"], writes=[f"part_l{b}"], is_output=True,
                           fn=lambda e, col=col, tt=tt: e.indirect_dma_start(out=pl_[b][:, :], out_offset=bass.IndirectOffsetOnAxis(ap=idxT[:, col:col + 1], axis=0),
                                                                              in_=ysb[:, tt, :], in_offset=None, compute_op=ALU.add))
                else:
                    kb.dma("pool", None, None, reads=[YK, "idxTC"], writes=[f"part_c{b}"], is_output=True,
                           fn=lambda e, tt=tt: e.indirect_dma_start(out=pc_[b][:, :], out_offset=bass.IndirectOffsetOnAxis(ap=idxTc[0:CAP_C, r:r + 1], axis=0),
                                                                    in_=ysb[0:CAP_C, tt, :], in_offset=None, compute_op=ALU.add))
    return kb


def expert_inputs(aff, h2, wts, core, has_ctx):
    m = {}
    e0 = 2 * core
    rows = []
    rows_c = []
    for el in range(2):
        for b in range(B):
            rows.append(aff[b * SEQ:(b + 1) * SEQ, e0 + el])
            rows_c.append(aff[2 * SEQ + b * CTX:2 * SEQ + (b + 1) * CTX, e0 + el])
    m["affT"] = np.ascontiguousarray(np.stack(rows, 0))
    for b in range(B):
        m[f"h2l{b}"] = np.ascontiguousarray(h2[b * SEQ:(b + 1) * SEQ])
    if has_ctx:
        m["affTc"] = np.ascontiguousarray(np.stack(rows_c, 0))
        for b in range(B):
            m[f"h2c{b}"] = np.ascontiguousarray(h2[2 * SEQ + b * CTX:2 * SEQ + (b + 1) * CTX])
    for n, w in zip(("w1", "w3"), wts[:2]):
        m[n] = np.ascontiguousarray(w[e0:e0 + 2].reshape(2, 16, 128, 16, 128).transpose(0, 3, 2, 1, 4))
    m["w2"] = np.ascontiguousarray(wts[2][e0:e0 + 2].reshape(2, 16, 128, 8, 256).transpose(0, 3, 2, 1, 4))
    return m


def expert_parts(res, has_ctx):
    outs = []
    for r in res:
        a = [r["part_l0"], r["part_l1"]]
        if has_ctx:
            a += [r["part_c0"], r["part_c1"]]
        else:
            a += [np.zeros((CTX, D), np.float32)] * 2
        outs.append(np.concatenate(a, 0))
    return outs


def dplr_banks3(kb):
    a0 = [(kb.ps(f"bankA0_{i}", [128, 512]), f"bankA0_{i}") for i in range(2)]
    a1 = [(kb.ps(f"bankA1_{i}", [128, 512]), f"bankA1_{i}") for i in range(2)]
    bb = [(kb.ps(f"bankB_{i}", [128, 512]), f"bankB_{i}") for i in range(4)]
    return ({f"b{i}": a0[i % 2] for i in range(8)}, {f"b{i}": a1[i % 2] for i in range(8)}, {f"b{i}": bb[i % 4] for i in range(8)})


def build_mixers():
    kb = KB()
    c = dplr_consts(kb)
    bA0, bA1, bB = dplr_banks3(kb)
    sync = {}

    def sA0():
        emit_rwkv(kb, c, bA0, bA1, sync)

    def sA1():
        emit_rwkv_head1(kb, bA1, sync)
        kb.spin(lambda: sync.get("attn_done", False))
        emit_mlstm(kb, c, bA1, yacc_ext=(kb._at_va[:, :, 0:128], "at_va"))

    def sB():
        emit_attn(kb, c, bB)
        sync["attn_done"] = True
        emit_gdn(kb, c, bB)
    kb.run_streams([sA0, sA1, sB])
    return kb


def mixer_inputs(pl_all, p, core):
    b, j = core // 4, core % 4
    m = {}
    m.update(rwkv_inputs(pl_all, p, b, j))
    m.update(mlstm_inputs(pl_all, p, b, j))
    m.update(gdn_inputs(pl_all, p, b, j))
    m.update(attn_inputs(pl_all, p, b, j))
    return m


def mixer_outputs(res):
    mix = np.zeros((TOT, D), np.float32)
    for core in range(NCORES):
        b, j = core // 4, core % 4
        for gi, nm in enumerate(("rw_out", "ml_out", "gd_out", "at_out")):
            o = res[core][nm]
            cs = slice(gi * GW + j * 128, gi * GW + (j + 1) * 128)
            mix[2 * SEQ + b * CTX:2 * SEQ + (b + 1) * CTX, cs] = o[:CTX]
            mix[b * SEQ:(b + 1) * SEQ, cs] = o[CTX:]
    return mix


_PROGS = {}


def _prog(name, fn):
    if name not in _PROGS:
        _PROGS[name] = fn().finish()
    return _PROGS[name]


LAYER_PARAMS = ['rw_mu', 'rw_w0', 'rw_w_up', 'rw_a0', 'rw_a_up', 'rw_g_up', 'rw_k_k', 'rw_k_a', 'rw_r_k', 'rw_ln_w', 'rw_ln_b',
                'ml_ib', 'ml_fb', 'ml_norm_g', 'gd_conv', 'gd_a_log', 'gd_dt_bias', 'gd_norm_g', 'at_q_norm', 'at_k_norm']


def kernel(**inp):
    inp = {k: np.asarray(v, np.float32) for k, v in inp.items()}
    mod = run_mod(inp["c"], inp["c_ctx"], inp["ada_w"], inp["ada_b"])
    x = np.concatenate([inp["x"].reshape(-1, D), inp["ctx"].reshape(-1, D)], 0)
    parts = None
    m5 = None
    for l in range(DEPTH):
        p = {n: inp[n][l] for n in LAYER_PARAMS}
        xs = rows_to_cores(x)
        mr = mod_rows_for(mod[l], [0, 1])
        g1 = np.ascontiguousarray(inp["norm1_g"][l][None, :])
        w_in = np.ascontiguousarray(inp["w_in"][l])
        if parts is None:
            nc = _prog("rows0", lambda: build_rows(False, "inproj"))
            maps = [{"xin": xs[c], "g": g1, "modrows": mr[c], "w": w_in} for c in range(NCORES)]
        else:
            nc = _prog("rows1", lambda: build_rows(True, "inproj"))
            maps = [{"xin": xs[c], "g": g1, "modrows": mr[c], "w": w_in, "parts": parts[c], "m5": m5[c]} for c in range(NCORES)]
        res = run(nc, maps)
        pl_all = cores_to_rows([r["out"] for r in res])
        if parts is not None:
            x = cores_to_rows([r["xout"] for r in res])
            xs = rows_to_cores(x)
        nc = _prog("mixers", build_mixers)
        res = run(nc, [mixer_inputs(pl_all, p, c) for c in range(NCORES)])
        mix = mixer_outputs(res)
        del pl_all
        nc = _prog("outproj", build_outproj)
        ms = rows_to_cores(mix)
        mr2 = mod_rows_for(mod[l], [2, 3, 4])
        g2 = np.ascontiguousarray(inp["norm2_g"][l][None, :])
        w_out = np.ascontiguousarray(inp["w_out"][l])
        wr = np.ascontiguousarray(inp["w_router"][l])
        res = run(nc, [{"mix": ms[c], "xin": xs[c], "w": w_out, "mods": mr2[c], "g": g2, "wr": wr} for c in range(NCORES)])
        x = cores_to_rows([r["xmid"] for r in res])
        h2 = cores_to_rows([r["h2"] for r in res])
        aff = cores_to_rows([r["aff"] for r in res])
        has_ctx = l < DEPTH - 1
        nc = _prog("experts%d" % has_ctx, lambda: build_experts(has_ctx))
        wts = (inp["w_exp1"][l], inp["w_exp3"][l], inp["w_exp2"][l])
        res = run(nc, [expert_inputs(aff, h2, wts, c, has_ctx) for c in range(NCORES)])
        pfull = expert_parts(res, has_ctx)
        pc = [rows_to_cores(a) for a in pfull]
        parts = [np.ascontiguousarray(np.stack([pc[e][c] for e in range(NCORES)], 0)) for c in range(NCORES)]
        m5 = [np.ascontiguousarray(a[:, 0, :]) for a in mod_rows_for(mod[l], [5])]
        del pfull, pc
    nc = _prog("final", lambda: build_rows(True, "final"))
    xs = rows_to_cores(x)
    gf = np.ascontiguousarray(inp["final_g"][None, :])
    res = run(nc, [{"xin": xs[c], "g": gf, "parts": parts[c], "m5": m5[c]} for c in range(NCORES)])
    out = cores_to_rows([r["out"] for r in res])[:B * SEQ]
    return np.ascontiguousarray(out.reshape(B, SEQ, D).astype(np.float32))
```
